# Optimizing a Trainium2 kernel written in Bass

```python
import math
import jax, jax.numpy as jnp
from jax import lax
import numpy as np

D_MODEL = 1024
BATCH = 16
SEQ = 2048
DEPTH = 2

GDN_HEADS = 4
GDN_DK = 128
GDN_DV = 128
GDN_CONV = 4
GDN_CHUNK = 64
MLSTM_HEADS = 4
MLSTM_DK = 128
MLSTM_DV = 128
MLSTM_CHUNK = 64
NSA_HEADS = 8
NSA_GROUPS = 2
NSA_HPG = NSA_HEADS // NSA_GROUPS
NSA_DH = 64
CMP_BLOCK = 32
CMP_STRIDE = 16
SEL_BLOCK = 64
SEL_TOPN = 4
WINDOW = 512
Q_BLOCK = 128
REL_BUCKETS = 32
REL_MAX_DIST = 128
N_BRANCH = 3
BRANCH_W = 512
D_FF = -(-8 * D_MODEL // (3 * 256)) * 256
PLE_DIM = 256
EPS = 1e-6
NEG = -1e30
FORCE_SCORE = 1e4

IN_SIZES = [
    GDN_HEADS * GDN_DK, GDN_HEADS * GDN_DK, GDN_HEADS * GDN_DV, GDN_HEADS * GDN_DV, GDN_HEADS, GDN_HEADS,
    MLSTM_HEADS * MLSTM_DK, MLSTM_HEADS * MLSTM_DK, MLSTM_HEADS * MLSTM_DV, MLSTM_HEADS * MLSTM_DV, MLSTM_HEADS, MLSTM_HEADS,
    NSA_HEADS * NSA_DH] + [NSA_GROUPS * NSA_DH] * 6 + [NSA_HEADS * 3, N_BRANCH * D_MODEL]
D_IN = sum(IN_SIZES)

kernel_name = "hybrid_gdn_mlstm_nsa_block"


def rms_f32(x, g):
    xf = x.astype(jnp.float32)
    return xf * lax.rsqrt(jnp.mean(xf * xf, -1, keepdims=True) + EPS) * g.astype(jnp.float32)


def rmsnorm(x, g):
    return rms_f32(x, g).astype(x.dtype)


def l2norm(x):
    return x * lax.rsqrt(jnp.sum(x * x, -1, keepdims=True) + 1e-6)


def split_cols(y, sizes):
    return jnp.split(y, np.cumsum(sizes)[:-1].tolist(), axis=-1)


def causal_conv(x, w):
    K, C = w.shape
    return lax.conv_general_dilated(x, w[:, None, :].astype(x.dtype), window_strides=(1,),
                                    padding=[(K - 1, 0)], dimension_numbers=('NWC', 'WIO', 'NWC'),
                                    feature_group_count=C)


def to_chunks(t, L):
    Bsz, S, H, d = t.shape
    return t.reshape(Bsz, S // L, L, H, d).transpose(1, 0, 3, 2, 4)


def from_chunks(t):
    N, Bsz, H, L, d = t.shape
    return t.transpose(1, 0, 3, 2, 4).reshape(Bsz, N * L, H, d)


def rel_bucket(dist):
    max_exact = REL_BUCKETS // 2
    d = jnp.maximum(dist, 0)
    df = jnp.maximum(d, 1).astype(jnp.float32)
    large = max_exact + (jnp.log(df / max_exact) / math.log(REL_MAX_DIST / max_exact)
                         * (REL_BUCKETS - max_exact)).astype(jnp.int32)
    large = jnp.minimum(large, REL_BUCKETS - 1)
    return jnp.where(d < max_exact, d, large)


def masked_softmax(s, valid):
    s = jnp.where(valid, s.astype(jnp.float32), NEG)
    e = jnp.exp(s - jnp.max(s, -1, keepdims=True)) * valid
    return e / jnp.maximum(jnp.sum(e, -1, keepdims=True), 1e-30)


def gated_deltanet(q, k, v, z, a, b, conv_w, a_log, dt_bias, norm_w):
    dt = q.dtype
    Bsz, S, _ = q.shape
    H, dk, dv, L = GDN_HEADS, GDN_DK, GDN_DV, GDN_CHUNK
    f32 = jnp.float32
    qkv = jax.nn.silu(causal_conv(jnp.concatenate([q, k, v], -1), conv_w)).astype(f32)
    q, k, v = jnp.split(qkv, [H * dk, 2 * H * dk], axis=-1)
    q = l2norm(q.reshape(Bsz, S, H, dk)) * (dk ** -0.5)
    k = l2norm(k.reshape(Bsz, S, H, dk))
    v = v.reshape(Bsz, S, H, dv)
    beta = jax.nn.sigmoid(b.astype(f32))
    g = -jnp.exp(a_log.astype(f32)) * jax.nn.softplus(a.astype(f32) + dt_bias.astype(f32))
    qc, kc, vc = to_chunks(q, L), to_chunks(k, L), to_chunks(v, L)
    bc = to_chunks(beta[..., None], L)
    gc = jnp.cumsum(to_chunks(g[..., None], L)[..., 0], axis=-1)
    tril = jnp.tril(jnp.ones((L, L), bool))
    strict = jnp.tril(jnp.ones((L, L), bool), -1)
    diff = gc[..., :, None] - gc[..., None, :]
    decay = jnp.where(tril, jnp.exp(jnp.where(tril, diff, 0.0)), 0.0)
    kb = kc * bc
    X = jnp.where(strict, jnp.einsum('nbhid,nbhjd->nbhij', kb, kc) * decay, 0.0)
    eye = jnp.eye(L, dtype=f32)
    T = lax.linalg.triangular_solve(eye + X, jnp.broadcast_to(eye, X.shape), left_side=True,
                                    lower=True, unit_diagonal=True)
    u = T @ (vc * bc)
    w = T @ (kb * jnp.exp(gc)[..., None])
    attn = jnp.einsum('nbhid,nbhjd->nbhij', qc, kc) * decay

    def step(S_, xs):
        q_, k_, u_, w_, attn_, g_ = xs
        v_new = u_ - w_ @ S_
        o = (q_ * jnp.exp(g_)[..., None]) @ S_ + attn_ @ v_new
        gl = g_[..., -1:]
        S_ = S_ * jnp.exp(gl)[..., None] + jnp.einsum('bhld,bhle->bhde', k_ * jnp.exp(gl - g_)[..., None], v_new)
        return S_, o

    S0 = jnp.zeros((Bsz, H, dk, dv), f32)
    _, o = lax.scan(step, S0, (qc, kc, u, w, attn, gc))
    o = from_chunks(o)
    o = rms_f32(o, norm_w) * jax.nn.silu(z.reshape(Bsz, S, H, dv).astype(f32))
    return o.reshape(Bsz, S, H * dv).astype(dt)


def mlstm(q, k, v, o_pre, i_pre, f_pre, b_i, b_f, norm_w):
    dt = q.dtype
    Bsz, S, _ = q.shape
    H, dk, dv, L = MLSTM_HEADS, MLSTM_DK, MLSTM_DV, MLSTM_CHUNK
    f32 = jnp.float32
    q = q.reshape(Bsz, S, H, dk).astype(f32)
    k = k.reshape(Bsz, S, H, dk).astype(f32) * (dk ** -0.5)
    v = v.reshape(Bsz, S, H, dv).astype(f32)
    it = i_pre.astype(f32) + b_i.astype(f32)
    lf = jax.nn.log_sigmoid(f_pre.astype(f32) + b_f.astype(f32))
    qc, kc, vc = to_chunks(q, L), to_chunks(k, L), to_chunks(v, L)
    itc = to_chunks(it[..., None], L)[..., 0]
    bcum = jnp.cumsum(to_chunks(lf[..., None], L)[..., 0], axis=-1)
    tril = jnp.tril(jnp.ones((L, L), bool))
    Dm = jnp.where(tril, bcum[..., :, None] - bcum[..., None, :] + itc[..., None, :], NEG)
    dmax = jnp.max(Dm, -1)
    qk = jnp.einsum('nbhid,nbhjd->nbhij', qc, kc)

    def step(carry, xs):
        Cb, nb, m = carry
        q_, k_, v_, it_, b_, D_, dmax_, qk_ = xs
        a = b_ + m[..., None]
        mt = jnp.maximum(a, dmax_)
        Sm = jnp.exp(D_ - mt[..., None]) * qk_
        si = jnp.exp(a - mt)
        num = si[..., None] * (q_ @ Cb) + Sm @ v_
        den = si * jnp.einsum('bhld,bhd->bhl', q_, nb) + jnp.sum(Sm, -1)
        h = num / jnp.maximum(jnp.abs(den), jnp.exp(-mt))[..., None]
        bl = b_[..., -1]
        ds = bl[..., None] - b_ + it_
        m_new = jnp.maximum(bl + m, jnp.max(ds, -1))
        wk = k_ * jnp.exp(ds - m_new[..., None])[..., None]
        sc = jnp.exp(bl + m - m_new)
        Cb = sc[..., None, None] * Cb + jnp.einsum('bhld,bhle->bhde', wk, v_)
        nb = sc[..., None] * nb + jnp.sum(wk, -2)
        return (Cb, nb, m_new), h

    init = (jnp.zeros((Bsz, H, dk, dv), f32), jnp.zeros((Bsz, H, dk), f32), jnp.zeros((Bsz, H), f32))
    _, h = lax.scan(step, init, (qc, kc, vc, itc, bcum, Dm, dmax, qk))
    h = rms_f32(from_chunks(h), norm_w) * jax.nn.sigmoid(o_pre.reshape(Bsz, S, H, dv).astype(f32))
    return h.reshape(Bsz, S, H * dv).astype(dt)


def nsa(q, k_cmp, v_cmp, k_slc, v_slc, k_win, v_win, gate_pre, q_norm, k_norm, cmp_pe, w_cmp, rel_bias):
    dt = q.dtype
    Bsz, S, _ = q.shape
    H, G, hpg, dh = NSA_HEADS, NSA_GROUPS, NSA_HPG, NSA_DH
    f32 = jnp.float32
    q = rms_f32(q.reshape(Bsz, S, G, hpg, dh), q_norm) * (dh ** -0.5)

    def kv(t):
        return t.reshape(Bsz, S, G, dh).astype(f32)

    n_cmp = (S - CMP_BLOCK) // CMP_STRIDE + 1
    cmp_start = np.arange(n_cmp) * CMP_STRIDE
    cmp_end = cmp_start + CMP_BLOCK - 1
    blk_idx = cmp_start[:, None] + np.arange(CMP_BLOCK)[None]

    def compress(t, pe, w):
        blocks = t[:, blk_idx] + pe.astype(f32)[:, None, :]
        return jnp.einsum('bnlgd,lde->bnge', blocks, w.astype(f32))

    ck = rms_f32(compress(kv(k_cmp), cmp_pe[0], w_cmp[0]), k_norm[0])
    cv = compress(kv(v_cmp), cmp_pe[1], w_cmp[1])
    t_pos = np.arange(S)
    dist_c = (t_pos[:, None] - cmp_end[None]).astype(np.int32)
    valid_c = dist_c >= 0
    bias_c = rel_bias[rel_bucket(jnp.asarray(dist_c))].reshape(S, n_cmp, G, hpg).transpose(2, 3, 0, 1)
    s_c = jnp.einsum('bsgkd,bngd->bgksn', q, ck) + bias_c
    p_c = masked_softmax(s_c, valid_c)
    o_cmp = jnp.einsum('bgksn,bngd->bsgkd', p_c, cv)

    n_sel = S // SEL_BLOCK
    topn = min(SEL_TOPN, n_sel)
    sel_start = np.arange(n_sel) * SEL_BLOCK
    sel_end = sel_start + SEL_BLOCK - 1
    overlap = ((cmp_start[:, None] <= sel_end[None]) & (cmp_end[:, None] >= sel_start[None])).astype(np.float32)
    imp = jnp.einsum('bgksn,nj->bgsj', p_c, jnp.asarray(overlap))
    cur = t_pos // SEL_BLOCK
    jj = np.arange(n_sel)
    forced = (jj[None] == 0) | (jj[None] == cur[:, None])
    causal_blk = jj[None] <= cur[:, None]
    imp = jnp.where(forced, FORCE_SCORE, jnp.where(causal_blk, imp, -1.0))
    _, sel_idx = lax.top_k(imp, topn)

    ks_b = rms_f32(kv(k_slc), k_norm[1]).reshape(Bsz, n_sel, SEL_BLOCK, G, dh).transpose(0, 3, 1, 2, 4)
    vs_b = kv(v_slc).reshape(Bsz, n_sel, SEL_BLOCK, G, dh).transpose(0, 3, 1, 2, 4)
    kw_pad = jnp.pad(rms_f32(kv(k_win), k_norm[2]), ((0, 0), (WINDOW, 0), (0, 0), (0, 0)))
    vw_pad = jnp.pad(kv(v_win), ((0, 0), (WINDOW, 0), (0, 0), (0, 0)))
    nq = S // Q_BLOCK
    q_blocks = q.reshape(Bsz, nq, Q_BLOCK, G, hpg, dh).transpose(1, 0, 2, 3, 4, 5)
    idx_blocks = sel_idx.reshape(Bsz, G, nq, Q_BLOCK, topn).transpose(2, 0, 1, 3, 4)
    rb = rel_bias.reshape(REL_BUCKETS, G, hpg)
    b_ar = jnp.arange(Bsz)[:, None, None, None]
    g_ar = jnp.arange(G)[None, :, None, None]
    Lw = WINDOW + Q_BLOCK

    def block_fn(xs):
        qb, ib, c = xs
        t = c * Q_BLOCK + jnp.arange(Q_BLOCK)
        Ksel = ks_b[b_ar, g_ar, ib].reshape(Bsz, G, Q_BLOCK, topn * SEL_BLOCK, dh)
        Vsel = vs_b[b_ar, g_ar, ib].reshape(Bsz, G, Q_BLOCK, topn * SEL_BLOCK, dh)
        kpos = (ib[..., None] * SEL_BLOCK + jnp.arange(SEL_BLOCK)).reshape(Bsz, G, Q_BLOCK, topn * SEL_BLOCK)
        dist = t[None, None, :, None] - kpos
        bias = rb[rel_bucket(dist), g_ar].transpose(0, 1, 4, 2, 3)
        s = jnp.einsum('bqgkd,bgqnd->bgkqn', qb, Ksel) + bias
        p = masked_softmax(s, (dist >= 0)[:, :, None])
        o_s = jnp.einsum('bgkqn,bgqnd->bqgkd', p, Vsel)
        start = c * Q_BLOCK
        Kw = lax.dynamic_slice_in_dim(kw_pad, start, Lw, axis=1)
        Vw = lax.dynamic_slice_in_dim(vw_pad, start, Lw, axis=1)
        spos = start - WINDOW + jnp.arange(Lw)
        dw = t[:, None] - spos[None]
        valid_w = (dw >= 0) & (dw < WINDOW) & (spos[None] >= 0)
        bias_w = rel_bias[rel_bucket(dw)].reshape(Q_BLOCK, Lw, G, hpg).transpose(2, 3, 0, 1)
        s = jnp.einsum('bqgkd,bngd->bgkqn', qb, Kw) + bias_w
        p = masked_softmax(s, valid_w)
        o_w = jnp.einsum('bgkqn,bngd->bqgkd', p, Vw)
        return o_s, o_w

    o_s, o_w = lax.map(block_fn, (q_blocks, idx_blocks, jnp.arange(nq)))
    o_s = o_s.transpose(1, 0, 2, 3, 4, 5).reshape(Bsz, S, G, hpg, dh)
    o_w = o_w.transpose(1, 0, 2, 3, 4, 5).reshape(Bsz, S, G, hpg, dh)
    gates = jax.nn.sigmoid(gate_pre.astype(f32)).reshape(Bsz, S, G, hpg, 3)
    out = gates[..., 0:1] * o_cmp + gates[..., 1:2] * o_s + gates[..., 2:3] * o_w
    return out.reshape(Bsz, S, H * dh).astype(dt)


def setup_inputs(seed: int = 0) -> dict:
    key = jax.random.key(seed)
    ks = jax.random.split(key, 26)
    f32 = jnp.float32

    def nrm(k, shape, scale):
        return jax.random.normal(k, shape, f32) * scale

    def gain(k, shape):
        return 1.0 + 0.02 * jax.random.normal(k, shape, f32)

    dt_init = jnp.exp(jax.random.uniform(ks[5], (DEPTH, GDN_HEADS), f32, math.log(1e-3), math.log(1e-1)))
    return {
        "x": nrm(ks[0], (BATCH, SEQ, D_MODEL), 1.0),
        "p": nrm(ks[1], (DEPTH, BATCH, SEQ, PLE_DIM), 1.0),
        "rel_bias": nrm(ks[2], (REL_BUCKETS, NSA_HEADS), 0.3),
        "norm_mix": gain(ks[3], (DEPTH, D_MODEL)),
        "w_in": nrm(ks[4], (DEPTH, D_MODEL, D_IN), D_MODEL ** -0.5),
        "conv_w": nrm(ks[6], (DEPTH, GDN_CONV, 2 * GDN_HEADS * GDN_DK + GDN_HEADS * GDN_DV), GDN_CONV ** -0.5),
        "gdn_a_log": jnp.log(jax.random.uniform(ks[7], (DEPTH, GDN_HEADS), f32, 1.0, 16.0)),
        "gdn_dt_bias": dt_init + jnp.log(-jnp.expm1(-dt_init)),
        "gdn_norm": gain(ks[8], (DEPTH, GDN_DV)),
        "mlstm_b_i": nrm(ks[9], (DEPTH, MLSTM_HEADS), 0.1),
        "mlstm_b_f": jnp.linspace(3.0, 6.0, MLSTM_HEADS, dtype=f32)[None] + nrm(ks[10], (DEPTH, MLSTM_HEADS), 0.1),
        "mlstm_norm": gain(ks[11], (DEPTH, MLSTM_DV)),
        "nsa_q_norm": gain(ks[12], (DEPTH, NSA_DH)),
        "nsa_k_norm": gain(ks[13], (DEPTH, 3, NSA_DH)),
        "nsa_cmp_pe": nrm(ks[14], (DEPTH, 2, CMP_BLOCK, NSA_DH), 0.02),
        "nsa_w_cmp": nrm(ks[15], (DEPTH, 2, CMP_BLOCK, NSA_DH, NSA_DH), (CMP_BLOCK * NSA_DH) ** -0.5),
        "w_branch": nrm(ks[16], (DEPTH, N_BRANCH, BRANCH_W, D_MODEL), BRANCH_W ** -0.5),
        "w_out": nrm(ks[17], (DEPTH, D_MODEL, D_MODEL), D_MODEL ** -0.5),
        "norm_ffn": gain(ks[18], (DEPTH, D_MODEL)),
        "w_ffn_in": nrm(ks[19], (DEPTH, D_MODEL, 2 * D_FF), D_MODEL ** -0.5),
        "w_ffn_out": nrm(ks[20], (DEPTH, D_FF, D_MODEL), D_FF ** -0.5),
        "norm_ple": gain(ks[21], (DEPTH, D_MODEL)),
        "w_ple_gate": nrm(ks[22], (DEPTH, D_MODEL, D_MODEL), D_MODEL ** -0.5),
        "w_ple_proj": nrm(ks[23], (DEPTH, PLE_DIM, D_MODEL), PLE_DIM ** -0.5),
    }


def reference(x, p, rel_bias, norm_mix, w_in, conv_w, gdn_a_log, gdn_dt_bias, gdn_norm, mlstm_b_i, mlstm_b_f,
              mlstm_norm, nsa_q_norm, nsa_k_norm, nsa_cmp_pe, nsa_w_cmp, w_branch, w_out, norm_ffn, w_ffn_in,
              w_ffn_out, norm_ple, w_ple_gate, w_ple_proj):
    Bsz, S, D = x.shape
    h = x
    for l in range(DEPTH):
        u = rmsnorm(h, norm_mix[l])
        (aq, ak, av, az, aa, ab,
         bq, bk, bv, bo, bi, bf,
         cq, ckc, cvc, cks, cvs, ckw, cvw, cg, mg) = split_cols(u @ w_in[l], IN_SIZES)
        y_a = gated_deltanet(aq, ak, av, az, aa, ab, conv_w[l], gdn_a_log[l], gdn_dt_bias[l], gdn_norm[l])
        y_b = mlstm(bq, bk, bv, bo, bi, bf, mlstm_b_i[l], mlstm_b_f[l], mlstm_norm[l])
        y_c = nsa(cq, ckc, cvc, cks, cvs, ckw, cvw, cg, nsa_q_norm[l], nsa_k_norm[l], nsa_cmp_pe[l],
                  nsa_w_cmp[l], rel_bias)
        ys = jnp.stack([y_a, y_b, y_c], axis=2)
        br = jnp.einsum('bsnc,ncd->bsnd', ys, w_branch[l])
        gates = jax.nn.sigmoid(mg.astype(jnp.float32)).reshape(Bsz, S, N_BRANCH, D).astype(br.dtype)
        merged = jnp.sum(gates * br, axis=2)
        h = h + merged @ w_out[l]
        u = rmsnorm(h, norm_ffn[l])
        gt, up = jnp.split(u @ w_ffn_in[l], 2, axis=-1)
        h = h + (jax.nn.silu(gt) * up) @ w_ffn_out[l]
        u = rmsnorm(h, norm_ple[l])
        h = h + jax.nn.sigmoid(u @ w_ple_gate[l]) * (p[l] @ w_ple_proj[l])
    return h
```

```python
import numpy as np
import concourse.bass as bass
import concourse.mybir as mybir
from concourse.bass_utils import run_bass_kernel_spmd

F32 = mybir.dt.float32
BF16 = mybir.dt.bfloat16
I32 = mybir.dt.int32
AF = mybir.ActivationFunctionType
ALU = mybir.AluOpType
AX = mybir.AxisListType

N_DMA_SEMS = 48


class V:
    __slots__ = ("buf", "ap")

    def __init__(self, buf, ap):
        self.buf = buf
        self.ap = ap

    def __getitem__(self, idx):
        return V(self.buf, self.ap[idx])

    def r(self, pat, **kw):
        return V(self.buf, self.ap.rearrange(pat, **kw))

    def bc(self, shape):
        return V(self.buf, self.ap.to_broadcast(list(shape)))


class Buf:
    __slots__ = ("ap", "name", "lw", "rd", "excl")

    def __init__(self, ap, name=""):
        self.ap = ap
        self.name = name
        self.lw = None
        self.rd = []
        self.excl = False

    def __getitem__(self, idx):
        return V(self, self.ap[idx])

    def v(self):
        return V(self, self.ap)

    def r(self, pat, **kw):
        return V(self, self.ap.rearrange(pat, **kw))

    def sub(self, idx, name=""):
        return Buf(self.ap[idx], name or self.name)


def _ap(x):
    return x.ap if isinstance(x, V) else x


class Prog:
    ENGS = ("pe", "dve", "act", "pool", "sp")

    def __init__(self, nc):
        self.nc = nc
        self.ops = []
        self.eng = {"pe": nc.tensor, "dve": nc.vector, "act": nc.scalar, "pool": nc.gpsimd, "sp": nc.sync}
        self._ctx = []
        self.all_bufs = []
        self.last_barrier = None
        self._uid = 0

    def _nm(self, name):
        self._uid += 1
        return "%s_%d" % (name, self._uid)

    def sb(self, name, shape, dt=F32):
        g = self.nc.sbuf_tensor(self._nm(name), list(shape), dt)
        t = g.__enter__()
        self._ctx.append(g)
        b = Buf(t.ap() if hasattr(t, "ap") else t[:], name)
        self.all_bufs.append(b)
        return b

    def ps(self, name, shape, dt=F32):
        g = self.nc.psum_tensor(self._nm(name), list(shape), dt)
        t = g.__enter__()
        self._ctx.append(g)
        b = Buf(t.ap() if hasattr(t, "ap") else t[:], name)
        b.excl = True
        self.all_bufs.append(b)
        return b

    def dram(self, name, shape, dt=F32, kind="Internal"):
        t = self.nc.dram_tensor(name, list(shape), dt, kind=kind).ap()
        b = Buf(t, name)
        self.all_bufs.append(b)
        return b

    def track(self, b):
        self.all_bufs.append(b)
        return b

    def mark(self):
        return len(self._ctx)

    def release(self, mark):
        self.barrier()
        while len(self._ctx) > mark:
            g = self._ctx.pop()
            g.__exit__(None, None, None)

    def op(self, eng, fn, reads=(), writes=()):
        self.ops.append((eng, fn, tuple(reads), tuple(writes), False, self.last_barrier))

    def barrier(self):
        idx = len(self.ops)
        self.ops.append(("sp", lambda e: e.nop(), (), (), "bar", self.last_barrier))
        self.last_barrier = idx

    def dma(self, eng, out, in_, **kw):
        oa, ia = _ap(out), _ap(in_)

        def fn(e, oa=oa, ia=ia, kw=kw):
            return e.dma_start(out=oa, in_=ia, **kw)
        self.ops.append((eng, fn, (in_.buf,), (out.buf,), True, self.last_barrier))

    @staticmethod
    def _rw(outs, ins):
        w = [o.buf for o in outs if isinstance(o, V)]
        r = [i.buf for i in ins if isinstance(i, V)]
        w += [b for b in r if b.excl and b not in w]
        return r, w

    def activation(self, out, in_, func, bias=0.0, scale=1.0, accum_out=None, eng="act"):
        r, w = self._rw([out, accum_out], [in_, bias, scale])
        kw = dict(out=_ap(out), in_=_ap(in_), func=func, bias=_ap(bias), scale=_ap(scale))
        if accum_out is not None:
            kw["accum_out"] = _ap(accum_out)
        self.op(eng, lambda e, kw=kw: e.activation(**kw), r, w)

    def tt(self, out, in0, in1, op, eng="dve"):
        r, w = self._rw([out], [in0, in1])
        kw = dict(out=_ap(out), in0=_ap(in0), in1=_ap(in1), op=op)
        self.op(eng, lambda e, kw=kw: e.tensor_tensor(**kw), r, w)

    def ts(self, out, in0, s1, op0, s2=None, op1=None, accum_out=None, eng="dve"):
        r, w = self._rw([out, accum_out], [in0, s1, s2])
        kw = dict(out=_ap(out), in0=_ap(in0), scalar1=_ap(s1), scalar2=_ap(s2), op0=op0)
        if op1 is not None:
            kw["op1"] = op1
        if accum_out is not None:
            kw["accum_out"] = _ap(accum_out)
        self.op(eng, lambda e, kw=kw: e.tensor_scalar(**kw), r, w)

    def stt(self, out, in0, scalar, in1, op0, op1, eng="dve"):
        r, w = self._rw([out], [in0, scalar, in1])
        kw = dict(out=_ap(out), in0=_ap(in0), scalar=_ap(scalar), in1=_ap(in1), op0=op0, op1=op1)
        self.op("dve", lambda e, kw=kw: e.scalar_tensor_tensor(**kw), r, w)

    def copy(self, out, in_, eng="dve"):
        r, w = self._rw([out], [in_])
        oa, ia = _ap(out), _ap(in_)
        if eng == "act":
            self.op(eng, lambda e: e.copy(out=oa, in_=ia), r, w)
        else:
            self.op(eng, lambda e: e.tensor_copy(out=oa, in_=ia), r, w)

    def memset(self, out, val, eng="pool"):
        r, w = self._rw([out], [])
        oa = _ap(out)
        self.op(eng, lambda e: e.memset(oa, val), r, w)

    def reduce(self, out, in_, op, axis=AX.X, eng="dve"):
        r, w = self._rw([out], [in_])
        oa, ia = _ap(out), _ap(in_)
        self.op(eng, lambda e: e.tensor_reduce(out=oa, in_=ia, axis=axis, op=op), r, w)

    def recip(self, out, in_):
        r, w = self._rw([out], [in_])
        oa, ia = _ap(out), _ap(in_)
        self.op("dve", lambda e: e.reciprocal(out=oa, in_=ia), r, w)

    def mm(self, out, lhsT, rhs, start=True, stop=True, skip=False):
        r, w = self._rw([out], [lhsT, rhs])
        oa, la, ra = _ap(out), _ap(lhsT), _ap(rhs)
        if skip:
            self.op("pe", lambda e: e.matmul(oa, la, ra, start=start, stop=stop, skip_group_check=True), r, w)
        else:
            self.op("pe", lambda e: e.matmul(oa, la, ra, start=start, stop=stop), r, w)

    def tr(self, out, in_, ident):
        r, w = self._rw([out], [in_, ident])
        oa, ia, da = _ap(out), _ap(in_), _ap(ident)
        self.op("pe", lambda e: e.transpose(oa, ia, da), r, w)

    def emit(self):
        nc = self.nc
        ops = self.ops
        n = len(ops)
        deps = [None] * n
        signal = [False] * n
        last_on = {}
        dmas_since = []
        for i, (eng, fn, reads, writes, is_dma, bar) in enumerate(ops):
            d = set()
            if bar is not None:
                d.add(bar)
            if is_dma == "bar":
                d.update(last_on.values())
                d.update(dmas_since)
                dmas_since = []
                is_dma = False
            elif is_dma:
                dmas_since.append(i)
            last_on[eng] = i
            for r in reads:
                if r.lw is not None:
                    d.add(r.lw)
            for w in writes:
                if w.lw is not None:
                    j = w.lw
                    if is_dma or ops[j][4] or ops[j][0] != eng:
                        d.add(j)
                for j in w.rd:
                    if is_dma or ops[j][4] or ops[j][0] != eng:
                        d.add(j)
            d.discard(i)
            if eng == "pe":
                d = {j for j in d if ops[j][0] != "pe" or ops[j][4]}
            for r in reads:
                r.rd.append(i)
            for w in writes:
                w.lw = i
                w.rd = []
            best = {}
            dd = set()
            for j in d:
                if ops[j][4] is True:
                    dd.add(j)
                else:
                    e2 = ops[j][0]
                    if e2 not in best or best[e2] < j:
                        best[e2] = j
            dd.update(best.values())
            deps[i] = dd
            for j in dd:
                signal[j] = True
        sems = {}
        for e in self.ENGS:
            g = nc.semaphore("s_" + e)
            sems[e] = g.__enter__()
            self._ctx.append(g)
        dsems = []
        for k in range(N_DMA_SEMS):
            g = nc.semaphore("d_%d" % k)
            dsems.append(g.__enter__())
            self._ctx.append(g)
        cnt = {e: 0 for e in self.ENGS}
        dcnt = [0] * N_DMA_SEMS
        ev = [None] * n
        dnext = 0
        dma_prev = [None] * N_DMA_SEMS
        dma_guard = [None] * n
        for i, (eng, fn, reads, writes, is_dma, bar) in enumerate(ops):
            if is_dma is True:
                k = dnext
                dnext = (dnext + 1) % N_DMA_SEMS
                dma_guard[i] = dma_prev[k]
                dcnt[k] += 16
                ev[i] = ("d", k, dcnt[k])
                dma_prev[k] = ev[i]
            elif signal[i]:
                cnt[eng] += 1
                ev[i] = ("c", eng, cnt[eng])
        waited = {e: {} for e in self.ENGS}
        nwaits = 0
        last_dma = {}
        for i, (eng, fn, reads, writes, is_dma, bar) in enumerate(ops):
            need = {}
            for j in deps[i]:
                kind, key, val = ev[j]
                if need.get((kind, key), 0) < val:
                    need[(kind, key)] = val
            if is_dma is True and dma_guard[i] is not None:
                kind, key, val = dma_guard[i]
                if need.get((kind, key), 0) < val:
                    need[(kind, key)] = val
            w = waited[eng]
            for (kind, key), val in need.items():
                if w.get((kind, key), 0) >= val:
                    continue
                w[(kind, key)] = val
                s = sems[key] if kind == "c" else dsems[key]
                self.eng[eng].wait_ge(s, val)
                nwaits += 1
            ins = fn(self.eng[eng])
            if ev[i] is not None:
                kind, key, val = ev[i]
                if kind == "c":
                    ins.then_inc(sems[key], 1)
                else:
                    ins.then_inc(dsems[key], 16)
                    last_dma[key] = val
        w = waited["sp"]
        for k, val in last_dma.items():
            if w.get(("d", k), 0) < val:
                self.eng["sp"].wait_ge(dsems[k], val)
        self.stats = dict(n_ops=n, n_waits=nwaits, cnt=dict(cnt))
        return self.stats

    def close(self):
        while self._ctx:
            g = self._ctx.pop()
            g.__exit__(None, None, None)


S = 2048
D = 1024
NT = S // 128
DEPTH = 2
NSEQ = 2
D_IN = 8488
D_FF = 2816
EPS = 1e-6
C_AQ, C_AK, C_AV, C_AZ, C_AA, C_AB = 0, 512, 1024, 1536, 2048, 2052
C_BQ, C_BK, C_BV, C_BO, C_BI, C_BF = 2056, 2568, 3080, 3592, 4104, 4108
C_CQ, C_CKC, C_CVC, C_CKS, C_CVS, C_CKW, C_CVW, C_CG, C_MG = 4112, 4624, 4752, 4880, 5008, 5136, 5264, 5392, 5416


TM_RANGES = [(C_AZ, 520), (C_BK, 512), (C_BV, 1032), (C_CQ, 1304)]
TM_OFF = {}
_o = 0
for _c, _w in TM_RANGES:
    TM_OFF[_c] = _o
    _o += _w
TM_W = _o


def tm_col(c):
    for c0, w in TM_RANGES:
        if c0 <= c < c0 + w:
            return TM_OFF[c0] + (c - c0)
    raise KeyError(c)


FM_RANGES = [(C_AQ, 1536), (C_BQ, 1024)]
FM_OFF = {C_AQ: 0, C_BQ: 1536}


class Ctx:
    pass


CM_TRI, CM_NEGM, CM_STRICT, CM_SEL127, CM_IDENT, CM_ONES = range(6)
NCM = 6
SM_ALOG, SM_DTB, SM_BI, SM_BF, SM_GDNN, SM_MLN, SM_QN, SM_KN = 0, 4, 8, 12, 16, 144, 272, 336
NSMALL = 336 + 192


def declare(P, dbg=()):
    nc = P.nc
    C = Ctx()

    def inp(name, shape, dt=F32):
        return P.track(Buf(nc.dram_tensor(name, list(shape), dt, kind="ExternalInput").ap(), name))

    C.x = inp("x", [NSEQ, S, D])
    C.pT = inp("pT", [DEPTH, NSEQ, 256, S])
    C.w_in = inp("w_in", [DEPTH, D, D_IN])
    C.w_branch = inp("w_branch", [DEPTH, 3, 512, D])
    C.w_out = inp("w_out", [DEPTH, D, D])
    C.w_ffn_in = inp("w_ffn_in", [DEPTH, D, 2 * D_FF])
    C.w_ffn_out = inp("w_ffn_out", [DEPTH, D_FF, D])
    C.w_ple_gate = inp("w_ple_gate", [DEPTH, D, D])
    C.w_ple_proj = inp("w_ple_proj", [DEPTH, 256, D])
    C.gains = inp("gains", [DEPTH, 3, 128, 8])
    C.ident = inp("ident", [128, 128])
    C.cmat = inp("cmat", [128, NCM, 128])
    C.small = inp("small", [DEPTH, 128, NSMALL])
    C.cw = inp("cw", [DEPTH, 128, 12, 4])
    C.tbls = inp("tbls", [4, 128, 8, 128])
    C.biasC = inp("biasC", [NT, 2, 128, 4, 128])
    C.selc = inp("selc", [128, NT, 2, 32])
    C.ovm = inp("ovm", [128, 32])
    C.rselc = inp("rselc", [32, NT, 128])
    C.wcmp = inp("wcmp", [DEPTH, 64, 2, 32, 64])
    C.peT = inp("peT", [DEPTH, 64, 2, 32])
    C.kn0 = inp("kn0", [DEPTH, 64, 1])
    C.out = P.track(Buf(nc.dram_tensor("out", [NSEQ, S, D], F32, kind="ExternalOutput").ap(), "out"))

    C.xt = [[C.x.sub((q, slice(t * 128, (t + 1) * 128), slice(None)), "x%d_%d" % (q, t)) for t in range(NT)] for q in range(NSEQ)]
    C.ot = [[C.out.sub((q, slice(t * 128, (t + 1) * 128), slice(None)), "o%d_%d" % (q, t)) for t in range(NT)] for q in range(NSEQ)]

    def scr(name, shape, dt=F32):
        kind = "ExternalOutput" if name in dbg else "Internal"
        return P.track(Buf(nc.dram_tensor(name, list(shape), dt, kind=kind).ap(), name))

    C.tm = scr("tm_scr", [S, TM_W])
    C.fm = scr("fm_scr", [2560, S])
    if "y" in dbg:
        C.ydbg = P.track(Buf(nc.dram_tensor("ydbg", [128, 12, S], F32, kind="ExternalOutput").ap(), "ydbg"))
    return C


def setup_consts(P, C):
    K = Ctx()
    K.identf = P.sb("identf", [128, 128])
    K.identb = P.sb("identb", [128, 128], BF16)
    P.dma("sp", K.identf.v(), C.ident.v())
    P.copy(K.identb.v(), K.identf.v())
    K.cm = P.sb("cmat", [128, NCM, 128])
    P.dma("sp", K.cm.v(), C.cmat.v())
    K.cm4 = P.sb("cmat4", [128, 3, 4, 128])
    for i, cmi in enumerate((CM_NEGM, CM_STRICT, CM_IDENT)):
        for h in range(4):
            P.copy(K.cm4[:, i, h, :], K.cm[:, cmi, :], eng="pool")
    K.small = P.sb("small", [128, DEPTH, NSMALL])
    P.dma("sp", K.small.v(), C.small.r("l p n -> p l n"))
    K.cw = P.sb("cw", [128, DEPTH, 12, 4])
    P.dma("sp", K.cw.v(), C.cw.r("l p b k -> p l b k"))
    K.tbl = P.sb("tbl", [128, 4, 8, 128], BF16)
    K.rsel = P.sb("rsel", [32, NT, 128], BF16)
    K.selc = P.sb("selc", [128, NT, 2, 32])
    mt = P.mark()
    tblf = P.sb("tblf", [128, 4, 8, 128])
    P.dma("sp", tblf.v(), C.tbls.r("a j h i -> j a h i"))
    P.copy(K.tbl.v(), tblf.v(), eng="pool")
    rself = P.sb("rself", [32, NT, 128])
    P.dma("sp", rself.v(), C.rselc.v())
    P.copy(K.rsel.v(), rself.v(), eng="pool")
    P.dma("sp", K.selc.v(), C.selc.v())
    P.release(mt)
    K.gains = P.sb("gains", [128, DEPTH, 3, 8])
    P.dma("sp", K.gains.v(), C.gains.r("l n p k -> p l n k"))
    return K


def norm_transpose(P, K, hsrc, uT, wq="sp"):
    m = P.mark()
    hA = [P.sb("hA", [128, D]) for _ in range(2)]
    junk = P.sb("junkA", [128, D], BF16)
    ub = [P.sb("ub", [128, D], BF16) for _ in range(2)]
    ss = P.sb("ssA", [128, NT])
    rs = P.sb("rsA", [128, NT])
    pT = [P.ps("pTA", [128, 8, 128], BF16) for _ in range(2)]
    for t in range(NT):
        b = t % 2
        P.dma(wq, hA[b].v(), hsrc[t].v())
        sst = ss.sub((slice(None), slice(t, t + 1)))
        rst = rs.sub((slice(None), slice(t, t + 1)))
        P.activation(junk.v(), hA[b].v(), AF.Square, accum_out=sst.v())
        P.activation(rst.v(), sst.v(), AF.Sqrt, bias=K.epsD.v(), scale=1.0 / D)
        P.recip(rst.v(), rst.v())
        P.ts(ub[b].v(), hA[b].v(), rst.v(), ALU.mult)
        for kc in range(8):
            P.tr(pT[b][:, kc, :], ub[b][:, kc * 128:(kc + 1) * 128], K.identb.v())
        if t % 2 == 0:
            P.copy(uT[:, :, t * 128:(t + 1) * 128], pT[b].v(), eng="act")
        else:
            P.copy(uT[:, :, t * 128:(t + 1) * 128], pT[b].v(), eng="dve")
    P.release(m)


class WStream:
    def __init__(self, P, nk, width, nstage=2, nbuf=2):
        self.P = P
        self.nk = nk
        self.width = width
        self.wf = [P.sb("wf", [128, nk, width]) for _ in range(nstage)]
        self.wb = [P.sb("wb", [128, nk, width], BF16) for _ in range(nbuf)]
        self.i = 0
        self.j = 0
        self.ceng = 0

    def load(self, wsrc_v, ncols, gain=None, nk=None):
        P = self.P
        nk = nk or self.nk
        wf = self.wf[self.i % len(self.wf)]
        wb = self.wb[self.j % len(self.wb)]
        self.i += 1
        self.j += 1
        P.dma("sp", wf[:, :nk, :ncols], wsrc_v.r("(k p) c -> p k c", p=128))
        for kc in range(nk):
            eng = ("pool", "dve")[self.ceng % 2]
            self.ceng += 1
            if gain is not None:
                P.ts(wb[:, kc, :ncols], wf[:, kc, :ncols], gain[:, kc:kc + 1], ALU.mult, eng=eng)
            else:
                P.copy(wb[:, kc, :ncols], wf[:, kc, :ncols], eng=eng)
        return wb[:, :nk, :ncols]


def phase_A(P, C, K, l, s, hsrc, uT):
    norm_transpose(P, K, hsrc, uT)
    m = P.mark()
    gain = K.gains[:, l, 0, :]
    ws = WStream(P, 8, 512)
    w_in = C.w_in[l]
    pfm = [P.ps("pfm", [128, 512]) for _ in range(2)]
    sfm = [P.sb("sfm", [128, S]) for _ in range(2)]
    blk = 0
    for c0, w in FM_RANGES:
        for cb in range(0, w, 512):
            wb = ws.load(w_in[:, c0 + cb:c0 + cb + 512], 512, gain)
            for j in range(4):
                st = sfm[blk % 2]
                for tg in range(4):
                    ps = pfm[tg % 2]
                    for kc in range(8):
                        P.mm(ps.v(), wb[:, kc, j * 128:(j + 1) * 128], uT[:, kc, tg * 512:(tg + 1) * 512],
                             start=(kc == 0), stop=(kc == 7))
                    P.copy(st[:, tg * 512:(tg + 1) * 512], ps.v(), eng=("act", "dve")[tg % 2])
                r0 = FM_OFF[c0] + cb + j * 128
                P.dma("pool", C.fm[r0:r0 + 128, :], st.v())
                blk += 1
    ptm = [P.ps("ptm", [128, 512]) for _ in range(2)]
    stm = [P.sb("stm", [128, 512]) for _ in range(2)]
    it = 0
    for c0, w in TM_RANGES:
        for cb in range(0, w, 512):
            nc_ = min(512, w - cb)
            wb = ws.load(w_in[:, c0 + cb:c0 + cb + nc_], nc_, gain)
            for t in range(NT):
                ps = ptm[it % 2]
                st = stm[it % 2]
                for kc in range(8):
                    P.mm(ps[:, :nc_], uT[:, kc, t * 128:(t + 1) * 128], wb[:, kc, :], start=(kc == 0), stop=(kc == 7))
                P.copy(st[:, :nc_], ps[:, :nc_], eng=("act", "dve")[it % 2])
                o = TM_OFF[c0] + cb
                P.dma("pool", C.tm[t * 128:(t + 1) * 128, o:o + nc_], st[:, :nc_])
                it += 1
    P.release(m)


STOPB = 0


class Banks:
    def __init__(self, P, n=8):
        self.t = [P.ps("bank", [128, 4, 128]) for _ in range(n)]
        self.i = 0

    def nxt(self):
        b = self.t[self.i % len(self.t)]
        self.i += 1
        return b


class RR:
    def __init__(self, engs):
        self.engs = engs
        self.i = 0

    def __call__(self):
        e = self.engs[self.i % len(self.engs)]
        self.i += 1
        return e


def scale_cols(P, out, in_, sc, eng):
    if eng == "act":
        P.activation(out, in_, AF.Copy, scale=sc)
    else:
        P.ts(out, in_, sc, ALU.mult, eng=eng)


def out_stage_bufs(P):
    OS = Ctx()
    OS.osq = P.sb("osq", [128, 4, 128])
    OS.oss = P.sb("oss", [128, 4])
    OS.zt = P.sb("zt", [128, 4, 128])
    return OS


def out_stage(P, C, K, bk, OS, osb, gcol, gfunc, gainB, yT, yoff, cg):
    zt, osq, oss = OS.zt, OS.osq, OS.oss
    P.dma("sp", zt.r("p h d -> p (h d)"), C.tm[cg * 128:(cg + 1) * 128, gcol:gcol + 512])
    P.activation(zt.v(), zt.v(), gfunc)
    P.activation(osq.v(), osb.v(), AF.Square)
    P.reduce(oss.v(), osq.v(), ALU.add)
    P.activation(oss.v(), oss.v(), AF.Sqrt, bias=K.epsD.v(), scale=1.0 / 128)
    P.recip(oss.v(), oss.v())
    for h in range(4):
        P.stt(osb[:, h, :], osb[:, h, :], oss[:, h:h + 1], gainB, ALU.mult, ALU.mult)
    P.tt(osb.v(), osb.v(), zt.v(), ALU.mult, eng="pool")
    y_ps = bk.nxt()
    for h in range(4):
        P.tr(y_ps[:, h, :], osb[:, h, :], K.identf.v())
    P.copy(yT[:, yoff:yoff + 4, cg * 128:(cg + 1) * 128], y_ps.v(), eng="act")


def phase_C(P, C, K, l, s, yT):
    m = P.mark()
    sm = K.small
    cm = K.cm
    NEGM4 = K.cm4[:, 0]
    bk = Banks(P, 8)
    flat = lambda b: b.r("p a b -> p (a b)")
    ext = lambda b, hh: flat(b)[:, 0:258].r("p (a b) -> p a b", b=129)[:, hh, :]
    gi = P.sb("gi", [128, NT, 8])
    ci = tm_col(C_BI)
    for c in range(NT):
        P.dma("sp", gi[:, c, :], C.tm[c * 128:(c + 1) * 128, ci:ci + 8])
    it = P.sb("it", [128, NT, 4])
    lf = P.sb("lf", [128, NT, 4])
    nbf = P.sb("nbf", [128, 4])
    P.ts(nbf.v(), sm[:, l, SM_BF:SM_BF + 4], -1.0, ALU.mult)
    for h in range(4):
        P.ts(it[:, :, h], gi[:, :, h], sm[:, l, SM_BI + h:SM_BI + h + 1], ALU.add)
        P.activation(lf[:, :, h], gi[:, :, 4 + h], AF.Exp, bias=nbf[:, h:h + 1], scale=-1.0)
    P.activation(lf.v(), lf.v(), AF.Ln, bias=1.0)
    P.ts(lf.v(), lf.v(), -1.0, ALU.mult)
    G1 = P.sb("G1", [128, NT, 4, 2])
    G2 = P.sb("G2", [128, NT, 4, 2])
    G3 = P.sb("G3", [128, NT, 4, 2])
    P.memset(G1.v(), 0.0)
    P.memset(G2.v(), 0.0)
    P.memset(G3.v(), 0.0)
    P.memset(G1[0:1, :, :, 1], 1.0)
    P.memset(G2[0:1, :, :, 0], 1.0)
    P.copy(G1[:, :, :, 0], lf.v())
    P.ts(G2[:, :, :, 1], lf.v(), -1.0, ALU.mult)
    P.copy(G3[:, :, :, 1], it.v())
    bc = P.sb("bc", [128, NT, 4])
    f64 = lambda b: b.r("p c h -> p (c h)")
    b0 = bk.nxt()
    P.mm(flat(b0)[:, 0:64], cm[:, CM_TRI, :], f64(lf))
    P.copy(f64(bc), flat(b0)[:, 0:64])
    b1 = bk.nxt()
    P.mm(flat(b1)[:, 0:64], cm[:, CM_SEL127, :], f64(bc))
    EBL = P.sb("EBL", [128, NT, 4])
    EKW = P.sb("EKW", [128, NT, 4])
    EB = P.sb("EB", [128, NT, 4])
    P.activation(f64(EBL), flat(b1)[:, 0:64], AF.Exp)
    P.tt(f64(EKW), flat(b1)[:, 0:64], f64(bc), ALU.subtract)
    P.tt(EKW.v(), EKW.v(), it.v(), ALU.add)
    P.activation(EKW.v(), EKW.v(), AF.Exp)
    P.activation(EB.v(), bc.v(), AF.Exp)
    qk = [P.sb("qk", [128, S]) for _ in range(8)]
    for blk in range(8):
        r0 = FM_OFF[C_BQ] + blk * 128
        P.dma("sp", qk[blk].v(), C.fm[r0:r0 + 128, :])
        if blk < 4:
            P.activation(qk[blk].v(), qk[blk].v(), AF.Copy, scale=128 ** -0.5)
    qT, kT = qk[0:4], qk[4:8]
    Cst = P.sb("Cst", [128, 4, 129])
    P.memset(Cst.v(), 0.0)
    k_tm = [P.sb("k_tm", [128, 4, 128]) for _ in range(2)]
    v_ext = [P.sb("v_ext", [128, 4, 129]) for _ in range(2)]
    for b in range(2):
        P.memset(v_ext[b][:, :, 128:129], 1.0)
    kw_tm = P.sb("kw_tm", [128, 4, 128])
    R1 = P.sb("R1", [2, 4, 128])
    R2 = P.sb("R2", [2, 4, 128])
    DT = P.sb("DT", [128, 4, 128])
    SmT = P.sb("SmT", [128, 4, 128])
    htmp = P.sb("htmp", [128, 4, 129])
    htot = P.sb("htot", [128, 4, 129])
    rden = P.sb("rden", [128, 4])
    osb = P.sb("osb", [128, 4, 128])
    OS = out_stage_bufs(P)
    gainB = sm[:, l, SM_MLN:SM_MLN + 128]
    ck, cv = tm_col(C_BK), tm_col(C_BV)
    for cg in range(NT):
        o = slice(cg * 128, (cg + 1) * 128)
        kt, ve = k_tm[cg % 2], v_ext[cg % 2]
        P.dma("sp", kt.r("p h d -> p (h d)"), C.tm[o, ck:ck + 512])
        P.dma("sp", ve[:, :, 0:128], C.tm[o, cv:cv + 512].r("p (h d) -> p h d", d=128))
        r_ps = bk.nxt()
        r2_ps = bk.nxt()
        for h in range(4):
            P.mm(r_ps[0:2, h, :], G1[:, cg, h, :], cm[:, CM_TRI, :])
        for h in range(4):
            P.mm(r2_ps[0:2, h, :], G2[:, cg, h, :], cm[:, CM_TRI, :], start=True, stop=False)
            P.mm(r2_ps[0:2, h, :], G3[:, cg, h, :], cm[:, CM_IDENT, :], start=False, stop=True)
        P.copy(R1.v(), r_ps[0:2, :, :], eng="dve")
        P.copy(R2.v(), r2_ps[0:2, :, :], eng="act")
        dt_ps = bk.nxt()
        for h in range(4):
            P.mm(dt_ps[:, h, :], R2[:, h, :], R1[:, h, :])
        P.tt(DT.v(), dt_ps.v(), NEGM4, ALU.add)
        P.activation(DT.v(), DT.v(), AF.Exp)
        kq_ps = bk.nxt()
        for h in range(4):
            P.mm(kq_ps[:, h, :], kT[h][:, o], qT[h][:, o])
        P.tt(SmT.v(), kq_ps.v(), DT.v(), ALU.mult)
        hq = [bk.nxt(), bk.nxt()]
        hs = [bk.nxt(), bk.nxt()]
        for h in range(4):
            P.mm(ext(hq[h // 2], h % 2), qT[h][:, o], Cst[:, h, :])
        for h in range(4):
            P.mm(ext(hs[h // 2], h % 2), SmT[:, h, :], ve[:, h, :])
        for h in range(4):
            P.activation(htmp[:, h, :], ext(hq[h // 2], h % 2), AF.Copy, scale=EB[:, cg, h:h + 1])
        for h in range(4):
            P.tt(htot[:, h, :], htmp[:, h, :], ext(hs[h // 2], h % 2), ALU.add)
        P.ts(rden.v(), htot[:, :, 128], -1.0, ALU.mult)
        P.tt(rden.v(), rden.v(), htot[:, :, 128], ALU.max)
        P.ts(rden.v(), rden.v(), 1.0, ALU.max)
        P.recip(rden.v(), rden.v())
        for h in range(4):
            P.ts(osb[:, h, :], htot[:, h, 0:128], rden[:, h:h + 1], ALU.mult, eng=("dve", "pool")[h % 2])
        for h in range(4):
            P.ts(kw_tm[:, h, :], kt[:, h, :], EKW[:, cg, h:h + 1], ALU.mult, eng=("pool", "dve")[h % 2])
        cps = [bk.nxt(), bk.nxt()]
        for h in range(4):
            P.mm(ext(cps[h // 2], h % 2), kw_tm[:, h, :], ve[:, h, :])
        for h in range(4):
            P.stt(Cst[:, h, :], Cst[:, h, :], EBL[:, cg, h:h + 1], ext(cps[h // 2], h % 2), ALU.mult, ALU.add)
        out_stage(P, C, K, bk, OS, osb, tm_col(C_BO), AF.Sigmoid, gainB, yT, 4, cg)
    P.release(m)


def phase_B(P, C, K, l, s, yT):
    m = P.mark()
    sm = K.small
    cm = K.cm
    NEGM4, STRICT4, IDENT4 = K.cm4[:, 0], K.cm4[:, 1], K.cm4[:, 2]
    bk = Banks(P, 8)
    rr = RR(("dve", "act", "pool"))
    rr2 = RR(("dve", "act"))
    ab = P.sb("ab", [128, NT, 8])
    ca = tm_col(C_AA)
    for c in range(NT):
        P.dma("sp", ab[:, c, :], C.tm[c * 128:(c + 1) * 128, ca:ca + 8])
    e1 = P.sb("e1", [128, NT, 4])
    g = P.sb("g", [128, NT, 4])
    nega = P.sb("nega", [128, 4])
    P.activation(nega.v(), sm[:, l, SM_ALOG:SM_ALOG + 4], AF.Exp)
    P.ts(nega.v(), nega.v(), -1.0, ALU.mult)
    for h in range(4):
        P.activation(e1[:, :, h], ab[:, :, h], AF.Exp, bias=sm[:, l, SM_DTB + h:SM_DTB + h + 1])
    P.activation(e1.v(), e1.v(), AF.Ln, bias=1.0)
    for h in range(4):
        P.ts(g[:, :, h], e1[:, :, h], nega[:, h:h + 1], ALU.mult)
    beta = P.sb("beta", [128, NT, 4])
    P.activation(beta.v(), ab[:, :, 4:8], AF.Sigmoid)
    nbeta = P.sb("nbeta", [128, NT, 4])
    P.ts(nbeta.v(), beta.v(), -1.0, ALU.mult)
    G1 = P.sb("G1", [128, NT, 4, 2])
    G2 = P.sb("G2", [128, NT, 4, 2])
    P.memset(G1.v(), 0.0)
    P.memset(G2.v(), 0.0)
    P.memset(G1[0:1, :, :, 1], 1.0)
    P.memset(G2[0:1, :, :, 0], 1.0)
    P.copy(G1[:, :, :, 0], g.v())
    P.ts(G2[:, :, :, 1], g.v(), -1.0, ALU.mult)
    gc = P.sb("gc", [128, NT, 4])
    b0 = bk.nxt()
    P.mm(b0.r("p a b -> p (a b)")[:, 0:64], cm[:, CM_TRI, :], g.r("p c h -> p (c h)"))
    P.copy(gc.r("p c h -> p (c h)"), b0.r("p a b -> p (a b)")[:, 0:64])
    b1 = bk.nxt()
    P.mm(b1.r("p a b -> p (a b)")[:, 0:64], cm[:, CM_SEL127, :], gc.r("p c h -> p (c h)"))
    EGL = P.sb("EGL", [128, NT, 4])
    ED = P.sb("ED", [128, NT, 4])
    EG = P.sb("EG", [128, NT, 4])
    P.activation(EGL.r("p c h -> p (c h)"), b1.r("p a b -> p (a b)")[:, 0:64], AF.Exp)
    P.tt(ED.r("p c h -> p (c h)"), b1.r("p a b -> p (a b)")[:, 0:64], gc.r("p c h -> p (c h)"), ALU.subtract)
    P.activation(ED.v(), ED.v(), AF.Exp)
    P.activation(EG.v(), gc.v(), AF.Exp)
    if STOPB == 1:
        P.release(m)
        return
    Sst = P.sb("Sst", [128, 4, 128])
    P.memset(Sst.v(), 0.0)
    HW = 1024
    qkv = [P.sb("qkv", [128, HW]) for _ in range(12)]
    raw = [P.sb("raw", [128, HW + 3]) for _ in range(2)]
    sqt = P.sb("sqt", [128, HW])
    rnt = P.sb("rnt", [128, 512])
    gainB = sm[:, l, SM_GDNN:SM_GDNN + 128]
    for hf in range(2):
        t0 = hf * HW
        for blk in range(12):
            r = raw[blk % 2]
            r0 = FM_OFF[C_AQ] + blk * 128
            if hf == 0:
                P.memset(r[:, 0:3], 0.0)
                P.dma("sp", r[:, 3:], C.fm[r0:r0 + 128, 0:HW])
            else:
                P.dma("sp", r.v(), C.fm[r0:r0 + 128, t0 - 3:t0 + HW])
            dst = qkv[blk]
            e = ("dve", "pool")[blk % 2]
            P.ts(dst.v(), r[:, 0:HW], K.cw[:, l, blk, 0:1], ALU.mult, eng=e)
            for k in range(1, 4):
                P.stt(dst.v(), r[:, k:k + HW], K.cw[:, l, blk, k:k + 1], dst.v(), ALU.mult, ALU.add, eng=e)
            P.activation(dst.v(), dst.v(), AF.Silu)
            if blk < 8:
                P.activation(sqt.v(), dst.v(), AF.Square)
                for hh in range(2):
                    bb = bk.nxt()
                    bbv = bb.r("p a b -> p (a b)")
                    P.mm(bbv, cm[:, CM_ONES, :], sqt[:, hh * 512:(hh + 1) * 512])
                    P.activation(rnt.v(), bbv, AF.Sqrt, bias=K.eps6.v())
                    P.recip(rnt.v(), rnt.v())
                    sc = (128 ** -0.5) if blk < 4 else 1.0
                    P.stt(dst[:, hh * 512:(hh + 1) * 512], dst[:, hh * 512:(hh + 1) * 512], sc, rnt.v(), ALU.mult, ALU.mult)
        if STOPB == 2:
            P.release(m)
            return
        qT = qkv[0:4]
        kT = qkv[4:8]
        vT = qkv[8:12]
        for c in range(8):
            cg = hf * 8 + c
            o = slice(c * 128, (c + 1) * 128)
            kt_ps = bk.nxt()
            vt_ps = bk.nxt()
            for h in range(4):
                P.tr(kt_ps[:, h, :], kT[h][:, o], K.identf.v())
                P.tr(vt_ps[:, h, :], vT[h][:, o], K.identf.v())
            v_tm = P.sb("v_tm", [128, 4, 128]) if c == 0 and hf == 0 else v_tm
            kg_tm = P.sb("kg_tm", [128, 4, 128]) if c == 0 and hf == 0 else kg_tm
            kd_tm = P.sb("kd_tm", [128, 4, 128]) if c == 0 and hf == 0 else kd_tm
            P.copy(v_tm.v(), vt_ps.v(), eng="act")
            for h in range(4):
                scale_cols(P, kg_tm[:, h, :], kt_ps[:, h, :], EG[:, cg, h:h + 1], "dve")
                scale_cols(P, kd_tm[:, h, :], kt_ps[:, h, :], ED[:, cg, h:h + 1], "dve")
            r_ps = bk.nxt()
            for h in range(4):
                P.mm(r_ps[0:2, h, :], G1[:, cg, h, :], cm[:, CM_TRI, :])
            r2_ps = bk.nxt()
            for h in range(4):
                P.mm(r2_ps[0:2, h, :], G2[:, cg, h, :], cm[:, CM_TRI, :])
            if c == 0 and hf == 0:
                R1 = P.sb("R1", [2, 4, 128])
                R2 = P.sb("R2", [2, 4, 128])
                DT = P.sb("DT", [128, 4, 128])
                DTS = P.sb("DTS", [128, 4, 128])
                attnT = P.sb("attnT", [128, 4, 128])
                Bm = P.sb("Bm", [128, 4, 128])
                BT = P.sb("BT", [128, 4, 128])
                Rm = [P.sb("Rm", [128, 4, 128]) for _ in range(2)]
                Pk = [P.sb("Pk", [128, 4, 128]) for _ in range(2)]
                PkT = [P.sb("PkT", [128, 4, 128]) for _ in range(2)]
                nwT = P.sb("nwT", [128, 4, 128])
                vn = P.sb("vn", [128, 4, 128])
                otmp = P.sb("otmp", [128, 4, 128])
                osb = P.sb("osb", [128, 4, 128])
                OS = out_stage_bufs(P)
            P.copy(R1.v(), r_ps[0:2, :, :], eng="dve")
            P.copy(R2.v(), r2_ps[0:2, :, :], eng="act")
            if STOPB == 3:
                P.release(m)
                return
            dt_ps = bk.nxt()
            for h in range(4):
                P.mm(dt_ps[:, h, :], R2[:, h, :], R1[:, h, :])
            P.tt(DT.v(), dt_ps.v(), NEGM4, ALU.add)
            P.activation(DT.v(), DT.v(), AF.Exp)
            P.tt(DTS.v(), DT.v(), STRICT4, ALU.mult, eng="pool")
            kq_ps = bk.nxt()
            kk_ps = bk.nxt()
            for h in range(4):
                P.mm(kq_ps[:, h, :], kT[h][:, o], qT[h][:, o])
                P.mm(kk_ps[:, h, :], kT[h][:, o], kT[h][:, o])
            P.tt(attnT.v(), kq_ps.v(), DT.v(), ALU.mult)
            for h in range(4):
                P.stt(Bm[:, h, :], kk_ps[:, h, :], nbeta[:, cg, h:h + 1], DTS[:, h, :], ALU.mult, ALU.mult)
            if STOPB == 4:
                P.release(m)
                return
            bt_ps = bk.nxt()
            for h in range(4):
                P.tr(bt_ps[:, h, :], Bm[:, h, :], K.identf.v())
            P.copy(BT.v(), bt_ps.v(), eng="act")
            P.tt(Rm[0].v(), Bm.v(), IDENT4, ALU.add, eng="pool")
            cur, curT, R = Bm, BT, Rm[0]
            for k in range(1, 7):
                nP, nPT, nR = Pk[k % 2], PkT[k % 2], Rm[k % 2]
                pT_ps = bk.nxt()
                for h in range(4):
                    P.mm(pT_ps[:, h, :], cur[:, h, :], curT[:, h, :])
                if k < 6:
                    p_ps = bk.nxt()
                    for h in range(4):
                        P.mm(p_ps[:, h, :], curT[:, h, :], cur[:, h, :])
                P.copy(nPT.v(), pT_ps.v(), eng="act")
                if k < 6:
                    P.copy(nP.v(), p_ps.v(), eng="dve")
                rr_ps = bk.nxt()
                for h in range(4):
                    P.mm(rr_ps[:, h, :], nPT[:, h, :], R[:, h, :])
                P.tt(nR.v(), R.v(), rr_ps.v(), ALU.add)
                cur, curT, R = nP, nPT, nR
            Tt = R
            w_ps = bk.nxt()
            for h in range(4):
                P.mm(w_ps[:, h, :], kg_tm[:, h, :], Tt[:, h, :])
            P.activation(nwT.v(), w_ps.v(), AF.Copy, scale=-1.0)
            if STOPB == 5:
                P.release(m)
                return
            vn_ps = bk.nxt()
            o1_ps = bk.nxt()
            for h in range(4):
                P.mm(vn_ps[:, h, :], Tt[:, h, :], v_tm[:, h, :], start=True, stop=False)
                P.mm(vn_ps[:, h, :], nwT[:, h, :], Sst[:, h, :], start=False, stop=True)
                P.mm(o1_ps[:, h, :], qT[h][:, o], Sst[:, h, :])
            for h in range(4):
                scale_cols(P, vn[:, h, :], vn_ps[:, h, :], beta[:, cg, h:h + 1], "dve")
                scale_cols(P, otmp[:, h, :], o1_ps[:, h, :], EG[:, cg, h:h + 1], "act")
            o2_ps = bk.nxt()
            s_ps = bk.nxt()
            for h in range(4):
                P.mm(o2_ps[:, h, :], attnT[:, h, :], vn[:, h, :])
                P.mm(s_ps[:, h, :], kd_tm[:, h, :], vn[:, h, :])
            P.tt(osb.v(), otmp.v(), o2_ps.v(), ALU.add)
            for h in range(4):
                P.stt(Sst[:, h, :], Sst[:, h, :], EGL[:, cg, h:h + 1], s_ps[:, h, :], ALU.mult, ALU.add)
            if STOPB == 6:
                P.release(m)
                return
            out_stage(P, C, K, bk, OS, osb, tm_col(C_AZ), AF.Silu, gainB, yT, 0, cg)
            if STOPB == 8 + cg:
                P.release(m)
                return
    P.release(m)


def phase_D(P, C, K, l, s, yT):
    m = P.mark()
    sm = K.small
    cm = K.cm
    bk = Banks(P, 2)
    bkA = Banks(P, 2)
    bkS = Banks(P, 4)
    flat = lambda b: b.r("p a b -> p (a b)")
    kTs = P.sb("kTs", [64, 2, S], BF16)
    kTw = P.sb("kTw", [64, 2, S], BF16)
    vs = P.sb("vs", [128, NT, 2, 65], BF16)
    vw = P.sb("vw", [128, NT, 2, 65], BF16)
    P.memset(vs[:, :, :, 64:65], 1.0)
    P.memset(vw[:, :, :, 64:65], 1.0)
    ckT = P.sb("ckT", [64, 2, 128])
    cvx = P.sb("cvx", [128, 2, 97])
    P.memset(ckT.v(), 0.0)
    P.memset(cvx.v(), 0.0)
    P.memset(cvx[:, :, 64:65], 1.0)
    for g in range(2):
        P.dma("sp", cvx[:, g, 65:97], C.ovm.v())
    gq8 = P.sb("gq8", [128, 8, 64])
    gk = P.sb("gk", [128, 4, 64])
    for h in range(8):
        P.ts(gq8[:, h, :], sm[:, l, SM_QN:SM_QN + 64], 0.125, ALU.mult, eng="pool")
    for a in range(2):
        for g in range(2):
            P.copy(gk[:, 2 * a + g, :], sm[:, l, SM_KN + 64 * (a + 1):SM_KN + 64 * (a + 2)], eng="pool")
    kn0 = P.sb("kn0", [64, 1])
    P.dma("sp", kn0.v(), C.kn0[l])
    ckc = tm_col(C_CKC)
    m1 = P.mark()
    kcT = P.sb("kcT", [64, 2, S])
    vcT = P.sb("vcT", [64, 2, S])
    Wc = P.sb("Wc", [64, 2, 32, 64])
    peT = P.sb("peT", [64, 2, 32])
    P.dma("sp", Wc.v(), C.wcmp[l])
    P.dma("sp", peT.v(), C.peT[l])
    kv = [P.sb("kv", [128, 6, 2, 64]) for _ in range(2)]
    ksq = P.sb("ksq", [128, 2, 2, 64])
    kss = P.sb("kss", [128, 2, 2])
    kn = P.sb("kn", [128, 2, 2, 64])
    for t in range(NT):
        b = kv[t % 2]
        o = slice(t * 128, (t + 1) * 128)
        P.dma("sp", b.r("p a g d -> p (a g d)"), C.tm[o, ckc:ckc + 768])
        P.copy(vs[:, t, :, 0:64], b[:, 3, :, :], eng="pool")
        P.copy(vw[:, t, :, 0:64], b[:, 5, :, :], eng="pool")
        P.activation(ksq.v(), b[:, 2:6:2, :, :], AF.Square)
        P.reduce(kss.v(), ksq.v(), ALU.add)
        P.activation(kss.v(), kss.v(), AF.Sqrt, bias=K.epsD.v(), scale=1.0 / 64)
        P.recip(kss.v(), kss.v())
        for a in range(2):
            for g in range(2):
                P.stt(kn[:, a, g, :], b[:, 2 + 2 * a, g, :], kss[:, a, g:g + 1], gk[:, 2 * a + g, :], ALU.mult, ALU.mult)
        ps1 = bk.nxt()
        ps2 = bk.nxt()
        for a in range(2):
            for g in range(2):
                P.tr(ps1[0:64, 2 * a + g, :], kn[:, a, g, :], K.identf.v())
                P.tr(ps2[0:64, 2 * a + g, :], b[:, a, g, :], K.identf.v())
        P.copy(kTs[:, :, o], ps1[0:64, 0:2, :], eng="act")
        P.copy(kTw[:, :, o], ps1[0:64, 2:4, :], eng="act")
        P.copy(kcT[:, :, o], ps2[0:64, 0:2, :], eng="dve")
        P.copy(vcT[:, :, o], ps2[0:64, 2:4, :], eng="dve")
    cst = P.sb("cst", [64, 2])
    raw = P.sb("rawc", [64, 127])
    sqc = P.sb("sqc", [64, 127])
    rnc = P.sb("rnc", [64, 127])
    for kvi in range(2):
        pc = bk.nxt()
        for lq in range(32):
            P.mm(flat(pc)[0:64, 0:1], Wc[:, kvi, lq, :], peT[:, kvi, lq:lq + 1], start=(lq == 0), stop=(lq == 31))
        P.copy(cst[:, kvi:kvi + 1], flat(pc)[0:64, 0:1])
    for kvi in range(2):
        src = (kcT, vcT)[kvi]
        for g in range(2):
            ps = bk.nxt()
            pv = flat(ps)[0:64, 0:127]
            for lq in range(32):
                P.mm(pv, Wc[:, kvi, lq, :], src[:, g, lq:lq + 16 * 126 + 1:16], start=(lq == 0), stop=(lq == 31))
            P.ts(raw.v(), pv, cst[:, kvi:kvi + 1], ALU.add)
            if kvi == 0:
                P.activation(sqc.v(), raw.v(), AF.Square)
                p2 = bk.nxt()
                P.mm(flat(p2)[0:64, 0:127], cm[0:64, CM_ONES, 0:64], sqc.v())
                P.activation(rnc.v(), flat(p2)[0:64, 0:127], AF.Sqrt, bias=K.epsD[0:64, :], scale=1.0 / 64)
                P.recip(rnc.v(), rnc.v())
                P.stt(ckT[:, g, 0:127], raw.v(), kn0[:, 0:1], rnc.v(), ALU.mult, ALU.mult)
            else:
                p2 = bk.nxt()
                P.tr(p2[0:127, 0, 0:64], raw.v(), K.identf[0:64, 0:64])
                P.copy(cvx[0:127, g, 0:64], p2[0:127, 0, 0:64])
    P.release(m1)
    qt = [P.sb("qt", [128, 8, 64]) for _ in range(2)]
    gt = [P.sb("gt", [128, 8, 3]) for _ in range(2)]
    qsq = P.sb("qsq", [128, 8, 64])
    qss = P.sb("qss", [128, 8])
    qn = P.sb("qn", [128, 8, 64])
    gs = P.sb("gs", [128, 8, 3])
    qTf = P.sb("qTf", [64, 4, 128])
    qTb = P.sb("qTb", [64, 4, 128], BF16)
    bct = [P.sb("bct", [128, 4, 128]) for _ in range(2)]
    ec = P.sb("ec", [128, 4, 128])
    esb = [P.sb("esb", [128, 4, 128], BF16) for _ in range(3)]
    rc = P.sb("rc", [128, 3, 4])
    imp = P.sb("imp", [128, 32])
    mx8 = P.sb("mx8", [128, 8])
    selm = P.sb("selm", [128, 32])
    selT = P.sb("selT", [32, 4, 128], BF16)
    yc = P.sb("yc", [128, 8, 64])
    cq_, cg_ = tm_col(C_CQ), tm_col(C_CG)
    ie = 0
    ib = 0
    for it in range(NT):
        o = slice(it * 128, (it + 1) * 128)
        q_, g_ = qt[it % 2], gt[it % 2]
        P.dma("sp", q_.r("p h d -> p (h d)"), C.tm[o, cq_:cq_ + 512])
        P.dma("sp", g_.r("p h k -> p (h k)"), C.tm[o, cg_:cg_ + 24])
        P.activation(qsq.v(), q_.v(), AF.Square)
        P.reduce(qss.v(), qsq.v(), ALU.add)
        P.activation(qss.v(), qss.v(), AF.Sqrt, bias=K.epsD.v(), scale=1.0 / 64)
        P.recip(qss.v(), qss.v())
        for h in range(8):
            P.stt(qn[:, h, :], q_[:, h, :], qss[:, h:h + 1], gq8[:, h, :], ALU.mult, ALU.mult)
        P.activation(gs.v(), g_.v(), AF.Sigmoid)
        for g in range(2):
            hs_ = slice(4 * g, 4 * g + 4)
            qps = bk.nxt()
            for hh in range(4):
                P.tr(qps[0:64, hh, :], qn[:, 4 * g + hh, :], K.identf.v())
            P.copy(qTf.v(), qps[0:64, :, :], eng="act")
            P.copy(qTb.v(), qps[0:64, :, :], eng="dve")
            bc_ = bct[ib % 2]
            ib += 1
            P.dma("sp", bc_.v(), C.biasC[it, g])
            sc = bk.nxt()
            P.mm(flat(sc), ckT[:, g, :], flat(qTf), start=True, stop=False)
            P.mm(flat(sc), cm[:, CM_IDENT, :], flat(bc_), start=False, stop=True)
            P.activation(ec.v(), sc.v(), AF.Exp)
            oc = bk.nxt()
            ocv = flat(oc)[:, 0:388].r("p (h c) -> p h c", c=97)
            for hh in range(4):
                P.mm(ocv[:, hh, :], ec[:, hh, :], cvx[:, g, :])
            P.ts(rc[:, 0, :], ocv[:, :, 64], 1e-30, ALU.max)
            P.recip(rc[:, 0, :], rc[:, 0, :])
            P.ts(imp.v(), ocv[:, 0, 65:97], rc[:, 0, 0:1], ALU.mult)
            for hh in range(1, 4):
                P.stt(imp.v(), ocv[:, hh, 65:97], rc[:, 0, hh:hh + 1], imp.v(), ALU.mult, ALU.add)
            P.tt(rc[:, 0, :], rc[:, 0, :], gs[:, hs_, 0], ALU.mult)
            for hh in range(4):
                P.ts(yc[:, 4 * g + hh, :], ocv[:, hh, 0:64], rc[:, 0, hh:hh + 1], ALU.mult)
            P.tt(imp.v(), imp.v(), K.selc[:, it, 0, :], ALU.mult)
            P.tt(imp.v(), imp.v(), K.selc[:, it, 1, :], ALU.add)
            ia, ma = imp.ap, mx8.ap
            P.op("dve", lambda e, ia=ia, ma=ma: e.max(out=ma, in_=ia), [imp], [mx8])
            P.ts(selm.v(), imp.v(), mx8[:, 3:4], ALU.is_ge, -1.0, ALU.add)
            sps = bk.nxt()
            P.tr(sps[0:32, 0, :], selm.v(), K.identf.v())
            for hh in range(4):
                P.copy(selT[:, hh, :], sps[0:32, 0, :], eng=("act", "dve")[hh % 2])
            osel = bkA.nxt()
            osv = flat(osel)[:, 0:260].r("p (h c) -> p h c", c=65)
            for jt in range(it + 1):
                off = it - jt
                ti = min(off, 2)
                sp_ = bkS.nxt()
                P.mm(flat(sp_), kTs[:, g, jt * 128:(jt + 1) * 128], flat(qTb), start=True, stop=False)
                P.mm(flat(sp_), K.identb.v(), K.tbl[:, ti, hs_, :].r("p h i -> p (h i)"), start=False, stop=False)
                P.mm(flat(sp_), K.rsel[:, jt, :], flat(selT), start=False, stop=True)
                es = esb[ie % 3]
                ie += 1
                P.activation(es.v(), sp_.v(), AF.Exp)
                for hh in range(4):
                    P.mm(osv[:, hh, :], es[:, hh, :], vs[:, jt, g, :], start=(jt == 0 and hh == 0), stop=(jt == it), skip=True)
            P.ts(rc[:, 1, :], osv[:, :, 64], 1e-30, ALU.max)
            P.recip(rc[:, 1, :], rc[:, 1, :])
            P.tt(rc[:, 1, :], rc[:, 1, :], gs[:, hs_, 1], ALU.mult)
            for hh in range(4):
                P.stt(yc[:, 4 * g + hh, :], osv[:, hh, 0:64], rc[:, 1, hh:hh + 1], yc[:, 4 * g + hh, :], ALU.mult, ALU.add)
            owin = bkA.nxt()
            owv = flat(owin)[:, 0:260].r("p (h c) -> p h c", c=65)
            j0 = max(0, it - 4)
            for jt in range(j0, it + 1):
                off = it - jt
                ti = (0, 1, 2, 2, 3)[off]
                sp_ = bkS.nxt()
                P.mm(flat(sp_), kTw[:, g, jt * 128:(jt + 1) * 128], flat(qTb), start=True, stop=False)
                P.mm(flat(sp_), K.identb.v(), K.tbl[:, ti, hs_, :].r("p h i -> p (h i)"), start=False, stop=True)
                es = esb[ie % 3]
                ie += 1
                P.activation(es.v(), sp_.v(), AF.Exp)
                for hh in range(4):
                    P.mm(owv[:, hh, :], es[:, hh, :], vw[:, jt, g, :], start=(jt == j0 and hh == 0), stop=(jt == it), skip=True)
            P.ts(rc[:, 2, :], owv[:, :, 64], 1e-30, ALU.max)
            P.recip(rc[:, 2, :], rc[:, 2, :])
            P.tt(rc[:, 2, :], rc[:, 2, :], gs[:, hs_, 2], ALU.mult)
            for hh in range(4):
                P.stt(yc[:, 4 * g + hh, :], owv[:, hh, 0:64], rc[:, 2, hh:hh + 1], yc[:, 4 * g + hh, :], ALU.mult, ALU.add)
        y_ps = bk.nxt()
        for c4 in range(4):
            P.tr(y_ps[:, c4, :], yc[:, 2 * c4:2 * c4 + 2, :].r("p h d -> p (h d)"), K.identf.v())
        P.copy(yT[:, 8:12, o], y_ps.v(), eng="act")
    P.release(m)


def phase_E(P, C, K, l, s, uT, yT, hin, hout):
    m0 = P.mark()
    mergedT = P.sb("mergedT", [128, 8, S], BF16)
    m1 = P.mark()
    wsg = WStream(P, 8, 256, nstage=2, nbuf=3)
    wsb = WStream(P, 4, 256, nstage=2, nbuf=3)
    pg = [P.ps("pg", [128, 512]) for _ in range(2)]
    pb = [P.ps("pb", [128, 512]) for _ in range(2)]
    gsb = [P.sb("gsb", [128, 512]) for _ in range(2)]
    acc = [P.sb("acc", [128, 512]) for _ in range(2)]
    gain = K.gains[:, l, 0, :]
    it = 0
    k3 = 0
    for q in range(4):
        wg = []
        wbr = []
        for n in range(3):
            c0 = C_MG + n * 1024 + q * 256
            wg.append(wsg.load(C.w_in[l][:, c0:c0 + 256], 256, gain))
            wbr.append(wsb.load(C.w_branch[l, n][:, q * 256:(q + 1) * 256], 256))
        for j in range(2):
            fblk = q * 2 + j
            for tg in range(4):
                a = acc[it % 2]
                tsl = slice(tg * 512, (tg + 1) * 512)
                for n in range(3):
                    g_ps, b_ps, gs = pg[k3 % 2], pb[k3 % 2], gsb[k3 % 2]
                    k3 += 1
                    for kc in range(8):
                        P.mm(g_ps.v(), wg[n][:, kc, j * 128:(j + 1) * 128], uT[:, kc, tsl], start=(kc == 0), stop=(kc == 7))
                    for kc in range(4):
                        P.mm(b_ps.v(), wbr[n][:, kc, j * 128:(j + 1) * 128], yT[:, n * 4 + kc, tsl], start=(kc == 0), stop=(kc == 3))
                    P.activation(gs.v(), g_ps.v(), AF.Sigmoid)
                    if n == 0:
                        P.tt(a.v(), gs.v(), b_ps.v(), ALU.mult)
                    else:
                        P.tt(gs.v(), gs.v(), b_ps.v(), ALU.mult)
                        if n == 1:
                            P.tt(a.v(), a.v(), gs.v(), ALU.add, eng="pool")
                        else:
                            P.tt(mergedT[:, fblk, tsl], a.v(), gs.v(), ALU.add, eng="pool")
                it += 1
    P.release(m1)
    ws = WStream(P, 8, 512)
    po = [P.ps("po", [128, 512]) for _ in range(2)]
    hsb = [P.sb("hsb", [128, 512]) for _ in range(3)]
    it = 0
    for half in range(2):
        csl = slice(half * 512, (half + 1) * 512)
        w = ws.load(C.w_out[l][:, csl], 512)
        for t in range(NT):
            ps = po[it % 2]
            hi = hsb[it % 3]
            it += 1
            P.dma("sp", hi.v(), hin[t][:, csl])
            for kc in range(8):
                P.mm(ps.v(), mergedT[:, kc, t * 128:(t + 1) * 128], w[:, kc, :], start=(kc == 0), stop=(kc == 7))
            P.tt(hi.v(), hi.v(), ps.v(), ALU.add)
            P.dma("pool", hout[t][:, csl], hi.v())
    P.release(m0)


def phase_F(P, C, K, l, s, uT, hout):
    norm_transpose(P, K, hout, uT)
    m = P.mark()
    actT = P.sb("actT", [128, 22, S], BF16)
    m2 = P.mark()
    gain = K.gains[:, l, 1, :]
    wsg = WStream(P, 8, 256)
    wsu = WStream(P, 8, 256)
    pgt = [P.ps("pgt", [128, 512]) for _ in range(2)]
    pup = [P.ps("pup", [128, 512]) for _ in range(2)]
    sg = [P.sb("sg", [128, 512]) for _ in range(2)]
    it = 0
    for q in range(11):
        wg = wsg.load(C.w_ffn_in[l][:, q * 256:(q + 1) * 256], 256, gain)
        wu = wsu.load(C.w_ffn_in[l][:, D_FF + q * 256:D_FF + (q + 1) * 256], 256, gain)
        for j in range(2):
            fblk = q * 2 + j
            for tg in range(4):
                tsl = slice(tg * 512, (tg + 1) * 512)
                g_ps, u_ps, sgt = pgt[it % 2], pup[it % 2], sg[it % 2]
                it += 1
                for kc in range(8):
                    P.mm(g_ps.v(), wg[:, kc, j * 128:(j + 1) * 128], uT[:, kc, tsl], start=(kc == 0), stop=(kc == 7))
                for kc in range(8):
                    P.mm(u_ps.v(), wu[:, kc, j * 128:(j + 1) * 128], uT[:, kc, tsl], start=(kc == 0), stop=(kc == 7))
                P.activation(sgt.v(), g_ps.v(), AF.Silu)
                P.tt(actT[:, fblk, tsl], sgt.v(), u_ps.v(), ALU.mult)
    P.release(m2)
    ws = WStream(P, 22, 256, nstage=1, nbuf=2)
    po = [P.ps("po2", [128, 512]) for _ in range(2)]
    hsb = [P.sb("hsb2", [128, 256]) for _ in range(3)]
    it = 0
    for qd in range(4):
        csl = slice(qd * 256, (qd + 1) * 256)
        w = ws.load(C.w_ffn_out[l][:, csl], 256)
        for t in range(NT):
            ps = po[it % 2]
            hi = hsb[it % 3]
            it += 1
            P.dma("sp", hi.v(), hout[t][:, csl])
            for kc in range(22):
                P.mm(ps[:, 0:256], actT[:, kc, t * 128:(t + 1) * 128], w[:, kc, :], start=(kc == 0), stop=(kc == 21))
            P.tt(hi.v(), hi.v(), ps[:, 0:256], ALU.add)
            P.dma("pool", hout[t][:, csl], hi.v())
    P.release(m)
    norm_transpose(P, K, hout, uT)
    m = P.mark()
    pTf = P.sb("pTf", [128, 2, S])
    pTb = P.sb("pTb", [128, 2, S], BF16)
    P.dma("sp", pTf.v(), C.pT[l, s].r("(k p) t -> p k t", p=128))
    P.copy(pTb.v(), pTf.v(), eng="pool")
    gain = K.gains[:, l, 2, :]
    wsg = WStream(P, 8, 512)
    wsp = WStream(P, 2, 512)
    pg = [P.ps("pg3", [128, 512]) for _ in range(2)]
    pp = [P.ps("pp3", [128, 512]) for _ in range(2)]
    gsb = [P.sb("gsb3", [128, 512]) for _ in range(2)]
    hsb = [P.sb("hsb3", [128, 512]) for _ in range(3)]
    it = 0
    for half in range(2):
        csl = slice(half * 512, (half + 1) * 512)
        wg = wsg.load(C.w_ple_gate[l][:, csl], 512, gain)
        wp = wsp.load(C.w_ple_proj[l][:, csl], 512)
        for t in range(NT):
            g_ps, p_ps, gs, hi = pg[it % 2], pp[it % 2], gsb[it % 2], hsb[it % 3]
            it += 1
            P.dma("sp", hi.v(), hout[t][:, csl])
            for kc in range(8):
                P.mm(g_ps.v(), uT[:, kc, t * 128:(t + 1) * 128], wg[:, kc, :], start=(kc == 0), stop=(kc == 7))
            for kc in range(2):
                P.mm(p_ps.v(), pTb[:, kc, t * 128:(t + 1) * 128], wp[:, kc, :], start=(kc == 0), stop=(kc == 1))
            P.activation(gs.v(), g_ps.v(), AF.Sigmoid)
            P.tt(gs.v(), gs.v(), p_ps.v(), ALU.mult)
            P.tt(hi.v(), hi.v(), gs.v(), ALU.add, eng="pool")
            P.dma("pool", hout[t][:, csl], hi.v())
    P.release(m)


def build(n_layers=DEPTH, n_seq=NSEQ, phases="AE", dbg=(), dbg_yT=False):
    nc = bass.Bass("TRN2", target_bir_lowering=False)
    P = Prog(nc)
    C = declare(P, dbg)
    K = setup_consts(P, C)
    K.epsD = P.sb("epsD", [128, 1])
    P.memset(K.epsD.v(), EPS)
    K.eps6 = P.sb("eps6", [128, 1])
    P.memset(K.eps6.v(), 1e-6)
    if dbg_yT:
        ydbg = P.track(Buf(nc.dram_tensor("yT_dbg", [128, 12, S], F32, kind="ExternalInput").ap(), "yT_dbg"))
    for s in range(n_seq):
        for l in range(n_layers):
            hin = C.xt[s] if l == 0 else C.ot[s]
            my = P.mark()
            yT = P.sb("yT", [128, 12, S], BF16)
            mu = P.mark()
            uT = P.sb("uT", [128, 8, S], BF16)
            if "A" in phases:
                phase_A(P, C, K, l, s, hin, uT)
            P.release(mu)
            if "B" in phases:
                phase_B(P, C, K, l, s, yT)
            if "C" in phases:
                phase_C(P, C, K, l, s, yT)
            if "D" in phases:
                phase_D(P, C, K, l, s, yT)
            if "y" in dbg:
                mm_ = P.mark()
                yf = P.sb("yf", [128, 12, S])
                P.copy(yf.v(), yT.v(), eng="pool")
                P.dma("sp", C.ydbg.v(), yf.v())
                P.release(mm_)
            uT = P.sb("uT", [128, 8, S], BF16)
            norm_transpose(P, K, hin, uT)
            if dbg_yT:
                m = P.mark()
                yf = P.sb("yf", [128, 12, S])
                P.dma("sp", yf.v(), ydbg.v())
                P.copy(yT.v(), yf.v(), eng="pool")
                P.release(m)
            if "E" in phases:
                phase_E(P, C, K, l, s, uT, yT, hin, C.ot[s])
            P.release(my)
            if "E" in phases:
                uT = P.sb("uT", [128, 8, S], BF16)
                phase_F(P, C, K, l, s, uT, C.ot[s])
                P.release(my)
    st = P.emit()
    P.close()
    return nc, st


import math
NEGB = -30000.0


def rel_bucket_np(dist):
    d = np.maximum(dist, 0)
    df = np.maximum(d, 1).astype(np.float32)
    large = 16 + (np.log(df / np.float32(16)) / np.float32(math.log(128 / 16)) * np.float32(16)).astype(np.int32)
    large = np.minimum(large, 31)
    return np.where(d < 16, d, large).astype(np.int64)


def nsa_consts(inp):
    rb = inp["rel_bias"].astype(np.float32)
    d = {}
    j = np.arange(128)[:, None]
    i = np.arange(128)[None, :]
    neg = np.float32(NEGB)

    def gat(dist, valid):
        g = rb[rel_bucket_np(dist)]
        g = np.where(valid[..., None], g, neg)
        return np.ascontiguousarray(g.transpose(0, 2, 1))
    tb = np.zeros((4, 128, 8, 128), np.float32)
    tb[0] = gat(i - j, (i - j) >= 0)
    tb[1] = gat(128 + i - j, np.ones((128, 128), bool))
    tb[2] = gat(np.full((128, 128), 1000), np.ones((128, 128), bool))
    tb[3] = gat(512 + i - j, i < j)
    d["tbls"] = tb
    n = np.arange(128)[:, None]
    bc = np.zeros((NT, 2, 128, 4, 128), np.float32)
    for it in range(NT):
        tq = it * 128 + np.arange(128)[None, :]
        dist = tq - (16 * n + 31)
        valid = (dist >= 0) & (n < 127)
        g = np.where(valid[..., None], rb[rel_bucket_np(dist)], neg)
        g = g.transpose(0, 2, 1)
        bc[it, 0] = g[:, 0:4]
        bc[it, 1] = g[:, 4:8]
    d["biasC"] = bc
    sc = np.zeros((128, NT, 2, 32), np.float32)
    jj = np.arange(32)[None, :]
    for it in range(NT):
        cur = (it * 128 + np.arange(128)[:, None]) // 64
        forced = (jj == 0) | (jj == cur)
        causal = jj <= cur
        sc[:, it, 0] = (causal & ~forced)
        sc[:, it, 1] = np.where(forced, 1e4, np.where(causal, 0.0, -1.0))
    d["selc"] = sc
    cs = np.arange(127) * 16
    ce = cs + 31
    ss = np.arange(32) * 64
    se = ss + 63
    ov = np.zeros((128, 32), np.float32)
    ov[:127] = ((cs[:, None] <= se[None]) & (ce[:, None] >= ss[None]))
    d["ovm"] = ov
    rs = np.zeros((32, NT, 128), np.float32)
    for jt in range(NT):
        for jr in range(128):
            rs[2 * jt + jr // 64, jt, jr] = 30000.0
    d["rselc"] = rs
    d["wcmp"] = np.ascontiguousarray(inp["nsa_w_cmp"].transpose(0, 3, 1, 2, 4))
    d["peT"] = np.ascontiguousarray(inp["nsa_cmp_pe"].transpose(0, 3, 1, 2))
    d["kn0"] = np.ascontiguousarray(inp["nsa_k_norm"][:, 0, :, None])
    return d


def host_consts(inp):
    d = {}
    j = np.arange(128)[:, None]
    i = np.arange(128)[None, :]
    cm = np.zeros((128, NCM, 128), np.float32)
    cm[:, CM_TRI] = (j <= i)
    cm[:, CM_NEGM] = np.where(i >= j, 0.0, -30000.0)
    cm[:, CM_STRICT] = (i > j)
    cm[:, CM_SEL127] = (j == 127) * np.ones((1, 128))
    cm[:, CM_IDENT] = (i == j)
    cm[:, CM_ONES] = 1.0
    d["cmat"] = cm
    sm = np.zeros((DEPTH, NSMALL), np.float32)
    sm[:, SM_ALOG:SM_ALOG + 4] = inp["gdn_a_log"]
    sm[:, SM_DTB:SM_DTB + 4] = inp["gdn_dt_bias"]
    sm[:, SM_BI:SM_BI + 4] = inp["mlstm_b_i"]
    sm[:, SM_BF:SM_BF + 4] = inp["mlstm_b_f"]
    sm[:, SM_GDNN:SM_GDNN + 128] = inp["gdn_norm"]
    sm[:, SM_MLN:SM_MLN + 128] = inp["mlstm_norm"]
    sm[:, SM_QN:SM_QN + 64] = inp["nsa_q_norm"]
    sm[:, SM_KN:SM_KN + 192] = inp["nsa_k_norm"].reshape(DEPTH, 192)
    d["small"] = np.ascontiguousarray(np.broadcast_to(sm[:, None, :], (DEPTH, 128, NSMALL)))
    d.update(nsa_consts(inp))
    d["cw"] = np.ascontiguousarray(inp["conv_w"].reshape(DEPTH, 4, 12, 128).transpose(0, 3, 2, 1))
    return d


def host_inputs(inp, core=0):
    b0 = core * 2
    d = {}
    d["x"] = np.ascontiguousarray(inp["x"][b0:b0 + 2])
    d["pT"] = np.ascontiguousarray(np.transpose(inp["p"][:, b0:b0 + 2], (0, 1, 3, 2)))
    for k in ["w_in", "w_branch", "w_out", "w_ffn_in", "w_ffn_out", "w_ple_gate", "w_ple_proj"]:
        d[k] = inp[k]
    g = np.stack([inp["norm_mix"], inp["norm_ffn"], inp["norm_ple"]], axis=1)
    d["gains"] = np.ascontiguousarray(g.reshape(DEPTH, 3, 8, 128).transpose(0, 1, 3, 2))
    d["ident"] = np.eye(128, dtype=np.float32)
    d.update(host_consts(inp))
    return d


_NC_CACHE = {}


def kernel(**inputs):
    inp = {k_: np.asarray(v) for k_, v in inputs.items()}
    if "full" not in _NC_CACHE:
        _NC_CACHE["full"] = build(n_layers=DEPTH, n_seq=NSEQ, phases="ABCDE")[0]
    nc = _NC_CACHE["full"]
    consts = host_consts(inp)
    shared = {}
    for k_ in ["w_in", "w_branch", "w_out", "w_ffn_in", "w_ffn_out", "w_ple_gate", "w_ple_proj"]:
        shared[k_] = np.ascontiguousarray(inp[k_], dtype=np.float32)
    g = np.stack([inp["norm_mix"], inp["norm_ffn"], inp["norm_ple"]], axis=1)
    shared["gains"] = np.ascontiguousarray(g.reshape(DEPTH, 3, 8, 128).transpose(0, 1, 3, 2), dtype=np.float32)
    shared["ident"] = np.eye(128, dtype=np.float32)
    shared.update(consts)
    in_maps = []
    for c in range(8):
        d = dict(shared)
        d["x"] = np.ascontiguousarray(inp["x"][2 * c:2 * c + 2], dtype=np.float32)
        d["pT"] = np.ascontiguousarray(np.transpose(inp["p"][:, 2 * c:2 * c + 2], (0, 1, 3, 2)), dtype=np.float32)
        in_maps.append(d)
    res = run_bass_kernel_spmd(nc, in_maps, core_ids=list(range(8)))
    out = np.concatenate([np.asarray(r["out"]) for r in res.results], axis=0)
    return out.astype(np.float32)
```

```python
import numpy as np
import concourse.bass as bass
import concourse.mybir as mybir
from concourse.bass_utils import run_bass_kernel_spmd

F32 = mybir.dt.float32
BF16 = mybir.dt.bfloat16
I32 = mybir.dt.int32
AF = mybir.ActivationFunctionType
ALU = mybir.AluOpType
AX = mybir.AxisListType

N_DMA_SEMS = 48


class V:
    __slots__ = ("buf", "ap")

    def __init__(self, buf, ap):
        self.buf = buf
        self.ap = ap

    def __getitem__(self, idx):
        return V(self.buf, self.ap[idx])

    def r(self, pat, **kw):
        return V(self.buf, self.ap.rearrange(pat, **kw))

    def bc(self, shape):
        return V(self.buf, self.ap.to_broadcast(list(shape)))


class Buf:
    __slots__ = ("ap", "name", "lw", "rd", "excl")

    def __init__(self, ap, name=""):
        self.ap = ap
        self.name = name
        self.lw = None
        self.rd = []
        self.excl = False

    def __getitem__(self, idx):
        return V(self, self.ap[idx])

    def v(self):
        return V(self, self.ap)

    def r(self, pat, **kw):
        return V(self, self.ap.rearrange(pat, **kw))

    def sub(self, idx, name=""):
        return Buf(self.ap[idx], name or self.name)


def _ap(x):
    return x.ap if isinstance(x, V) else x


class Prog:
    ENGS = ("pe", "dve", "act", "pool", "sp")

    def __init__(self, nc):
        self.nc = nc
        self.ops = []
        self.eng = {"pe": nc.tensor, "dve": nc.vector, "act": nc.scalar, "pool": nc.gpsimd, "sp": nc.sync}
        self._ctx = []
        self.all_bufs = []
        self.last_barrier = None
        self._uid = 0

    def _nm(self, name):
        self._uid += 1
        return "%s_%d" % (name, self._uid)

    def sb(self, name, shape, dt=F32):
        g = self.nc.sbuf_tensor(self._nm(name), list(shape), dt)
        t = g.__enter__()
        self._ctx.append(g)
        b = Buf(t.ap() if hasattr(t, "ap") else t[:], name)
        self.all_bufs.append(b)
        return b

    def ps(self, name, shape, dt=F32):
        g = self.nc.psum_tensor(self._nm(name), list(shape), dt)
        t = g.__enter__()
        self._ctx.append(g)
        b = Buf(t.ap() if hasattr(t, "ap") else t[:], name)
        b.excl = True
        self.all_bufs.append(b)
        return b

    def dram(self, name, shape, dt=F32, kind="Internal"):
        t = self.nc.dram_tensor(name, list(shape), dt, kind=kind).ap()
        b = Buf(t, name)
        self.all_bufs.append(b)
        return b

    def track(self, b):
        self.all_bufs.append(b)
        return b

    def mark(self):
        return len(self._ctx)

    def release(self, mark):
        self.barrier()
        while len(self._ctx) > mark:
            g = self._ctx.pop()
            g.__exit__(None, None, None)

    def op(self, eng, fn, reads=(), writes=()):
        self.ops.append((eng, fn, tuple(reads), tuple(writes), False, self.last_barrier))

    def barrier(self):
        idx = len(self.ops)
        self.ops.append(("sp", lambda e: e.nop(), (), (), "bar", self.last_barrier))
        self.last_barrier = idx

    def dma(self, eng, out, in_, **kw):
        oa, ia = _ap(out), _ap(in_)

        def fn(e, oa=oa, ia=ia, kw=kw):
            return e.dma_start(out=oa, in_=ia, **kw)
        self.ops.append((eng, fn, (in_.buf,), (out.buf,), True, self.last_barrier))

    @staticmethod
    def _rw(outs, ins):
        w = [o.buf for o in outs if isinstance(o, V)]
        r = [i.buf for i in ins if isinstance(i, V)]
        w += [b for b in r if b.excl and b not in w]
        return r, w

    def activation(self, out, in_, func, bias=0.0, scale=1.0, accum_out=None, eng="act"):
        r, w = self._rw([out, accum_out], [in_, bias, scale])
        kw = dict(out=_ap(out), in_=_ap(in_), func=func, bias=_ap(bias), scale=_ap(scale))
        if accum_out is not None:
            kw["accum_out"] = _ap(accum_out)
        self.op(eng, lambda e, kw=kw: e.activation(**kw), r, w)

    def tt(self, out, in0, in1, op, eng="dve"):
        r, w = self._rw([out], [in0, in1])
        kw = dict(out=_ap(out), in0=_ap(in0), in1=_ap(in1), op=op)
        self.op(eng, lambda e, kw=kw: e.tensor_tensor(**kw), r, w)

    def ts(self, out, in0, s1, op0, s2=None, op1=None, accum_out=None, eng="dve"):
        r, w = self._rw([out, accum_out], [in0, s1, s2])
        kw = dict(out=_ap(out), in0=_ap(in0), scalar1=_ap(s1), scalar2=_ap(s2), op0=op0)
        if op1 is not None:
            kw["op1"] = op1
        if accum_out is not None:
            kw["accum_out"] = _ap(accum_out)
        self.op(eng, lambda e, kw=kw: e.tensor_scalar(**kw), r, w)

    def stt(self, out, in0, scalar, in1, op0, op1, eng="dve"):
        r, w = self._rw([out], [in0, scalar, in1])
        kw = dict(out=_ap(out), in0=_ap(in0), scalar=_ap(scalar), in1=_ap(in1), op0=op0, op1=op1)
        self.op("dve", lambda e, kw=kw: e.scalar_tensor_tensor(**kw), r, w)

    def copy(self, out, in_, eng="dve"):
        r, w = self._rw([out], [in_])
        oa, ia = _ap(out), _ap(in_)
        if eng == "act":
            self.op(eng, lambda e: e.copy(out=oa, in_=ia), r, w)
        else:
            self.op(eng, lambda e: e.tensor_copy(out=oa, in_=ia), r, w)

    def memset(self, out, val, eng="pool"):
        r, w = self._rw([out], [])
        oa = _ap(out)
        self.op(eng, lambda e: e.memset(oa, val), r, w)

    def reduce(self, out, in_, op, axis=AX.X, eng="dve"):
        r, w = self._rw([out], [in_])
        oa, ia = _ap(out), _ap(in_)
        self.op(eng, lambda e: e.tensor_reduce(out=oa, in_=ia, axis=axis, op=op), r, w)

    def recip(self, out, in_):
        r, w = self._rw([out], [in_])
        oa, ia = _ap(out), _ap(in_)
        self.op("dve", lambda e: e.reciprocal(out=oa, in_=ia), r, w)

    def mm(self, out, lhsT, rhs, start=True, stop=True, skip=False):
        r, w = self._rw([out], [lhsT, rhs])
        oa, la, ra = _ap(out), _ap(lhsT), _ap(rhs)
        if skip:
            self.op("pe", lambda e: e.matmul(oa, la, ra, start=start, stop=stop, skip_group_check=True), r, w)
        else:
            self.op("pe", lambda e: e.matmul(oa, la, ra, start=start, stop=stop), r, w)

    def tr(self, out, in_, ident):
        r, w = self._rw([out], [in_, ident])
        oa, ia, da = _ap(out), _ap(in_), _ap(ident)
        self.op("pe", lambda e: e.transpose(oa, ia, da), r, w)

    def emit(self):
        nc = self.nc
        ops = self.ops
        n = len(ops)
        deps = [None] * n
        signal = [False] * n
        last_on = {}
        dmas_since = []
        for i, (eng, fn, reads, writes, is_dma, bar) in enumerate(ops):
            d = set()
            if bar is not None:
                d.add(bar)
            if is_dma == "bar":
                d.update(last_on.values())
                d.update(dmas_since)
                dmas_since = []
                is_dma = False
            elif is_dma:
                dmas_since.append(i)
            last_on[eng] = i
            for r in reads:
                if r.lw is not None:
                    d.add(r.lw)
            for w in writes:
                if w.lw is not None:
                    j = w.lw
                    if is_dma or ops[j][4] or ops[j][0] != eng:
                        d.add(j)
                for j in w.rd:
                    if is_dma or ops[j][4] or ops[j][0] != eng:
                        d.add(j)
            d.discard(i)
            if eng == "pe":
                d = {j for j in d if ops[j][0] != "pe" or ops[j][4]}
            for r in reads:
                r.rd.append(i)
            for w in writes:
                w.lw = i
                w.rd = []
            best = {}
            dd = set()
            for j in d:
                if ops[j][4] is True:
                    dd.add(j)
                else:
                    e2 = ops[j][0]
                    if e2 not in best or best[e2] < j:
                        best[e2] = j
            dd.update(best.values())
            deps[i] = dd
            for j in dd:
                signal[j] = True
        sems = {}
        for e in self.ENGS:
            g = nc.semaphore("s_" + e)
            sems[e] = g.__enter__()
            self._ctx.append(g)
        dsems = []
        for k in range(N_DMA_SEMS):
            g = nc.semaphore("d_%d" % k)
            dsems.append(g.__enter__())
            self._ctx.append(g)
        cnt = {e: 0 for e in self.ENGS}
        dcnt = [0] * N_DMA_SEMS
        ev = [None] * n
        dnext = 0
        dma_prev = [None] * N_DMA_SEMS
        dma_guard = [None] * n
        for i, (eng, fn, reads, writes, is_dma, bar) in enumerate(ops):
            if is_dma is True:
                k = dnext
                dnext = (dnext + 1) % N_DMA_SEMS
                dma_guard[i] = dma_prev[k]
                dcnt[k] += 16
                ev[i] = ("d", k, dcnt[k])
                dma_prev[k] = ev[i]
            elif signal[i]:
                cnt[eng] += 1
                ev[i] = ("c", eng, cnt[eng])
        waited = {e: {} for e in self.ENGS}
        nwaits = 0
        last_dma = {}
        for i, (eng, fn, reads, writes, is_dma, bar) in enumerate(ops):
            need = {}
            for j in deps[i]:
                kind, key, val = ev[j]
                if need.get((kind, key), 0) < val:
                    need[(kind, key)] = val
            if is_dma is True and dma_guard[i] is not None:
                kind, key, val = dma_guard[i]
                if need.get((kind, key), 0) < val:
                    need[(kind, key)] = val
            w = waited[eng]
            for (kind, key), val in need.items():
                if w.get((kind, key), 0) >= val:
                    continue
                w[(kind, key)] = val
                s = sems[key] if kind == "c" else dsems[key]
                self.eng[eng].wait_ge(s, val)
                nwaits += 1
            ins = fn(self.eng[eng])
            if ev[i] is not None:
                kind, key, val = ev[i]
                if kind == "c":
                    ins.then_inc(sems[key], 1)
                else:
                    ins.then_inc(dsems[key], 16)
                    last_dma[key] = val
        w = waited["sp"]
        for k, val in last_dma.items():
            if w.get(("d", k), 0) < val:
                self.eng["sp"].wait_ge(dsems[k], val)
        self.stats = dict(n_ops=n, n_waits=nwaits, cnt=dict(cnt))
        return self.stats

    def close(self):
        while self._ctx:
            g = self._ctx.pop()
            g.__exit__(None, None, None)


S = 2048
D = 1024
NT = S // 128
DEPTH = 2
NSEQ = 2
D_IN = 8488
D_FF = 2816
EPS = 1e-6
C_AQ, C_AK, C_AV, C_AZ, C_AA, C_AB = 0, 512, 1024, 1536, 2048, 2052
C_BQ, C_BK, C_BV, C_BO, C_BI, C_BF = 2056, 2568, 3080, 3592, 4104, 4108
C_CQ, C_CKC, C_CVC, C_CKS, C_CVS, C_CKW, C_CVW, C_CG, C_MG = 4112, 4624, 4752, 4880, 5008, 5136, 5264, 5392, 5416


TM_RANGES = [(C_AZ, 520), (C_BK, 512), (C_BV, 1032), (C_CQ, 1304)]
TM_OFF = {}
_o = 0
for _c, _w in TM_RANGES:
    TM_OFF[_c] = _o
    _o += _w
TM_W = _o


def tm_col(c):
    for c0, w in TM_RANGES:
        if c0 <= c < c0 + w:
            return TM_OFF[c0] + (c - c0)
    raise KeyError(c)


FM_RANGES = [(C_AQ, 1536), (C_BQ, 1024)]
FM_OFF = {C_AQ: 0, C_BQ: 1536}


class Ctx:
    pass


CM_TRI, CM_NEGM, CM_STRICT, CM_SEL127, CM_IDENT, CM_ONES = range(6)
NCM = 6
SM_ALOG, SM_DTB, SM_BI, SM_BF, SM_GDNN, SM_MLN, SM_QN, SM_KN = 0, 4, 8, 12, 16, 144, 272, 336
NSMALL = 336 + 192


def declare(P, dbg=()):
    nc = P.nc
    C = Ctx()

    def inp(name, shape, dt=F32):
        return P.track(Buf(nc.dram_tensor(name, list(shape), dt, kind="ExternalInput").ap(), name))

    C.x = inp("x", [NSEQ, S, D])
    C.pT = inp("pT", [DEPTH, NSEQ, 256, S])
    C.w_in = inp("w_in", [DEPTH, D, D_IN])
    C.w_branch = inp("w_branch", [DEPTH, 3, 512, D])
    C.w_out = inp("w_out", [DEPTH, D, D])
    C.w_ffn_in = inp("w_ffn_in", [DEPTH, D, 2 * D_FF])
    C.w_ffn_out = inp("w_ffn_out", [DEPTH, D_FF, D])
    C.w_ple_gate = inp("w_ple_gate", [DEPTH, D, D])
    C.w_ple_proj = inp("w_ple_proj", [DEPTH, 256, D])
    C.gains_b = inp("gains_b", [DEPTH, 3, 128, D])
    C.ident = inp("ident", [128, 128])
    C.cmat = inp("cmat", [128, NCM, 128])
    C.small = inp("small", [DEPTH, 128, NSMALL])
    C.cw = inp("cw", [DEPTH, 128, 12, 4])
    C.tbls = inp("tbls", [4, 128, 8, 128])
    C.biasC = inp("biasC", [NT, 2, 128, 4, 128])
    C.selc = inp("selc", [128, NT, 2, 32])
    C.ovm = inp("ovm", [128, 32])
    C.rselc = inp("rselc", [32, NT, 128])
    C.wcmp = inp("wcmp", [DEPTH, 64, 2, 32, 64])
    C.peT = inp("peT", [DEPTH, 64, 2, 32])
    C.kn0 = inp("kn0", [DEPTH, 64, 1])
    C.out = P.track(Buf(nc.dram_tensor("out", [NSEQ, S, D], F32, kind="ExternalOutput").ap(), "out"))

    C.xt = [[C.x.sub((q, slice(t * 128, (t + 1) * 128), slice(None)), "x%d_%d" % (q, t)) for t in range(NT)] for q in range(NSEQ)]
    C.ot = [[C.out.sub((q, slice(t * 128, (t + 1) * 128), slice(None)), "o%d_%d" % (q, t)) for t in range(NT)] for q in range(NSEQ)]

    def scr(name, shape, dt=F32):
        kind = "ExternalOutput" if name in dbg else "Internal"
        return P.track(Buf(nc.dram_tensor(name, list(shape), dt, kind=kind).ap(), name))

    C.tm = scr("tm_scr", [S, TM_W])
    C.fm = scr("fm_scr", [2560, S])
    if "y" in dbg:
        C.ydbg = P.track(Buf(nc.dram_tensor("ydbg", [128, 12, S], F32, kind="ExternalOutput").ap(), "ydbg"))
    return C


def setup_consts(P, C):
    K = Ctx()
    K.identf = P.sb("identf", [128, 128])
    K.identb = P.sb("identb", [128, 128], BF16)
    P.dma("sp", K.identf.v(), C.ident.v())
    P.copy(K.identb.v(), K.identf.v())
    K.cm = P.sb("cmat", [128, NCM, 128])
    P.dma("sp", K.cm.v(), C.cmat.v())
    K.cm4 = P.sb("cmat4", [128, 3, 4, 128])
    for i, cmi in enumerate((CM_NEGM, CM_STRICT, CM_IDENT)):
        for h in range(4):
            P.copy(K.cm4[:, i, h, :], K.cm[:, cmi, :], eng="pool")
    K.small = P.sb("small", [128, DEPTH, NSMALL])
    P.dma("sp", K.small.v(), C.small.r("l p n -> p l n"))
    K.cw = P.sb("cw", [128, DEPTH, 12, 4])
    P.dma("sp", K.cw.v(), C.cw.r("l p b k -> p l b k"))
    K.tbl = P.sb("tbl", [128, 4, 8, 128], BF16)
    K.rsel = P.sb("rsel", [32, NT, 128], BF16)
    K.selc = P.sb("selc", [128, NT, 2, 32])
    mt = P.mark()
    tblf = P.sb("tblf", [128, 4, 8, 128])
    P.dma("sp", tblf.v(), C.tbls.r("a j h i -> j a h i"))
    P.copy(K.tbl.v(), tblf.v(), eng="pool")
    rself = P.sb("rself", [32, NT, 128])
    P.dma("sp", rself.v(), C.rselc.v())
    P.copy(K.rsel.v(), rself.v(), eng="pool")
    P.dma("sp", K.selc.v(), C.selc.v())
    P.release(mt)
    return K


def norm_transpose(P, K, C, l, gi, hsrc, uT, wq="sp"):
    m = P.mark()
    hA = [P.sb("hA", [128, D]) for _ in range(2)]
    junk = P.sb("junkA", [128, D], BF16)
    ub = [P.sb("ub", [128, D], BF16) for _ in range(2)]
    ss = P.sb("ssA", [128, NT])
    rs = P.sb("rsA", [128, NT])
    pT = [P.ps("pTA", [128, 8, 128], BF16) for _ in range(2)]
    Gt = P.sb("Gt", [128, D])
    P.dma(wq, Gt.v(), C.gains_b[l, gi])
    P.dma(wq, hA[0].v(), hsrc[0].v())
    for t in range(NT):
        b = t % 2
        if t + 1 < NT:
            P.dma(wq, hA[(t + 1) % 2].v(), hsrc[t + 1].v())
        sst = ss.sub((slice(None), slice(t, t + 1)))
        rst = rs.sub((slice(None), slice(t, t + 1)))
        P.activation(junk.v(), hA[b].v(), AF.Square, accum_out=sst.v())
        P.activation(rst.v(), sst.v(), AF.Sqrt, bias=K.epsD.v(), scale=1.0 / D)
        P.recip(rst.v(), rst.v())
        P.stt(ub[b].v(), hA[b].v(), rst.v(), Gt.v(), ALU.mult, ALU.mult)
        for kc in range(8):
            P.tr(pT[b][:, kc, :], ub[b][:, kc * 128:(kc + 1) * 128], K.identb.v())
        if t % 2 == 0:
            P.copy(uT[:, :, t * 128:(t + 1) * 128], pT[b].v(), eng="act")
        else:
            P.copy(uT[:, :, t * 128:(t + 1) * 128], pT[b].v(), eng="dve")
    P.release(m)


class WStream:
    def __init__(self, P, nk, width, nstage=0, nbuf=3):
        self.P = P
        self.nk = nk
        self.width = width
        self.wb = [P.sb("wb", [128, nk, width], BF16) for _ in range(nbuf)]
        self.j = 0

    def load(self, wsrc_v, ncols, gain=None, nk=None):
        P = self.P
        nk = nk or self.nk
        wb = self.wb[self.j % len(self.wb)]
        self.j += 1
        P.dma("pool", wb[:, :nk, :ncols], wsrc_v.r("(k p) c -> p k c", p=128))
        return wb[:, :nk, :ncols]


def prefetch_iter(items, loader):
    nxt = loader(items[0]) if items else None
    for i, it_ in enumerate(items):
        cur = nxt
        if i + 1 < len(items):
            nxt = loader(items[i + 1])
        yield it_, cur


def phase_A(P, C, K, l, s, hsrc, uT):
    norm_transpose(P, K, C, l, 0, hsrc, uT)
    m = P.mark()
    ws = WStream(P, 8, 512, nbuf=3)
    w_in = C.w_in[l]
    blocks = []
    for c0, w in FM_RANGES:
        for cb in range(0, w, 512):
            blocks.append(("fm", c0, cb, 512))
    for c0, w in TM_RANGES:
        for cb in range(0, w, 512):
            blocks.append(("tm", c0, cb, min(512, w - cb)))
    pfm = [P.ps("pfm", [128, 512]) for _ in range(3)]
    sfm = [P.sb("sfm", [128, S]) for _ in range(2)]
    stm = [P.sb("stm", [128, 512]) for _ in range(4)]
    blk = 0
    it = 0
    ip = 0
    for (kind, c0, cb, nc_), wb in prefetch_iter(blocks, lambda b: ws.load(w_in[:, b[1] + b[2]:b[1] + b[2] + b[3]], b[3])):
        if kind == "fm":
            for j in range(4):
                st = sfm[blk % 2]
                for tg in range(4):
                    ps = pfm[ip % 3]
                    ip += 1
                    for kc in range(8):
                        P.mm(ps.v(), wb[:, kc, j * 128:(j + 1) * 128], uT[:, kc, tg * 512:(tg + 1) * 512],
                             start=(kc == 0), stop=(kc == 7))
                    P.copy(st[:, tg * 512:(tg + 1) * 512], ps.v(), eng=("act", "dve")[tg % 2])
                r0 = FM_OFF[c0] + cb + j * 128
                P.dma("sp", C.fm[r0:r0 + 128, :], st.v())
                blk += 1
        else:
            for t in range(NT):
                ps = pfm[ip % 3]
                ip += 1
                st = stm[it % 4]
                for kc in range(8):
                    P.mm(ps[:, :nc_], uT[:, kc, t * 128:(t + 1) * 128], wb[:, kc, :], start=(kc == 0), stop=(kc == 7))
                P.copy(st[:, :nc_], ps[:, :nc_], eng=("act", "dve")[it % 2])
                o = TM_OFF[c0] + cb
                P.dma("sp", C.tm[t * 128:(t + 1) * 128, o:o + nc_], st[:, :nc_])
                it += 1
    P.release(m)


STOPB = 0


class Banks:
    def __init__(self, P, n=8):
        self.t = [P.ps("bank", [128, 4, 128]) for _ in range(n)]
        self.i = 0

    def nxt(self):
        b = self.t[self.i % len(self.t)]
        self.i += 1
        return b


class RR:
    def __init__(self, engs):
        self.engs = engs
        self.i = 0

    def __call__(self):
        e = self.engs[self.i % len(self.engs)]
        self.i += 1
        return e


def scale_cols(P, out, in_, sc, eng):
    if eng == "act":
        P.activation(out, in_, AF.Copy, scale=sc)
    else:
        P.ts(out, in_, sc, ALU.mult, eng=eng)


def out_stage_bufs(P):
    OS = Ctx()
    OS.osq = P.sb("osq", [128, 4, 128])
    OS.oss = P.sb("oss", [128, 4])
    OS.zt = P.sb("zt", [128, 4, 128])
    return OS


def out_stage(P, C, K, bk, OS, osb, gcol, gfunc, gainB, yT, yoff, cg):
    zt, osq, oss = OS.zt, OS.osq, OS.oss
    P.dma("sp", zt.r("p h d -> p (h d)"), C.tm[cg * 128:(cg + 1) * 128, gcol:gcol + 512])
    P.activation(zt.v(), zt.v(), gfunc)
    P.activation(osq.v(), osb.v(), AF.Square)
    P.reduce(oss.v(), osq.v(), ALU.add)
    P.activation(oss.v(), oss.v(), AF.Sqrt, bias=K.epsD.v(), scale=1.0 / 128)
    P.recip(oss.v(), oss.v())
    for h in range(4):
        P.stt(osb[:, h, :], osb[:, h, :], oss[:, h:h + 1], gainB, ALU.mult, ALU.mult)
    P.tt(osb.v(), osb.v(), zt.v(), ALU.mult)
    y_ps = bk.nxt()
    for h in range(4):
        P.tr(y_ps[:, h, :], osb[:, h, :], K.identf.v())
    P.copy(yT[:, yoff:yoff + 4, cg * 128:(cg + 1) * 128], y_ps.v(), eng="act")


def phase_C(P, C, K, l, s, yT):
    m = P.mark()
    sm = K.small
    cm = K.cm
    NEGM4 = K.cm4[:, 0]
    bk = Banks(P, 8)
    flat = lambda b: b.r("p a b -> p (a b)")
    ext = lambda b, hh: flat(b)[:, 0:258].r("p (a b) -> p a b", b=129)[:, hh, :]
    gi = P.sb("gi", [128, NT, 8])
    ci = tm_col(C_BI)
    for c in range(NT):
        P.dma("sp", gi[:, c, :], C.tm[c * 128:(c + 1) * 128, ci:ci + 8])
    it = P.sb("it", [128, NT, 4])
    lf = P.sb("lf", [128, NT, 4])
    nbf = P.sb("nbf", [128, 4])
    P.ts(nbf.v(), sm[:, l, SM_BF:SM_BF + 4], -1.0, ALU.mult)
    for h in range(4):
        P.ts(it[:, :, h], gi[:, :, h], sm[:, l, SM_BI + h:SM_BI + h + 1], ALU.add)
        P.activation(lf[:, :, h], gi[:, :, 4 + h], AF.Exp, bias=nbf[:, h:h + 1], scale=-1.0)
    P.activation(lf.v(), lf.v(), AF.Ln, bias=1.0)
    P.ts(lf.v(), lf.v(), -1.0, ALU.mult)
    G1 = P.sb("G1", [128, NT, 4, 2])
    G2 = P.sb("G2", [128, NT, 4, 2])
    G3 = P.sb("G3", [128, NT, 4, 2])
    P.memset(G1.v(), 0.0)
    P.memset(G2.v(), 0.0)
    P.memset(G3.v(), 0.0)
    P.memset(G1[0:1, :, :, 1], 1.0)
    P.memset(G2[0:1, :, :, 0], 1.0)
    P.copy(G1[:, :, :, 0], lf.v())
    P.ts(G2[:, :, :, 1], lf.v(), -1.0, ALU.mult)
    P.copy(G3[:, :, :, 1], it.v())
    bc = P.sb("bc", [128, NT, 4])
    f64 = lambda b: b.r("p c h -> p (c h)")
    b0 = bk.nxt()
    P.mm(flat(b0)[:, 0:64], cm[:, CM_TRI, :], f64(lf))
    P.copy(f64(bc), flat(b0)[:, 0:64])
    b1 = bk.nxt()
    P.mm(flat(b1)[:, 0:64], cm[:, CM_SEL127, :], f64(bc))
    EBL = P.sb("EBL", [128, NT, 4])
    EKW = P.sb("EKW", [128, NT, 4])
    EB = P.sb("EB", [128, NT, 4])
    P.activation(f64(EBL), flat(b1)[:, 0:64], AF.Exp)
    P.tt(f64(EKW), flat(b1)[:, 0:64], f64(bc), ALU.subtract)
    P.tt(EKW.v(), EKW.v(), it.v(), ALU.add)
    P.activation(EKW.v(), EKW.v(), AF.Exp)
    P.activation(EB.v(), bc.v(), AF.Exp)
    qk = [P.sb("qk", [128, S]) for _ in range(8)]
    for blk in range(8):
        r0 = FM_OFF[C_BQ] + blk * 128
        P.dma("sp", qk[blk].v(), C.fm[r0:r0 + 128, :])
        if blk < 4:
            P.activation(qk[blk].v(), qk[blk].v(), AF.Copy, scale=128 ** -0.5)
    qT, kT = qk[0:4], qk[4:8]
    Cst = P.sb("Cst", [128, 4, 129])
    P.memset(Cst.v(), 0.0)
    k_tm = [P.sb("k_tm", [128, 4, 128]) for _ in range(2)]
    v_ext = [P.sb("v_ext", [128, 4, 129]) for _ in range(2)]
    for b in range(2):
        P.memset(v_ext[b][:, :, 128:129], 1.0)
    kw_tm = P.sb("kw_tm", [128, 4, 128])
    R1 = P.sb("R1", [2, 4, 128])
    R2 = P.sb("R2", [2, 4, 128])
    DT = P.sb("DT", [128, 4, 128])
    SmT = P.sb("SmT", [128, 4, 128])
    htmp = P.sb("htmp", [128, 4, 129])
    htot = P.sb("htot", [128, 4, 129])
    rden = P.sb("rden", [128, 4])
    osb = P.sb("osb", [128, 4, 128])
    OS = out_stage_bufs(P)
    gainB = sm[:, l, SM_MLN:SM_MLN + 128]
    ck, cv = tm_col(C_BK), tm_col(C_BV)
    for cg in range(NT):
        o = slice(cg * 128, (cg + 1) * 128)
        kt, ve = k_tm[cg % 2], v_ext[cg % 2]
        P.dma("sp", kt.r("p h d -> p (h d)"), C.tm[o, ck:ck + 512])
        P.dma("sp", ve[:, :, 0:128], C.tm[o, cv:cv + 512].r("p (h d) -> p h d", d=128))
        r_ps = bk.nxt()
        r2_ps = bk.nxt()
        for h in range(4):
            P.mm(r_ps[0:2, h, :], G1[:, cg, h, :], cm[:, CM_TRI, :])
        for h in range(4):
            P.mm(r2_ps[0:2, h, :], G2[:, cg, h, :], cm[:, CM_TRI, :], start=True, stop=False)
            P.mm(r2_ps[0:2, h, :], G3[:, cg, h, :], cm[:, CM_IDENT, :], start=False, stop=True)
        P.copy(R1.v(), r_ps[0:2, :, :], eng="dve")
        P.copy(R2.v(), r2_ps[0:2, :, :], eng="act")
        dt_ps = bk.nxt()
        for h in range(4):
            P.mm(dt_ps[:, h, :], R2[:, h, :], R1[:, h, :])
        P.tt(DT.v(), dt_ps.v(), NEGM4, ALU.add)
        P.activation(DT.v(), DT.v(), AF.Exp)
        kq_ps = bk.nxt()
        for h in range(4):
            P.mm(kq_ps[:, h, :], kT[h][:, o], qT[h][:, o])
        P.tt(SmT.v(), kq_ps.v(), DT.v(), ALU.mult)
        hq = [bk.nxt(), bk.nxt()]
        hs = [bk.nxt(), bk.nxt()]
        for h in range(4):
            P.mm(ext(hq[h // 2], h % 2), qT[h][:, o], Cst[:, h, :])
        for h in range(4):
            P.mm(ext(hs[h // 2], h % 2), SmT[:, h, :], ve[:, h, :])
        for h in range(4):
            P.activation(htmp[:, h, :], ext(hq[h // 2], h % 2), AF.Copy, scale=EB[:, cg, h:h + 1])
        for h in range(4):
            P.tt(htot[:, h, :], htmp[:, h, :], ext(hs[h // 2], h % 2), ALU.add)
        P.ts(rden.v(), htot[:, :, 128], -1.0, ALU.mult)
        P.tt(rden.v(), rden.v(), htot[:, :, 128], ALU.max)
        P.ts(rden.v(), rden.v(), 1.0, ALU.max)
        P.recip(rden.v(), rden.v())
        for h in range(4):
            scale_cols(P, osb[:, h, :], htot[:, h, 0:128], rden[:, h:h + 1], ("dve", "act")[h % 2])
        for h in range(4):
            scale_cols(P, kw_tm[:, h, :], kt[:, h, :], EKW[:, cg, h:h + 1], ("act", "dve")[h % 2])
        cps = [bk.nxt(), bk.nxt()]
        for h in range(4):
            P.mm(ext(cps[h // 2], h % 2), kw_tm[:, h, :], ve[:, h, :])
        for h in range(4):
            P.stt(Cst[:, h, :], Cst[:, h, :], EBL[:, cg, h:h + 1], ext(cps[h // 2], h % 2), ALU.mult, ALU.add)
        out_stage(P, C, K, bk, OS, osb, tm_col(C_BO), AF.Sigmoid, gainB, yT, 4, cg)
    P.release(m)


def phase_B(P, C, K, l, s, yT):
    m = P.mark()
    sm = K.small
    cm = K.cm
    NEGM4, STRICT4, IDENT4 = K.cm4[:, 0], K.cm4[:, 1], K.cm4[:, 2]
    bk = Banks(P, 8)
    rr = RR(("dve", "act", "pool"))
    rr2 = RR(("dve", "act"))
    ab = P.sb("ab", [128, NT, 8])
    ca = tm_col(C_AA)
    for c in range(NT):
        P.dma("sp", ab[:, c, :], C.tm[c * 128:(c + 1) * 128, ca:ca + 8])
    e1 = P.sb("e1", [128, NT, 4])
    g = P.sb("g", [128, NT, 4])
    nega = P.sb("nega", [128, 4])
    P.activation(nega.v(), sm[:, l, SM_ALOG:SM_ALOG + 4], AF.Exp)
    P.ts(nega.v(), nega.v(), -1.0, ALU.mult)
    for h in range(4):
        P.activation(e1[:, :, h], ab[:, :, h], AF.Exp, bias=sm[:, l, SM_DTB + h:SM_DTB + h + 1])
    P.activation(e1.v(), e1.v(), AF.Ln, bias=1.0)
    for h in range(4):
        P.ts(g[:, :, h], e1[:, :, h], nega[:, h:h + 1], ALU.mult)
    beta = P.sb("beta", [128, NT, 4])
    P.activation(beta.v(), ab[:, :, 4:8], AF.Sigmoid)
    nbeta = P.sb("nbeta", [128, NT, 4])
    P.ts(nbeta.v(), beta.v(), -1.0, ALU.mult)
    G1 = P.sb("G1", [128, NT, 4, 2])
    G2 = P.sb("G2", [128, NT, 4, 2])
    P.memset(G1.v(), 0.0)
    P.memset(G2.v(), 0.0)
    P.memset(G1[0:1, :, :, 1], 1.0)
    P.memset(G2[0:1, :, :, 0], 1.0)
    P.copy(G1[:, :, :, 0], g.v())
    P.ts(G2[:, :, :, 1], g.v(), -1.0, ALU.mult)
    gc = P.sb("gc", [128, NT, 4])
    b0 = bk.nxt()
    P.mm(b0.r("p a b -> p (a b)")[:, 0:64], cm[:, CM_TRI, :], g.r("p c h -> p (c h)"))
    P.copy(gc.r("p c h -> p (c h)"), b0.r("p a b -> p (a b)")[:, 0:64])
    b1 = bk.nxt()
    P.mm(b1.r("p a b -> p (a b)")[:, 0:64], cm[:, CM_SEL127, :], gc.r("p c h -> p (c h)"))
    EGL = P.sb("EGL", [128, NT, 4])
    ED = P.sb("ED", [128, NT, 4])
    EG = P.sb("EG", [128, NT, 4])
    P.activation(EGL.r("p c h -> p (c h)"), b1.r("p a b -> p (a b)")[:, 0:64], AF.Exp)
    P.tt(ED.r("p c h -> p (c h)"), b1.r("p a b -> p (a b)")[:, 0:64], gc.r("p c h -> p (c h)"), ALU.subtract)
    P.activation(ED.v(), ED.v(), AF.Exp)
    P.activation(EG.v(), gc.v(), AF.Exp)
    if STOPB == 1:
        P.release(m)
        return
    Sst = P.sb("Sst", [128, 4, 128])
    P.memset(Sst.v(), 0.0)
    HW = 1024
    qkv = [P.sb("qkv", [128, HW]) for _ in range(12)]
    raw = [P.sb("raw", [128, HW + 3]) for _ in range(2)]
    sqt = P.sb("sqt", [128, HW])
    rnt = P.sb("rnt", [128, 512])
    gainB = sm[:, l, SM_GDNN:SM_GDNN + 128]
    for hf in range(2):
        t0 = hf * HW
        for blk in range(12):
            r = raw[blk % 2]
            r0 = FM_OFF[C_AQ] + blk * 128
            if hf == 0:
                P.memset(r[:, 0:3], 0.0)
                P.dma("sp", r[:, 3:], C.fm[r0:r0 + 128, 0:HW])
            else:
                P.dma("sp", r.v(), C.fm[r0:r0 + 128, t0 - 3:t0 + HW])
            dst = qkv[blk]
            e = "dve"
            P.ts(dst.v(), r[:, 0:HW], K.cw[:, l, blk, 0:1], ALU.mult, eng=e)
            for k in range(1, 4):
                P.stt(dst.v(), r[:, k:k + HW], K.cw[:, l, blk, k:k + 1], dst.v(), ALU.mult, ALU.add, eng=e)
            P.activation(dst.v(), dst.v(), AF.Silu)
            if blk < 8:
                P.activation(sqt.v(), dst.v(), AF.Square)
                for hh in range(2):
                    bb = bk.nxt()
                    bbv = bb.r("p a b -> p (a b)")
                    P.mm(bbv, cm[:, CM_ONES, :], sqt[:, hh * 512:(hh + 1) * 512])
                    P.activation(rnt.v(), bbv, AF.Sqrt, bias=K.eps6.v())
                    P.recip(rnt.v(), rnt.v())
                    sc = (128 ** -0.5) if blk < 4 else 1.0
                    P.stt(dst[:, hh * 512:(hh + 1) * 512], dst[:, hh * 512:(hh + 1) * 512], sc, rnt.v(), ALU.mult, ALU.mult)
        if STOPB == 2:
            P.release(m)
            return
        qT = qkv[0:4]
        kT = qkv[4:8]
        vT = qkv[8:12]
        for c in range(8):
            cg = hf * 8 + c
            o = slice(c * 128, (c + 1) * 128)
            kt_ps = bk.nxt()
            vt_ps = bk.nxt()
            for h in range(4):
                P.tr(kt_ps[:, h, :], kT[h][:, o], K.identf.v())
                P.tr(vt_ps[:, h, :], vT[h][:, o], K.identf.v())
            v_tm = P.sb("v_tm", [128, 4, 128]) if c == 0 and hf == 0 else v_tm
            kg_tm = P.sb("kg_tm", [128, 4, 128]) if c == 0 and hf == 0 else kg_tm
            kd_tm = P.sb("kd_tm", [128, 4, 128]) if c == 0 and hf == 0 else kd_tm
            P.copy(v_tm.v(), vt_ps.v(), eng="act")
            for h in range(4):
                scale_cols(P, kg_tm[:, h, :], kt_ps[:, h, :], EG[:, cg, h:h + 1], "dve")
                scale_cols(P, kd_tm[:, h, :], kt_ps[:, h, :], ED[:, cg, h:h + 1], "dve")
            r_ps = bk.nxt()
            for h in range(4):
                P.mm(r_ps[0:2, h, :], G1[:, cg, h, :], cm[:, CM_TRI, :])
            r2_ps = bk.nxt()
            for h in range(4):
                P.mm(r2_ps[0:2, h, :], G2[:, cg, h, :], cm[:, CM_TRI, :])
            if c == 0 and hf == 0:
                R1 = P.sb("R1", [2, 4, 128])
                R2 = P.sb("R2", [2, 4, 128])
                DT = P.sb("DT", [128, 4, 128])
                DTS = P.sb("DTS", [128, 4, 128])
                attnT = P.sb("attnT", [128, 4, 128])
                Bm = P.sb("Bm", [128, 4, 128])
                BT = P.sb("BT", [128, 4, 128])
                Rm = [P.sb("Rm", [128, 4, 128]) for _ in range(2)]
                Pk = [P.sb("Pk", [128, 4, 128]) for _ in range(2)]
                PkT = [P.sb("PkT", [128, 4, 128]) for _ in range(2)]
                nwT = P.sb("nwT", [128, 4, 128])
                vn = P.sb("vn", [128, 4, 128])
                otmp = P.sb("otmp", [128, 4, 128])
                osb = P.sb("osb", [128, 4, 128])
                OS = out_stage_bufs(P)
            P.copy(R1.v(), r_ps[0:2, :, :], eng="dve")
            P.copy(R2.v(), r2_ps[0:2, :, :], eng="act")
            if STOPB == 3:
                P.release(m)
                return
            dt_ps = bk.nxt()
            for h in range(4):
                P.mm(dt_ps[:, h, :], R2[:, h, :], R1[:, h, :])
            P.tt(DT.v(), dt_ps.v(), NEGM4, ALU.add)
            P.activation(DT.v(), DT.v(), AF.Exp)
            P.tt(DTS.v(), DT.v(), STRICT4, ALU.mult)
            kq_ps = bk.nxt()
            kk_ps = bk.nxt()
            for h in range(4):
                P.mm(kq_ps[:, h, :], kT[h][:, o], qT[h][:, o])
                P.mm(kk_ps[:, h, :], kT[h][:, o], kT[h][:, o])
            P.tt(attnT.v(), kq_ps.v(), DT.v(), ALU.mult)
            for h in range(4):
                P.stt(Bm[:, h, :], kk_ps[:, h, :], nbeta[:, cg, h:h + 1], DTS[:, h, :], ALU.mult, ALU.mult)
            if STOPB == 4:
                P.release(m)
                return
            bt_ps = bk.nxt()
            for h in range(4):
                P.tr(bt_ps[:, h, :], Bm[:, h, :], K.identf.v())
            P.copy(BT.v(), bt_ps.v(), eng="act")
            P.tt(Rm[0].v(), Bm.v(), IDENT4, ALU.add)
            cur, curT, R = Bm, BT, Rm[0]
            for k in range(1, 7):
                nP, nPT, nR = Pk[k % 2], PkT[k % 2], Rm[k % 2]
                pT_ps = bk.nxt()
                for h in range(4):
                    P.mm(pT_ps[:, h, :], cur[:, h, :], curT[:, h, :])
                if k < 6:
                    p_ps = bk.nxt()
                    for h in range(4):
                        P.mm(p_ps[:, h, :], curT[:, h, :], cur[:, h, :])
                P.copy(nPT.v(), pT_ps.v(), eng="act")
                if k < 6:
                    P.copy(nP.v(), p_ps.v(), eng="dve")
                rr_ps = bk.nxt()
                for h in range(4):
                    P.mm(rr_ps[:, h, :], nPT[:, h, :], R[:, h, :])
                P.tt(nR.v(), R.v(), rr_ps.v(), ALU.add)
                cur, curT, R = nP, nPT, nR
            Tt = R
            w_ps = bk.nxt()
            for h in range(4):
                P.mm(w_ps[:, h, :], kg_tm[:, h, :], Tt[:, h, :])
            P.activation(nwT.v(), w_ps.v(), AF.Copy, scale=-1.0)
            if STOPB == 5:
                P.release(m)
                return
            vn_ps = bk.nxt()
            o1_ps = bk.nxt()
            for h in range(4):
                P.mm(vn_ps[:, h, :], Tt[:, h, :], v_tm[:, h, :], start=True, stop=False)
                P.mm(vn_ps[:, h, :], nwT[:, h, :], Sst[:, h, :], start=False, stop=True)
                P.mm(o1_ps[:, h, :], qT[h][:, o], Sst[:, h, :])
            for h in range(4):
                scale_cols(P, vn[:, h, :], vn_ps[:, h, :], beta[:, cg, h:h + 1], "dve")
                scale_cols(P, otmp[:, h, :], o1_ps[:, h, :], EG[:, cg, h:h + 1], "act")
            o2_ps = bk.nxt()
            s_ps = bk.nxt()
            for h in range(4):
                P.mm(o2_ps[:, h, :], attnT[:, h, :], vn[:, h, :])
                P.mm(s_ps[:, h, :], kd_tm[:, h, :], vn[:, h, :])
            P.tt(osb.v(), otmp.v(), o2_ps.v(), ALU.add)
            for h in range(4):
                P.stt(Sst[:, h, :], Sst[:, h, :], EGL[:, cg, h:h + 1], s_ps[:, h, :], ALU.mult, ALU.add)
            if STOPB == 6:
                P.release(m)
                return
            out_stage(P, C, K, bk, OS, osb, tm_col(C_AZ), AF.Silu, gainB, yT, 0, cg)
            if STOPB == 8 + cg:
                P.release(m)
                return
    P.release(m)


def phase_D(P, C, K, l, s, yT):
    m = P.mark()
    sm = K.small
    cm = K.cm
    bk = Banks(P, 2)
    bkA = Banks(P, 2)
    bkS = Banks(P, 4)
    flat = lambda b: b.r("p a b -> p (a b)")
    kTs = P.sb("kTs", [64, 2, S], BF16)
    kTw = P.sb("kTw", [64, 2, S], BF16)
    vs = P.sb("vs", [128, NT, 2, 65], BF16)
    vw = P.sb("vw", [128, NT, 2, 65], BF16)
    P.memset(vs[:, :, :, 64:65], 1.0)
    P.memset(vw[:, :, :, 64:65], 1.0)
    ckT = P.sb("ckT", [64, 2, 128])
    cvx = P.sb("cvx", [128, 2, 97])
    P.memset(ckT.v(), 0.0)
    P.memset(cvx.v(), 0.0)
    P.memset(cvx[:, :, 64:65], 1.0)
    for g in range(2):
        P.dma("sp", cvx[:, g, 65:97], C.ovm.v())
    gq8 = P.sb("gq8", [128, 8, 64])
    gk = P.sb("gk", [128, 4, 64])
    for h in range(8):
        P.ts(gq8[:, h, :], sm[:, l, SM_QN:SM_QN + 64], 0.125, ALU.mult)
    for a in range(2):
        for g in range(2):
            P.copy(gk[:, 2 * a + g, :], sm[:, l, SM_KN + 64 * (a + 1):SM_KN + 64 * (a + 2)])
    kn0 = P.sb("kn0", [64, 1])
    P.dma("sp", kn0.v(), C.kn0[l])
    ckc = tm_col(C_CKC)
    m1 = P.mark()
    kcT = P.sb("kcT", [64, 2, S])
    vcT = P.sb("vcT", [64, 2, S])
    Wc = P.sb("Wc", [64, 2, 32, 64])
    peT = P.sb("peT", [64, 2, 32])
    P.dma("sp", Wc.v(), C.wcmp[l])
    P.dma("sp", peT.v(), C.peT[l])
    kv = [P.sb("kv", [128, 6, 2, 64]) for _ in range(2)]
    ksq = P.sb("ksq", [128, 2, 2, 64])
    kss = P.sb("kss", [128, 2, 2])
    kn = P.sb("kn", [128, 2, 2, 64])
    for t in range(NT):
        b = kv[t % 2]
        o = slice(t * 128, (t + 1) * 128)
        P.dma("sp", b.r("p a g d -> p (a g d)"), C.tm[o, ckc:ckc + 768])
        P.copy(vs[:, t, :, 0:64], b[:, 3, :, :], eng="act")
        P.copy(vw[:, t, :, 0:64], b[:, 5, :, :], eng="dve")
        P.activation(ksq.v(), b[:, 2:6:2, :, :], AF.Square)
        P.reduce(kss.v(), ksq.v(), ALU.add)
        P.activation(kss.v(), kss.v(), AF.Sqrt, bias=K.epsD.v(), scale=1.0 / 64)
        P.recip(kss.v(), kss.v())
        for a in range(2):
            for g in range(2):
                P.stt(kn[:, a, g, :], b[:, 2 + 2 * a, g, :], kss[:, a, g:g + 1], gk[:, 2 * a + g, :], ALU.mult, ALU.mult)
        ps1 = bk.nxt()
        ps2 = bk.nxt()
        for a in range(2):
            for g in range(2):
                P.tr(ps1[0:64, 2 * a + g, :], kn[:, a, g, :], K.identf.v())
                P.tr(ps2[0:64, 2 * a + g, :], b[:, a, g, :], K.identf.v())
        P.copy(kTs[:, :, o], ps1[0:64, 0:2, :], eng="act")
        P.copy(kTw[:, :, o], ps1[0:64, 2:4, :], eng="act")
        P.copy(kcT[:, :, o], ps2[0:64, 0:2, :], eng="dve")
        P.copy(vcT[:, :, o], ps2[0:64, 2:4, :], eng="dve")
    cst = P.sb("cst", [64, 2])
    raw = P.sb("rawc", [64, 127])
    sqc = P.sb("sqc", [64, 127])
    rnc = P.sb("rnc", [64, 127])
    for kvi in range(2):
        pc = bk.nxt()
        for lq in range(32):
            P.mm(flat(pc)[0:64, 0:1], Wc[:, kvi, lq, :], peT[:, kvi, lq:lq + 1], start=(lq == 0), stop=(lq == 31))
        P.copy(cst[:, kvi:kvi + 1], flat(pc)[0:64, 0:1])
    for kvi in range(2):
        src = (kcT, vcT)[kvi]
        for g in range(2):
            ps = bk.nxt()
            pv = flat(ps)[0:64, 0:127]
            for lq in range(32):
                P.mm(pv, Wc[:, kvi, lq, :], src[:, g, lq:lq + 16 * 126 + 1:16], start=(lq == 0), stop=(lq == 31))
            P.ts(raw.v(), pv, cst[:, kvi:kvi + 1], ALU.add)
            if kvi == 0:
                P.activation(sqc.v(), raw.v(), AF.Square)
                p2 = bk.nxt()
                P.mm(flat(p2)[0:64, 0:127], cm[0:64, CM_ONES, 0:64], sqc.v())
                P.activation(rnc.v(), flat(p2)[0:64, 0:127], AF.Sqrt, bias=K.epsD[0:64, :], scale=1.0 / 64)
                P.recip(rnc.v(), rnc.v())
                P.stt(ckT[:, g, 0:127], raw.v(), kn0[:, 0:1], rnc.v(), ALU.mult, ALU.mult)
            else:
                p2 = bk.nxt()
                P.tr(p2[0:127, 0, 0:64], raw.v(), K.identf[0:64, 0:64])
                P.copy(cvx[0:127, g, 0:64], p2[0:127, 0, 0:64])
    P.release(m1)
    qt = [P.sb("qt", [128, 8, 64]) for _ in range(2)]
    gt = [P.sb("gt", [128, 8, 3]) for _ in range(2)]
    qsq = P.sb("qsq", [128, 8, 64])
    qss = P.sb("qss", [128, 8])
    qn = P.sb("qn", [128, 8, 64])
    gs = P.sb("gs", [128, 8, 3])
    qTf = P.sb("qTf", [64, 4, 128])
    qTb = P.sb("qTb", [64, 4, 128], BF16)
    bct = [P.sb("bct", [128, 4, 128]) for _ in range(2)]
    ec = P.sb("ec", [128, 4, 128])
    esb = [P.sb("esb", [128, 4, 128], BF16) for _ in range(3)]
    rc = P.sb("rc", [128, 3, 4])
    imp = P.sb("imp", [128, 32])
    mx8 = P.sb("mx8", [128, 8])
    selm = P.sb("selm", [128, 32])
    selT = P.sb("selT", [32, 4, 128], BF16)
    yc = P.sb("yc", [128, 8, 64])
    cq_, cg_ = tm_col(C_CQ), tm_col(C_CG)
    ie = 0
    ib = 0
    for it in range(NT):
        o = slice(it * 128, (it + 1) * 128)
        q_, g_ = qt[it % 2], gt[it % 2]
        P.dma("sp", q_.r("p h d -> p (h d)"), C.tm[o, cq_:cq_ + 512])
        P.dma("sp", g_.r("p h k -> p (h k)"), C.tm[o, cg_:cg_ + 24])
        P.activation(qsq.v(), q_.v(), AF.Square)
        P.reduce(qss.v(), qsq.v(), ALU.add)
        P.activation(qss.v(), qss.v(), AF.Sqrt, bias=K.epsD.v(), scale=1.0 / 64)
        P.recip(qss.v(), qss.v())
        for h in range(8):
            P.stt(qn[:, h, :], q_[:, h, :], qss[:, h:h + 1], gq8[:, h, :], ALU.mult, ALU.mult)
        P.activation(gs.v(), g_.v(), AF.Sigmoid)
        for g in range(2):
            hs_ = slice(4 * g, 4 * g + 4)
            qps = bk.nxt()
            for hh in range(4):
                P.tr(qps[0:64, hh, :], qn[:, 4 * g + hh, :], K.identf.v())
            P.copy(qTf.v(), qps[0:64, :, :], eng="act")
            P.copy(qTb.v(), qps[0:64, :, :], eng="dve")
            bc_ = bct[ib % 2]
            ib += 1
            P.dma("sp", bc_.v(), C.biasC[it, g])
            sc = bk.nxt()
            P.mm(flat(sc), ckT[:, g, :], flat(qTf), start=True, stop=False)
            P.mm(flat(sc), cm[:, CM_IDENT, :], flat(bc_), start=False, stop=True)
            P.activation(ec.v(), sc.v(), AF.Exp)
            oc = bk.nxt()
            ocv = flat(oc)[:, 0:388].r("p (h c) -> p h c", c=97)
            for hh in range(4):
                P.mm(ocv[:, hh, :], ec[:, hh, :], cvx[:, g, :])
            P.ts(rc[:, 0, :], ocv[:, :, 64], 1e-30, ALU.max)
            P.recip(rc[:, 0, :], rc[:, 0, :])
            P.ts(imp.v(), ocv[:, 0, 65:97], rc[:, 0, 0:1], ALU.mult)
            for hh in range(1, 4):
                P.stt(imp.v(), ocv[:, hh, 65:97], rc[:, 0, hh:hh + 1], imp.v(), ALU.mult, ALU.add)
            P.tt(rc[:, 0, :], rc[:, 0, :], gs[:, hs_, 0], ALU.mult)
            for hh in range(4):
                P.ts(yc[:, 4 * g + hh, :], ocv[:, hh, 0:64], rc[:, 0, hh:hh + 1], ALU.mult)
            P.tt(imp.v(), imp.v(), K.selc[:, it, 0, :], ALU.mult)
            P.tt(imp.v(), imp.v(), K.selc[:, it, 1, :], ALU.add)
            ia, ma = imp.ap, mx8.ap
            P.op("dve", lambda e, ia=ia, ma=ma: e.max(out=ma, in_=ia), [imp], [mx8])
            P.ts(selm.v(), imp.v(), mx8[:, 3:4], ALU.is_ge, -1.0, ALU.add)
            sps = bk.nxt()
            P.tr(sps[0:32, 0, :], selm.v(), K.identf.v())
            for hh in range(4):
                P.copy(selT[:, hh, :], sps[0:32, 0, :], eng=("act", "dve")[hh % 2])
            osel = bkA.nxt()
            osv = flat(osel)[:, 0:260].r("p (h c) -> p h c", c=65)
            for jt in range(it + 1):
                off = it - jt
                ti = min(off, 2)
                sp_ = bkS.nxt()
                P.mm(flat(sp_), kTs[:, g, jt * 128:(jt + 1) * 128], flat(qTb), start=True, stop=False)
                P.mm(flat(sp_), K.identb.v(), K.tbl[:, ti, hs_, :].r("p h i -> p (h i)"), start=False, stop=False)
                P.mm(flat(sp_), K.rsel[:, jt, :], flat(selT), start=False, stop=True)
                es = esb[ie % 3]
                ie += 1
                P.activation(es.v(), sp_.v(), AF.Exp)
                for hh in range(4):
                    P.mm(osv[:, hh, :], es[:, hh, :], vs[:, jt, g, :], start=(jt == 0 and hh == 0), stop=(jt == it), skip=True)
            P.ts(rc[:, 1, :], osv[:, :, 64], 1e-30, ALU.max)
            P.recip(rc[:, 1, :], rc[:, 1, :])
            P.tt(rc[:, 1, :], rc[:, 1, :], gs[:, hs_, 1], ALU.mult)
            for hh in range(4):
                P.stt(yc[:, 4 * g + hh, :], osv[:, hh, 0:64], rc[:, 1, hh:hh + 1], yc[:, 4 * g + hh, :], ALU.mult, ALU.add)
            owin = bkA.nxt()
            owv = flat(owin)[:, 0:260].r("p (h c) -> p h c", c=65)
            j0 = max(0, it - 4)
            for jt in range(j0, it + 1):
                off = it - jt
                ti = (0, 1, 2, 2, 3)[off]
                sp_ = bkS.nxt()
                P.mm(flat(sp_), kTw[:, g, jt * 128:(jt + 1) * 128], flat(qTb), start=True, stop=False)
                P.mm(flat(sp_), K.identb.v(), K.tbl[:, ti, hs_, :].r("p h i -> p (h i)"), start=False, stop=True)
                es = esb[ie % 3]
                ie += 1
                P.activation(es.v(), sp_.v(), AF.Exp)
                for hh in range(4):
                    P.mm(owv[:, hh, :], es[:, hh, :], vw[:, jt, g, :], start=(jt == j0 and hh == 0), stop=(jt == it), skip=True)
            P.ts(rc[:, 2, :], owv[:, :, 64], 1e-30, ALU.max)
            P.recip(rc[:, 2, :], rc[:, 2, :])
            P.tt(rc[:, 2, :], rc[:, 2, :], gs[:, hs_, 2], ALU.mult)
            for hh in range(4):
                P.stt(yc[:, 4 * g + hh, :], owv[:, hh, 0:64], rc[:, 2, hh:hh + 1], yc[:, 4 * g + hh, :], ALU.mult, ALU.add)
        y_ps = bk.nxt()
        for c4 in range(4):
            P.tr(y_ps[:, c4, :], yc[:, 2 * c4:2 * c4 + 2, :].r("p h d -> p (h d)"), K.identf.v())
        P.copy(yT[:, 8:12, o], y_ps.v(), eng="act")
    P.release(m)


def phase_E(P, C, K, l, s, uT, yT, hin, hout):
    m0 = P.mark()
    mergedT = P.sb("mergedT", [128, 8, S], BF16)
    m1 = P.mark()
    wsg = WStream(P, 8, 256, nbuf=6)
    wsb = WStream(P, 4, 256, nbuf=6)
    pg = [P.ps("pg", [128, 512]) for _ in range(3)]
    pb = [P.ps("pb", [128, 512]) for _ in range(3)]
    gsb = [P.sb("gsb", [128, 512]) for _ in range(3)]
    acc = [P.sb("acc", [128, 512]) for _ in range(2)]
    it = 0
    k3 = 0

    def ld(q):
        wg, wbr = [], []
        for n in range(3):
            c0 = C_MG + n * 1024 + q * 256
            wg.append(wsg.load(C.w_in[l][:, c0:c0 + 256], 256))
            wbr.append(wsb.load(C.w_branch[l, n][:, q * 256:(q + 1) * 256], 256))
        return wg, wbr
    for q, (wg, wbr) in prefetch_iter(list(range(4)), ld):
        for j in range(2):
            fblk = q * 2 + j
            for tg in range(4):
                a = acc[it % 2]
                tsl = slice(tg * 512, (tg + 1) * 512)
                for n in range(3):
                    g_ps, b_ps, gs = pg[k3 % 3], pb[k3 % 3], gsb[k3 % 3]
                    k3 += 1
                    for kc in range(8):
                        P.mm(g_ps.v(), wg[n][:, kc, j * 128:(j + 1) * 128], uT[:, kc, tsl], start=(kc == 0), stop=(kc == 7))
                    for kc in range(4):
                        P.mm(b_ps.v(), wbr[n][:, kc, j * 128:(j + 1) * 128], yT[:, n * 4 + kc, tsl], start=(kc == 0), stop=(kc == 3))
                    P.activation(gs.v(), g_ps.v(), AF.Sigmoid)
                    if n == 0:
                        P.tt(a.v(), gs.v(), b_ps.v(), ALU.mult)
                    else:
                        P.tt(gs.v(), gs.v(), b_ps.v(), ALU.mult)
                        if n == 1:
                            P.tt(a.v(), a.v(), gs.v(), ALU.add)
                        else:
                            P.tt(mergedT[:, fblk, tsl], a.v(), gs.v(), ALU.add)
                it += 1
    P.release(m1)
    ws = WStream(P, 8, 512, nbuf=2)
    po = [P.ps("po", [128, 512]) for _ in range(3)]
    hsb = [P.sb("hsb", [128, 512]) for _ in range(4)]
    items = [(half, t) for half in range(2) for t in range(NT)]
    wts = [ws.load(C.w_out[l][:, half * 512:(half + 1) * 512], 512) for half in range(2)]

    def ldh(x):
        half, t = x
        hi = hsb[(half * NT + t) % 4]
        P.dma("sp", hi.v(), hin[t][:, half * 512:(half + 1) * 512])
        return hi
    it = 0
    for (half, t), hi in prefetch_iter(items, ldh):
        csl = slice(half * 512, (half + 1) * 512)
        w = wts[half]
        ps = po[it % 3]
        it += 1
        for kc in range(8):
            P.mm(ps.v(), mergedT[:, kc, t * 128:(t + 1) * 128], w[:, kc, :], start=(kc == 0), stop=(kc == 7))
        P.tt(hi.v(), hi.v(), ps.v(), ALU.add)
        P.dma("sp", hout[t][:, csl], hi.v())
    P.release(m0)


def phase_F(P, C, K, l, s, uT, hout):
    norm_transpose(P, K, C, l, 1, hout, uT)
    m = P.mark()
    actT = P.sb("actT", [128, 22, S], BF16)
    m2 = P.mark()
    wsg = WStream(P, 8, 256, nbuf=3)
    wsu = WStream(P, 8, 256, nbuf=3)
    pgt = [P.ps("pgt", [128, 512]) for _ in range(3)]
    pup = [P.ps("pup", [128, 512]) for _ in range(3)]
    sg = [P.sb("sg", [128, 512]) for _ in range(3)]
    it = 0

    def ldf(q):
        return (wsg.load(C.w_ffn_in[l][:, q * 256:(q + 1) * 256], 256),
                wsu.load(C.w_ffn_in[l][:, D_FF + q * 256:D_FF + (q + 1) * 256], 256))
    for q, (wg, wu) in prefetch_iter(list(range(11)), ldf):
        for j in range(2):
            fblk = q * 2 + j
            for tg in range(4):
                tsl = slice(tg * 512, (tg + 1) * 512)
                g_ps, u_ps, sgt = pgt[it % 3], pup[it % 3], sg[it % 3]
                it += 1
                for kc in range(8):
                    P.mm(g_ps.v(), wg[:, kc, j * 128:(j + 1) * 128], uT[:, kc, tsl], start=(kc == 0), stop=(kc == 7))
                for kc in range(8):
                    P.mm(u_ps.v(), wu[:, kc, j * 128:(j + 1) * 128], uT[:, kc, tsl], start=(kc == 0), stop=(kc == 7))
                P.activation(sgt.v(), g_ps.v(), AF.Silu)
                P.tt(actT[:, fblk, tsl], sgt.v(), u_ps.v(), ALU.mult)
    P.release(m2)
    ws = WStream(P, 22, 256, nbuf=2)
    po = [P.ps("po2", [128, 512]) for _ in range(3)]
    hsb = [P.sb("hsb2", [128, 256]) for _ in range(4)]
    items = [(qd, t) for qd in range(4) for t in range(NT)]
    wts = {}

    def ldh2(x):
        qd, t = x
        if t == 0:
            wts[qd] = ws.load(C.w_ffn_out[l][:, qd * 256:(qd + 1) * 256], 256)
        hi = hsb[(qd * NT + t) % 4]
        P.dma("sp", hi.v(), hout[t][:, qd * 256:(qd + 1) * 256])
        return hi
    it = 0
    for (qd, t), hi in prefetch_iter(items, ldh2):
        csl = slice(qd * 256, (qd + 1) * 256)
        w = wts[qd]
        ps = po[it % 3]
        it += 1
        for kc in range(22):
            P.mm(ps[:, 0:256], actT[:, kc, t * 128:(t + 1) * 128], w[:, kc, :], start=(kc == 0), stop=(kc == 21))
        P.tt(hi.v(), hi.v(), ps[:, 0:256], ALU.add)
        P.dma("sp", hout[t][:, csl], hi.v())
    P.release(m)
    norm_transpose(P, K, C, l, 2, hout, uT)
    m = P.mark()
    pTb = P.sb("pTb", [128, 2, S], BF16)
    P.dma("pool", pTb.v(), C.pT[l, s].r("(k p) t -> p k t", p=128))
    wsg = WStream(P, 8, 512, nbuf=2)
    wsp = WStream(P, 2, 512, nbuf=2)
    pg = [P.ps("pg3", [128, 512]) for _ in range(2)]
    pp = [P.ps("pp3", [128, 512]) for _ in range(2)]
    gsb = [P.sb("gsb3", [128, 512]) for _ in range(3)]
    hsb = [P.sb("hsb3", [128, 512]) for _ in range(4)]
    wgs = [wsg.load(C.w_ple_gate[l][:, half * 512:(half + 1) * 512], 512) for half in range(2)]
    wps = [wsp.load(C.w_ple_proj[l][:, half * 512:(half + 1) * 512], 512) for half in range(2)]
    items = [(half, t) for half in range(2) for t in range(NT)]

    def ldh3(x):
        half, t = x
        hi = hsb[(half * NT + t) % 4]
        P.dma("sp", hi.v(), hout[t][:, half * 512:(half + 1) * 512])
        return hi
    it = 0
    for (half, t), hi in prefetch_iter(items, ldh3):
        csl = slice(half * 512, (half + 1) * 512)
        wg, wp = wgs[half], wps[half]
        g_ps, p_ps, gs = pg[it % 2], pp[it % 2], gsb[it % 3]
        it += 1
        for kc in range(8):
            P.mm(g_ps.v(), uT[:, kc, t * 128:(t + 1) * 128], wg[:, kc, :], start=(kc == 0), stop=(kc == 7))
        for kc in range(2):
            P.mm(p_ps.v(), pTb[:, kc, t * 128:(t + 1) * 128], wp[:, kc, :], start=(kc == 0), stop=(kc == 1))
        P.activation(gs.v(), g_ps.v(), AF.Sigmoid)
        P.tt(gs.v(), gs.v(), p_ps.v(), ALU.mult)
        P.tt(hi.v(), hi.v(), gs.v(), ALU.add)
        P.dma("sp", hout[t][:, csl], hi.v())
    P.release(m)


def build(n_layers=DEPTH, n_seq=NSEQ, phases="AE", dbg=(), dbg_yT=False):
    nc = bass.Bass("TRN2", target_bir_lowering=False)
    P = Prog(nc)
    C = declare(P, dbg)
    K = setup_consts(P, C)
    K.epsD = P.sb("epsD", [128, 1])
    P.memset(K.epsD.v(), EPS)
    K.eps6 = P.sb("eps6", [128, 1])
    P.memset(K.eps6.v(), 1e-6)
    if dbg_yT:
        ydbg = P.track(Buf(nc.dram_tensor("yT_dbg", [128, 12, S], F32, kind="ExternalInput").ap(), "yT_dbg"))
    for s in range(n_seq):
        for l in range(n_layers):
            hin = C.xt[s] if l == 0 else C.ot[s]
            my = P.mark()
            yT = P.sb("yT", [128, 12, S], BF16)
            mu = P.mark()
            uT = P.sb("uT", [128, 8, S], BF16)
            if "A" in phases:
                phase_A(P, C, K, l, s, hin, uT)
            P.release(mu)
            if "B" in phases:
                phase_B(P, C, K, l, s, yT)
            if "C" in phases:
                phase_C(P, C, K, l, s, yT)
            if "D" in phases:
                phase_D(P, C, K, l, s, yT)
            if "y" in dbg:
                mm_ = P.mark()
                yf = P.sb("yf", [128, 12, S])
                P.copy(yf.v(), yT.v(), eng="pool")
                P.dma("sp", C.ydbg.v(), yf.v())
                P.release(mm_)
            uT = P.sb("uT", [128, 8, S], BF16)
            norm_transpose(P, K, C, l, 0, hin, uT)
            if dbg_yT:
                m = P.mark()
                yf = P.sb("yf", [128, 12, S])
                P.dma("sp", yf.v(), ydbg.v())
                P.copy(yT.v(), yf.v(), eng="pool")
                P.release(m)
            if "E" in phases:
                phase_E(P, C, K, l, s, uT, yT, hin, C.ot[s])
            P.release(my)
            if "E" in phases:
                uT = P.sb("uT", [128, 8, S], BF16)
                phase_F(P, C, K, l, s, uT, C.ot[s])
                P.release(my)
    st = P.emit()
    P.close()
    return nc, st


import math
NEGB = -30000.0


def rel_bucket_np(dist):
    d = np.maximum(dist, 0)
    df = np.maximum(d, 1).astype(np.float32)
    large = 16 + (np.log(df / np.float32(16)) / np.float32(math.log(128 / 16)) * np.float32(16)).astype(np.int32)
    large = np.minimum(large, 31)
    return np.where(d < 16, d, large).astype(np.int64)


def nsa_consts(inp):
    rb = inp["rel_bias"].astype(np.float32)
    d = {}
    j = np.arange(128)[:, None]
    i = np.arange(128)[None, :]
    neg = np.float32(NEGB)

    def gat(dist, valid):
        g = rb[rel_bucket_np(dist)]
        g = np.where(valid[..., None], g, neg)
        return np.ascontiguousarray(g.transpose(0, 2, 1))
    tb = np.zeros((4, 128, 8, 128), np.float32)
    tb[0] = gat(i - j, (i - j) >= 0)
    tb[1] = gat(128 + i - j, np.ones((128, 128), bool))
    tb[2] = gat(np.full((128, 128), 1000), np.ones((128, 128), bool))
    tb[3] = gat(512 + i - j, i < j)
    d["tbls"] = tb
    n = np.arange(128)[:, None]
    bc = np.zeros((NT, 2, 128, 4, 128), np.float32)
    for it in range(NT):
        tq = it * 128 + np.arange(128)[None, :]
        dist = tq - (16 * n + 31)
        valid = (dist >= 0) & (n < 127)
        g = np.where(valid[..., None], rb[rel_bucket_np(dist)], neg)
        g = g.transpose(0, 2, 1)
        bc[it, 0] = g[:, 0:4]
        bc[it, 1] = g[:, 4:8]
    d["biasC"] = bc
    sc = np.zeros((128, NT, 2, 32), np.float32)
    jj = np.arange(32)[None, :]
    for it in range(NT):
        cur = (it * 128 + np.arange(128)[:, None]) // 64
        forced = (jj == 0) | (jj == cur)
        causal = jj <= cur
        sc[:, it, 0] = (causal & ~forced)
        sc[:, it, 1] = np.where(forced, 1e4, np.where(causal, 0.0, -1.0))
    d["selc"] = sc
    cs = np.arange(127) * 16
    ce = cs + 31
    ss = np.arange(32) * 64
    se = ss + 63
    ov = np.zeros((128, 32), np.float32)
    ov[:127] = ((cs[:, None] <= se[None]) & (ce[:, None] >= ss[None]))
    d["ovm"] = ov
    rs = np.zeros((32, NT, 128), np.float32)
    for jt in range(NT):
        for jr in range(128):
            rs[2 * jt + jr // 64, jt, jr] = 30000.0
    d["rselc"] = rs
    d["wcmp"] = np.ascontiguousarray(inp["nsa_w_cmp"].transpose(0, 3, 1, 2, 4))
    d["peT"] = np.ascontiguousarray(inp["nsa_cmp_pe"].transpose(0, 3, 1, 2))
    d["kn0"] = np.ascontiguousarray(inp["nsa_k_norm"][:, 0, :, None])
    return d


def host_consts(inp):
    d = {}
    j = np.arange(128)[:, None]
    i = np.arange(128)[None, :]
    cm = np.zeros((128, NCM, 128), np.float32)
    cm[:, CM_TRI] = (j <= i)
    cm[:, CM_NEGM] = np.where(i >= j, 0.0, -30000.0)
    cm[:, CM_STRICT] = (i > j)
    cm[:, CM_SEL127] = (j == 127) * np.ones((1, 128))
    cm[:, CM_IDENT] = (i == j)
    cm[:, CM_ONES] = 1.0
    d["cmat"] = cm
    sm = np.zeros((DEPTH, NSMALL), np.float32)
    sm[:, SM_ALOG:SM_ALOG + 4] = inp["gdn_a_log"]
    sm[:, SM_DTB:SM_DTB + 4] = inp["gdn_dt_bias"]
    sm[:, SM_BI:SM_BI + 4] = inp["mlstm_b_i"]
    sm[:, SM_BF:SM_BF + 4] = inp["mlstm_b_f"]
    sm[:, SM_GDNN:SM_GDNN + 128] = inp["gdn_norm"]
    sm[:, SM_MLN:SM_MLN + 128] = inp["mlstm_norm"]
    sm[:, SM_QN:SM_QN + 64] = inp["nsa_q_norm"]
    sm[:, SM_KN:SM_KN + 192] = inp["nsa_k_norm"].reshape(DEPTH, 192)
    d["small"] = np.ascontiguousarray(np.broadcast_to(sm[:, None, :], (DEPTH, 128, NSMALL)))
    d.update(nsa_consts(inp))
    d["cw"] = np.ascontiguousarray(inp["conv_w"].reshape(DEPTH, 4, 12, 128).transpose(0, 3, 2, 1))
    return d


def host_inputs(inp, core=0):
    b0 = core * 2
    d = {}
    d["x"] = np.ascontiguousarray(inp["x"][b0:b0 + 2])
    d["pT"] = np.ascontiguousarray(np.transpose(inp["p"][:, b0:b0 + 2], (0, 1, 3, 2)))
    for k in ["w_in", "w_branch", "w_out", "w_ffn_in", "w_ffn_out", "w_ple_gate", "w_ple_proj"]:
        d[k] = inp[k]
    g = np.stack([inp["norm_mix"], inp["norm_ffn"], inp["norm_ple"]], axis=1)
    d["gains_b"] = np.ascontiguousarray(np.broadcast_to(g[:, :, None, :], (DEPTH, 3, 128, D)), dtype=np.float32)
    d["ident"] = np.eye(128, dtype=np.float32)
    d.update(host_consts(inp))
    return d


_NC_CACHE = {}


def kernel(**inputs):
    inp = {k_: np.asarray(v) for k_, v in inputs.items()}
    if "full" not in _NC_CACHE:
        _NC_CACHE["full"] = build(n_layers=DEPTH, n_seq=NSEQ, phases="ABCDE")[0]
    nc = _NC_CACHE["full"]
    consts = host_consts(inp)
    shared = {}
    for k_ in ["w_in", "w_branch", "w_out", "w_ffn_in", "w_ffn_out", "w_ple_gate", "w_ple_proj"]:
        shared[k_] = np.ascontiguousarray(inp[k_], dtype=np.float32)
    g = np.stack([inp["norm_mix"], inp["norm_ffn"], inp["norm_ple"]], axis=1)
    shared["gains_b"] = np.ascontiguousarray(np.broadcast_to(g[:, :, None, :], (DEPTH, 3, 128, D)), dtype=np.float32)
    shared["ident"] = np.eye(128, dtype=np.float32)
    shared.update(consts)
    in_maps = []
    for c in range(8):
        d = dict(shared)
        d["x"] = np.ascontiguousarray(inp["x"][2 * c:2 * c + 2], dtype=np.float32)
        d["pT"] = np.ascontiguousarray(np.transpose(inp["p"][:, 2 * c:2 * c + 2], (0, 1, 3, 2)), dtype=np.float32)
        in_maps.append(d)
    res = run_bass_kernel_spmd(nc, in_maps, core_ids=list(range(8)))
    out = np.concatenate([np.asarray(r["out"]) for r in res.results], axis=0)
    return out.astype(np.float32)
```

```python
import numpy as np
import concourse.bass as bass
import concourse.mybir as mybir
from concourse.bass_utils import run_bass_kernel_spmd

F32 = mybir.dt.float32
BF16 = mybir.dt.bfloat16
I32 = mybir.dt.int32
AF = mybir.ActivationFunctionType
ALU = mybir.AluOpType
AX = mybir.AxisListType

N_DMA_SEMS = 48


class V:
    __slots__ = ("buf", "ap")

    def __init__(self, buf, ap):
        self.buf = buf
        self.ap = ap

    def __getitem__(self, idx):
        return V(self.buf, self.ap[idx])

    def r(self, pat, **kw):
        return V(self.buf, self.ap.rearrange(pat, **kw))

    def bc(self, shape):
        return V(self.buf, self.ap.to_broadcast(list(shape)))


class Buf:
    __slots__ = ("ap", "name", "lw", "rd", "excl")

    def __init__(self, ap, name=""):
        self.ap = ap
        self.name = name
        self.lw = None
        self.rd = []
        self.excl = False

    def __getitem__(self, idx):
        return V(self, self.ap[idx])

    def v(self):
        return V(self, self.ap)

    def r(self, pat, **kw):
        return V(self, self.ap.rearrange(pat, **kw))

    def sub(self, idx, name=""):
        return Buf(self.ap[idx], name or self.name)


def _ap(x):
    return x.ap if isinstance(x, V) else x


class Prog:
    ENGS = ("pe", "dve", "act", "pool", "sp")

    def __init__(self, nc):
        self.nc = nc
        self.ops = []
        self.eng = {"pe": nc.tensor, "dve": nc.vector, "act": nc.scalar, "pool": nc.gpsimd, "sp": nc.sync}
        self._ctx = []
        self.all_bufs = []
        self.last_barrier = None
        self._uid = 0

    def _nm(self, name):
        self._uid += 1
        return "%s_%d" % (name, self._uid)

    def sb(self, name, shape, dt=F32):
        g = self.nc.sbuf_tensor(self._nm(name), list(shape), dt)
        t = g.__enter__()
        self._ctx.append(g)
        b = Buf(t.ap() if hasattr(t, "ap") else t[:], name)
        self.all_bufs.append(b)
        return b

    def ps(self, name, shape, dt=F32):
        g = self.nc.psum_tensor(self._nm(name), list(shape), dt)
        t = g.__enter__()
        self._ctx.append(g)
        b = Buf(t.ap() if hasattr(t, "ap") else t[:], name)
        b.excl = True
        self.all_bufs.append(b)
        return b

    def dram(self, name, shape, dt=F32, kind="Internal"):
        t = self.nc.dram_tensor(name, list(shape), dt, kind=kind).ap()
        b = Buf(t, name)
        self.all_bufs.append(b)
        return b

    def track(self, b):
        self.all_bufs.append(b)
        return b

    def mark(self):
        return len(self._ctx)

    def release(self, mark):
        self.barrier()
        while len(self._ctx) > mark:
            g = self._ctx.pop()
            g.__exit__(None, None, None)

    def op(self, eng, fn, reads=(), writes=()):
        self.ops.append((eng, fn, tuple(reads), tuple(writes), False, self.last_barrier))

    def barrier(self):
        idx = len(self.ops)
        self.ops.append(("sp", lambda e: e.nop(), (), (), "bar", self.last_barrier))
        self.last_barrier = idx

    def dma(self, eng, out, in_, **kw):
        oa, ia = _ap(out), _ap(in_)

        def fn(e, oa=oa, ia=ia, kw=kw):
            return e.dma_start(out=oa, in_=ia, **kw)
        self.ops.append((eng, fn, (in_.buf,), (out.buf,), True, self.last_barrier))

    @staticmethod
    def _rw(outs, ins):
        w = [o.buf for o in outs if isinstance(o, V)]
        r = [i.buf for i in ins if isinstance(i, V)]
        w += [b for b in r if b.excl and b not in w]
        return r, w

    def activation(self, out, in_, func, bias=0.0, scale=1.0, accum_out=None, eng="act"):
        r, w = self._rw([out, accum_out], [in_, bias, scale])
        kw = dict(out=_ap(out), in_=_ap(in_), func=func, bias=_ap(bias), scale=_ap(scale))
        if accum_out is not None:
            kw["accum_out"] = _ap(accum_out)
        self.op(eng, lambda e, kw=kw: e.activation(**kw), r, w)

    def tt(self, out, in0, in1, op, eng="dve"):
        r, w = self._rw([out], [in0, in1])
        kw = dict(out=_ap(out), in0=_ap(in0), in1=_ap(in1), op=op)
        self.op(eng, lambda e, kw=kw: e.tensor_tensor(**kw), r, w)

    def ts(self, out, in0, s1, op0, s2=None, op1=None, accum_out=None, eng="dve"):
        r, w = self._rw([out, accum_out], [in0, s1, s2])
        kw = dict(out=_ap(out), in0=_ap(in0), scalar1=_ap(s1), scalar2=_ap(s2), op0=op0)
        if op1 is not None:
            kw["op1"] = op1
        if accum_out is not None:
            kw["accum_out"] = _ap(accum_out)
        self.op(eng, lambda e, kw=kw: e.tensor_scalar(**kw), r, w)

    def stt(self, out, in0, scalar, in1, op0, op1, eng="dve"):
        r, w = self._rw([out], [in0, scalar, in1])
        kw = dict(out=_ap(out), in0=_ap(in0), scalar=_ap(scalar), in1=_ap(in1), op0=op0, op1=op1)
        self.op("dve", lambda e, kw=kw: e.scalar_tensor_tensor(**kw), r, w)

    def copy(self, out, in_, eng="dve"):
        r, w = self._rw([out], [in_])
        oa, ia = _ap(out), _ap(in_)
        if eng == "act":
            self.op(eng, lambda e: e.copy(out=oa, in_=ia), r, w)
        else:
            self.op(eng, lambda e: e.tensor_copy(out=oa, in_=ia), r, w)

    def memset(self, out, val, eng="pool"):
        r, w = self._rw([out], [])
        oa = _ap(out)
        self.op(eng, lambda e: e.memset(oa, val), r, w)

    def reduce(self, out, in_, op, axis=AX.X, eng="dve"):
        r, w = self._rw([out], [in_])
        oa, ia = _ap(out), _ap(in_)
        self.op(eng, lambda e: e.tensor_reduce(out=oa, in_=ia, axis=axis, op=op), r, w)

    def recip(self, out, in_):
        r, w = self._rw([out], [in_])
        oa, ia = _ap(out), _ap(in_)
        self.op("dve", lambda e: e.reciprocal(out=oa, in_=ia), r, w)

    def mm(self, out, lhsT, rhs, start=True, stop=True, skip=False):
        r, w = self._rw([out], [lhsT, rhs])
        oa, la, ra = _ap(out), _ap(lhsT), _ap(rhs)
        if skip:
            self.op("pe", lambda e: e.matmul(oa, la, ra, start=start, stop=stop, skip_group_check=True), r, w)
        else:
            self.op("pe", lambda e: e.matmul(oa, la, ra, start=start, stop=stop), r, w)

    def tr(self, out, in_, ident):
        r, w = self._rw([out], [in_, ident])
        oa, ia, da = _ap(out), _ap(in_), _ap(ident)
        self.op("pe", lambda e: e.transpose(oa, ia, da), r, w)

    def emit(self):
        nc = self.nc
        ops = self.ops
        n = len(ops)
        deps = [None] * n
        signal = [False] * n
        last_on = {}
        dmas_since = []
        for i, (eng, fn, reads, writes, is_dma, bar) in enumerate(ops):
            d = set()
            if bar is not None:
                d.add(bar)
            if is_dma == "bar":
                d.update(last_on.values())
                d.update(dmas_since)
                dmas_since = []
                is_dma = False
            elif is_dma:
                dmas_since.append(i)
            last_on[eng] = i
            for r in reads:
                if r.lw is not None:
                    d.add(r.lw)
            for w in writes:
                if w.lw is not None:
                    j = w.lw
                    if is_dma or ops[j][4] or ops[j][0] != eng:
                        d.add(j)
                for j in w.rd:
                    if is_dma or ops[j][4] or ops[j][0] != eng:
                        d.add(j)
            d.discard(i)
            if eng == "pe":
                d = {j for j in d if ops[j][0] != "pe" or ops[j][4]}
            for r in reads:
                r.rd.append(i)
            for w in writes:
                w.lw = i
                w.rd = []
            best = {}
            dd = set()
            for j in d:
                if ops[j][4] is True:
                    dd.add(j)
                else:
                    e2 = ops[j][0]
                    if e2 not in best or best[e2] < j:
                        best[e2] = j
            dd.update(best.values())
            deps[i] = dd
            for j in dd:
                signal[j] = True
        sems = {}
        for e in self.ENGS:
            g = nc.semaphore("s_" + e)
            sems[e] = g.__enter__()
            self._ctx.append(g)
        dsems = []
        for k in range(N_DMA_SEMS):
            g = nc.semaphore("d_%d" % k)
            dsems.append(g.__enter__())
            self._ctx.append(g)
        cnt = {e: 0 for e in self.ENGS}
        dcnt = [0] * N_DMA_SEMS
        ev = [None] * n
        dnext = 0
        dma_prev = [None] * N_DMA_SEMS
        dma_guard = [None] * n
        for i, (eng, fn, reads, writes, is_dma, bar) in enumerate(ops):
            if is_dma is True:
                k = dnext
                dnext = (dnext + 1) % N_DMA_SEMS
                dma_guard[i] = dma_prev[k]
                dcnt[k] += 16
                ev[i] = ("d", k, dcnt[k])
                dma_prev[k] = ev[i]
            elif signal[i]:
                cnt[eng] += 1
                ev[i] = ("c", eng, cnt[eng])
        waited = {e: {} for e in self.ENGS}
        nwaits = 0
        last_dma = {}
        for i, (eng, fn, reads, writes, is_dma, bar) in enumerate(ops):
            need = {}
            for j in deps[i]:
                kind, key, val = ev[j]
                if need.get((kind, key), 0) < val:
                    need[(kind, key)] = val
            if is_dma is True and dma_guard[i] is not None:
                kind, key, val = dma_guard[i]
                if need.get((kind, key), 0) < val:
                    need[(kind, key)] = val
            w = waited[eng]
            for (kind, key), val in need.items():
                if w.get((kind, key), 0) >= val:
                    continue
                w[(kind, key)] = val
                s = sems[key] if kind == "c" else dsems[key]
                self.eng[eng].wait_ge(s, val)
                nwaits += 1
            ins = fn(self.eng[eng])
            if ev[i] is not None:
                kind, key, val = ev[i]
                if kind == "c":
                    ins.then_inc(sems[key], 1)
                else:
                    ins.then_inc(dsems[key], 16)
                    last_dma[key] = val
        w = waited["sp"]
        for k, val in last_dma.items():
            if w.get(("d", k), 0) < val:
                self.eng["sp"].wait_ge(dsems[k], val)
        self.stats = dict(n_ops=n, n_waits=nwaits, cnt=dict(cnt))
        return self.stats

    def close(self):
        while self._ctx:
            g = self._ctx.pop()
            g.__exit__(None, None, None)


S = 2048
D = 1024
NT = S // 128
DEPTH = 2
NSEQ = 2
D_IN = 8488
D_FF = 2816
EPS = 1e-6
C_AQ, C_AK, C_AV, C_AZ, C_AA, C_AB = 0, 512, 1024, 1536, 2048, 2052
C_BQ, C_BK, C_BV, C_BO, C_BI, C_BF = 2056, 2568, 3080, 3592, 4104, 4108
C_CQ, C_CKC, C_CVC, C_CKS, C_CVS, C_CKW, C_CVW, C_CG, C_MG = 4112, 4624, 4752, 4880, 5008, 5136, 5264, 5392, 5416


TM_RANGES = [(C_AZ, 520), (C_BK, 512), (C_BV, 1032), (C_CQ, 1304)]
TM_OFF = {}
_o = 0
for _c, _w in TM_RANGES:
    TM_OFF[_c] = _o
    _o += _w
TM_W = _o


def tm_col(c):
    for c0, w in TM_RANGES:
        if c0 <= c < c0 + w:
            return TM_OFF[c0] + (c - c0)
    raise KeyError(c)


FM_RANGES = [(C_AQ, 1536), (C_BQ, 1024)]
FM_OFF = {C_AQ: 0, C_BQ: 1536}


class Ctx:
    pass


CM_TRI, CM_NEGM, CM_STRICT, CM_SEL127, CM_IDENT, CM_ONES = range(6)
NCM = 6
SM_ALOG, SM_DTB, SM_BI, SM_BF, SM_GDNN, SM_MLN, SM_QN, SM_KN = 0, 4, 8, 12, 16, 144, 272, 336
NSMALL = 336 + 192


def declare(P, dbg=()):
    nc = P.nc
    C = Ctx()

    def inp(name, shape, dt=F32):
        return P.track(Buf(nc.dram_tensor(name, list(shape), dt, kind="ExternalInput").ap(), name))

    C.x = inp("x", [NSEQ, S, D])
    C.pT = inp("pT", [DEPTH, NSEQ, 256, S])
    C.w_in = inp("w_in", [DEPTH, D, D_IN])
    C.w_branch = inp("w_branch", [DEPTH, 3, 512, D])
    C.w_out = inp("w_out", [DEPTH, D, D])
    C.w_ffn_in = inp("w_ffn_in", [DEPTH, D, 2 * D_FF])
    C.w_ffn_out = inp("w_ffn_out", [DEPTH, D_FF, D])
    C.w_ple_gate = inp("w_ple_gate", [DEPTH, D, D])
    C.w_ple_proj = inp("w_ple_proj", [DEPTH, 256, D])
    C.gains_b = inp("gains_b", [DEPTH, 3, 128, D])
    C.ident = inp("ident", [128, 128])
    C.cmat = inp("cmat", [128, NCM, 128])
    C.small = inp("small", [DEPTH, 128, NSMALL])
    C.cw = inp("cw", [DEPTH, 128, 12, 4])
    C.tbls = inp("tbls", [4, 128, 8, 128])
    C.biasC = inp("biasC", [NT, 2, 128, 4, 128])
    C.selc = inp("selc", [128, NT, 2, 32])
    C.ovm = inp("ovm", [128, 32])
    C.rselc = inp("rselc", [32, NT, 128])
    C.wcmp = inp("wcmp", [DEPTH, 64, 2, 32, 64])
    C.peT = inp("peT", [DEPTH, 64, 2, 32])
    C.kn0 = inp("kn0", [DEPTH, 64, 1])
    C.out = P.track(Buf(nc.dram_tensor("out", [NSEQ, S, D], F32, kind="ExternalOutput").ap(), "out"))

    C.xt = [[C.x.sub((q, slice(t * 128, (t + 1) * 128), slice(None)), "x%d_%d" % (q, t)) for t in range(NT)] for q in range(NSEQ)]
    C.ot = [[C.out.sub((q, slice(t * 128, (t + 1) * 128), slice(None)), "o%d_%d" % (q, t)) for t in range(NT)] for q in range(NSEQ)]

    def scr(name, shape, dt=F32):
        kind = "ExternalOutput" if name in dbg else "Internal"
        return P.track(Buf(nc.dram_tensor(name, list(shape), dt, kind=kind).ap(), name))

    C.tm = scr("tm_scr", [S, TM_W])
    C.fm = scr("fm_scr", [2560, S])
    if "y" in dbg:
        C.ydbg = P.track(Buf(nc.dram_tensor("ydbg", [128, 12, S], F32, kind="ExternalOutput").ap(), "ydbg"))
    return C


def setup_consts(P, C):
    K = Ctx()
    K.identf = P.sb("identf", [128, 128])
    K.identb = P.sb("identb", [128, 128], BF16)
    P.dma("sp", K.identf.v(), C.ident.v())
    P.copy(K.identb.v(), K.identf.v())
    K.cm = P.sb("cmat", [128, NCM, 128])
    P.dma("sp", K.cm.v(), C.cmat.v())
    K.cm4 = P.sb("cmat4", [128, 3, 4, 128])
    for i, cmi in enumerate((CM_NEGM, CM_STRICT, CM_IDENT)):
        for h in range(4):
            P.copy(K.cm4[:, i, h, :], K.cm[:, cmi, :], eng="pool")
    K.small = P.sb("small", [128, DEPTH, NSMALL])
    P.dma("sp", K.small.v(), C.small.r("l p n -> p l n"))
    K.cw = P.sb("cw", [128, DEPTH, 12, 4])
    P.dma("sp", K.cw.v(), C.cw.r("l p b k -> p l b k"))
    K.tbl = P.sb("tbl", [128, 4, 8, 128], BF16)
    K.rsel = P.sb("rsel", [32, NT, 128], BF16)
    K.selc = P.sb("selc", [128, NT, 2, 32])
    mt = P.mark()
    tblf = P.sb("tblf", [128, 4, 8, 128])
    P.dma("sp", tblf.v(), C.tbls.r("a j h i -> j a h i"))
    P.copy(K.tbl.v(), tblf.v(), eng="pool")
    rself = P.sb("rself", [32, NT, 128])
    P.dma("sp", rself.v(), C.rselc.v())
    P.copy(K.rsel.v(), rself.v(), eng="pool")
    P.dma("sp", K.selc.v(), C.selc.v())
    P.release(mt)
    return K


def norm_transpose(P, K, C, l, gi, hsrc, uT, wq="sp"):
    m = P.mark()
    NPF = 4
    hA = [P.sb("hA", [128, D]) for _ in range(NPF)]
    junk = P.sb("junkA", [128, D], BF16)
    ub = [P.sb("ub", [128, D], BF16) for _ in range(2)]
    ss = P.sb("ssA", [128, NT])
    rs = P.sb("rsA", [128, NT])
    pT = [P.ps("pTA", [128, 8, 128], BF16) for _ in range(2)]
    Gt = P.sb("Gt", [128, D])
    P.dma(wq, Gt.v(), C.gains_b[l, gi])
    for t in range(NPF - 1):
        P.dma(wq, hA[t].v(), hsrc[t].v())
    for t in range(NT):
        b = t % 2
        if t + NPF - 1 < NT:
            P.dma(wq, hA[(t + NPF - 1) % NPF].v(), hsrc[t + NPF - 1].v())
        sst = ss.sub((slice(None), slice(t, t + 1)))
        rst = rs.sub((slice(None), slice(t, t + 1)))
        P.activation(junk.v(), hA[t % NPF].v(), AF.Square, accum_out=sst.v())
        P.activation(rst.v(), sst.v(), AF.Sqrt, bias=K.epsD.v(), scale=1.0 / D)
        P.recip(rst.v(), rst.v())
        P.stt(ub[b].v(), hA[t % NPF].v(), rst.v(), Gt.v(), ALU.mult, ALU.mult)
        for kc in range(8):
            P.tr(pT[b][:, kc, :], ub[b][:, kc * 128:(kc + 1) * 128], K.identb.v())
        if t % 2 == 0:
            P.copy(uT[:, :, t * 128:(t + 1) * 128], pT[b].v(), eng="act")
        else:
            P.copy(uT[:, :, t * 128:(t + 1) * 128], pT[b].v(), eng="dve")
    P.release(m)


class WStream:
    def __init__(self, P, nk, width, nstage=0, nbuf=3):
        self.P = P
        self.nk = nk
        self.width = width
        self.wb = [P.sb("wb", [128, nk, width], BF16) for _ in range(nbuf)]
        self.j = 0

    def load(self, wsrc_v, ncols, gain=None, nk=None):
        P = self.P
        nk = nk or self.nk
        wb = self.wb[self.j % len(self.wb)]
        self.j += 1
        P.dma("pool", wb[:, :nk, :ncols], wsrc_v.r("(k p) c -> p k c", p=128))
        return wb[:, :nk, :ncols]


def prefetch_iter(items, loader):
    nxt = loader(items[0]) if items else None
    for i, it_ in enumerate(items):
        cur = nxt
        if i + 1 < len(items):
            nxt = loader(items[i + 1])
        yield it_, cur


def phase_A(P, C, K, l, s, hsrc, uT):
    norm_transpose(P, K, C, l, 0, hsrc, uT)
    m = P.mark()
    ws = WStream(P, 8, 512, nbuf=3)
    w_in = C.w_in[l]
    blocks = []
    for c0, w in FM_RANGES:
        for cb in range(0, w, 512):
            blocks.append(("fm", c0, cb, 512))
    for c0, w in TM_RANGES:
        for cb in range(0, w, 512):
            blocks.append(("tm", c0, cb, min(512, w - cb)))
    pfm = [P.ps("pfm", [128, 512]) for _ in range(3)]
    sfm = [P.sb("sfm", [128, S]) for _ in range(2)]
    stm = [P.sb("stm", [128, 512]) for _ in range(4)]
    blk = 0
    it = 0
    ip = 0
    for (kind, c0, cb, nc_), wb in prefetch_iter(blocks, lambda b: ws.load(w_in[:, b[1] + b[2]:b[1] + b[2] + b[3]], b[3])):
        if kind == "fm":
            for j in range(4):
                st = sfm[blk % 2]
                for tg in range(4):
                    ps = pfm[ip % 3]
                    ip += 1
                    for kc in range(8):
                        P.mm(ps.v(), wb[:, kc, j * 128:(j + 1) * 128], uT[:, kc, tg * 512:(tg + 1) * 512],
                             start=(kc == 0), stop=(kc == 7))
                    P.copy(st[:, tg * 512:(tg + 1) * 512], ps.v(), eng=("act", "dve")[tg % 2])
                r0 = FM_OFF[c0] + cb + j * 128
                P.dma("sp", C.fm[r0:r0 + 128, :], st.v())
                blk += 1
        else:
            for t in range(NT):
                ps = pfm[ip % 3]
                ip += 1
                st = stm[it % 4]
                for kc in range(8):
                    P.mm(ps[:, :nc_], uT[:, kc, t * 128:(t + 1) * 128], wb[:, kc, :], start=(kc == 0), stop=(kc == 7))
                P.copy(st[:, :nc_], ps[:, :nc_], eng=("act", "dve")[it % 2])
                o = TM_OFF[c0] + cb
                P.dma("sp", C.tm[t * 128:(t + 1) * 128, o:o + nc_], st[:, :nc_])
                it += 1
    P.release(m)


STOPB = 0


class Banks:
    def __init__(self, P, n=8):
        self.t = [P.ps("bank", [128, 4, 128]) for _ in range(n)]
        self.i = 0

    def nxt(self):
        b = self.t[self.i % len(self.t)]
        self.i += 1
        return b


class RR:
    def __init__(self, engs):
        self.engs = engs
        self.i = 0

    def __call__(self):
        e = self.engs[self.i % len(self.engs)]
        self.i += 1
        return e


def scale_cols(P, out, in_, sc, eng):
    if eng == "act":
        P.activation(out, in_, AF.Copy, scale=sc)
    else:
        P.ts(out, in_, sc, ALU.mult, eng=eng)


def out_stage_bufs(P):
    OS = Ctx()
    OS.osq = P.sb("osq", [128, 4, 128])
    OS.oss = P.sb("oss", [128, 4])
    OS.zt = P.sb("zt", [128, 4, 128])
    return OS


def out_stage(P, C, K, bk, OS, osb, gcol, gfunc, gainB, yT, yoff, cg):
    zt, osq, oss = OS.zt, OS.osq, OS.oss
    P.dma("sp", zt.r("p h d -> p (h d)"), C.tm[cg * 128:(cg + 1) * 128, gcol:gcol + 512])
    P.activation(zt.v(), zt.v(), gfunc)
    P.activation(osq.v(), osb.v(), AF.Square)
    P.reduce(oss.v(), osq.v(), ALU.add)
    P.activation(oss.v(), oss.v(), AF.Sqrt, bias=K.epsD.v(), scale=1.0 / 128)
    P.recip(oss.v(), oss.v())
    for h in range(4):
        P.stt(osb[:, h, :], osb[:, h, :], oss[:, h:h + 1], gainB, ALU.mult, ALU.mult)
    P.tt(osb.v(), osb.v(), zt.v(), ALU.mult)
    y_ps = bk.nxt()
    for h in range(4):
        P.tr(y_ps[:, h, :], osb[:, h, :], K.identf.v())
    P.copy(yT[:, yoff:yoff + 4, cg * 128:(cg + 1) * 128], y_ps.v(), eng="act")


class SubBanks:
    def __init__(self, tiles):
        self.t = list(tiles)
        self.i = 0

    def nxt(self):
        b = self.t[self.i % len(self.t)]
        self.i += 1
        return b


def run_streams(gens, weights):
    gens = list(gens)
    weights = list(weights)
    while gens:
        for gi in range(len(gens) - 1, -1, -1):
            pass
        alive = []
        for g_, w_ in zip(gens, weights):
            done = False
            for _ in range(w_):
                try:
                    next(g_)
                except StopIteration:
                    done = True
                    break
            if not done:
                alive.append((g_, w_))
        gens = [a[0] for a in alive]
        weights = [a[1] for a in alive]


def gate_prefetch(P, C, zt, gcol, gfunc, cg):
    P.dma("sp", zt.r("p h d -> p (h d)"), C.tm[cg * 128:(cg + 1) * 128, gcol:gcol + 512])
    P.activation(zt.v(), zt.v(), gfunc)


def out_stage_gen(P, C, K, bk, OS, osb, gcol, gfunc, gainB, yT, yoff, cg, zt=None):
    osq, oss = OS.osq, OS.oss
    if zt is None:
        zt = OS.zt
        gate_prefetch(P, C, zt, gcol, gfunc, cg)
    P.activation(osq.v(), osb.v(), AF.Square)
    yield
    P.reduce(oss.v(), osq.v(), ALU.add)
    yield
    P.activation(oss.v(), oss.v(), AF.Sqrt, bias=K.epsD.v(), scale=1.0 / 128)
    yield
    P.recip(oss.v(), oss.v())
    for h in range(4):
        P.stt(osb[:, h, :], osb[:, h, :], oss[:, h:h + 1], gainB, ALU.mult, ALU.mult)
    P.tt(osb.v(), osb.v(), zt.v(), ALU.mult)
    yield
    y_ps = bk.nxt()
    for h in range(4):
        P.tr(y_ps[:, h, :], osb[:, h, :], K.identf.v())
    yield
    P.copy(yT[:, yoff:yoff + 4, cg * 128:(cg + 1) * 128], y_ps.v(), eng="act")


def phase_C(P, C, K, l, s, yT):
    m = P.mark()
    sm = K.small
    cm = K.cm
    NEGM4 = K.cm4[:, 0]
    bk = Banks(P, 8)
    flat = lambda b: b.r("p a b -> p (a b)")
    ext = lambda b, hh: flat(b)[:, 0:258].r("p (a b) -> p a b", b=129)[:, hh, :]
    gi = P.sb("gi", [128, NT, 8])
    ci = tm_col(C_BI)
    for c in range(NT):
        P.dma("sp", gi[:, c, :], C.tm[c * 128:(c + 1) * 128, ci:ci + 8])
    it = P.sb("it", [128, NT, 4])
    lf = P.sb("lf", [128, NT, 4])
    nbf = P.sb("nbf", [128, 4])
    P.ts(nbf.v(), sm[:, l, SM_BF:SM_BF + 4], -1.0, ALU.mult)
    for h in range(4):
        P.ts(it[:, :, h], gi[:, :, h], sm[:, l, SM_BI + h:SM_BI + h + 1], ALU.add)
        P.activation(lf[:, :, h], gi[:, :, 4 + h], AF.Exp, bias=nbf[:, h:h + 1], scale=-1.0)
    P.activation(lf.v(), lf.v(), AF.Ln, bias=1.0)
    P.ts(lf.v(), lf.v(), -1.0, ALU.mult)
    G1 = P.sb("G1", [128, NT, 4, 2])
    G2 = P.sb("G2", [128, NT, 4, 2])
    G3 = P.sb("G3", [128, NT, 4, 2])
    P.memset(G1.v(), 0.0)
    P.memset(G2.v(), 0.0)
    P.memset(G3.v(), 0.0)
    P.memset(G1[0:1, :, :, 1], 1.0)
    P.memset(G2[0:1, :, :, 0], 1.0)
    P.copy(G1[:, :, :, 0], lf.v())
    P.ts(G2[:, :, :, 1], lf.v(), -1.0, ALU.mult)
    P.copy(G3[:, :, :, 1], it.v())
    bc = P.sb("bc", [128, NT, 4])
    f64 = lambda b: b.r("p c h -> p (c h)")
    b0 = bk.nxt()
    P.mm(flat(b0)[:, 0:64], cm[:, CM_TRI, :], f64(lf))
    P.copy(f64(bc), flat(b0)[:, 0:64])
    b1 = bk.nxt()
    P.mm(flat(b1)[:, 0:64], cm[:, CM_SEL127, :], f64(bc))
    EBL = P.sb("EBL", [128, NT, 4])
    EKW = P.sb("EKW", [128, NT, 4])
    EB = P.sb("EB", [128, NT, 4])
    P.activation(f64(EBL), flat(b1)[:, 0:64], AF.Exp)
    P.tt(f64(EKW), flat(b1)[:, 0:64], f64(bc), ALU.subtract)
    P.tt(EKW.v(), EKW.v(), it.v(), ALU.add)
    P.activation(EKW.v(), EKW.v(), AF.Exp)
    P.activation(EB.v(), bc.v(), AF.Exp)
    qk = [P.sb("qk", [128, S]) for _ in range(8)]
    for blk in range(8):
        r0 = FM_OFF[C_BQ] + blk * 128
        P.dma("sp", qk[blk].v(), C.fm[r0:r0 + 128, :])
        if blk < 4:
            P.activation(qk[blk].v(), qk[blk].v(), AF.Copy, scale=128 ** -0.5)
    qT, kT = qk[0:4], qk[4:8]
    Cst = P.sb("Cst", [128, 4, 129])
    P.memset(Cst.v(), 0.0)
    k_tm = [P.sb("k_tm", [128, 4, 128]) for _ in range(2)]
    v_ext = [P.sb("v_ext", [128, 4, 129]) for _ in range(2)]
    for b in range(2):
        P.memset(v_ext[b][:, :, 128:129], 1.0)
    kw_tm = P.sb("kw_tm", [128, 4, 128])
    R1 = P.sb("R1", [2, 4, 128])
    R2 = P.sb("R2", [2, 4, 128])
    DT = P.sb("DT", [128, 4, 128])
    SmT = P.sb("SmT", [128, 4, 128])
    htmp = P.sb("htmp", [128, 4, 129])
    htot = P.sb("htot", [128, 4, 129])
    rden = P.sb("rden", [128, 4])
    osb = P.sb("osb", [128, 4, 128])
    OS = out_stage_bufs(P)
    gainB = sm[:, l, SM_MLN:SM_MLN + 128]
    ck, cv = tm_col(C_BK), tm_col(C_BV)
    bkX = SubBanks(bk.t[0:3])
    bkY = SubBanks(bk.t[3:8])
    SmT2 = [SmT, P.sb("SmT", [128, 4, 128])]
    kw2 = [kw_tm, P.sb("kw_tm", [128, 4, 128])]
    zt2 = [P.sb("ztC", [128, 4, 128]) for _ in range(2)]

    def stageX(cg):
        par = cg % 2
        o = slice(cg * 128, (cg + 1) * 128)
        kt, ve = k_tm[par], v_ext[par]
        P.dma("sp", kt.r("p h d -> p (h d)"), C.tm[o, ck:ck + 512])
        P.dma("sp", ve[:, :, 0:128], C.tm[o, cv:cv + 512].r("p (h d) -> p h d", d=128))
        gate_prefetch(P, C, zt2[par], tm_col(C_BO), AF.Sigmoid, cg)
        r_ps = bkX.nxt()
        r2_ps = bkX.nxt()
        for h in range(4):
            P.mm(r_ps[0:2, h, :], G1[:, cg, h, :], cm[:, CM_TRI, :])
        for h in range(4):
            P.mm(r2_ps[0:2, h, :], G2[:, cg, h, :], cm[:, CM_TRI, :], start=True, stop=False)
            P.mm(r2_ps[0:2, h, :], G3[:, cg, h, :], cm[:, CM_IDENT, :], start=False, stop=True)
        yield
        P.copy(R1.v(), r_ps[0:2, :, :], eng="dve")
        P.copy(R2.v(), r2_ps[0:2, :, :], eng="act")
        yield
        dt_ps = bkX.nxt()
        for h in range(4):
            P.mm(dt_ps[:, h, :], R2[:, h, :], R1[:, h, :])
        kq_ps = bkX.nxt()
        for h in range(4):
            P.mm(kq_ps[:, h, :], kT[h][:, o], qT[h][:, o])
        yield
        P.tt(DT.v(), dt_ps.v(), NEGM4, ALU.add)
        yield
        P.activation(DT.v(), DT.v(), AF.Exp)
        for h in range(4):
            scale_cols(P, kw2[par][:, h, :], kt[:, h, :], EKW[:, cg, h:h + 1], ("act", "dve")[h % 2])
        yield
        P.tt(SmT2[par].v(), kq_ps.v(), DT.v(), ALU.mult)

    def stageY(cg):
        par = cg % 2
        o = slice(cg * 128, (cg + 1) * 128)
        ve = v_ext[par]
        hq = [bkY.nxt(), bkY.nxt()]
        hs = [bkY.nxt(), bkY.nxt()]
        for h in range(4):
            P.mm(ext(hq[h // 2], h % 2), qT[h][:, o], Cst[:, h, :])
        for h in range(4):
            P.mm(ext(hs[h // 2], h % 2), SmT2[par][:, h, :], ve[:, h, :])
        yield
        for h in range(4):
            P.activation(htmp[:, h, :], ext(hq[h // 2], h % 2), AF.Copy, scale=EB[:, cg, h:h + 1])
        cps = [bkY.nxt(), bkY.nxt()]
        for h in range(4):
            P.mm(ext(cps[h // 2], h % 2), kw2[par][:, h, :], ve[:, h, :])
        yield
        for h in range(4):
            P.tt(htot[:, h, :], htmp[:, h, :], ext(hs[h // 2], h % 2), ALU.add)
        P.ts(rden.v(), htot[:, :, 128], -1.0, ALU.mult)
        P.tt(rden.v(), rden.v(), htot[:, :, 128], ALU.max)
        P.ts(rden.v(), rden.v(), 1.0, ALU.max)
        P.recip(rden.v(), rden.v())
        for h in range(4):
            scale_cols(P, osb[:, h, :], htot[:, h, 0:128], rden[:, h:h + 1], ("dve", "act")[h % 2])
        for h in range(4):
            P.stt(Cst[:, h, :], Cst[:, h, :], EBL[:, cg, h:h + 1], ext(cps[h // 2], h % 2), ALU.mult, ALU.add)
        yield
        yield from out_stage_gen(P, C, K, bkY, OS, osb, tm_col(C_BO), AF.Sigmoid, gainB, yT, 4, cg, zt=zt2[par])

    run_streams([stageX(0)], [1])
    for cg in range(NT):
        if cg + 1 < NT:
            run_streams([stageX(cg + 1), stageY(cg)], [1, 2])
        else:
            run_streams([stageY(cg)], [1])
    P.release(m)


def phase_B(P, C, K, l, s, yT):
    m = P.mark()
    sm = K.small
    cm = K.cm
    NEGM4, STRICT4, IDENT4 = K.cm4[:, 0], K.cm4[:, 1], K.cm4[:, 2]
    bk = Banks(P, 8)
    rr = RR(("dve", "act", "pool"))
    rr2 = RR(("dve", "act"))
    ab = P.sb("ab", [128, NT, 8])
    ca = tm_col(C_AA)
    for c in range(NT):
        P.dma("sp", ab[:, c, :], C.tm[c * 128:(c + 1) * 128, ca:ca + 8])
    e1 = P.sb("e1", [128, NT, 4])
    g = P.sb("g", [128, NT, 4])
    nega = P.sb("nega", [128, 4])
    P.activation(nega.v(), sm[:, l, SM_ALOG:SM_ALOG + 4], AF.Exp)
    P.ts(nega.v(), nega.v(), -1.0, ALU.mult)
    for h in range(4):
        P.activation(e1[:, :, h], ab[:, :, h], AF.Exp, bias=sm[:, l, SM_DTB + h:SM_DTB + h + 1])
    P.activation(e1.v(), e1.v(), AF.Ln, bias=1.0)
    for h in range(4):
        P.ts(g[:, :, h], e1[:, :, h], nega[:, h:h + 1], ALU.mult)
    beta = P.sb("beta", [128, NT, 4])
    P.activation(beta.v(), ab[:, :, 4:8], AF.Sigmoid)
    nbeta = P.sb("nbeta", [128, NT, 4])
    P.ts(nbeta.v(), beta.v(), -1.0, ALU.mult)
    G1 = P.sb("G1", [128, NT, 4, 2])
    G2 = P.sb("G2", [128, NT, 4, 2])
    P.memset(G1.v(), 0.0)
    P.memset(G2.v(), 0.0)
    P.memset(G1[0:1, :, :, 1], 1.0)
    P.memset(G2[0:1, :, :, 0], 1.0)
    P.copy(G1[:, :, :, 0], g.v())
    P.ts(G2[:, :, :, 1], g.v(), -1.0, ALU.mult)
    gc = P.sb("gc", [128, NT, 4])
    b0 = bk.nxt()
    P.mm(b0.r("p a b -> p (a b)")[:, 0:64], cm[:, CM_TRI, :], g.r("p c h -> p (c h)"))
    P.copy(gc.r("p c h -> p (c h)"), b0.r("p a b -> p (a b)")[:, 0:64])
    b1 = bk.nxt()
    P.mm(b1.r("p a b -> p (a b)")[:, 0:64], cm[:, CM_SEL127, :], gc.r("p c h -> p (c h)"))
    EGL = P.sb("EGL", [128, NT, 4])
    ED = P.sb("ED", [128, NT, 4])
    EG = P.sb("EG", [128, NT, 4])
    P.activation(EGL.r("p c h -> p (c h)"), b1.r("p a b -> p (a b)")[:, 0:64], AF.Exp)
    P.tt(ED.r("p c h -> p (c h)"), b1.r("p a b -> p (a b)")[:, 0:64], gc.r("p c h -> p (c h)"), ALU.subtract)
    P.activation(ED.v(), ED.v(), AF.Exp)
    P.activation(EG.v(), gc.v(), AF.Exp)
    if STOPB == 1:
        P.release(m)
        return
    Sst = P.sb("Sst", [128, 4, 128])
    P.memset(Sst.v(), 0.0)
    HW = 1024
    qkv = [P.sb("qkv", [128, HW]) for _ in range(12)]
    raw = [P.sb("raw", [128, HW + 3]) for _ in range(2)]
    sqt = P.sb("sqt", [128, HW])
    rnt = P.sb("rnt", [128, 512])
    gainB = sm[:, l, SM_GDNN:SM_GDNN + 128]
    for hf in range(2):
        t0 = hf * HW
        for blk in range(12):
            r = raw[blk % 2]
            r0 = FM_OFF[C_AQ] + blk * 128
            if hf == 0:
                P.memset(r[:, 0:3], 0.0)
                P.dma("sp", r[:, 3:], C.fm[r0:r0 + 128, 0:HW])
            else:
                P.dma("sp", r.v(), C.fm[r0:r0 + 128, t0 - 3:t0 + HW])
            dst = qkv[blk]
            e = "dve"
            P.ts(dst.v(), r[:, 0:HW], K.cw[:, l, blk, 0:1], ALU.mult, eng=e)
            for k in range(1, 4):
                P.stt(dst.v(), r[:, k:k + HW], K.cw[:, l, blk, k:k + 1], dst.v(), ALU.mult, ALU.add, eng=e)
            P.activation(dst.v(), dst.v(), AF.Silu)
            if blk < 8:
                P.activation(sqt.v(), dst.v(), AF.Square)
                for hh in range(2):
                    bb = bk.nxt()
                    bbv = bb.r("p a b -> p (a b)")
                    P.mm(bbv, cm[:, CM_ONES, :], sqt[:, hh * 512:(hh + 1) * 512])
                    P.activation(rnt.v(), bbv, AF.Sqrt, bias=K.eps6.v())
                    P.recip(rnt.v(), rnt.v())
                    sc = (128 ** -0.5) if blk < 4 else 1.0
                    P.stt(dst[:, hh * 512:(hh + 1) * 512], dst[:, hh * 512:(hh + 1) * 512], sc, rnt.v(), ALU.mult, ALU.mult)
        if STOPB == 2:
            P.release(m)
            return
        qT = qkv[0:4]
        kT = qkv[4:8]
        vT = qkv[8:12]
        if hf == 0:
            bkX = SubBanks(bk.t[0:4])
            bkY = SubBanks(bk.t[4:8])
            v_tm = [P.sb("v_tm", [128, 4, 128]) for _ in range(2)]
            kd_tm = [P.sb("kd_tm", [128, 4, 128]) for _ in range(2)]
            attnT = [P.sb("attnT", [128, 4, 128]) for _ in range(2)]
            nwT = [P.sb("nwT", [128, 4, 128]) for _ in range(2)]
            Rm = [[P.sb("Rm", [128, 4, 128]) for _ in range(2)] for _ in range(2)]
            kg_tm = P.sb("kg_tm", [128, 4, 128])
            R1 = P.sb("R1", [2, 4, 128])
            R2 = P.sb("R2", [2, 4, 128])
            DT = P.sb("DT", [128, 4, 128])
            DTS = P.sb("DTS", [128, 4, 128])
            Bm = P.sb("Bm", [128, 4, 128])
            BT = P.sb("BT", [128, 4, 128])
            Pk = [P.sb("Pk", [128, 4, 128]) for _ in range(2)]
            PkT = [P.sb("PkT", [128, 4, 128]) for _ in range(2)]
            vn = P.sb("vn", [128, 4, 128])
            otmp = P.sb("otmp", [128, 4, 128])
            osb = P.sb("osb", [128, 4, 128])
            OS = out_stage_bufs(P)

        def stageX(c, hf=hf, qT=qT, kT=kT, vT=vT):
            cg = hf * 8 + c
            par = cg % 2
            o = slice(c * 128, (c + 1) * 128)
            kt_ps = bkX.nxt()
            vt_ps = bkX.nxt()
            for h in range(4):
                P.tr(kt_ps[:, h, :], kT[h][:, o], K.identf.v())
                P.tr(vt_ps[:, h, :], vT[h][:, o], K.identf.v())
            yield
            P.copy(v_tm[par].v(), vt_ps.v(), eng="act")
            for h in range(4):
                scale_cols(P, kg_tm[:, h, :], kt_ps[:, h, :], EG[:, cg, h:h + 1], "dve")
                scale_cols(P, kd_tm[par][:, h, :], kt_ps[:, h, :], ED[:, cg, h:h + 1], "dve")
            r_ps = bkX.nxt()
            for h in range(4):
                P.mm(r_ps[0:2, h, :], G1[:, cg, h, :], cm[:, CM_TRI, :])
            r2_ps = bkX.nxt()
            for h in range(4):
                P.mm(r2_ps[0:2, h, :], G2[:, cg, h, :], cm[:, CM_TRI, :])
            yield
            P.copy(R1.v(), r_ps[0:2, :, :], eng="dve")
            P.copy(R2.v(), r2_ps[0:2, :, :], eng="act")
            yield
            dt_ps = bkX.nxt()
            for h in range(4):
                P.mm(dt_ps[:, h, :], R2[:, h, :], R1[:, h, :])
            kq_ps = bkX.nxt()
            kk_ps = bkX.nxt()
            for h in range(4):
                P.mm(kq_ps[:, h, :], kT[h][:, o], qT[h][:, o])
                P.mm(kk_ps[:, h, :], kT[h][:, o], kT[h][:, o])
            yield
            P.tt(DT.v(), dt_ps.v(), NEGM4, ALU.add)
            yield
            P.activation(DT.v(), DT.v(), AF.Exp)
            yield
            P.tt(DTS.v(), DT.v(), STRICT4, ALU.mult)
            P.tt(attnT[par].v(), kq_ps.v(), DT.v(), ALU.mult)
            for h in range(4):
                P.stt(Bm[:, h, :], kk_ps[:, h, :], nbeta[:, cg, h:h + 1], DTS[:, h, :], ALU.mult, ALU.mult)
            yield
            bt_ps = bkX.nxt()
            for h in range(4):
                P.tr(bt_ps[:, h, :], Bm[:, h, :], K.identf.v())
            yield
            P.copy(BT.v(), bt_ps.v(), eng="act")
            P.tt(Rm[par][0].v(), Bm.v(), IDENT4, ALU.add)
            yield
            cur, curT, R = Bm, BT, Rm[par][0]
            for k in range(1, 7):
                nP, nPT, nR = Pk[k % 2], PkT[k % 2], Rm[par][k % 2]
                pT_ps = bkX.nxt()
                for h in range(4):
                    P.mm(pT_ps[:, h, :], cur[:, h, :], curT[:, h, :])
                if k < 6:
                    p_ps = bkX.nxt()
                    for h in range(4):
                        P.mm(p_ps[:, h, :], curT[:, h, :], cur[:, h, :])
                yield
                P.copy(nPT.v(), pT_ps.v(), eng="act")
                if k < 6:
                    P.copy(nP.v(), p_ps.v(), eng="dve")
                yield
                rr_ps = bkX.nxt()
                for h in range(4):
                    P.mm(rr_ps[:, h, :], nPT[:, h, :], R[:, h, :])
                yield
                P.tt(nR.v(), R.v(), rr_ps.v(), ALU.add)
                yield
                cur, curT, R = nP, nPT, nR
            Tt = R
            w_ps = bkX.nxt()
            for h in range(4):
                P.mm(w_ps[:, h, :], kg_tm[:, h, :], Tt[:, h, :])
            yield
            P.activation(nwT[par].v(), w_ps.v(), AF.Copy, scale=-1.0)

        def stageY(c, hf=hf, qT=qT):
            cg = hf * 8 + c
            par = cg % 2
            o = slice(c * 128, (c + 1) * 128)
            Tt = Rm[par][0]
            vn_ps = bkY.nxt()
            o1_ps = bkY.nxt()
            for h in range(4):
                P.mm(vn_ps[:, h, :], Tt[:, h, :], v_tm[par][:, h, :], start=True, stop=False)
                P.mm(vn_ps[:, h, :], nwT[par][:, h, :], Sst[:, h, :], start=False, stop=True)
                P.mm(o1_ps[:, h, :], qT[h][:, o], Sst[:, h, :])
            yield
            for h in range(4):
                scale_cols(P, vn[:, h, :], vn_ps[:, h, :], beta[:, cg, h:h + 1], "dve")
                scale_cols(P, otmp[:, h, :], o1_ps[:, h, :], EG[:, cg, h:h + 1], "act")
            yield
            o2_ps = bkY.nxt()
            s_ps = bkY.nxt()
            for h in range(4):
                P.mm(o2_ps[:, h, :], attnT[par][:, h, :], vn[:, h, :])
                P.mm(s_ps[:, h, :], kd_tm[par][:, h, :], vn[:, h, :])
            yield
            P.tt(osb.v(), otmp.v(), o2_ps.v(), ALU.add)
            for h in range(4):
                P.stt(Sst[:, h, :], Sst[:, h, :], EGL[:, cg, h:h + 1], s_ps[:, h, :], ALU.mult, ALU.add)
            yield
            yield from out_stage_gen(P, C, K, bkY, OS, osb, tm_col(C_AZ), AF.Silu, gainB, yT, 0, cg)

        run_streams([stageX(0)], [1])
        for c in range(8):
            if c + 1 < 8:
                run_streams([stageX(c + 1), stageY(c)], [2, 1])
            else:
                run_streams([stageY(c)], [1])
    P.release(m)


def phase_D(P, C, K, l, s, yT):
    m = P.mark()
    sm = K.small
    cm = K.cm
    bk = Banks(P, 2)
    bkA = Banks(P, 2)
    bkS = Banks(P, 4)
    flat = lambda b: b.r("p a b -> p (a b)")
    kTs = P.sb("kTs", [64, 2, S], BF16)
    kTw = P.sb("kTw", [64, 2, S], BF16)
    vs = P.sb("vs", [128, NT, 2, 65], BF16)
    vw = P.sb("vw", [128, NT, 2, 65], BF16)
    P.memset(vs[:, :, :, 64:65], 1.0)
    P.memset(vw[:, :, :, 64:65], 1.0)
    ckT = P.sb("ckT", [64, 2, 128])
    cvx = P.sb("cvx", [128, 2, 97])
    P.memset(ckT.v(), 0.0)
    P.memset(cvx.v(), 0.0)
    P.memset(cvx[:, :, 64:65], 1.0)
    for g in range(2):
        P.dma("sp", cvx[:, g, 65:97], C.ovm.v())
    gq8 = P.sb("gq8", [128, 8, 64])
    gk = P.sb("gk", [128, 4, 64])
    for h in range(8):
        P.ts(gq8[:, h, :], sm[:, l, SM_QN:SM_QN + 64], 0.125, ALU.mult)
    for a in range(2):
        for g in range(2):
            P.copy(gk[:, 2 * a + g, :], sm[:, l, SM_KN + 64 * (a + 1):SM_KN + 64 * (a + 2)])
    kn0 = P.sb("kn0", [64, 1])
    P.dma("sp", kn0.v(), C.kn0[l])
    ckc = tm_col(C_CKC)
    m1 = P.mark()
    kcT = P.sb("kcT", [64, 2, S])
    vcT = P.sb("vcT", [64, 2, S])
    Wc = P.sb("Wc", [64, 2, 32, 64])
    peT = P.sb("peT", [64, 2, 32])
    P.dma("sp", Wc.v(), C.wcmp[l])
    P.dma("sp", peT.v(), C.peT[l])
    kv = [P.sb("kv", [128, 6, 2, 64]) for _ in range(2)]
    ksq = P.sb("ksq", [128, 2, 2, 64])
    kss = P.sb("kss", [128, 2, 2])
    kn = P.sb("kn", [128, 2, 2, 64])
    for t in range(NT):
        b = kv[t % 2]
        o = slice(t * 128, (t + 1) * 128)
        P.dma("sp", b.r("p a g d -> p (a g d)"), C.tm[o, ckc:ckc + 768])
        P.copy(vs[:, t, :, 0:64], b[:, 3, :, :], eng="act")
        P.copy(vw[:, t, :, 0:64], b[:, 5, :, :], eng="dve")
        P.activation(ksq.v(), b[:, 2:6:2, :, :], AF.Square)
        P.reduce(kss.v(), ksq.v(), ALU.add)
        P.activation(kss.v(), kss.v(), AF.Sqrt, bias=K.epsD.v(), scale=1.0 / 64)
        P.recip(kss.v(), kss.v())
        for a in range(2):
            for g in range(2):
                P.stt(kn[:, a, g, :], b[:, 2 + 2 * a, g, :], kss[:, a, g:g + 1], gk[:, 2 * a + g, :], ALU.mult, ALU.mult)
        ps1 = bk.nxt()
        ps2 = bk.nxt()
        for a in range(2):
            for g in range(2):
                P.tr(ps1[0:64, 2 * a + g, :], kn[:, a, g, :], K.identf.v())
                P.tr(ps2[0:64, 2 * a + g, :], b[:, a, g, :], K.identf.v())
        P.copy(kTs[:, :, o], ps1[0:64, 0:2, :], eng="act")
        P.copy(kTw[:, :, o], ps1[0:64, 2:4, :], eng="act")
        P.copy(kcT[:, :, o], ps2[0:64, 0:2, :], eng="dve")
        P.copy(vcT[:, :, o], ps2[0:64, 2:4, :], eng="dve")
    cst = P.sb("cst", [64, 2])
    raw = P.sb("rawc", [64, 127])
    sqc = P.sb("sqc", [64, 127])
    rnc = P.sb("rnc", [64, 127])
    for kvi in range(2):
        pc = bk.nxt()
        for lq in range(32):
            P.mm(flat(pc)[0:64, 0:1], Wc[:, kvi, lq, :], peT[:, kvi, lq:lq + 1], start=(lq == 0), stop=(lq == 31))
        P.copy(cst[:, kvi:kvi + 1], flat(pc)[0:64, 0:1])
    for kvi in range(2):
        src = (kcT, vcT)[kvi]
        for g in range(2):
            ps = bk.nxt()
            pv = flat(ps)[0:64, 0:127]
            for lq in range(32):
                P.mm(pv, Wc[:, kvi, lq, :], src[:, g, lq:lq + 16 * 126 + 1:16], start=(lq == 0), stop=(lq == 31))
            P.ts(raw.v(), pv, cst[:, kvi:kvi + 1], ALU.add)
            if kvi == 0:
                P.activation(sqc.v(), raw.v(), AF.Square)
                p2 = bk.nxt()
                P.mm(flat(p2)[0:64, 0:127], cm[0:64, CM_ONES, 0:64], sqc.v())
                P.activation(rnc.v(), flat(p2)[0:64, 0:127], AF.Sqrt, bias=K.epsD[0:64, :], scale=1.0 / 64)
                P.recip(rnc.v(), rnc.v())
                P.stt(ckT[:, g, 0:127], raw.v(), kn0[:, 0:1], rnc.v(), ALU.mult, ALU.mult)
            else:
                p2 = bk.nxt()
                P.tr(p2[0:127, 0, 0:64], raw.v(), K.identf[0:64, 0:64])
                P.copy(cvx[0:127, g, 0:64], p2[0:127, 0, 0:64])
    P.release(m1)
    qt = [P.sb("qt", [128, 8, 64]) for _ in range(2)]
    gt = [P.sb("gt", [128, 8, 3]) for _ in range(2)]
    qsq = P.sb("qsq", [128, 8, 64])
    qss = P.sb("qss", [128, 8])
    qn = [P.sb("qn", [128, 8, 64]) for _ in range(2)]
    gs = [P.sb("gs", [128, 8, 3]) for _ in range(2)]
    qTf = P.sb("qTf", [64, 4, 128])
    qTb = [P.sb("qTb", [64, 4, 128], BF16) for _ in range(2)]
    bct = [P.sb("bct", [128, 4, 128]) for _ in range(2)]
    ec = P.sb("ec", [128, 4, 128])
    esb = [P.sb("esb", [128, 4, 128], BF16) for _ in range(4)]
    rc = P.sb("rc", [128, 3, 4])
    imp = P.sb("imp", [128, 32])
    mx8 = P.sb("mx8", [128, 8])
    selm = P.sb("selm", [128, 32])
    selT = P.sb("selT", [32, 4, 128], BF16)
    yc = [P.sb("yc", [128, 8, 64]) for _ in range(2)]
    cq_, cg_ = tm_col(C_CQ), tm_col(C_CG)
    st_ = dict(ie=0, ib=0)
    LOOK = 2

    def qprep(it):
        o = slice(it * 128, (it + 1) * 128)
        q_, g_ = qt[it % 2], gt[it % 2]
        P.dma("sp", q_.r("p h d -> p (h d)"), C.tm[o, cq_:cq_ + 512])
        P.dma("sp", g_.r("p h k -> p (h k)"), C.tm[o, cg_:cg_ + 24])
        P.activation(qsq.v(), q_.v(), AF.Square)
        P.reduce(qss.v(), qsq.v(), ALU.add)
        P.activation(qss.v(), qss.v(), AF.Sqrt, bias=K.epsD.v(), scale=1.0 / 64)
        P.recip(qss.v(), qss.v())
        for h in range(8):
            P.stt(qn[it % 2][:, h, :], q_[:, h, :], qss[:, h:h + 1], gq8[:, h, :], ALU.mult, ALU.mult)
        P.activation(gs[it % 2].v(), g_.v(), AF.Sigmoid)

    def ytrans(it):
        o = slice(it * 128, (it + 1) * 128)
        y_ps = bk.nxt()
        for c4 in range(4):
            P.tr(y_ps[:, c4, :], yc[it % 2][:, 2 * c4:2 * c4 + 2, :].r("p h d -> p (h d)"), K.identf.v())
        P.copy(yT[:, 8:12, o], y_ps.v(), eng="act")

    qprep(0)
    deferred = None
    for it in range(NT):
        qn_, gs_, yc_ = qn[it % 2], gs[it % 2], yc[it % 2]
        for g in range(2):
            hs_ = slice(4 * g, 4 * g + 4)
            qTb_ = qTb[g]
            qps = bk.nxt()
            for hh in range(4):
                P.tr(qps[0:64, hh, :], qn_[:, 4 * g + hh, :], K.identf.v())
            P.copy(qTf.v(), qps[0:64, :, :], eng="act")
            P.copy(qTb_.v(), qps[0:64, :, :], eng="dve")
            bc_ = bct[st_["ib"] % 2]
            st_["ib"] += 1
            P.dma("sp", bc_.v(), C.biasC[it, g])
            sc = bk.nxt()
            P.mm(flat(sc), ckT[:, g, :], flat(qTf), start=True, stop=False)
            P.mm(flat(sc), cm[:, CM_IDENT, :], flat(bc_), start=False, stop=True)
            P.activation(ec.v(), sc.v(), AF.Exp)
            oc = bk.nxt()
            ocv = flat(oc)[:, 0:388].r("p (h c) -> p h c", c=97)
            for hh in range(4):
                P.mm(ocv[:, hh, :], ec[:, hh, :], cvx[:, g, :])
            if deferred is not None:
                ytrans(deferred)
                deferred = None
            P.ts(rc[:, 0, :], ocv[:, :, 64], 1e-30, ALU.max)
            P.recip(rc[:, 0, :], rc[:, 0, :])
            P.ts(imp.v(), ocv[:, 0, 65:97], rc[:, 0, 0:1], ALU.mult)
            for hh in range(1, 4):
                P.stt(imp.v(), ocv[:, hh, 65:97], rc[:, 0, hh:hh + 1], imp.v(), ALU.mult, ALU.add)
            P.tt(rc[:, 0, :], rc[:, 0, :], gs_[:, hs_, 0], ALU.mult)
            for hh in range(4):
                P.ts(yc_[:, 4 * g + hh, :], ocv[:, hh, 0:64], rc[:, 0, hh:hh + 1], ALU.mult)
            P.tt(imp.v(), imp.v(), K.selc[:, it, 0, :], ALU.mult)
            P.tt(imp.v(), imp.v(), K.selc[:, it, 1, :], ALU.add)
            ia, ma = imp.ap, mx8.ap
            P.op("dve", lambda e, ia=ia, ma=ma: e.max(out=ma, in_=ia), [imp], [mx8])
            P.ts(selm.v(), imp.v(), mx8[:, 3:4], ALU.is_ge, -1.0, ALU.add)
            osel = bkA.nxt()
            osv = flat(osel)[:, 0:260].r("p (h c) -> p h c", c=65)
            owin = bkA.nxt()
            owv = flat(owin)[:, 0:260].r("p (h c) -> p h c", c=65)
            j0 = max(0, it - 4)
            items = [("w", jt) for jt in range(j0, it + 1)] + [("s", jt) for jt in range(it + 1)]

            def scores(item):
                br, jt = item
                off = it - jt
                if br == "s" and jt == 0:
                    sps = bk.nxt()
                    P.tr(sps[0:32, 0, :], selm.v(), K.identf.v())
                    for hh in range(4):
                        P.copy(selT[:, hh, :], sps[0:32, 0, :], eng=("act", "dve")[hh % 2])
                    if g == 1 and it + 1 < NT:
                        qprep(it + 1)
                sp_ = bkS.nxt()
                if br == "s":
                    ti = min(off, 2)
                    P.mm(flat(sp_), kTs[:, g, jt * 128:(jt + 1) * 128], flat(qTb_), start=True, stop=False)
                    P.mm(flat(sp_), K.identb.v(), K.tbl[:, ti, hs_, :].r("p h i -> p (h i)"), start=False, stop=False)
                    P.mm(flat(sp_), K.rsel[:, jt, :], flat(selT), start=False, stop=True)
                else:
                    ti = (0, 1, 2, 2, 3)[off]
                    P.mm(flat(sp_), kTw[:, g, jt * 128:(jt + 1) * 128], flat(qTb_), start=True, stop=False)
                    P.mm(flat(sp_), K.identb.v(), K.tbl[:, ti, hs_, :].r("p h i -> p (h i)"), start=False, stop=True)
                return sp_

            def finish(item, sp_):
                br, jt = item
                es = esb[st_["ie"] % 4]
                st_["ie"] += 1
                P.activation(es.v(), sp_.v(), AF.Exp)
                if br == "s":
                    for hh in range(4):
                        P.mm(osv[:, hh, :], es[:, hh, :], vs[:, jt, g, :], start=(jt == 0 and hh == 0), stop=(jt == it), skip=True)
                    if jt == it:
                        P.ts(rc[:, 1, :], osv[:, :, 64], 1e-30, ALU.max)
                        P.recip(rc[:, 1, :], rc[:, 1, :])
                        P.tt(rc[:, 1, :], rc[:, 1, :], gs_[:, hs_, 1], ALU.mult)
                        for hh in range(4):
                            P.stt(yc_[:, 4 * g + hh, :], osv[:, hh, 0:64], rc[:, 1, hh:hh + 1], yc_[:, 4 * g + hh, :], ALU.mult, ALU.add)
                else:
                    for hh in range(4):
                        P.mm(owv[:, hh, :], es[:, hh, :], vw[:, jt, g, :], start=(jt == j0 and hh == 0), stop=(jt == it), skip=True)
                    if jt == it:
                        P.ts(rc[:, 2, :], owv[:, :, 64], 1e-30, ALU.max)
                        P.recip(rc[:, 2, :], rc[:, 2, :])
                        P.tt(rc[:, 2, :], rc[:, 2, :], gs_[:, hs_, 2], ALU.mult)
                        for hh in range(4):
                            P.stt(yc_[:, 4 * g + hh, :], owv[:, hh, 0:64], rc[:, 2, hh:hh + 1], yc_[:, 4 * g + hh, :], ALU.mult, ALU.add)

            pending = []
            for item in items:
                pending.append((item, scores(item)))
                if len(pending) > LOOK:
                    finish(*pending.pop(0))
            while pending:
                finish(*pending.pop(0))
        deferred = it
    ytrans(deferred)
    P.release(m)


def phase_E(P, C, K, l, s, uT, yT, hin, hout):
    m0 = P.mark()
    mergedT = P.sb("mergedT", [128, 8, S], BF16)
    m1 = P.mark()
    wsg = WStream(P, 8, 256, nbuf=6)
    wsb = WStream(P, 4, 256, nbuf=6)
    pg = [P.ps("pg", [128, 512]) for _ in range(3)]
    pb = [P.ps("pb", [128, 512]) for _ in range(3)]
    gsb = [P.sb("gsb", [128, 512]) for _ in range(3)]
    acc = [P.sb("acc", [128, 512]) for _ in range(2)]
    it = 0
    k3 = 0

    def ld(q):
        wg, wbr = [], []
        for n in range(3):
            c0 = C_MG + n * 1024 + q * 256
            wg.append(wsg.load(C.w_in[l][:, c0:c0 + 256], 256))
            wbr.append(wsb.load(C.w_branch[l, n][:, q * 256:(q + 1) * 256], 256))
        return wg, wbr
    for q, (wg, wbr) in prefetch_iter(list(range(4)), ld):
        for j in range(2):
            fblk = q * 2 + j
            for tg in range(4):
                a = acc[it % 2]
                tsl = slice(tg * 512, (tg + 1) * 512)
                for n in range(3):
                    g_ps, b_ps, gs = pg[k3 % 3], pb[k3 % 3], gsb[k3 % 3]
                    k3 += 1
                    for kc in range(8):
                        P.mm(g_ps.v(), wg[n][:, kc, j * 128:(j + 1) * 128], uT[:, kc, tsl], start=(kc == 0), stop=(kc == 7))
                    for kc in range(4):
                        P.mm(b_ps.v(), wbr[n][:, kc, j * 128:(j + 1) * 128], yT[:, n * 4 + kc, tsl], start=(kc == 0), stop=(kc == 3))
                    P.activation(gs.v(), g_ps.v(), AF.Sigmoid)
                    if n == 0:
                        P.tt(a.v(), gs.v(), b_ps.v(), ALU.mult)
                    else:
                        P.tt(gs.v(), gs.v(), b_ps.v(), ALU.mult)
                        if n == 1:
                            P.tt(a.v(), a.v(), gs.v(), ALU.add)
                        else:
                            P.tt(mergedT[:, fblk, tsl], a.v(), gs.v(), ALU.add)
                it += 1
    P.release(m1)
    ws = WStream(P, 8, 512, nbuf=2)
    po = [P.ps("po", [128, 512]) for _ in range(3)]
    hsb = [P.sb("hsb", [128, 512]) for _ in range(4)]
    items = [(half, t) for half in range(2) for t in range(NT)]
    wts = [ws.load(C.w_out[l][:, half * 512:(half + 1) * 512], 512) for half in range(2)]

    def ldh(x):
        half, t = x
        hi = hsb[(half * NT + t) % 4]
        P.dma("sp", hi.v(), hin[t][:, half * 512:(half + 1) * 512])
        return hi
    it = 0
    for (half, t), hi in prefetch_iter(items, ldh):
        csl = slice(half * 512, (half + 1) * 512)
        w = wts[half]
        ps = po[it % 3]
        it += 1
        for kc in range(8):
            P.mm(ps.v(), mergedT[:, kc, t * 128:(t + 1) * 128], w[:, kc, :], start=(kc == 0), stop=(kc == 7))
        P.tt(hi.v(), hi.v(), ps.v(), ALU.add)
        P.dma("sp", hout[t][:, csl], hi.v())
    P.release(m0)


def phase_F(P, C, K, l, s, uT, hout):
    norm_transpose(P, K, C, l, 1, hout, uT)
    m = P.mark()
    actT = P.sb("actT", [128, 22, S], BF16)
    m2 = P.mark()
    wsg = WStream(P, 8, 256, nbuf=3)
    wsu = WStream(P, 8, 256, nbuf=3)
    pgt = [P.ps("pgt", [128, 512]) for _ in range(3)]
    pup = [P.ps("pup", [128, 512]) for _ in range(3)]
    sg = [P.sb("sg", [128, 512]) for _ in range(3)]
    it = 0

    def ldf(q):
        return (wsg.load(C.w_ffn_in[l][:, q * 256:(q + 1) * 256], 256),
                wsu.load(C.w_ffn_in[l][:, D_FF + q * 256:D_FF + (q + 1) * 256], 256))
    for q, (wg, wu) in prefetch_iter(list(range(11)), ldf):
        for j in range(2):
            fblk = q * 2 + j
            for tg in range(4):
                tsl = slice(tg * 512, (tg + 1) * 512)
                g_ps, u_ps, sgt = pgt[it % 3], pup[it % 3], sg[it % 3]
                it += 1
                for kc in range(8):
                    P.mm(g_ps.v(), wg[:, kc, j * 128:(j + 1) * 128], uT[:, kc, tsl], start=(kc == 0), stop=(kc == 7))
                for kc in range(8):
                    P.mm(u_ps.v(), wu[:, kc, j * 128:(j + 1) * 128], uT[:, kc, tsl], start=(kc == 0), stop=(kc == 7))
                P.activation(sgt.v(), g_ps.v(), AF.Silu)
                P.tt(actT[:, fblk, tsl], sgt.v(), u_ps.v(), ALU.mult)
    P.release(m2)
    ws = WStream(P, 22, 256, nbuf=2)
    po = [P.ps("po2", [128, 512]) for _ in range(3)]
    hsb = [P.sb("hsb2", [128, 256]) for _ in range(4)]
    items = [(qd, t) for qd in range(4) for t in range(NT)]
    wts = {}

    def ldh2(x):
        qd, t = x
        if t == 0:
            wts[qd] = ws.load(C.w_ffn_out[l][:, qd * 256:(qd + 1) * 256], 256)
        hi = hsb[(qd * NT + t) % 4]
        P.dma("sp", hi.v(), hout[t][:, qd * 256:(qd + 1) * 256])
        return hi
    it = 0
    for (qd, t), hi in prefetch_iter(items, ldh2):
        csl = slice(qd * 256, (qd + 1) * 256)
        w = wts[qd]
        ps = po[it % 3]
        it += 1
        for kc in range(22):
            P.mm(ps[:, 0:256], actT[:, kc, t * 128:(t + 1) * 128], w[:, kc, :], start=(kc == 0), stop=(kc == 21))
        P.tt(hi.v(), hi.v(), ps[:, 0:256], ALU.add)
        P.dma("sp", hout[t][:, csl], hi.v())
    P.release(m)
    norm_transpose(P, K, C, l, 2, hout, uT)
    m = P.mark()
    pTb = P.sb("pTb", [128, 2, S], BF16)
    P.dma("pool", pTb.v(), C.pT[l, s].r("(k p) t -> p k t", p=128))
    wsg = WStream(P, 8, 512, nbuf=2)
    wsp = WStream(P, 2, 512, nbuf=2)
    pg = [P.ps("pg3", [128, 512]) for _ in range(2)]
    pp = [P.ps("pp3", [128, 512]) for _ in range(2)]
    gsb = [P.sb("gsb3", [128, 512]) for _ in range(3)]
    hsb = [P.sb("hsb3", [128, 512]) for _ in range(4)]
    wgs = [wsg.load(C.w_ple_gate[l][:, half * 512:(half + 1) * 512], 512) for half in range(2)]
    wps = [wsp.load(C.w_ple_proj[l][:, half * 512:(half + 1) * 512], 512) for half in range(2)]
    items = [(half, t) for half in range(2) for t in range(NT)]

    def ldh3(x):
        half, t = x
        hi = hsb[(half * NT + t) % 4]
        P.dma("sp", hi.v(), hout[t][:, half * 512:(half + 1) * 512])
        return hi
    it = 0
    for (half, t), hi in prefetch_iter(items, ldh3):
        csl = slice(half * 512, (half + 1) * 512)
        wg, wp = wgs[half], wps[half]
        g_ps, p_ps, gs = pg[it % 2], pp[it % 2], gsb[it % 3]
        it += 1
        for kc in range(8):
            P.mm(g_ps.v(), uT[:, kc, t * 128:(t + 1) * 128], wg[:, kc, :], start=(kc == 0), stop=(kc == 7))
        for kc in range(2):
            P.mm(p_ps.v(), pTb[:, kc, t * 128:(t + 1) * 128], wp[:, kc, :], start=(kc == 0), stop=(kc == 1))
        P.activation(gs.v(), g_ps.v(), AF.Sigmoid)
        P.tt(gs.v(), gs.v(), p_ps.v(), ALU.mult)
        P.tt(hi.v(), hi.v(), gs.v(), ALU.add)
        P.dma("sp", hout[t][:, csl], hi.v())
    P.release(m)


def build(n_layers=DEPTH, n_seq=NSEQ, phases="AE", dbg=(), dbg_yT=False):
    nc = bass.Bass("TRN2", target_bir_lowering=False)
    P = Prog(nc)
    C = declare(P, dbg)
    K = setup_consts(P, C)
    K.epsD = P.sb("epsD", [128, 1])
    P.memset(K.epsD.v(), EPS)
    K.eps6 = P.sb("eps6", [128, 1])
    P.memset(K.eps6.v(), 1e-6)
    if dbg_yT:
        ydbg = P.track(Buf(nc.dram_tensor("yT_dbg", [128, 12, S], F32, kind="ExternalInput").ap(), "yT_dbg"))
    for s in range(n_seq):
        for l in range(n_layers):
            hin = C.xt[s] if l == 0 else C.ot[s]
            my = P.mark()
            yT = P.sb("yT", [128, 12, S], BF16)
            mu = P.mark()
            uT = P.sb("uT", [128, 8, S], BF16)
            if "A" in phases:
                phase_A(P, C, K, l, s, hin, uT)
            P.release(mu)
            if "B" in phases:
                phase_B(P, C, K, l, s, yT)
            if "C" in phases:
                phase_C(P, C, K, l, s, yT)
            if "D" in phases:
                phase_D(P, C, K, l, s, yT)
            if "y" in dbg:
                mm_ = P.mark()
                yf = P.sb("yf", [128, 12, S])
                P.copy(yf.v(), yT.v(), eng="pool")
                P.dma("sp", C.ydbg.v(), yf.v())
                P.release(mm_)
            uT = P.sb("uT", [128, 8, S], BF16)
            norm_transpose(P, K, C, l, 0, hin, uT)
            if dbg_yT:
                m = P.mark()
                yf = P.sb("yf", [128, 12, S])
                P.dma("sp", yf.v(), ydbg.v())
                P.copy(yT.v(), yf.v(), eng="pool")
                P.release(m)
            if "E" in phases:
                phase_E(P, C, K, l, s, uT, yT, hin, C.ot[s])
            P.release(my)
            if "E" in phases:
                uT = P.sb("uT", [128, 8, S], BF16)
                phase_F(P, C, K, l, s, uT, C.ot[s])
                P.release(my)
    st = P.emit()
    P.close()
    return nc, st


import math
NEGB = -30000.0


def rel_bucket_np(dist):
    d = np.maximum(dist, 0)
    df = np.maximum(d, 1).astype(np.float32)
    large = 16 + (np.log(df / np.float32(16)) / np.float32(math.log(128 / 16)) * np.float32(16)).astype(np.int32)
    large = np.minimum(large, 31)
    return np.where(d < 16, d, large).astype(np.int64)


def nsa_consts(inp):
    rb = inp["rel_bias"].astype(np.float32)
    d = {}
    j = np.arange(128)[:, None]
    i = np.arange(128)[None, :]
    neg = np.float32(NEGB)

    def gat(dist, valid):
        g = rb[rel_bucket_np(dist)]
        g = np.where(valid[..., None], g, neg)
        return np.ascontiguousarray(g.transpose(0, 2, 1))
    tb = np.zeros((4, 128, 8, 128), np.float32)
    tb[0] = gat(i - j, (i - j) >= 0)
    tb[1] = gat(128 + i - j, np.ones((128, 128), bool))
    tb[2] = gat(np.full((128, 128), 1000), np.ones((128, 128), bool))
    tb[3] = gat(512 + i - j, i < j)
    d["tbls"] = tb
    n = np.arange(128)[:, None]
    bc = np.zeros((NT, 2, 128, 4, 128), np.float32)
    for it in range(NT):
        tq = it * 128 + np.arange(128)[None, :]
        dist = tq - (16 * n + 31)
        valid = (dist >= 0) & (n < 127)
        g = np.where(valid[..., None], rb[rel_bucket_np(dist)], neg)
        g = g.transpose(0, 2, 1)
        bc[it, 0] = g[:, 0:4]
        bc[it, 1] = g[:, 4:8]
    d["biasC"] = bc
    sc = np.zeros((128, NT, 2, 32), np.float32)
    jj = np.arange(32)[None, :]
    for it in range(NT):
        cur = (it * 128 + np.arange(128)[:, None]) // 64
        forced = (jj == 0) | (jj == cur)
        causal = jj <= cur
        sc[:, it, 0] = (causal & ~forced)
        sc[:, it, 1] = np.where(forced, 1e4, np.where(causal, 0.0, -1.0))
    d["selc"] = sc
    cs = np.arange(127) * 16
    ce = cs + 31
    ss = np.arange(32) * 64
    se = ss + 63
    ov = np.zeros((128, 32), np.float32)
    ov[:127] = ((cs[:, None] <= se[None]) & (ce[:, None] >= ss[None]))
    d["ovm"] = ov
    rs = np.zeros((32, NT, 128), np.float32)
    for jt in range(NT):
        for jr in range(128):
            rs[2 * jt + jr // 64, jt, jr] = 30000.0
    d["rselc"] = rs
    d["wcmp"] = np.ascontiguousarray(inp["nsa_w_cmp"].transpose(0, 3, 1, 2, 4))
    d["peT"] = np.ascontiguousarray(inp["nsa_cmp_pe"].transpose(0, 3, 1, 2))
    d["kn0"] = np.ascontiguousarray(inp["nsa_k_norm"][:, 0, :, None])
    return d


def host_consts(inp):
    d = {}
    j = np.arange(128)[:, None]
    i = np.arange(128)[None, :]
    cm = np.zeros((128, NCM, 128), np.float32)
    cm[:, CM_TRI] = (j <= i)
    cm[:, CM_NEGM] = np.where(i >= j, 0.0, -30000.0)
    cm[:, CM_STRICT] = (i > j)
    cm[:, CM_SEL127] = (j == 127) * np.ones((1, 128))
    cm[:, CM_IDENT] = (i == j)
    cm[:, CM_ONES] = 1.0
    d["cmat"] = cm
    sm = np.zeros((DEPTH, NSMALL), np.float32)
    sm[:, SM_ALOG:SM_ALOG + 4] = inp["gdn_a_log"]
    sm[:, SM_DTB:SM_DTB + 4] = inp["gdn_dt_bias"]
    sm[:, SM_BI:SM_BI + 4] = inp["mlstm_b_i"]
    sm[:, SM_BF:SM_BF + 4] = inp["mlstm_b_f"]
    sm[:, SM_GDNN:SM_GDNN + 128] = inp["gdn_norm"]
    sm[:, SM_MLN:SM_MLN + 128] = inp["mlstm_norm"]
    sm[:, SM_QN:SM_QN + 64] = inp["nsa_q_norm"]
    sm[:, SM_KN:SM_KN + 192] = inp["nsa_k_norm"].reshape(DEPTH, 192)
    d["small"] = np.ascontiguousarray(np.broadcast_to(sm[:, None, :], (DEPTH, 128, NSMALL)))
    d.update(nsa_consts(inp))
    d["cw"] = np.ascontiguousarray(inp["conv_w"].reshape(DEPTH, 4, 12, 128).transpose(0, 3, 2, 1))
    return d


def host_inputs(inp, core=0):
    b0 = core * 2
    d = {}
    d["x"] = np.ascontiguousarray(inp["x"][b0:b0 + 2])
    d["pT"] = np.ascontiguousarray(np.transpose(inp["p"][:, b0:b0 + 2], (0, 1, 3, 2)))
    for k in ["w_in", "w_branch", "w_out", "w_ffn_in", "w_ffn_out", "w_ple_gate", "w_ple_proj"]:
        d[k] = inp[k]
    g = np.stack([inp["norm_mix"], inp["norm_ffn"], inp["norm_ple"]], axis=1)
    d["gains_b"] = np.ascontiguousarray(np.broadcast_to(g[:, :, None, :], (DEPTH, 3, 128, D)), dtype=np.float32)
    d["ident"] = np.eye(128, dtype=np.float32)
    d.update(host_consts(inp))
    return d


_NC_CACHE = {}


def kernel(**inputs):
    inp = {k_: np.asarray(v) for k_, v in inputs.items()}
    if "full" not in _NC_CACHE:
        _NC_CACHE["full"] = build(n_layers=DEPTH, n_seq=NSEQ, phases="ABCDE")[0]
    nc = _NC_CACHE["full"]
    consts = host_consts(inp)
    shared = {}
    for k_ in ["w_in", "w_branch", "w_out", "w_ffn_in", "w_ffn_out", "w_ple_gate", "w_ple_proj"]:
        shared[k_] = np.ascontiguousarray(inp[k_], dtype=np.float32)
    g = np.stack([inp["norm_mix"], inp["norm_ffn"], inp["norm_ple"]], axis=1)
    shared["gains_b"] = np.ascontiguousarray(np.broadcast_to(g[:, :, None, :], (DEPTH, 3, 128, D)), dtype=np.float32)
    shared["ident"] = np.eye(128, dtype=np.float32)
    shared.update(consts)
    in_maps = []
    for c in range(8):
        d = dict(shared)
        d["x"] = np.ascontiguousarray(inp["x"][2 * c:2 * c + 2], dtype=np.float32)
        d["pT"] = np.ascontiguousarray(np.transpose(inp["p"][:, 2 * c:2 * c + 2], (0, 1, 3, 2)), dtype=np.float32)
        in_maps.append(d)
    res = run_bass_kernel_spmd(nc, in_maps, core_ids=list(range(8)))
    out = np.concatenate([np.asarray(r["out"]) for r in res.results], axis=0)
    return out.astype(np.float32)
```

```python
import numpy as np
import concourse.bass as bass
import concourse.mybir as mybir
from concourse.bass_utils import run_bass_kernel_spmd

F32 = mybir.dt.float32
BF16 = mybir.dt.bfloat16
I32 = mybir.dt.int32
AF = mybir.ActivationFunctionType
ALU = mybir.AluOpType
AX = mybir.AxisListType

N_DMA_SEMS = 48


class V:
    __slots__ = ("buf", "ap")

    def __init__(self, buf, ap):
        self.buf = buf
        self.ap = ap

    def __getitem__(self, idx):
        return V(self.buf, self.ap[idx])

    def r(self, pat, **kw):
        return V(self.buf, self.ap.rearrange(pat, **kw))

    def bc(self, shape):
        return V(self.buf, self.ap.to_broadcast(list(shape)))


class Buf:
    __slots__ = ("ap", "name", "lw", "rd", "excl")

    def __init__(self, ap, name=""):
        self.ap = ap
        self.name = name
        self.lw = None
        self.rd = []
        self.excl = False

    def __getitem__(self, idx):
        return V(self, self.ap[idx])

    def v(self):
        return V(self, self.ap)

    def r(self, pat, **kw):
        return V(self, self.ap.rearrange(pat, **kw))

    def sub(self, idx, name=""):
        return Buf(self.ap[idx], name or self.name)


def _ap(x):
    return x.ap if isinstance(x, V) else x


class Prog:
    ENGS = ("pe", "dve", "act", "pool", "sp")

    def __init__(self, nc):
        self.nc = nc
        self.ops = []
        self.eng = {"pe": nc.tensor, "dve": nc.vector, "act": nc.scalar, "pool": nc.gpsimd, "sp": nc.sync}
        self._ctx = []
        self.all_bufs = []
        self.last_barrier = None
        self._uid = 0

    def _nm(self, name):
        self._uid += 1
        return "%s_%d" % (name, self._uid)

    def sb(self, name, shape, dt=F32):
        g = self.nc.sbuf_tensor(self._nm(name), list(shape), dt)
        t = g.__enter__()
        self._ctx.append(g)
        b = Buf(t.ap() if hasattr(t, "ap") else t[:], name)
        self.all_bufs.append(b)
        return b

    def ps(self, name, shape, dt=F32):
        g = self.nc.psum_tensor(self._nm(name), list(shape), dt)
        t = g.__enter__()
        self._ctx.append(g)
        b = Buf(t.ap() if hasattr(t, "ap") else t[:], name)
        b.excl = True
        self.all_bufs.append(b)
        return b

    def dram(self, name, shape, dt=F32, kind="Internal"):
        t = self.nc.dram_tensor(name, list(shape), dt, kind=kind).ap()
        b = Buf(t, name)
        self.all_bufs.append(b)
        return b

    def track(self, b):
        self.all_bufs.append(b)
        return b

    def mark(self):
        return len(self._ctx)

    def release(self, mark):
        self.barrier()
        while len(self._ctx) > mark:
            g = self._ctx.pop()
            g.__exit__(None, None, None)

    def op(self, eng, fn, reads=(), writes=()):
        self.ops.append((eng, fn, tuple(reads), tuple(writes), False, self.last_barrier))

    def barrier(self):
        idx = len(self.ops)
        self.ops.append(("sp", lambda e: e.nop(), (), (), "bar", self.last_barrier))
        self.last_barrier = idx

    def dma(self, eng, out, in_, **kw):
        oa, ia = _ap(out), _ap(in_)

        def fn(e, oa=oa, ia=ia, kw=kw):
            return e.dma_start(out=oa, in_=ia, **kw)
        self.ops.append((eng, fn, (in_.buf,), (out.buf,), True, self.last_barrier))

    @staticmethod
    def _rw(outs, ins):
        w = [o.buf for o in outs if isinstance(o, V)]
        r = [i.buf for i in ins if isinstance(i, V)]
        w += [b for b in r if b.excl and b not in w]
        return r, w

    def activation(self, out, in_, func, bias=0.0, scale=1.0, accum_out=None, eng="act"):
        r, w = self._rw([out, accum_out], [in_, bias, scale])
        kw = dict(out=_ap(out), in_=_ap(in_), func=func, bias=_ap(bias), scale=_ap(scale))
        if accum_out is not None:
            kw["accum_out"] = _ap(accum_out)
        self.op(eng, lambda e, kw=kw: e.activation(**kw), r, w)

    def tt(self, out, in0, in1, op, eng="dve"):
        r, w = self._rw([out], [in0, in1])
        kw = dict(out=_ap(out), in0=_ap(in0), in1=_ap(in1), op=op)
        self.op(eng, lambda e, kw=kw: e.tensor_tensor(**kw), r, w)

    def ts(self, out, in0, s1, op0, s2=None, op1=None, accum_out=None, eng="dve"):
        r, w = self._rw([out, accum_out], [in0, s1, s2])
        kw = dict(out=_ap(out), in0=_ap(in0), scalar1=_ap(s1), scalar2=_ap(s2), op0=op0)
        if op1 is not None:
            kw["op1"] = op1
        if accum_out is not None:
            kw["accum_out"] = _ap(accum_out)
        self.op(eng, lambda e, kw=kw: e.tensor_scalar(**kw), r, w)

    def stt(self, out, in0, scalar, in1, op0, op1, eng="dve"):
        r, w = self._rw([out], [in0, scalar, in1])
        kw = dict(out=_ap(out), in0=_ap(in0), scalar=_ap(scalar), in1=_ap(in1), op0=op0, op1=op1)
        self.op("dve", lambda e, kw=kw: e.scalar_tensor_tensor(**kw), r, w)

    def copy(self, out, in_, eng="dve"):
        r, w = self._rw([out], [in_])
        oa, ia = _ap(out), _ap(in_)
        if eng == "act":
            self.op(eng, lambda e: e.copy(out=oa, in_=ia), r, w)
        else:
            self.op(eng, lambda e: e.tensor_copy(out=oa, in_=ia), r, w)

    def memset(self, out, val, eng="pool"):
        r, w = self._rw([out], [])
        oa = _ap(out)
        self.op(eng, lambda e: e.memset(oa, val), r, w)

    def reduce(self, out, in_, op, axis=AX.X, eng="dve"):
        r, w = self._rw([out], [in_])
        oa, ia = _ap(out), _ap(in_)
        self.op(eng, lambda e: e.tensor_reduce(out=oa, in_=ia, axis=axis, op=op), r, w)

    def recip(self, out, in_):
        r, w = self._rw([out], [in_])
        oa, ia = _ap(out), _ap(in_)
        self.op("dve", lambda e: e.reciprocal(out=oa, in_=ia), r, w)

    def mm(self, out, lhsT, rhs, start=True, stop=True, skip=False):
        r, w = self._rw([out], [lhsT, rhs])
        oa, la, ra = _ap(out), _ap(lhsT), _ap(rhs)
        if skip:
            self.op("pe", lambda e: e.matmul(oa, la, ra, start=start, stop=stop, skip_group_check=True), r, w)
        else:
            self.op("pe", lambda e: e.matmul(oa, la, ra, start=start, stop=stop), r, w)

    def tr(self, out, in_, ident):
        r, w = self._rw([out], [in_, ident])
        oa, ia, da = _ap(out), _ap(in_), _ap(ident)
        self.op("pe", lambda e: e.transpose(oa, ia, da), r, w)

    def emit(self):
        nc = self.nc
        ops = self.ops
        n = len(ops)
        deps = [None] * n
        signal = [False] * n
        last_on = {}
        dmas_since = []
        for i, (eng, fn, reads, writes, is_dma, bar) in enumerate(ops):
            d = set()
            if bar is not None:
                d.add(bar)
            if is_dma == "bar":
                d.update(last_on.values())
                d.update(dmas_since)
                dmas_since = []
                is_dma = False
            elif is_dma:
                dmas_since.append(i)
            last_on[eng] = i
            for r in reads:
                if r.lw is not None:
                    d.add(r.lw)
            for w in writes:
                if w.lw is not None:
                    j = w.lw
                    if is_dma or ops[j][4] or ops[j][0] != eng:
                        d.add(j)
                for j in w.rd:
                    if is_dma or ops[j][4] or ops[j][0] != eng:
                        d.add(j)
            d.discard(i)
            if eng == "pe":
                d = {j for j in d if ops[j][0] != "pe" or ops[j][4]}
            for r in reads:
                r.rd.append(i)
            for w in writes:
                w.lw = i
                w.rd = []
            best = {}
            dd = set()
            for j in d:
                if ops[j][4] is True:
                    dd.add(j)
                else:
                    e2 = ops[j][0]
                    if e2 not in best or best[e2] < j:
                        best[e2] = j
            dd.update(best.values())
            deps[i] = dd
            for j in dd:
                signal[j] = True
        sems = {}
        for e in self.ENGS:
            g = nc.semaphore("s_" + e)
            sems[e] = g.__enter__()
            self._ctx.append(g)
        dsems = []
        for k in range(N_DMA_SEMS):
            g = nc.semaphore("d_%d" % k)
            dsems.append(g.__enter__())
            self._ctx.append(g)
        cnt = {e: 0 for e in self.ENGS}
        dcnt = [0] * N_DMA_SEMS
        ev = [None] * n
        dnext = 0
        dma_prev = [None] * N_DMA_SEMS
        dma_guard = [None] * n
        for i, (eng, fn, reads, writes, is_dma, bar) in enumerate(ops):
            if is_dma is True:
                k = dnext
                dnext = (dnext + 1) % N_DMA_SEMS
                dma_guard[i] = dma_prev[k]
                dcnt[k] += 16
                ev[i] = ("d", k, dcnt[k])
                dma_prev[k] = ev[i]
            elif signal[i]:
                cnt[eng] += 1
                ev[i] = ("c", eng, cnt[eng])
        waited = {e: {} for e in self.ENGS}
        nwaits = 0
        last_dma = {}
        for i, (eng, fn, reads, writes, is_dma, bar) in enumerate(ops):
            need = {}
            for j in deps[i]:
                kind, key, val = ev[j]
                if need.get((kind, key), 0) < val:
                    need[(kind, key)] = val
            if is_dma is True and dma_guard[i] is not None:
                kind, key, val = dma_guard[i]
                if need.get((kind, key), 0) < val:
                    need[(kind, key)] = val
            w = waited[eng]
            for (kind, key), val in need.items():
                if w.get((kind, key), 0) >= val:
                    continue
                w[(kind, key)] = val
                s = sems[key] if kind == "c" else dsems[key]
                self.eng[eng].wait_ge(s, val)
                nwaits += 1
            ins = fn(self.eng[eng])
            if ev[i] is not None:
                kind, key, val = ev[i]
                if kind == "c":
                    ins.then_inc(sems[key], 1)
                else:
                    ins.then_inc(dsems[key], 16)
                    last_dma[key] = val
        w = waited["sp"]
        for k, val in last_dma.items():
            if w.get(("d", k), 0) < val:
                self.eng["sp"].wait_ge(dsems[k], val)
        self.stats = dict(n_ops=n, n_waits=nwaits, cnt=dict(cnt))
        return self.stats

    def close(self):
        while self._ctx:
            g = self._ctx.pop()
            g.__exit__(None, None, None)


S = 2048
D = 1024
NT = S // 128
DEPTH = 2
NSEQ = 2
D_IN = 8488
D_FF = 2816
EPS = 1e-6
C_AQ, C_AK, C_AV, C_AZ, C_AA, C_AB = 0, 512, 1024, 1536, 2048, 2052
C_BQ, C_BK, C_BV, C_BO, C_BI, C_BF = 2056, 2568, 3080, 3592, 4104, 4108
C_CQ, C_CKC, C_CVC, C_CKS, C_CVS, C_CKW, C_CVW, C_CG, C_MG = 4112, 4624, 4752, 4880, 5008, 5136, 5264, 5392, 5416


TM_RANGES = [(C_AZ, 520), (C_BK, 512), (C_BV, 1032), (C_CQ, 1304)]
TM_OFF = {}
_o = 0
for _c, _w in TM_RANGES:
    TM_OFF[_c] = _o
    _o += _w
TM_W = _o


def tm_col(c):
    for c0, w in TM_RANGES:
        if c0 <= c < c0 + w:
            return TM_OFF[c0] + (c - c0)
    raise KeyError(c)


FM_RANGES = [(C_AQ, 1536), (C_BQ, 1024)]
FM_OFF = {C_AQ: 0, C_BQ: 1536}


class Ctx:
    pass


CM_TRI, CM_NEGM, CM_STRICT, CM_SEL127, CM_IDENT, CM_ONES = range(6)
NCM = 6
SM_ALOG, SM_DTB, SM_BI, SM_BF, SM_GDNN, SM_MLN, SM_QN, SM_KN = 0, 4, 8, 12, 16, 144, 272, 336
NSMALL = 336 + 192


def declare(P, dbg=()):
    nc = P.nc
    C = Ctx()

    def inp(name, shape, dt=F32):
        return P.track(Buf(nc.dram_tensor(name, list(shape), dt, kind="ExternalInput").ap(), name))

    C.x = inp("x", [NSEQ, S, D])
    C.pT = inp("pT", [DEPTH, NSEQ, 256, S])
    C.w_in = inp("w_in", [DEPTH, D, D_IN])
    C.w_branch = inp("w_branch", [DEPTH, 3, 512, D])
    C.w_out = inp("w_out", [DEPTH, D, D])
    C.w_ffn_in = inp("w_ffn_in", [DEPTH, D, 2 * D_FF])
    C.w_ffn_out = inp("w_ffn_out", [DEPTH, D_FF, D])
    C.w_ple_gate = inp("w_ple_gate", [DEPTH, D, D])
    C.w_ple_proj = inp("w_ple_proj", [DEPTH, 256, D])
    C.gains_b = inp("gains_b", [DEPTH, 3, 128, D])
    C.ident = inp("ident", [128, 128])
    C.cmat = inp("cmat", [128, NCM, 128])
    C.small = inp("small", [DEPTH, 128, NSMALL])
    C.cw = inp("cw", [DEPTH, 128, 12, 4])
    C.tbls = inp("tbls", [4, 128, 8, 128])
    C.biasC = inp("biasC", [NT, 2, 128, 4, 128])
    C.selc = inp("selc", [128, NT, 2, 32])
    C.ovm = inp("ovm", [128, 32])
    C.rselc = inp("rselc", [32, NT, 128])
    C.wcmp = inp("wcmp", [DEPTH, 64, 2, 32, 64])
    C.peT = inp("peT", [DEPTH, 64, 2, 32])
    C.kn0 = inp("kn0", [DEPTH, 64, 1])
    C.out = P.track(Buf(nc.dram_tensor("out", [NSEQ, S, D], F32, kind="ExternalOutput").ap(), "out"))

    C.xt = [[C.x.sub((q, slice(t * 128, (t + 1) * 128), slice(None)), "x%d_%d" % (q, t)) for t in range(NT)] for q in range(NSEQ)]
    C.ot = [[C.out.sub((q, slice(t * 128, (t + 1) * 128), slice(None)), "o%d_%d" % (q, t)) for t in range(NT)] for q in range(NSEQ)]

    def scr(name, shape, dt=F32):
        kind = "ExternalOutput" if name in dbg else "Internal"
        return P.track(Buf(nc.dram_tensor(name, list(shape), dt, kind=kind).ap(), name))

    C.tm = scr("tm_scr", [S, TM_W])
    C.fm = scr("fm_scr", [2560, S])
    if "y" in dbg:
        C.ydbg = P.track(Buf(nc.dram_tensor("ydbg", [128, 12, S], F32, kind="ExternalOutput").ap(), "ydbg"))
    return C


def setup_consts(P, C):
    K = Ctx()
    K.identf = P.sb("identf", [128, 128])
    K.identb = P.sb("identb", [128, 128], BF16)
    P.dma("sp", K.identf.v(), C.ident.v())
    P.copy(K.identb.v(), K.identf.v())
    K.cm = P.sb("cmat", [128, NCM, 128])
    P.dma("sp", K.cm.v(), C.cmat.v())
    K.cm4 = P.sb("cmat4", [128, 3, 4, 128])
    for i, cmi in enumerate((CM_NEGM, CM_STRICT, CM_IDENT)):
        for h in range(4):
            P.copy(K.cm4[:, i, h, :], K.cm[:, cmi, :], eng="pool")
    K.small = P.sb("small", [128, DEPTH, NSMALL])
    P.dma("sp", K.small.v(), C.small.r("l p n -> p l n"))
    K.cw = P.sb("cw", [128, DEPTH, 12, 4])
    P.dma("sp", K.cw.v(), C.cw.r("l p b k -> p l b k"))
    etbl = P.sb("etbl", [128, 4, 8, 128], BF16)
    K.rsel = P.sb("rsel", [32, NT, 128], BF16)
    K.selc = P.sb("selc", [128, NT, 2, 32])
    mt = P.mark()
    tblf = P.sb("tblf", [128, 4, 8, 128])
    P.dma("sp", tblf.v(), C.tbls.r("a j h i -> j a h i"))
    K.etbl = etbl
    for a in range(4):
        P.activation(etbl[:, a], tblf[:, a], AF.Exp)
    rself = P.sb("rself", [32, NT, 128])
    P.dma("sp", rself.v(), C.rselc.v())
    P.copy(K.rsel.v(), rself.v(), eng="pool")
    P.dma("sp", K.selc.v(), C.selc.v())
    P.release(mt)
    return K


def norm_transpose(P, K, C, l, gi, hsrc, uT, wq="sp"):
    m = P.mark()
    NPF = 4
    hA = [P.sb("hA", [128, D]) for _ in range(NPF)]
    junk = P.sb("junkA", [128, D], BF16)
    ub = [P.sb("ub", [128, D], BF16) for _ in range(2)]
    ss = P.sb("ssA", [128, NT])
    rs = P.sb("rsA", [128, NT])
    pT = [P.ps("pTA", [128, 8, 128], BF16) for _ in range(2)]
    Gt = P.sb("Gt", [128, D])
    P.dma(wq, Gt.v(), C.gains_b[l, gi])
    for t in range(NPF - 1):
        P.dma(wq, hA[t].v(), hsrc[t].v())
    def stage1(t):
        b = t % 2
        if t + NPF - 1 < NT:
            P.dma(wq, hA[(t + NPF - 1) % NPF].v(), hsrc[t + NPF - 1].v())
        sst = ss.sub((slice(None), slice(t, t + 1)))
        rst = rs.sub((slice(None), slice(t, t + 1)))
        P.activation(junk.v(), hA[t % NPF].v(), AF.Square, accum_out=sst.v())
        P.activation(rst.v(), sst.v(), AF.Sqrt, bias=K.epsD.v(), scale=1.0 / D)
        P.recip(rst.v(), rst.v())
        P.stt(ub[b].v(), hA[t % NPF].v(), rst.v(), Gt.v(), ALU.mult, ALU.mult)

    def stage2(t):
        b = t % 2
        for kc in range(8):
            P.tr(pT[b][:, kc, :], ub[b][:, kc * 128:(kc + 1) * 128], K.identb.v())
        if t % 2 == 0:
            P.copy(uT[:, :, t * 128:(t + 1) * 128], pT[b].v(), eng="act")
        else:
            P.copy(uT[:, :, t * 128:(t + 1) * 128], pT[b].v(), eng="dve")

    stage1(0)
    for t in range(NT):
        if t + 1 < NT:
            stage1(t + 1)
        stage2(t)
    P.release(m)


class WStream:
    def __init__(self, P, nk, width, nstage=0, nbuf=3):
        self.P = P
        self.nk = nk
        self.width = width
        self.wb = [P.sb("wb", [128, nk, width], BF16) for _ in range(nbuf)]
        self.j = 0

    def load(self, wsrc_v, ncols, gain=None, nk=None):
        P = self.P
        nk = nk or self.nk
        wb = self.wb[self.j % len(self.wb)]
        self.j += 1
        P.dma("pool", wb[:, :nk, :ncols], wsrc_v.r("(k p) c -> p k c", p=128))
        return wb[:, :nk, :ncols]


def prefetch_iter(items, loader):
    nxt = loader(items[0]) if items else None
    for i, it_ in enumerate(items):
        cur = nxt
        if i + 1 < len(items):
            nxt = loader(items[i + 1])
        yield it_, cur


def phase_A(P, C, K, l, s, hsrc, uT):
    norm_transpose(P, K, C, l, 0, hsrc, uT)
    m = P.mark()
    ws = WStream(P, 8, 512, nbuf=3)
    w_in = C.w_in[l]
    blocks = []
    for c0, w in FM_RANGES:
        for cb in range(0, w, 512):
            blocks.append(("fm", c0, cb, 512))
    for c0, w in TM_RANGES:
        for cb in range(0, w, 512):
            blocks.append(("tm", c0, cb, min(512, w - cb)))
    pfm = [P.ps("pfm", [128, 512]) for _ in range(3)]
    sfm = [P.sb("sfm", [128, S]) for _ in range(2)]
    stm = [P.sb("stm", [128, 512]) for _ in range(4)]
    blk = 0
    it = 0
    ip = 0
    for (kind, c0, cb, nc_), wb in prefetch_iter(blocks, lambda b: ws.load(w_in[:, b[1] + b[2]:b[1] + b[2] + b[3]], b[3])):
        if kind == "fm":
            for j in range(4):
                st = sfm[blk % 2]
                for tg in range(4):
                    ps = pfm[ip % 3]
                    ip += 1
                    for kc in range(8):
                        P.mm(ps.v(), wb[:, kc, j * 128:(j + 1) * 128], uT[:, kc, tg * 512:(tg + 1) * 512],
                             start=(kc == 0), stop=(kc == 7))
                    P.copy(st[:, tg * 512:(tg + 1) * 512], ps.v(), eng=("act", "dve")[tg % 2])
                r0 = FM_OFF[c0] + cb + j * 128
                P.dma("sp", C.fm[r0:r0 + 128, :], st.v())
                blk += 1
        else:
            for t in range(NT):
                ps = pfm[ip % 3]
                ip += 1
                st = stm[it % 4]
                for kc in range(8):
                    P.mm(ps[:, :nc_], uT[:, kc, t * 128:(t + 1) * 128], wb[:, kc, :], start=(kc == 0), stop=(kc == 7))
                P.copy(st[:, :nc_], ps[:, :nc_], eng=("act", "dve")[it % 2])
                o = TM_OFF[c0] + cb
                P.dma("sp", C.tm[t * 128:(t + 1) * 128, o:o + nc_], st[:, :nc_])
                it += 1
    P.release(m)


STOPB = 0


class Banks:
    def __init__(self, P, n=8):
        self.t = [P.ps("bank", [128, 4, 128]) for _ in range(n)]
        self.i = 0

    def nxt(self):
        b = self.t[self.i % len(self.t)]
        self.i += 1
        return b


class RR:
    def __init__(self, engs):
        self.engs = engs
        self.i = 0

    def __call__(self):
        e = self.engs[self.i % len(self.engs)]
        self.i += 1
        return e


def scale_cols(P, out, in_, sc, eng):
    if eng == "act":
        P.activation(out, in_, AF.Copy, scale=sc)
    else:
        P.ts(out, in_, sc, ALU.mult, eng=eng)


def out_stage_bufs(P):
    OS = Ctx()
    OS.osq = P.sb("osq", [128, 4, 128])
    OS.oss = P.sb("oss", [128, 4])
    OS.zt = P.sb("zt", [128, 4, 128])
    return OS


def out_stage(P, C, K, bk, OS, osb, gcol, gfunc, gainB, yT, yoff, cg):
    zt, osq, oss = OS.zt, OS.osq, OS.oss
    P.dma("sp", zt.r("p h d -> p (h d)"), C.tm[cg * 128:(cg + 1) * 128, gcol:gcol + 512])
    P.activation(zt.v(), zt.v(), gfunc)
    P.activation(osq.v(), osb.v(), AF.Square)
    P.reduce(oss.v(), osq.v(), ALU.add)
    P.activation(oss.v(), oss.v(), AF.Sqrt, bias=K.epsD.v(), scale=1.0 / 128)
    P.recip(oss.v(), oss.v())
    for h in range(4):
        P.stt(osb[:, h, :], osb[:, h, :], oss[:, h:h + 1], gainB, ALU.mult, ALU.mult)
    P.tt(osb.v(), osb.v(), zt.v(), ALU.mult)
    y_ps = bk.nxt()
    for h in range(4):
        P.tr(y_ps[:, h, :], osb[:, h, :], K.identf.v())
    P.copy(yT[:, yoff:yoff + 4, cg * 128:(cg + 1) * 128], y_ps.v(), eng="act")


class SubBanks:
    def __init__(self, tiles):
        self.t = list(tiles)
        self.i = 0

    def nxt(self):
        b = self.t[self.i % len(self.t)]
        self.i += 1
        return b


def run_streams(gens, weights):
    gens = list(gens)
    weights = list(weights)
    while gens:
        for gi in range(len(gens) - 1, -1, -1):
            pass
        alive = []
        for g_, w_ in zip(gens, weights):
            done = False
            for _ in range(w_):
                try:
                    next(g_)
                except StopIteration:
                    done = True
                    break
            if not done:
                alive.append((g_, w_))
        gens = [a[0] for a in alive]
        weights = [a[1] for a in alive]


def gate_prefetch(P, C, zt, gcol, gfunc, cg):
    P.dma("sp", zt.r("p h d -> p (h d)"), C.tm[cg * 128:(cg + 1) * 128, gcol:gcol + 512])
    P.activation(zt.v(), zt.v(), gfunc)


def out_stage_gen(P, C, K, bk, OS, osb, gcol, gfunc, gainB, yT, yoff, cg, zt=None):
    osq, oss = OS.osq, OS.oss
    if zt is None:
        zt = OS.zt
        gate_prefetch(P, C, zt, gcol, gfunc, cg)
    P.activation(osq.v(), osb.v(), AF.Square)
    yield
    P.reduce(oss.v(), osq.v(), ALU.add)
    yield
    P.activation(oss.v(), oss.v(), AF.Sqrt, bias=K.epsD.v(), scale=1.0 / 128)
    yield
    P.recip(oss.v(), oss.v())
    for h in range(4):
        P.stt(osb[:, h, :], osb[:, h, :], oss[:, h:h + 1], gainB, ALU.mult, ALU.mult)
    P.tt(osb.v(), osb.v(), zt.v(), ALU.mult)
    yield
    y_ps = bk.nxt()
    for h in range(4):
        P.tr(y_ps[:, h, :], osb[:, h, :], K.identf.v())
    yield
    P.copy(yT[:, yoff:yoff + 4, cg * 128:(cg + 1) * 128], y_ps.v(), eng="act")


def phase_C(P, C, K, l, s, yT):
    m = P.mark()
    sm = K.small
    cm = K.cm
    NEGM4 = K.cm4[:, 0]
    bk = Banks(P, 8)
    flat = lambda b: b.r("p a b -> p (a b)")
    ext = lambda b, hh: flat(b)[:, 0:258].r("p (a b) -> p a b", b=129)[:, hh, :]
    gi = P.sb("gi", [128, NT, 8])
    ci = tm_col(C_BI)
    for c in range(NT):
        P.dma("sp", gi[:, c, :], C.tm[c * 128:(c + 1) * 128, ci:ci + 8])
    it = P.sb("it", [128, NT, 4])
    lf = P.sb("lf", [128, NT, 4])
    nbf = P.sb("nbf", [128, 4])
    P.ts(nbf.v(), sm[:, l, SM_BF:SM_BF + 4], -1.0, ALU.mult)
    for h in range(4):
        P.ts(it[:, :, h], gi[:, :, h], sm[:, l, SM_BI + h:SM_BI + h + 1], ALU.add)
        P.activation(lf[:, :, h], gi[:, :, 4 + h], AF.Exp, bias=nbf[:, h:h + 1], scale=-1.0)
    P.activation(lf.v(), lf.v(), AF.Ln, bias=1.0)
    P.ts(lf.v(), lf.v(), -1.0, ALU.mult)
    G1 = P.sb("G1", [128, NT, 4, 2])
    G2 = P.sb("G2", [128, NT, 4, 2])
    G3 = P.sb("G3", [128, NT, 4, 2])
    P.memset(G1.v(), 0.0)
    P.memset(G2.v(), 0.0)
    P.memset(G3.v(), 0.0)
    P.memset(G1[0:1, :, :, 1], 1.0)
    P.memset(G2[0:1, :, :, 0], 1.0)
    P.copy(G1[:, :, :, 0], lf.v())
    P.ts(G2[:, :, :, 1], lf.v(), -1.0, ALU.mult)
    P.copy(G3[:, :, :, 1], it.v())
    bc = P.sb("bc", [128, NT, 4])
    f64 = lambda b: b.r("p c h -> p (c h)")
    b0 = bk.nxt()
    P.mm(flat(b0)[:, 0:64], cm[:, CM_TRI, :], f64(lf))
    P.copy(f64(bc), flat(b0)[:, 0:64])
    b1 = bk.nxt()
    P.mm(flat(b1)[:, 0:64], cm[:, CM_SEL127, :], f64(bc))
    EBL = P.sb("EBL", [128, NT, 4])
    EKW = P.sb("EKW", [128, NT, 4])
    EB = P.sb("EB", [128, NT, 4])
    P.activation(f64(EBL), flat(b1)[:, 0:64], AF.Exp)
    P.tt(f64(EKW), flat(b1)[:, 0:64], f64(bc), ALU.subtract)
    P.tt(EKW.v(), EKW.v(), it.v(), ALU.add)
    P.activation(EKW.v(), EKW.v(), AF.Exp)
    P.activation(EB.v(), bc.v(), AF.Exp)
    qk = [P.sb("qk", [128, S]) for _ in range(8)]
    for blk in range(8):
        r0 = FM_OFF[C_BQ] + blk * 128
        P.dma("sp", qk[blk].v(), C.fm[r0:r0 + 128, :])
        if blk < 4:
            P.activation(qk[blk].v(), qk[blk].v(), AF.Copy, scale=128 ** -0.5)
    qT, kT = qk[0:4], qk[4:8]
    Cst = P.sb("Cst", [128, 4, 129])
    P.memset(Cst.v(), 0.0)
    k_tm = [P.sb("k_tm", [128, 4, 128]) for _ in range(2)]
    v_ext = [P.sb("v_ext", [128, 4, 129]) for _ in range(2)]
    for b in range(2):
        P.memset(v_ext[b][:, :, 128:129], 1.0)
    kw_tm = P.sb("kw_tm", [128, 4, 128])
    R1 = P.sb("R1", [2, 4, 128])
    R2 = P.sb("R2", [2, 4, 128])
    DT = P.sb("DT", [128, 4, 128])
    SmT = P.sb("SmT", [128, 4, 128])
    htmp = P.sb("htmp", [128, 4, 129])
    htot = P.sb("htot", [128, 4, 129])
    rden = P.sb("rden", [128, 4])
    osb = P.sb("osb", [128, 4, 128])
    OS = out_stage_bufs(P)
    gainB = sm[:, l, SM_MLN:SM_MLN + 128]
    ck, cv = tm_col(C_BK), tm_col(C_BV)
    bkX = SubBanks(bk.t[0:3])
    bkY = SubBanks(bk.t[3:8])
    SmT2 = [SmT, P.sb("SmT", [128, 4, 128])]
    kw2 = [kw_tm, P.sb("kw_tm", [128, 4, 128])]
    zt2 = [P.sb("ztC", [128, 4, 128]) for _ in range(2)]

    def stageX(cg):
        par = cg % 2
        o = slice(cg * 128, (cg + 1) * 128)
        kt, ve = k_tm[par], v_ext[par]
        P.dma("sp", kt.r("p h d -> p (h d)"), C.tm[o, ck:ck + 512])
        P.dma("sp", ve[:, :, 0:128], C.tm[o, cv:cv + 512].r("p (h d) -> p h d", d=128))
        gate_prefetch(P, C, zt2[par], tm_col(C_BO), AF.Sigmoid, cg)
        r_ps = bkX.nxt()
        r2_ps = bkX.nxt()
        for h in range(4):
            P.mm(r_ps[0:2, h, :], G1[:, cg, h, :], cm[:, CM_TRI, :])
        for h in range(4):
            P.mm(r2_ps[0:2, h, :], G2[:, cg, h, :], cm[:, CM_TRI, :], start=True, stop=False)
            P.mm(r2_ps[0:2, h, :], G3[:, cg, h, :], cm[:, CM_IDENT, :], start=False, stop=True)
        yield
        P.copy(R1.v(), r_ps[0:2, :, :], eng="dve")
        P.copy(R2.v(), r2_ps[0:2, :, :], eng="act")
        yield
        dt_ps = bkX.nxt()
        for h in range(4):
            P.mm(dt_ps[:, h, :], R2[:, h, :], R1[:, h, :])
        kq_ps = bkX.nxt()
        for h in range(4):
            P.mm(kq_ps[:, h, :], kT[h][:, o], qT[h][:, o])
        yield
        P.tt(DT.v(), dt_ps.v(), NEGM4, ALU.add)
        yield
        P.activation(DT.v(), DT.v(), AF.Exp)
        for h in range(4):
            scale_cols(P, kw2[par][:, h, :], kt[:, h, :], EKW[:, cg, h:h + 1], ("act", "dve")[h % 2])
        yield
        P.tt(SmT2[par].v(), kq_ps.v(), DT.v(), ALU.mult)

    def stageY(cg):
        par = cg % 2
        o = slice(cg * 128, (cg + 1) * 128)
        ve = v_ext[par]
        hq = [bkY.nxt(), bkY.nxt()]
        hs = [bkY.nxt(), bkY.nxt()]
        for h in range(4):
            P.mm(ext(hq[h // 2], h % 2), qT[h][:, o], Cst[:, h, :])
        for h in range(4):
            P.mm(ext(hs[h // 2], h % 2), SmT2[par][:, h, :], ve[:, h, :])
        yield
        for h in range(4):
            P.activation(htmp[:, h, :], ext(hq[h // 2], h % 2), AF.Copy, scale=EB[:, cg, h:h + 1])
        cps = [bkY.nxt(), bkY.nxt()]
        for h in range(4):
            P.mm(ext(cps[h // 2], h % 2), kw2[par][:, h, :], ve[:, h, :])
        yield
        for h in range(4):
            P.tt(htot[:, h, :], htmp[:, h, :], ext(hs[h // 2], h % 2), ALU.add)
        P.ts(rden.v(), htot[:, :, 128], -1.0, ALU.mult)
        P.tt(rden.v(), rden.v(), htot[:, :, 128], ALU.max)
        P.ts(rden.v(), rden.v(), 1.0, ALU.max)
        P.recip(rden.v(), rden.v())
        for h in range(4):
            scale_cols(P, osb[:, h, :], htot[:, h, 0:128], rden[:, h:h + 1], ("dve", "act")[h % 2])
        for h in range(4):
            P.stt(Cst[:, h, :], Cst[:, h, :], EBL[:, cg, h:h + 1], ext(cps[h // 2], h % 2), ALU.mult, ALU.add)
        yield
        yield from out_stage_gen(P, C, K, bkY, OS, osb, tm_col(C_BO), AF.Sigmoid, gainB, yT, 4, cg, zt=zt2[par])

    run_streams([stageX(0)], [1])
    for cg in range(NT):
        if cg + 1 < NT:
            run_streams([stageX(cg + 1), stageY(cg)], [1, 2])
        else:
            run_streams([stageY(cg)], [1])
    P.release(m)


def phase_B(P, C, K, l, s, yT):
    m = P.mark()
    sm = K.small
    cm = K.cm
    NEGM4, STRICT4, IDENT4 = K.cm4[:, 0], K.cm4[:, 1], K.cm4[:, 2]
    bk = Banks(P, 8)
    rr = RR(("dve", "act", "pool"))
    rr2 = RR(("dve", "act"))
    ab = P.sb("ab", [128, NT, 8])
    ca = tm_col(C_AA)
    for c in range(NT):
        P.dma("sp", ab[:, c, :], C.tm[c * 128:(c + 1) * 128, ca:ca + 8])
    e1 = P.sb("e1", [128, NT, 4])
    g = P.sb("g", [128, NT, 4])
    nega = P.sb("nega", [128, 4])
    P.activation(nega.v(), sm[:, l, SM_ALOG:SM_ALOG + 4], AF.Exp)
    P.ts(nega.v(), nega.v(), -1.0, ALU.mult)
    for h in range(4):
        P.activation(e1[:, :, h], ab[:, :, h], AF.Exp, bias=sm[:, l, SM_DTB + h:SM_DTB + h + 1])
    P.activation(e1.v(), e1.v(), AF.Ln, bias=1.0)
    for h in range(4):
        P.ts(g[:, :, h], e1[:, :, h], nega[:, h:h + 1], ALU.mult)
    beta = P.sb("beta", [128, NT, 4])
    P.activation(beta.v(), ab[:, :, 4:8], AF.Sigmoid)
    nbeta = P.sb("nbeta", [128, NT, 4])
    P.ts(nbeta.v(), beta.v(), -1.0, ALU.mult)
    G1 = P.sb("G1", [128, NT, 4, 2])
    G2 = P.sb("G2", [128, NT, 4, 2])
    P.memset(G1.v(), 0.0)
    P.memset(G2.v(), 0.0)
    P.memset(G1[0:1, :, :, 1], 1.0)
    P.memset(G2[0:1, :, :, 0], 1.0)
    P.copy(G1[:, :, :, 0], g.v())
    P.ts(G2[:, :, :, 1], g.v(), -1.0, ALU.mult)
    gc = P.sb("gc", [128, NT, 4])
    b0 = bk.nxt()
    P.mm(b0.r("p a b -> p (a b)")[:, 0:64], cm[:, CM_TRI, :], g.r("p c h -> p (c h)"))
    P.copy(gc.r("p c h -> p (c h)"), b0.r("p a b -> p (a b)")[:, 0:64])
    b1 = bk.nxt()
    P.mm(b1.r("p a b -> p (a b)")[:, 0:64], cm[:, CM_SEL127, :], gc.r("p c h -> p (c h)"))
    EGL = P.sb("EGL", [128, NT, 4])
    ED = P.sb("ED", [128, NT, 4])
    EG = P.sb("EG", [128, NT, 4])
    P.activation(EGL.r("p c h -> p (c h)"), b1.r("p a b -> p (a b)")[:, 0:64], AF.Exp)
    P.tt(ED.r("p c h -> p (c h)"), b1.r("p a b -> p (a b)")[:, 0:64], gc.r("p c h -> p (c h)"), ALU.subtract)
    P.activation(ED.v(), ED.v(), AF.Exp)
    P.activation(EG.v(), gc.v(), AF.Exp)
    if STOPB == 1:
        P.release(m)
        return
    Sst = P.sb("Sst", [128, 4, 128])
    P.memset(Sst.v(), 0.0)
    HW = 1024
    qkv = [P.sb("qkv", [128, HW]) for _ in range(12)]
    raw = [P.sb("raw", [128, HW + 3]) for _ in range(2)]
    sqt = P.sb("sqt", [128, HW])
    rnt = P.sb("rnt", [128, 512])
    gainB = sm[:, l, SM_GDNN:SM_GDNN + 128]
    for hf in range(2):
        t0 = hf * HW
        def prepA(blk, hf=hf, t0=t0):
            r = raw[blk % 2]
            r0 = FM_OFF[C_AQ] + blk * 128
            if hf == 0:
                P.memset(r[:, 0:3], 0.0)
                P.dma("sp", r[:, 3:], C.fm[r0:r0 + 128, 0:HW])
            else:
                P.dma("sp", r.v(), C.fm[r0:r0 + 128, t0 - 3:t0 + HW])
            dst = qkv[blk]
            e = "dve"
            P.ts(dst.v(), r[:, 0:HW], K.cw[:, l, blk, 0:1], ALU.mult, eng=e)
            for k in range(1, 4):
                P.stt(dst.v(), r[:, k:k + HW], K.cw[:, l, blk, k:k + 1], dst.v(), ALU.mult, ALU.add, eng=e)
            P.activation(dst.v(), dst.v(), AF.Silu)

        def prepB(blk):
            dst = qkv[blk]
            if blk < 8:
                P.activation(sqt.v(), dst.v(), AF.Square)
                for hh in range(2):
                    bb = bk.nxt()
                    bbv = bb.r("p a b -> p (a b)")
                    P.mm(bbv, cm[:, CM_ONES, :], sqt[:, hh * 512:(hh + 1) * 512])
                    P.activation(rnt.v(), bbv, AF.Sqrt, bias=K.eps6.v())
                    P.recip(rnt.v(), rnt.v())
                    sc = (128 ** -0.5) if blk < 4 else 1.0
                    P.stt(dst[:, hh * 512:(hh + 1) * 512], dst[:, hh * 512:(hh + 1) * 512], sc, rnt.v(), ALU.mult, ALU.mult)

        prepA(0)
        for blk in range(12):
            if blk + 1 < 12:
                prepA(blk + 1)
            prepB(blk)
        if STOPB == 2:
            P.release(m)
            return
        qT = qkv[0:4]
        kT = qkv[4:8]
        vT = qkv[8:12]
        if hf == 0:
            bkX = SubBanks(bk.t[0:4])
            bkY = SubBanks(bk.t[4:8])
            v_tm = [P.sb("v_tm", [128, 4, 128]) for _ in range(2)]
            kd_tm = [P.sb("kd_tm", [128, 4, 128]) for _ in range(2)]
            attnT = [P.sb("attnT", [128, 4, 128]) for _ in range(2)]
            nwT = [P.sb("nwT", [128, 4, 128]) for _ in range(2)]
            Rm = [[P.sb("Rm", [128, 4, 128]) for _ in range(2)] for _ in range(2)]
            kg_tm = P.sb("kg_tm", [128, 4, 128])
            R1 = P.sb("R1", [2, 4, 128])
            R2 = P.sb("R2", [2, 4, 128])
            DT = P.sb("DT", [128, 4, 128])
            DTS = P.sb("DTS", [128, 4, 128])
            Bm = P.sb("Bm", [128, 4, 128])
            BT = P.sb("BT", [128, 4, 128])
            Pk = [P.sb("Pk", [128, 4, 128]) for _ in range(2)]
            PkT = [P.sb("PkT", [128, 4, 128]) for _ in range(2)]
            vn = P.sb("vn", [128, 4, 128])
            otmp = P.sb("otmp", [128, 4, 128])
            osb = P.sb("osb", [128, 4, 128])
            OS = out_stage_bufs(P)

        def stageX(c, hf=hf, qT=qT, kT=kT, vT=vT):
            cg = hf * 8 + c
            par = cg % 2
            o = slice(c * 128, (c + 1) * 128)
            kt_ps = bkX.nxt()
            vt_ps = bkX.nxt()
            for h in range(4):
                P.tr(kt_ps[:, h, :], kT[h][:, o], K.identf.v())
                P.tr(vt_ps[:, h, :], vT[h][:, o], K.identf.v())
            yield
            P.copy(v_tm[par].v(), vt_ps.v(), eng="act")
            for h in range(4):
                scale_cols(P, kg_tm[:, h, :], kt_ps[:, h, :], EG[:, cg, h:h + 1], "dve")
                scale_cols(P, kd_tm[par][:, h, :], kt_ps[:, h, :], ED[:, cg, h:h + 1], "dve")
            r_ps = bkX.nxt()
            for h in range(4):
                P.mm(r_ps[0:2, h, :], G1[:, cg, h, :], cm[:, CM_TRI, :])
            r2_ps = bkX.nxt()
            for h in range(4):
                P.mm(r2_ps[0:2, h, :], G2[:, cg, h, :], cm[:, CM_TRI, :])
            yield
            P.copy(R1.v(), r_ps[0:2, :, :], eng="dve")
            P.copy(R2.v(), r2_ps[0:2, :, :], eng="act")
            yield
            dt_ps = bkX.nxt()
            for h in range(4):
                P.mm(dt_ps[:, h, :], R2[:, h, :], R1[:, h, :])
            kq_ps = bkX.nxt()
            kk_ps = bkX.nxt()
            for h in range(4):
                P.mm(kq_ps[:, h, :], kT[h][:, o], qT[h][:, o])
                P.mm(kk_ps[:, h, :], kT[h][:, o], kT[h][:, o])
            yield
            P.tt(DT.v(), dt_ps.v(), NEGM4, ALU.add)
            yield
            P.activation(DT.v(), DT.v(), AF.Exp)
            yield
            P.tt(DTS.v(), DT.v(), STRICT4, ALU.mult)
            P.tt(attnT[par].v(), kq_ps.v(), DT.v(), ALU.mult)
            for h in range(4):
                P.stt(Bm[:, h, :], kk_ps[:, h, :], nbeta[:, cg, h:h + 1], DTS[:, h, :], ALU.mult, ALU.mult)
            yield
            bt_ps = bkX.nxt()
            for h in range(4):
                P.tr(bt_ps[:, h, :], Bm[:, h, :], K.identf.v())
            yield
            P.copy(BT.v(), bt_ps.v(), eng="act")
            P.tt(Rm[par][0].v(), Bm.v(), IDENT4, ALU.add)
            yield
            cur, curT, R = Bm, BT, Rm[par][0]
            for k in range(1, 7):
                nP, nPT, nR = Pk[k % 2], PkT[k % 2], Rm[par][k % 2]
                pT_ps = bkX.nxt()
                for h in range(4):
                    P.mm(pT_ps[:, h, :], cur[:, h, :], curT[:, h, :])
                if k < 6:
                    p_ps = bkX.nxt()
                    for h in range(4):
                        P.mm(p_ps[:, h, :], curT[:, h, :], cur[:, h, :])
                yield
                P.copy(nPT.v(), pT_ps.v(), eng="act")
                if k < 6:
                    P.copy(nP.v(), p_ps.v(), eng="dve")
                yield
                rr_ps = bkX.nxt()
                for h in range(4):
                    P.mm(rr_ps[:, h, :], nPT[:, h, :], R[:, h, :])
                yield
                P.tt(nR.v(), R.v(), rr_ps.v(), ALU.add)
                yield
                cur, curT, R = nP, nPT, nR
            Tt = R
            w_ps = bkX.nxt()
            for h in range(4):
                P.mm(w_ps[:, h, :], kg_tm[:, h, :], Tt[:, h, :])
            yield
            P.activation(nwT[par].v(), w_ps.v(), AF.Copy, scale=-1.0)

        def stageY(c, hf=hf, qT=qT):
            cg = hf * 8 + c
            par = cg % 2
            o = slice(c * 128, (c + 1) * 128)
            Tt = Rm[par][0]
            vn_ps = bkY.nxt()
            o1_ps = bkY.nxt()
            for h in range(4):
                P.mm(vn_ps[:, h, :], Tt[:, h, :], v_tm[par][:, h, :], start=True, stop=False)
                P.mm(vn_ps[:, h, :], nwT[par][:, h, :], Sst[:, h, :], start=False, stop=True)
                P.mm(o1_ps[:, h, :], qT[h][:, o], Sst[:, h, :])
            yield
            for h in range(4):
                scale_cols(P, vn[:, h, :], vn_ps[:, h, :], beta[:, cg, h:h + 1], "dve")
                scale_cols(P, otmp[:, h, :], o1_ps[:, h, :], EG[:, cg, h:h + 1], "act")
            yield
            o2_ps = bkY.nxt()
            s_ps = bkY.nxt()
            for h in range(4):
                P.mm(o2_ps[:, h, :], attnT[par][:, h, :], vn[:, h, :])
                P.mm(s_ps[:, h, :], kd_tm[par][:, h, :], vn[:, h, :])
            yield
            P.tt(osb.v(), otmp.v(), o2_ps.v(), ALU.add)
            for h in range(4):
                P.stt(Sst[:, h, :], Sst[:, h, :], EGL[:, cg, h:h + 1], s_ps[:, h, :], ALU.mult, ALU.add)
            yield
            yield from out_stage_gen(P, C, K, bkY, OS, osb, tm_col(C_AZ), AF.Silu, gainB, yT, 0, cg)

        run_streams([stageX(0)], [1])
        for c in range(8):
            if c + 1 < 8:
                run_streams([stageX(c + 1), stageY(c)], [2, 1])
            else:
                run_streams([stageY(c)], [1])
    P.release(m)


def phase_D(P, C, K, l, s, yT):
    m = P.mark()
    sm = K.small
    cm = K.cm
    bk = Banks(P, 2)
    bkA = Banks(P, 2)
    bkS = Banks(P, 4)
    flat = lambda b: b.r("p a b -> p (a b)")
    kTs = P.sb("kTs", [64, 2, S], BF16)
    kTw = P.sb("kTw", [64, 2, S], BF16)
    vs = P.sb("vs", [128, NT, 2, 65], BF16)
    vw = P.sb("vw", [128, NT, 2, 65], BF16)
    P.memset(vs[:, :, :, 64:65], 1.0)
    P.memset(vw[:, :, :, 64:65], 1.0)
    ckT = P.sb("ckT", [64, 2, 128])
    cvx = P.sb("cvx", [128, 2, 97])
    P.memset(ckT.v(), 0.0)
    P.memset(cvx.v(), 0.0)
    P.memset(cvx[:, :, 64:65], 1.0)
    for g in range(2):
        P.dma("sp", cvx[:, g, 65:97], C.ovm.v())
    gq8 = P.sb("gq8", [128, 8, 64])
    gk = P.sb("gk", [128, 4, 64])
    for h in range(8):
        P.ts(gq8[:, h, :], sm[:, l, SM_QN:SM_QN + 64], 0.125, ALU.mult)
    for a in range(2):
        for g in range(2):
            P.copy(gk[:, 2 * a + g, :], sm[:, l, SM_KN + 64 * (a + 1):SM_KN + 64 * (a + 2)])
    kn0 = P.sb("kn0", [64, 1])
    P.dma("sp", kn0.v(), C.kn0[l])
    ckc = tm_col(C_CKC)
    m1 = P.mark()
    kcT = P.sb("kcT", [64, 2, S])
    vcT = P.sb("vcT", [64, 2, S])
    Wc = P.sb("Wc", [64, 2, 32, 64])
    peT = P.sb("peT", [64, 2, 32])
    P.dma("sp", Wc.v(), C.wcmp[l])
    P.dma("sp", peT.v(), C.peT[l])
    kv = [P.sb("kv", [128, 6, 2, 64]) for _ in range(2)]
    ksq = P.sb("ksq", [128, 2, 2, 64])
    kss = P.sb("kss", [128, 2, 2])
    kn = P.sb("kn", [128, 2, 2, 64])
    kn2 = [kn, P.sb("kn", [128, 2, 2, 64])]

    def d1_stage1(t):
        b = kv[t % 2]
        o = slice(t * 128, (t + 1) * 128)
        knt = kn2[t % 2]
        P.dma("sp", b.r("p a g d -> p (a g d)"), C.tm[o, ckc:ckc + 768])
        P.copy(vs[:, t, :, 0:64], b[:, 3, :, :], eng="act")
        P.copy(vw[:, t, :, 0:64], b[:, 5, :, :], eng="dve")
        P.activation(ksq.v(), b[:, 2:6:2, :, :], AF.Square)
        P.reduce(kss.v(), ksq.v(), ALU.add)
        P.activation(kss.v(), kss.v(), AF.Sqrt, bias=K.epsD.v(), scale=1.0 / 64)
        P.recip(kss.v(), kss.v())
        for a in range(2):
            for g in range(2):
                P.stt(knt[:, a, g, :], b[:, 2 + 2 * a, g, :], kss[:, a, g:g + 1], gk[:, 2 * a + g, :], ALU.mult, ALU.mult)

    def d1_stage2(t):
        b = kv[t % 2]
        o = slice(t * 128, (t + 1) * 128)
        knt = kn2[t % 2]
        ps1 = bk.nxt()
        ps2 = bk.nxt()
        for a in range(2):
            for g in range(2):
                P.tr(ps1[0:64, 2 * a + g, :], knt[:, a, g, :], K.identf.v())
                P.tr(ps2[0:64, 2 * a + g, :], b[:, a, g, :], K.identf.v())
        P.copy(kTs[:, :, o], ps1[0:64, 0:2, :], eng="act")
        P.copy(kTw[:, :, o], ps1[0:64, 2:4, :], eng="act")
        P.copy(kcT[:, :, o], ps2[0:64, 0:2, :], eng="dve")
        P.copy(vcT[:, :, o], ps2[0:64, 2:4, :], eng="dve")

    d1_stage1(0)
    for t in range(NT):
        if t + 1 < NT:
            d1_stage1(t + 1)
        d1_stage2(t)
    cst = P.sb("cst", [64, 2])
    raw = P.sb("rawc", [64, 127])
    sqc = P.sb("sqc", [64, 127])
    rnc = P.sb("rnc", [64, 127])
    for kvi in range(2):
        pc = bk.nxt()
        for lq in range(32):
            P.mm(flat(pc)[0:64, 0:1], Wc[:, kvi, lq, :], peT[:, kvi, lq:lq + 1], start=(lq == 0), stop=(lq == 31))
        P.copy(cst[:, kvi:kvi + 1], flat(pc)[0:64, 0:1])
    for kvi in range(2):
        src = (kcT, vcT)[kvi]
        for g in range(2):
            ps = bk.nxt()
            pv = flat(ps)[0:64, 0:127]
            for lq in range(32):
                P.mm(pv, Wc[:, kvi, lq, :], src[:, g, lq:lq + 16 * 126 + 1:16], start=(lq == 0), stop=(lq == 31))
            P.ts(raw.v(), pv, cst[:, kvi:kvi + 1], ALU.add)
            if kvi == 0:
                P.activation(sqc.v(), raw.v(), AF.Square)
                p2 = bk.nxt()
                P.mm(flat(p2)[0:64, 0:127], cm[0:64, CM_ONES, 0:64], sqc.v())
                P.activation(rnc.v(), flat(p2)[0:64, 0:127], AF.Sqrt, bias=K.epsD[0:64, :], scale=1.0 / 64)
                P.recip(rnc.v(), rnc.v())
                P.stt(ckT[:, g, 0:127], raw.v(), kn0[:, 0:1], rnc.v(), ALU.mult, ALU.mult)
            else:
                p2 = bk.nxt()
                P.tr(p2[0:127, 0, 0:64], raw.v(), K.identf[0:64, 0:64])
                P.copy(cvx[0:127, g, 0:64], p2[0:127, 0, 0:64])
    P.release(m1)
    qt = [P.sb("qt", [128, 8, 64]) for _ in range(2)]
    gt = [P.sb("gt", [128, 8, 3]) for _ in range(2)]
    qsq = P.sb("qsq", [128, 8, 64])
    qss = P.sb("qss", [128, 8])
    qn = [P.sb("qn", [128, 8, 64]) for _ in range(2)]
    gs = [P.sb("gs", [128, 8, 3]) for _ in range(2)]
    qTf = P.sb("qTf", [64, 4, 128])
    qTb = [P.sb("qTb", [64, 4, 128], BF16) for _ in range(2)]
    bct = [P.sb("bct", [128, 4, 128]) for _ in range(2)]
    ec = P.sb("ec", [128, 4, 128])
    esb = [P.sb("esb", [128, 4, 128], BF16) for _ in range(4)]
    rc = P.sb("rc", [128, 3, 4])
    imp = P.sb("imp", [128, 32])
    mx8 = P.sb("mx8", [128, 8])
    selm = P.sb("selm", [128, 32])
    selT = P.sb("selT", [32, 4, 128], BF16)
    yc = [P.sb("yc", [128, 8, 64]) for _ in range(2)]
    cq_, cg_ = tm_col(C_CQ), tm_col(C_CG)
    st_ = dict(ie=0, ib=0)
    LOOK = 3

    def qprep(it):
        o = slice(it * 128, (it + 1) * 128)
        q_, g_ = qt[it % 2], gt[it % 2]
        P.dma("sp", q_.r("p h d -> p (h d)"), C.tm[o, cq_:cq_ + 512])
        P.dma("sp", g_.r("p h k -> p (h k)"), C.tm[o, cg_:cg_ + 24])
        P.activation(qsq.v(), q_.v(), AF.Square)
        P.reduce(qss.v(), qsq.v(), ALU.add)
        P.activation(qss.v(), qss.v(), AF.Sqrt, bias=K.epsD.v(), scale=1.0 / 64)
        P.recip(qss.v(), qss.v())
        for h in range(8):
            P.stt(qn[it % 2][:, h, :], q_[:, h, :], qss[:, h:h + 1], gq8[:, h, :], ALU.mult, ALU.mult)
        P.activation(gs[it % 2].v(), g_.v(), AF.Sigmoid)

    def ytrans(it):
        o = slice(it * 128, (it + 1) * 128)
        y_ps = bk.nxt()
        for c4 in range(4):
            P.tr(y_ps[:, c4, :], yc[it % 2][:, 2 * c4:2 * c4 + 2, :].r("p h d -> p (h d)"), K.identf.v())
        P.copy(yT[:, 8:12, o], y_ps.v(), eng="act")

    qprep(0)
    deferred = None
    for it in range(NT):
        qn_, gs_, yc_ = qn[it % 2], gs[it % 2], yc[it % 2]
        for g in range(2):
            hs_ = slice(4 * g, 4 * g + 4)
            qTb_ = qTb[g]
            qps = bk.nxt()
            for hh in range(4):
                P.tr(qps[0:64, hh, :], qn_[:, 4 * g + hh, :], K.identf.v())
            P.copy(qTf.v(), qps[0:64, :, :], eng="act")
            P.copy(qTb_.v(), qps[0:64, :, :], eng="dve")
            bc_ = bct[st_["ib"] % 2]
            st_["ib"] += 1
            P.dma("sp", bc_.v(), C.biasC[it, g])
            sc = bk.nxt()
            P.mm(flat(sc), ckT[:, g, :], flat(qTf), start=True, stop=False)
            P.mm(flat(sc), cm[:, CM_IDENT, :], flat(bc_), start=False, stop=True)
            P.activation(ec.v(), sc.v(), AF.Exp)
            oc = bk.nxt()
            ocv = flat(oc)[:, 0:388].r("p (h c) -> p h c", c=97)
            for hh in range(4):
                P.mm(ocv[:, hh, :], ec[:, hh, :], cvx[:, g, :])
            if deferred is not None:
                ytrans(deferred)
                deferred = None
            P.ts(rc[:, 0, :], ocv[:, :, 64], 1e-30, ALU.max)
            P.recip(rc[:, 0, :], rc[:, 0, :])
            P.ts(imp.v(), ocv[:, 0, 65:97], rc[:, 0, 0:1], ALU.mult)
            for hh in range(1, 4):
                P.stt(imp.v(), ocv[:, hh, 65:97], rc[:, 0, hh:hh + 1], imp.v(), ALU.mult, ALU.add)
            P.tt(rc[:, 0, :], rc[:, 0, :], gs_[:, hs_, 0], ALU.mult)
            for hh in range(4):
                P.ts(yc_[:, 4 * g + hh, :], ocv[:, hh, 0:64], rc[:, 0, hh:hh + 1], ALU.mult)
            P.tt(imp.v(), imp.v(), K.selc[:, it, 0, :], ALU.mult)
            P.tt(imp.v(), imp.v(), K.selc[:, it, 1, :], ALU.add)
            ia, ma = imp.ap, mx8.ap
            P.op("dve", lambda e, ia=ia, ma=ma: e.max(out=ma, in_=ia), [imp], [mx8])
            P.ts(selm.v(), imp.v(), mx8[:, 3:4], ALU.is_ge, -1.0, ALU.add)
            osel = bkA.nxt()
            osv = flat(osel)[:, 0:260].r("p (h c) -> p h c", c=65)
            owin = bkA.nxt()
            owv = flat(owin)[:, 0:260].r("p (h c) -> p h c", c=65)
            j0 = max(0, it - 4)
            items = [("w", jt) for jt in range(j0, it + 1)] + [("s", jt) for jt in range(it + 1)]

            def scores(item):
                br, jt = item
                off = it - jt
                if br == "s" and jt == 0:
                    sps = bk.nxt()
                    P.tr(sps[0:32, 0, :], selm.v(), K.identf.v())
                    for hh in range(4):
                        P.copy(selT[:, hh, :], sps[0:32, 0, :], eng=("act", "dve")[hh % 2])
                    if g == 1 and it + 1 < NT:
                        qprep(it + 1)
                sp_ = bkS.nxt()
                if br == "s":
                    P.mm(flat(sp_), kTs[:, g, jt * 128:(jt + 1) * 128], flat(qTb_), start=True, stop=False)
                    P.mm(flat(sp_), K.rsel[:, jt, :], flat(selT), start=False, stop=True)
                else:
                    P.mm(flat(sp_), kTw[:, g, jt * 128:(jt + 1) * 128], flat(qTb_), start=True, stop=True)
                return sp_

            def finish(item, sp_):
                br, jt = item
                es = esb[st_["ie"] % 4]
                st_["ie"] += 1
                P.activation(es.v(), sp_.v(), AF.Exp)
                off = it - jt
                ti = min(off, 2) if br == "s" else (0, 1, 2, 2, 3)[off]
                P.tt(es.v(), es.v(), K.etbl[:, ti, hs_, :], ALU.mult)
                if br == "s":
                    for hh in range(4):
                        P.mm(osv[:, hh, :], es[:, hh, :], vs[:, jt, g, :], start=(jt == 0 and hh == 0), stop=(jt == it), skip=True)
                    if jt == it:
                        P.ts(rc[:, 1, :], osv[:, :, 64], 1e-30, ALU.max)
                        P.recip(rc[:, 1, :], rc[:, 1, :])
                        P.tt(rc[:, 1, :], rc[:, 1, :], gs_[:, hs_, 1], ALU.mult)
                        for hh in range(4):
                            P.stt(yc_[:, 4 * g + hh, :], osv[:, hh, 0:64], rc[:, 1, hh:hh + 1], yc_[:, 4 * g + hh, :], ALU.mult, ALU.add)
                else:
                    for hh in range(4):
                        P.mm(owv[:, hh, :], es[:, hh, :], vw[:, jt, g, :], start=(jt == j0 and hh == 0), stop=(jt == it), skip=True)
                    if jt == it:
                        P.ts(rc[:, 2, :], owv[:, :, 64], 1e-30, ALU.max)
                        P.recip(rc[:, 2, :], rc[:, 2, :])
                        P.tt(rc[:, 2, :], rc[:, 2, :], gs_[:, hs_, 2], ALU.mult)
                        for hh in range(4):
                            P.stt(yc_[:, 4 * g + hh, :], owv[:, hh, 0:64], rc[:, 2, hh:hh + 1], yc_[:, 4 * g + hh, :], ALU.mult, ALU.add)

            pending = []
            for item in items:
                pending.append((item, scores(item)))
                if len(pending) > LOOK:
                    finish(*pending.pop(0))
            while pending:
                finish(*pending.pop(0))
        deferred = it
    ytrans(deferred)
    P.release(m)


def phase_E(P, C, K, l, s, uT, yT, hin, hout):
    m0 = P.mark()
    mergedT = P.sb("mergedT", [128, 8, S], BF16)
    m1 = P.mark()
    wsg = WStream(P, 8, 256, nbuf=6)
    wsb = WStream(P, 4, 256, nbuf=6)
    pg = [P.ps("pg", [128, 512]) for _ in range(3)]
    pb = [P.ps("pb", [128, 512]) for _ in range(3)]
    gsb = [P.sb("gsb", [128, 512]) for _ in range(3)]
    acc = [P.sb("acc", [128, 512]) for _ in range(2)]
    it = 0
    k3 = 0

    def ld(q):
        wg, wbr = [], []
        for n in range(3):
            c0 = C_MG + n * 1024 + q * 256
            wg.append(wsg.load(C.w_in[l][:, c0:c0 + 256], 256))
            wbr.append(wsb.load(C.w_branch[l, n][:, q * 256:(q + 1) * 256], 256))
        return wg, wbr
    for q, (wg, wbr) in prefetch_iter(list(range(4)), ld):
        for j in range(2):
            fblk = q * 2 + j
            for tg in range(4):
                a = acc[it % 2]
                tsl = slice(tg * 512, (tg + 1) * 512)
                for n in range(3):
                    g_ps, b_ps, gs = pg[k3 % 3], pb[k3 % 3], gsb[k3 % 3]
                    k3 += 1
                    for kc in range(8):
                        P.mm(g_ps.v(), wg[n][:, kc, j * 128:(j + 1) * 128], uT[:, kc, tsl], start=(kc == 0), stop=(kc == 7))
                    for kc in range(4):
                        P.mm(b_ps.v(), wbr[n][:, kc, j * 128:(j + 1) * 128], yT[:, n * 4 + kc, tsl], start=(kc == 0), stop=(kc == 3))
                    P.activation(gs.v(), g_ps.v(), AF.Sigmoid)
                    if n == 0:
                        P.tt(a.v(), gs.v(), b_ps.v(), ALU.mult)
                    else:
                        P.tt(gs.v(), gs.v(), b_ps.v(), ALU.mult)
                        if n == 1:
                            P.tt(a.v(), a.v(), gs.v(), ALU.add)
                        else:
                            P.tt(mergedT[:, fblk, tsl], a.v(), gs.v(), ALU.add)
                it += 1
    P.release(m1)
    ws = WStream(P, 8, 512, nbuf=2)
    po = [P.ps("po", [128, 512]) for _ in range(3)]
    hsb = [P.sb("hsb", [128, 512]) for _ in range(4)]
    items = [(half, t) for half in range(2) for t in range(NT)]
    wts = [ws.load(C.w_out[l][:, half * 512:(half + 1) * 512], 512) for half in range(2)]

    def ldh(x):
        half, t = x
        hi = hsb[(half * NT + t) % 4]
        P.dma("sp", hi.v(), hin[t][:, half * 512:(half + 1) * 512])
        return hi
    it = 0
    for (half, t), hi in prefetch_iter(items, ldh):
        csl = slice(half * 512, (half + 1) * 512)
        w = wts[half]
        ps = po[it % 3]
        it += 1
        for kc in range(8):
            P.mm(ps.v(), mergedT[:, kc, t * 128:(t + 1) * 128], w[:, kc, :], start=(kc == 0), stop=(kc == 7))
        P.tt(hi.v(), hi.v(), ps.v(), ALU.add)
        P.dma("sp", hout[t][:, csl], hi.v())
    P.release(m0)


def phase_F(P, C, K, l, s, uT, hout):
    norm_transpose(P, K, C, l, 1, hout, uT)
    m = P.mark()
    actT = P.sb("actT", [128, 22, S], BF16)
    m2 = P.mark()
    wsg = WStream(P, 8, 256, nbuf=3)
    wsu = WStream(P, 8, 256, nbuf=3)
    pgt = [P.ps("pgt", [128, 512]) for _ in range(3)]
    pup = [P.ps("pup", [128, 512]) for _ in range(3)]
    sg = [P.sb("sg", [128, 512]) for _ in range(3)]
    it = 0

    def ldf(q):
        return (wsg.load(C.w_ffn_in[l][:, q * 256:(q + 1) * 256], 256),
                wsu.load(C.w_ffn_in[l][:, D_FF + q * 256:D_FF + (q + 1) * 256], 256))
    for q, (wg, wu) in prefetch_iter(list(range(11)), ldf):
        for j in range(2):
            fblk = q * 2 + j
            for tg in range(4):
                tsl = slice(tg * 512, (tg + 1) * 512)
                g_ps, u_ps, sgt = pgt[it % 3], pup[it % 3], sg[it % 3]
                it += 1
                for kc in range(8):
                    P.mm(g_ps.v(), wg[:, kc, j * 128:(j + 1) * 128], uT[:, kc, tsl], start=(kc == 0), stop=(kc == 7))
                for kc in range(8):
                    P.mm(u_ps.v(), wu[:, kc, j * 128:(j + 1) * 128], uT[:, kc, tsl], start=(kc == 0), stop=(kc == 7))
                P.activation(sgt.v(), g_ps.v(), AF.Silu)
                P.tt(actT[:, fblk, tsl], sgt.v(), u_ps.v(), ALU.mult)
    P.release(m2)
    ws = WStream(P, 22, 256, nbuf=2)
    po = [P.ps("po2", [128, 512]) for _ in range(3)]
    hsb = [P.sb("hsb2", [128, 256]) for _ in range(4)]
    items = [(qd, t) for qd in range(4) for t in range(NT)]
    wts = {}

    def ldh2(x):
        qd, t = x
        if t == 0:
            wts[qd] = ws.load(C.w_ffn_out[l][:, qd * 256:(qd + 1) * 256], 256)
        hi = hsb[(qd * NT + t) % 4]
        P.dma("sp", hi.v(), hout[t][:, qd * 256:(qd + 1) * 256])
        return hi
    it = 0
    for (qd, t), hi in prefetch_iter(items, ldh2):
        csl = slice(qd * 256, (qd + 1) * 256)
        w = wts[qd]
        ps = po[it % 3]
        it += 1
        for kc in range(22):
            P.mm(ps[:, 0:256], actT[:, kc, t * 128:(t + 1) * 128], w[:, kc, :], start=(kc == 0), stop=(kc == 21))
        P.tt(hi.v(), hi.v(), ps[:, 0:256], ALU.add)
        P.dma("sp", hout[t][:, csl], hi.v())
    P.release(m)
    norm_transpose(P, K, C, l, 2, hout, uT)
    m = P.mark()
    pTb = P.sb("pTb", [128, 2, S], BF16)
    P.dma("pool", pTb.v(), C.pT[l, s].r("(k p) t -> p k t", p=128))
    wsg = WStream(P, 8, 512, nbuf=2)
    wsp = WStream(P, 2, 512, nbuf=2)
    pg = [P.ps("pg3", [128, 512]) for _ in range(2)]
    pp = [P.ps("pp3", [128, 512]) for _ in range(2)]
    gsb = [P.sb("gsb3", [128, 512]) for _ in range(3)]
    hsb = [P.sb("hsb3", [128, 512]) for _ in range(4)]
    wgs = [wsg.load(C.w_ple_gate[l][:, half * 512:(half + 1) * 512], 512) for half in range(2)]
    wps = [wsp.load(C.w_ple_proj[l][:, half * 512:(half + 1) * 512], 512) for half in range(2)]
    items = [(half, t) for half in range(2) for t in range(NT)]

    def ldh3(x):
        half, t = x
        hi = hsb[(half * NT + t) % 4]
        P.dma("sp", hi.v(), hout[t][:, half * 512:(half + 1) * 512])
        return hi
    it = 0
    for (half, t), hi in prefetch_iter(items, ldh3):
        csl = slice(half * 512, (half + 1) * 512)
        wg, wp = wgs[half], wps[half]
        g_ps, p_ps, gs = pg[it % 2], pp[it % 2], gsb[it % 3]
        it += 1
        for kc in range(8):
            P.mm(g_ps.v(), uT[:, kc, t * 128:(t + 1) * 128], wg[:, kc, :], start=(kc == 0), stop=(kc == 7))
        for kc in range(2):
            P.mm(p_ps.v(), pTb[:, kc, t * 128:(t + 1) * 128], wp[:, kc, :], start=(kc == 0), stop=(kc == 1))
        P.activation(gs.v(), g_ps.v(), AF.Sigmoid)
        P.tt(gs.v(), gs.v(), p_ps.v(), ALU.mult)
        P.tt(hi.v(), hi.v(), gs.v(), ALU.add)
        P.dma("sp", hout[t][:, csl], hi.v())
    P.release(m)


def build(n_layers=DEPTH, n_seq=NSEQ, phases="AE", dbg=(), dbg_yT=False):
    nc = bass.Bass("TRN2", target_bir_lowering=False)
    P = Prog(nc)
    C = declare(P, dbg)
    K = setup_consts(P, C)
    K.epsD = P.sb("epsD", [128, 1])
    P.memset(K.epsD.v(), EPS)
    K.eps6 = P.sb("eps6", [128, 1])
    P.memset(K.eps6.v(), 1e-6)
    if dbg_yT:
        ydbg = P.track(Buf(nc.dram_tensor("yT_dbg", [128, 12, S], F32, kind="ExternalInput").ap(), "yT_dbg"))
    for s in range(n_seq):
        for l in range(n_layers):
            hin = C.xt[s] if l == 0 else C.ot[s]
            my = P.mark()
            yT = P.sb("yT", [128, 12, S], BF16)
            mu = P.mark()
            uT = P.sb("uT", [128, 8, S], BF16)
            if "A" in phases:
                phase_A(P, C, K, l, s, hin, uT)
            P.release(mu)
            if "B" in phases:
                phase_B(P, C, K, l, s, yT)
            if "C" in phases:
                phase_C(P, C, K, l, s, yT)
            if "D" in phases:
                phase_D(P, C, K, l, s, yT)
            if "y" in dbg:
                mm_ = P.mark()
                yf = P.sb("yf", [128, 12, S])
                P.copy(yf.v(), yT.v(), eng="pool")
                P.dma("sp", C.ydbg.v(), yf.v())
                P.release(mm_)
            uT = P.sb("uT", [128, 8, S], BF16)
            norm_transpose(P, K, C, l, 0, hin, uT)
            if dbg_yT:
                m = P.mark()
                yf = P.sb("yf", [128, 12, S])
                P.dma("sp", yf.v(), ydbg.v())
                P.copy(yT.v(), yf.v(), eng="pool")
                P.release(m)
            if "E" in phases:
                phase_E(P, C, K, l, s, uT, yT, hin, C.ot[s])
            P.release(my)
            if "E" in phases:
                uT = P.sb("uT", [128, 8, S], BF16)
                phase_F(P, C, K, l, s, uT, C.ot[s])
                P.release(my)
    st = P.emit()
    P.close()
    return nc, st


import math
NEGB = -30000.0


def rel_bucket_np(dist):
    d = np.maximum(dist, 0)
    df = np.maximum(d, 1).astype(np.float32)
    large = 16 + (np.log(df / np.float32(16)) / np.float32(math.log(128 / 16)) * np.float32(16)).astype(np.int32)
    large = np.minimum(large, 31)
    return np.where(d < 16, d, large).astype(np.int64)


def nsa_consts(inp):
    rb = inp["rel_bias"].astype(np.float32)
    d = {}
    j = np.arange(128)[:, None]
    i = np.arange(128)[None, :]
    neg = np.float32(NEGB)

    def gat(dist, valid):
        g = rb[rel_bucket_np(dist)]
        g = np.where(valid[..., None], g, neg)
        return np.ascontiguousarray(g.transpose(0, 2, 1))
    tb = np.zeros((4, 128, 8, 128), np.float32)
    tb[0] = gat(i - j, (i - j) >= 0)
    tb[1] = gat(128 + i - j, np.ones((128, 128), bool))
    tb[2] = gat(np.full((128, 128), 1000), np.ones((128, 128), bool))
    tb[3] = gat(512 + i - j, i < j)
    d["tbls"] = tb
    n = np.arange(128)[:, None]
    bc = np.zeros((NT, 2, 128, 4, 128), np.float32)
    for it in range(NT):
        tq = it * 128 + np.arange(128)[None, :]
        dist = tq - (16 * n + 31)
        valid = (dist >= 0) & (n < 127)
        g = np.where(valid[..., None], rb[rel_bucket_np(dist)], neg)
        g = g.transpose(0, 2, 1)
        bc[it, 0] = g[:, 0:4]
        bc[it, 1] = g[:, 4:8]
    d["biasC"] = bc
    sc = np.zeros((128, NT, 2, 32), np.float32)
    jj = np.arange(32)[None, :]
    for it in range(NT):
        cur = (it * 128 + np.arange(128)[:, None]) // 64
        forced = (jj == 0) | (jj == cur)
        causal = jj <= cur
        sc[:, it, 0] = (causal & ~forced)
        sc[:, it, 1] = np.where(forced, 1e4, np.where(causal, 0.0, -1.0))
    d["selc"] = sc
    cs = np.arange(127) * 16
    ce = cs + 31
    ss = np.arange(32) * 64
    se = ss + 63
    ov = np.zeros((128, 32), np.float32)
    ov[:127] = ((cs[:, None] <= se[None]) & (ce[:, None] >= ss[None]))
    d["ovm"] = ov
    rs = np.zeros((32, NT, 128), np.float32)
    for jt in range(NT):
        for jr in range(128):
            rs[2 * jt + jr // 64, jt, jr] = 30000.0
    d["rselc"] = rs
    d["wcmp"] = np.ascontiguousarray(inp["nsa_w_cmp"].transpose(0, 3, 1, 2, 4))
    d["peT"] = np.ascontiguousarray(inp["nsa_cmp_pe"].transpose(0, 3, 1, 2))
    d["kn0"] = np.ascontiguousarray(inp["nsa_k_norm"][:, 0, :, None])
    return d


def host_consts(inp):
    d = {}
    j = np.arange(128)[:, None]
    i = np.arange(128)[None, :]
    cm = np.zeros((128, NCM, 128), np.float32)
    cm[:, CM_TRI] = (j <= i)
    cm[:, CM_NEGM] = np.where(i >= j, 0.0, -30000.0)
    cm[:, CM_STRICT] = (i > j)
    cm[:, CM_SEL127] = (j == 127) * np.ones((1, 128))
    cm[:, CM_IDENT] = (i == j)
    cm[:, CM_ONES] = 1.0
    d["cmat"] = cm
    sm = np.zeros((DEPTH, NSMALL), np.float32)
    sm[:, SM_ALOG:SM_ALOG + 4] = inp["gdn_a_log"]
    sm[:, SM_DTB:SM_DTB + 4] = inp["gdn_dt_bias"]
    sm[:, SM_BI:SM_BI + 4] = inp["mlstm_b_i"]
    sm[:, SM_BF:SM_BF + 4] = inp["mlstm_b_f"]
    sm[:, SM_GDNN:SM_GDNN + 128] = inp["gdn_norm"]
    sm[:, SM_MLN:SM_MLN + 128] = inp["mlstm_norm"]
    sm[:, SM_QN:SM_QN + 64] = inp["nsa_q_norm"]
    sm[:, SM_KN:SM_KN + 192] = inp["nsa_k_norm"].reshape(DEPTH, 192)
    d["small"] = np.ascontiguousarray(np.broadcast_to(sm[:, None, :], (DEPTH, 128, NSMALL)))
    d.update(nsa_consts(inp))
    d["cw"] = np.ascontiguousarray(inp["conv_w"].reshape(DEPTH, 4, 12, 128).transpose(0, 3, 2, 1))
    return d


def host_inputs(inp, core=0):
    b0 = core * 2
    d = {}
    d["x"] = np.ascontiguousarray(inp["x"][b0:b0 + 2])
    d["pT"] = np.ascontiguousarray(np.transpose(inp["p"][:, b0:b0 + 2], (0, 1, 3, 2)))
    for k in ["w_in", "w_branch", "w_out", "w_ffn_in", "w_ffn_out", "w_ple_gate", "w_ple_proj"]:
        d[k] = inp[k]
    g = np.stack([inp["norm_mix"], inp["norm_ffn"], inp["norm_ple"]], axis=1)
    d["gains_b"] = np.ascontiguousarray(np.broadcast_to(g[:, :, None, :], (DEPTH, 3, 128, D)), dtype=np.float32)
    d["ident"] = np.eye(128, dtype=np.float32)
    d.update(host_consts(inp))
    return d


_NC_CACHE = {}


def kernel(**inputs):
    inp = {k_: np.asarray(v) for k_, v in inputs.items()}
    if "full" not in _NC_CACHE:
        _NC_CACHE["full"] = build(n_layers=DEPTH, n_seq=NSEQ, phases="ABCDE")[0]
    nc = _NC_CACHE["full"]
    consts = host_consts(inp)
    shared = {}
    for k_ in ["w_in", "w_branch", "w_out", "w_ffn_in", "w_ffn_out", "w_ple_gate", "w_ple_proj"]:
        shared[k_] = np.ascontiguousarray(inp[k_], dtype=np.float32)
    g = np.stack([inp["norm_mix"], inp["norm_ffn"], inp["norm_ple"]], axis=1)
    shared["gains_b"] = np.ascontiguousarray(np.broadcast_to(g[:, :, None, :], (DEPTH, 3, 128, D)), dtype=np.float32)
    shared["ident"] = np.eye(128, dtype=np.float32)
    shared.update(consts)
    in_maps = []
    for c in range(8):
        d = dict(shared)
        d["x"] = np.ascontiguousarray(inp["x"][2 * c:2 * c + 2], dtype=np.float32)
        d["pT"] = np.ascontiguousarray(np.transpose(inp["p"][:, 2 * c:2 * c + 2], (0, 1, 3, 2)), dtype=np.float32)
        in_maps.append(d)
    res = run_bass_kernel_spmd(nc, in_maps, core_ids=list(range(8)))
    out = np.concatenate([np.asarray(r["out"]) for r in res.results], axis=0)
    return out.astype(np.float32)
```

```python
import numpy as np
import concourse.bass as bass
import concourse.mybir as mybir
from concourse.bass_utils import run_bass_kernel_spmd

F32 = mybir.dt.float32
BF16 = mybir.dt.bfloat16
I32 = mybir.dt.int32
AF = mybir.ActivationFunctionType
ALU = mybir.AluOpType
AX = mybir.AxisListType

N_DMA_SEMS = 48


class V:
    __slots__ = ("buf", "ap")

    def __init__(self, buf, ap):
        self.buf = buf
        self.ap = ap

    def __getitem__(self, idx):
        return V(self.buf, self.ap[idx])

    def r(self, pat, **kw):
        return V(self.buf, self.ap.rearrange(pat, **kw))

    def bc(self, shape):
        return V(self.buf, self.ap.to_broadcast(list(shape)))


class Buf:
    __slots__ = ("ap", "name", "lw", "rd", "excl")

    def __init__(self, ap, name=""):
        self.ap = ap
        self.name = name
        self.lw = None
        self.rd = []
        self.excl = False

    def __getitem__(self, idx):
        return V(self, self.ap[idx])

    def v(self):
        return V(self, self.ap)

    def r(self, pat, **kw):
        return V(self, self.ap.rearrange(pat, **kw))

    def sub(self, idx, name=""):
        return Buf(self.ap[idx], name or self.name)


def _ap(x):
    return x.ap if isinstance(x, V) else x


class Prog:
    ENGS = ("pe", "dve", "act", "pool", "sp")

    def __init__(self, nc):
        self.nc = nc
        self.ops = []
        self.eng = {"pe": nc.tensor, "dve": nc.vector, "act": nc.scalar, "pool": nc.gpsimd, "sp": nc.sync}
        self._ctx = []
        self.all_bufs = []
        self.last_barrier = None
        self._uid = 0

    def _nm(self, name):
        self._uid += 1
        return "%s_%d" % (name, self._uid)

    def sb(self, name, shape, dt=F32):
        g = self.nc.sbuf_tensor(self._nm(name), list(shape), dt)
        t = g.__enter__()
        self._ctx.append(g)
        b = Buf(t.ap() if hasattr(t, "ap") else t[:], name)
        self.all_bufs.append(b)
        return b

    def ps(self, name, shape, dt=F32):
        g = self.nc.psum_tensor(self._nm(name), list(shape), dt)
        t = g.__enter__()
        self._ctx.append(g)
        b = Buf(t.ap() if hasattr(t, "ap") else t[:], name)
        b.excl = True
        self.all_bufs.append(b)
        return b

    def dram(self, name, shape, dt=F32, kind="Internal"):
        t = self.nc.dram_tensor(name, list(shape), dt, kind=kind).ap()
        b = Buf(t, name)
        self.all_bufs.append(b)
        return b

    def track(self, b):
        self.all_bufs.append(b)
        return b

    def mark(self):
        return len(self._ctx)

    def release(self, mark):
        self.barrier()
        while len(self._ctx) > mark:
            g = self._ctx.pop()
            g.__exit__(None, None, None)

    def op(self, eng, fn, reads=(), writes=()):
        self.ops.append((eng, fn, tuple(reads), tuple(writes), False, self.last_barrier))

    def barrier(self):
        idx = len(self.ops)
        self.ops.append(("sp", lambda e: e.nop(), (), (), "bar", self.last_barrier))
        self.last_barrier = idx

    def dma(self, eng, out, in_, **kw):
        oa, ia = _ap(out), _ap(in_)

        def fn(e, oa=oa, ia=ia, kw=kw):
            return e.dma_start(out=oa, in_=ia, **kw)
        self.ops.append((eng, fn, (in_.buf,), (out.buf,), True, self.last_barrier))

    @staticmethod
    def _rw(outs, ins):
        w = [o.buf for o in outs if isinstance(o, V)]
        r = [i.buf for i in ins if isinstance(i, V)]
        w += [b for b in r if b.excl and b not in w]
        return r, w

    def activation(self, out, in_, func, bias=0.0, scale=1.0, accum_out=None, eng="act"):
        r, w = self._rw([out, accum_out], [in_, bias, scale])
        kw = dict(out=_ap(out), in_=_ap(in_), func=func, bias=_ap(bias), scale=_ap(scale))
        if accum_out is not None:
            kw["accum_out"] = _ap(accum_out)
        self.op(eng, lambda e, kw=kw: e.activation(**kw), r, w)

    def tt(self, out, in0, in1, op, eng="dve"):
        r, w = self._rw([out], [in0, in1])
        kw = dict(out=_ap(out), in0=_ap(in0), in1=_ap(in1), op=op)
        self.op(eng, lambda e, kw=kw: e.tensor_tensor(**kw), r, w)

    def ts(self, out, in0, s1, op0, s2=None, op1=None, accum_out=None, eng="dve"):
        r, w = self._rw([out, accum_out], [in0, s1, s2])
        kw = dict(out=_ap(out), in0=_ap(in0), scalar1=_ap(s1), scalar2=_ap(s2), op0=op0)
        if op1 is not None:
            kw["op1"] = op1
        if accum_out is not None:
            kw["accum_out"] = _ap(accum_out)
        self.op(eng, lambda e, kw=kw: e.tensor_scalar(**kw), r, w)

    def stt(self, out, in0, scalar, in1, op0, op1, eng="dve"):
        r, w = self._rw([out], [in0, scalar, in1])
        kw = dict(out=_ap(out), in0=_ap(in0), scalar=_ap(scalar), in1=_ap(in1), op0=op0, op1=op1)
        self.op("dve", lambda e, kw=kw: e.scalar_tensor_tensor(**kw), r, w)

    def copy(self, out, in_, eng="dve"):
        r, w = self._rw([out], [in_])
        oa, ia = _ap(out), _ap(in_)
        if eng == "act":
            self.op(eng, lambda e: e.copy(out=oa, in_=ia), r, w)
        else:
            self.op(eng, lambda e: e.tensor_copy(out=oa, in_=ia), r, w)

    def memset(self, out, val, eng="pool"):
        r, w = self._rw([out], [])
        oa = _ap(out)
        self.op(eng, lambda e: e.memset(oa, val), r, w)

    def reduce(self, out, in_, op, axis=AX.X, eng="dve"):
        r, w = self._rw([out], [in_])
        oa, ia = _ap(out), _ap(in_)
        self.op(eng, lambda e: e.tensor_reduce(out=oa, in_=ia, axis=axis, op=op), r, w)

    def recip(self, out, in_):
        r, w = self._rw([out], [in_])
        oa, ia = _ap(out), _ap(in_)
        self.op("dve", lambda e: e.reciprocal(out=oa, in_=ia), r, w)

    def mm(self, out, lhsT, rhs, start=True, stop=True, skip=False):
        r, w = self._rw([out], [lhsT, rhs])
        oa, la, ra = _ap(out), _ap(lhsT), _ap(rhs)
        if skip:
            self.op("pe", lambda e: e.matmul(oa, la, ra, start=start, stop=stop, skip_group_check=True), r, w)
        else:
            self.op("pe", lambda e: e.matmul(oa, la, ra, start=start, stop=stop), r, w)

    def tr(self, out, in_, ident):
        r, w = self._rw([out], [in_, ident])
        oa, ia, da = _ap(out), _ap(in_), _ap(ident)
        self.op("pe", lambda e: e.transpose(oa, ia, da), r, w)

    def emit(self):
        nc = self.nc
        ops = self.ops
        n = len(ops)
        deps = [None] * n
        signal = [False] * n
        last_on = {}
        dmas_since = []
        for i, (eng, fn, reads, writes, is_dma, bar) in enumerate(ops):
            d = set()
            if bar is not None:
                d.add(bar)
            if is_dma == "bar":
                d.update(last_on.values())
                d.update(dmas_since)
                dmas_since = []
                is_dma = False
            elif is_dma:
                dmas_since.append(i)
            last_on[eng] = i
            for r in reads:
                if r.lw is not None:
                    d.add(r.lw)
            for w in writes:
                if w.lw is not None:
                    j = w.lw
                    if is_dma or ops[j][4] or ops[j][0] != eng:
                        d.add(j)
                for j in w.rd:
                    if is_dma or ops[j][4] or ops[j][0] != eng:
                        d.add(j)
            d.discard(i)
            if eng == "pe":
                d = {j for j in d if ops[j][0] != "pe" or ops[j][4]}
            for r in reads:
                r.rd.append(i)
            for w in writes:
                w.lw = i
                w.rd = []
            best = {}
            dd = set()
            for j in d:
                if ops[j][4] is True:
                    dd.add(j)
                else:
                    e2 = ops[j][0]
                    if e2 not in best or best[e2] < j:
                        best[e2] = j
            dd.update(best.values())
            deps[i] = dd
            for j in dd:
                signal[j] = True
        sems = {}
        for e in self.ENGS:
            g = nc.semaphore("s_" + e)
            sems[e] = g.__enter__()
            self._ctx.append(g)
        dsems = []
        for k in range(N_DMA_SEMS):
            g = nc.semaphore("d_%d" % k)
            dsems.append(g.__enter__())
            self._ctx.append(g)
        cnt = {e: 0 for e in self.ENGS}
        dcnt = [0] * N_DMA_SEMS
        ev = [None] * n
        dnext = 0
        dma_prev = [None] * N_DMA_SEMS
        dma_guard = [None] * n
        for i, (eng, fn, reads, writes, is_dma, bar) in enumerate(ops):
            if is_dma is True:
                k = dnext
                dnext = (dnext + 1) % N_DMA_SEMS
                dma_guard[i] = dma_prev[k]
                dcnt[k] += 16
                ev[i] = ("d", k, dcnt[k])
                dma_prev[k] = ev[i]
            elif signal[i]:
                cnt[eng] += 1
                ev[i] = ("c", eng, cnt[eng])
        waited = {e: {} for e in self.ENGS}
        nwaits = 0
        last_dma = {}
        for i, (eng, fn, reads, writes, is_dma, bar) in enumerate(ops):
            need = {}
            for j in deps[i]:
                kind, key, val = ev[j]
                if need.get((kind, key), 0) < val:
                    need[(kind, key)] = val
            if is_dma is True and dma_guard[i] is not None:
                kind, key, val = dma_guard[i]
                if need.get((kind, key), 0) < val:
                    need[(kind, key)] = val
            w = waited[eng]
            for (kind, key), val in need.items():
                if w.get((kind, key), 0) >= val:
                    continue
                w[(kind, key)] = val
                s = sems[key] if kind == "c" else dsems[key]
                self.eng[eng].wait_ge(s, val)
                nwaits += 1
            ins = fn(self.eng[eng])
            if ev[i] is not None:
                kind, key, val = ev[i]
                if kind == "c":
                    ins.then_inc(sems[key], 1)
                else:
                    ins.then_inc(dsems[key], 16)
                    last_dma[key] = val
        w = waited["sp"]
        for k, val in last_dma.items():
            if w.get(("d", k), 0) < val:
                self.eng["sp"].wait_ge(dsems[k], val)
        self.stats = dict(n_ops=n, n_waits=nwaits, cnt=dict(cnt))
        return self.stats

    def close(self):
        while self._ctx:
            g = self._ctx.pop()
            g.__exit__(None, None, None)


S = 2048
D = 1024
NT = S // 128
DEPTH = 2
NSEQ = 2
D_IN = 8488
D_FF = 2816
EPS = 1e-6
C_AQ, C_AK, C_AV, C_AZ, C_AA, C_AB = 0, 512, 1024, 1536, 2048, 2052
C_BQ, C_BK, C_BV, C_BO, C_BI, C_BF = 2056, 2568, 3080, 3592, 4104, 4108
C_CQ, C_CKC, C_CVC, C_CKS, C_CVS, C_CKW, C_CVW, C_CG, C_MG = 4112, 4624, 4752, 4880, 5008, 5136, 5264, 5392, 5416


TM_RANGES = [(C_AZ, 520), (C_BK, 512), (C_BV, 1032), (C_CQ, 1304)]
TM_OFF = {}
_o = 0
for _c, _w in TM_RANGES:
    TM_OFF[_c] = _o
    _o += _w
TM_W = _o


def tm_col(c):
    for c0, w in TM_RANGES:
        if c0 <= c < c0 + w:
            return TM_OFF[c0] + (c - c0)
    raise KeyError(c)


FM_RANGES = [(C_AQ, 1536), (C_BQ, 1024)]
FM_OFF = {C_AQ: 0, C_BQ: 1536}


class Ctx:
    pass


CM_TRI, CM_NEGM, CM_STRICT, CM_SEL127, CM_IDENT, CM_ONES = range(6)
NCM = 6
SM_ALOG, SM_DTB, SM_BI, SM_BF, SM_GDNN, SM_MLN, SM_QN, SM_KN = 0, 4, 8, 12, 16, 144, 272, 336
NSMALL = 336 + 192


def declare(P, dbg=()):
    nc = P.nc
    C = Ctx()

    def inp(name, shape, dt=F32):
        return P.track(Buf(nc.dram_tensor(name, list(shape), dt, kind="ExternalInput").ap(), name))

    C.x = inp("x", [NSEQ, S, D])
    C.pT = inp("pT", [DEPTH, NSEQ, 256, S])
    C.w_in = inp("w_in", [DEPTH, D, D_IN])
    C.w_branch = inp("w_branch", [DEPTH, 3, 512, D])
    C.w_out = inp("w_out", [DEPTH, D, D])
    C.w_ffn_in = inp("w_ffn_in", [DEPTH, D, 2 * D_FF])
    C.w_ffn_out = inp("w_ffn_out", [DEPTH, D_FF, D])
    C.w_ple_gate = inp("w_ple_gate", [DEPTH, D, D])
    C.w_ple_proj = inp("w_ple_proj", [DEPTH, 256, D])
    C.gains_b = inp("gains_b", [DEPTH, 3, 128, D])
    C.ident = inp("ident", [128, 128])
    C.cmat = inp("cmat", [128, NCM, 128])
    C.small = inp("small", [DEPTH, 128, NSMALL])
    C.cw = inp("cw", [DEPTH, 128, 12, 4])
    C.tbls = inp("tbls", [4, 128, 8, 128])
    C.biasC = inp("biasC", [NT, 2, 128, 4, 128])
    C.selc = inp("selc", [128, NT, 2, 32])
    C.ovm = inp("ovm", [128, 32])
    C.rselc = inp("rselc", [32, NT, 128])
    C.wcmp = inp("wcmp", [DEPTH, 64, 2, 32, 64])
    C.peT = inp("peT", [DEPTH, 64, 2, 32])
    C.kn0 = inp("kn0", [DEPTH, 64, 1])
    C.out = P.track(Buf(nc.dram_tensor("out", [NSEQ, S, D], F32, kind="ExternalOutput").ap(), "out"))

    C.xt = [[C.x.sub((q, slice(t * 128, (t + 1) * 128), slice(None)), "x%d_%d" % (q, t)) for t in range(NT)] for q in range(NSEQ)]
    C.ot = [[C.out.sub((q, slice(t * 128, (t + 1) * 128), slice(None)), "o%d_%d" % (q, t)) for t in range(NT)] for q in range(NSEQ)]

    def scr(name, shape, dt=F32):
        kind = "ExternalOutput" if name in dbg else "Internal"
        return P.track(Buf(nc.dram_tensor(name, list(shape), dt, kind=kind).ap(), name))

    C.tm = scr("tm_scr", [S, TM_W])
    C.fm = scr("fm_scr", [2560, S])
    if "y" in dbg:
        C.ydbg = P.track(Buf(nc.dram_tensor("ydbg", [128, 12, S], F32, kind="ExternalOutput").ap(), "ydbg"))
    return C


def setup_consts(P, C):
    K = Ctx()
    K.identf = P.sb("identf", [128, 128])
    K.identb = P.sb("identb", [128, 128], BF16)
    P.dma("sp", K.identf.v(), C.ident.v())
    P.copy(K.identb.v(), K.identf.v())
    K.cm = P.sb("cmat", [128, NCM, 128])
    P.dma("sp", K.cm.v(), C.cmat.v())
    K.cm4 = P.sb("cmat4", [128, 3, 4, 128])
    for i, cmi in enumerate((CM_NEGM, CM_STRICT, CM_IDENT)):
        for h in range(4):
            P.copy(K.cm4[:, i, h, :], K.cm[:, cmi, :], eng="pool")
    K.small = P.sb("small", [128, DEPTH, NSMALL])
    P.dma("sp", K.small.v(), C.small.r("l p n -> p l n"))
    K.cw = P.sb("cw", [128, DEPTH, 12, 4])
    P.dma("sp", K.cw.v(), C.cw.r("l p b k -> p l b k"))
    K.tbl = P.sb("tbl", [128, 4, 8, 128], BF16)
    K.rsel = P.sb("rsel", [128, NT, 128], BF16)
    P.memset(K.rsel.v(), 0.0)
    K.selc = P.sb("selc", [128, NT, 2, 32])
    mt = P.mark()
    tblf = P.sb("tblf", [128, 4, 8, 128])
    P.dma("sp", tblf.v(), C.tbls.r("a j h i -> j a h i"))
    P.copy(K.tbl.v(), tblf.v(), eng="pool")
    rself = P.sb("rself", [32, NT, 128])
    P.dma("sp", rself.v(), C.rselc.v())
    P.copy(K.rsel[0:32, :, :], rself.v(), eng="pool")
    P.dma("sp", K.selc.v(), C.selc.v())
    P.release(mt)
    return K


def norm_transpose(P, K, C, l, gi, hsrc, uT, wq="sp"):
    m = P.mark()
    NPF = 4
    hA = [P.sb("hA", [128, D]) for _ in range(NPF)]
    junk = P.sb("junkA", [128, D], BF16)
    ub = [P.sb("ub", [128, D], BF16) for _ in range(2)]
    ss = P.sb("ssA", [128, NT])
    rs = P.sb("rsA", [128, NT])
    pT = [P.ps("pTA", [128, 8, 128], BF16) for _ in range(2)]
    Gt = P.sb("Gt", [128, D])
    P.dma(wq, Gt.v(), C.gains_b[l, gi])
    for t in range(NPF - 1):
        P.dma(wq, hA[t].v(), hsrc[t].v())
    def stage1(t):
        b = t % 2
        if t + NPF - 1 < NT:
            P.dma(wq, hA[(t + NPF - 1) % NPF].v(), hsrc[t + NPF - 1].v())
        sst = ss.sub((slice(None), slice(t, t + 1)))
        rst = rs.sub((slice(None), slice(t, t + 1)))
        P.activation(junk.v(), hA[t % NPF].v(), AF.Square, accum_out=sst.v())
        P.activation(rst.v(), sst.v(), AF.Sqrt, bias=K.epsD.v(), scale=1.0 / D)
        P.recip(rst.v(), rst.v())
        P.stt(ub[b].v(), hA[t % NPF].v(), rst.v(), Gt.v(), ALU.mult, ALU.mult)

    def stage2(t):
        b = t % 2
        for kc in range(8):
            P.tr(pT[b][:, kc, :], ub[b][:, kc * 128:(kc + 1) * 128], K.identb.v())
        if t % 2 == 0:
            P.copy(uT[:, :, t * 128:(t + 1) * 128], pT[b].v(), eng="act")
        else:
            P.copy(uT[:, :, t * 128:(t + 1) * 128], pT[b].v(), eng="dve")

    stage1(0)
    for t in range(NT):
        if t + 1 < NT:
            stage1(t + 1)
        stage2(t)
    P.release(m)


class WStream:
    def __init__(self, P, nk, width, nstage=0, nbuf=3):
        self.P = P
        self.nk = nk
        self.width = width
        self.wb = [P.sb("wb", [128, nk, width], BF16) for _ in range(nbuf)]
        self.j = 0

    def load(self, wsrc_v, ncols, gain=None, nk=None):
        P = self.P
        nk = nk or self.nk
        wb = self.wb[self.j % len(self.wb)]
        self.j += 1
        P.dma("pool", wb[:, :nk, :ncols], wsrc_v.r("(k p) c -> p k c", p=128))
        return wb[:, :nk, :ncols]


def prefetch_iter(items, loader):
    nxt = loader(items[0]) if items else None
    for i, it_ in enumerate(items):
        cur = nxt
        if i + 1 < len(items):
            nxt = loader(items[i + 1])
        yield it_, cur


def phase_A(P, C, K, l, s, hsrc, uT):
    norm_transpose(P, K, C, l, 0, hsrc, uT)
    m = P.mark()
    ws = WStream(P, 8, 512, nbuf=3)
    w_in = C.w_in[l]
    blocks = []
    for c0, w in FM_RANGES:
        for cb in range(0, w, 512):
            blocks.append(("fm", c0, cb, 512))
    for c0, w in TM_RANGES:
        for cb in range(0, w, 512):
            blocks.append(("tm", c0, cb, min(512, w - cb)))
    pfm = [P.ps("pfm", [128, 512]) for _ in range(3)]
    sfm = [P.sb("sfm", [128, S]) for _ in range(2)]
    stm = [P.sb("stm", [128, 512]) for _ in range(4)]
    blk = 0
    it = 0
    ip = 0
    for (kind, c0, cb, nc_), wb in prefetch_iter(blocks, lambda b: ws.load(w_in[:, b[1] + b[2]:b[1] + b[2] + b[3]], b[3])):
        if kind == "fm":
            for j in range(4):
                st = sfm[blk % 2]
                for tg in range(4):
                    ps = pfm[ip % 3]
                    ip += 1
                    for kc in range(8):
                        P.mm(ps.v(), wb[:, kc, j * 128:(j + 1) * 128], uT[:, kc, tg * 512:(tg + 1) * 512],
                             start=(kc == 0), stop=(kc == 7))
                    P.copy(st[:, tg * 512:(tg + 1) * 512], ps.v(), eng=("act", "dve")[tg % 2])
                r0 = FM_OFF[c0] + cb + j * 128
                P.dma("sp", C.fm[r0:r0 + 128, :], st.v())
                blk += 1
        else:
            for t in range(NT):
                ps = pfm[ip % 3]
                ip += 1
                st = stm[it % 4]
                for kc in range(8):
                    P.mm(ps[:, :nc_], uT[:, kc, t * 128:(t + 1) * 128], wb[:, kc, :], start=(kc == 0), stop=(kc == 7))
                P.copy(st[:, :nc_], ps[:, :nc_], eng=("act", "dve")[it % 2])
                o = TM_OFF[c0] + cb
                P.dma("sp", C.tm[t * 128:(t + 1) * 128, o:o + nc_], st[:, :nc_])
                it += 1
    P.release(m)


STOPB = 0


class Banks:
    def __init__(self, P, n=8):
        self.t = [P.ps("bank", [128, 4, 128]) for _ in range(n)]
        self.i = 0

    def nxt(self):
        b = self.t[self.i % len(self.t)]
        self.i += 1
        return b


class RR:
    def __init__(self, engs):
        self.engs = engs
        self.i = 0

    def __call__(self):
        e = self.engs[self.i % len(self.engs)]
        self.i += 1
        return e


def scale_cols(P, out, in_, sc, eng):
    if eng == "act":
        P.activation(out, in_, AF.Copy, scale=sc)
    else:
        P.ts(out, in_, sc, ALU.mult, eng=eng)


def out_stage_bufs(P):
    OS = Ctx()
    OS.osq = P.sb("osq", [128, 4, 128])
    OS.oss = P.sb("oss", [128, 4])
    OS.zt = P.sb("zt", [128, 4, 128])
    return OS


def out_stage(P, C, K, bk, OS, osb, gcol, gfunc, gainB, yT, yoff, cg):
    zt, osq, oss = OS.zt, OS.osq, OS.oss
    P.dma("sp", zt.r("p h d -> p (h d)"), C.tm[cg * 128:(cg + 1) * 128, gcol:gcol + 512])
    P.activation(zt.v(), zt.v(), gfunc)
    P.activation(osq.v(), osb.v(), AF.Square)
    P.reduce(oss.v(), osq.v(), ALU.add)
    P.activation(oss.v(), oss.v(), AF.Sqrt, bias=K.epsD.v(), scale=1.0 / 128)
    P.recip(oss.v(), oss.v())
    for h in range(4):
        P.stt(osb[:, h, :], osb[:, h, :], oss[:, h:h + 1], gainB, ALU.mult, ALU.mult)
    P.tt(osb.v(), osb.v(), zt.v(), ALU.mult)
    y_ps = bk.nxt()
    for h in range(4):
        P.tr(y_ps[:, h, :], osb[:, h, :], K.identf.v())
    P.copy(yT[:, yoff:yoff + 4, cg * 128:(cg + 1) * 128], y_ps.v(), eng="act")


class SubBanks:
    def __init__(self, tiles):
        self.t = list(tiles)
        self.i = 0

    def nxt(self):
        b = self.t[self.i % len(self.t)]
        self.i += 1
        return b


def run_streams(gens, weights):
    gens = list(gens)
    weights = list(weights)
    while gens:
        for gi in range(len(gens) - 1, -1, -1):
            pass
        alive = []
        for g_, w_ in zip(gens, weights):
            done = False
            for _ in range(w_):
                try:
                    next(g_)
                except StopIteration:
                    done = True
                    break
            if not done:
                alive.append((g_, w_))
        gens = [a[0] for a in alive]
        weights = [a[1] for a in alive]


def gate_prefetch(P, C, zt, gcol, gfunc, cg):
    P.dma("sp", zt.r("p h d -> p (h d)"), C.tm[cg * 128:(cg + 1) * 128, gcol:gcol + 512])
    P.activation(zt.v(), zt.v(), gfunc)


def out_stage_gen(P, C, K, bk, OS, osb, gcol, gfunc, gainB, yT, yoff, cg, zt=None):
    osq, oss = OS.osq, OS.oss
    if zt is None:
        zt = OS.zt
        gate_prefetch(P, C, zt, gcol, gfunc, cg)
    P.activation(osq.v(), osb.v(), AF.Square)
    yield
    P.reduce(oss.v(), osq.v(), ALU.add)
    yield
    P.activation(oss.v(), oss.v(), AF.Sqrt, bias=K.epsD.v(), scale=1.0 / 128)
    yield
    P.recip(oss.v(), oss.v())
    for h in range(4):
        P.stt(osb[:, h, :], osb[:, h, :], oss[:, h:h + 1], gainB, ALU.mult, ALU.mult)
    P.tt(osb.v(), osb.v(), zt.v(), ALU.mult)
    yield
    y_ps = bk.nxt()
    for h in range(4):
        P.tr(y_ps[:, h, :], osb[:, h, :], K.identf.v())
    yield
    P.copy(yT[:, yoff:yoff + 4, cg * 128:(cg + 1) * 128], y_ps.v(), eng="act")


def phase_C(P, C, K, l, s, yT):
    m = P.mark()
    sm = K.small
    cm = K.cm
    NEGM4 = K.cm4[:, 0]
    bk = Banks(P, 8)
    flat = lambda b: b.r("p a b -> p (a b)")
    ext = lambda b, hh: flat(b)[:, 0:258].r("p (a b) -> p a b", b=129)[:, hh, :]
    gi = P.sb("gi", [128, NT, 8])
    ci = tm_col(C_BI)
    for c in range(NT):
        P.dma("sp", gi[:, c, :], C.tm[c * 128:(c + 1) * 128, ci:ci + 8])
    it = P.sb("it", [128, NT, 4])
    lf = P.sb("lf", [128, NT, 4])
    nbf = P.sb("nbf", [128, 4])
    P.ts(nbf.v(), sm[:, l, SM_BF:SM_BF + 4], -1.0, ALU.mult)
    for h in range(4):
        P.ts(it[:, :, h], gi[:, :, h], sm[:, l, SM_BI + h:SM_BI + h + 1], ALU.add)
        P.activation(lf[:, :, h], gi[:, :, 4 + h], AF.Exp, bias=nbf[:, h:h + 1], scale=-1.0)
    P.activation(lf.v(), lf.v(), AF.Ln, bias=1.0)
    P.ts(lf.v(), lf.v(), -1.0, ALU.mult)
    G1 = P.sb("G1", [128, NT, 4, 2])
    G2 = P.sb("G2", [128, NT, 4, 2])
    G3 = P.sb("G3", [128, NT, 4, 2])
    P.memset(G1.v(), 0.0)
    P.memset(G2.v(), 0.0)
    P.memset(G3.v(), 0.0)
    P.memset(G1[0:1, :, :, 1], 1.0)
    P.memset(G2[0:1, :, :, 0], 1.0)
    P.copy(G1[:, :, :, 0], lf.v())
    P.ts(G2[:, :, :, 1], lf.v(), -1.0, ALU.mult)
    P.copy(G3[:, :, :, 1], it.v())
    bc = P.sb("bc", [128, NT, 4])
    f64 = lambda b: b.r("p c h -> p (c h)")
    b0 = bk.nxt()
    P.mm(flat(b0)[:, 0:64], cm[:, CM_TRI, :], f64(lf))
    P.copy(f64(bc), flat(b0)[:, 0:64])
    b1 = bk.nxt()
    P.mm(flat(b1)[:, 0:64], cm[:, CM_SEL127, :], f64(bc))
    EBL = P.sb("EBL", [128, NT, 4])
    EKW = P.sb("EKW", [128, NT, 4])
    EB = P.sb("EB", [128, NT, 4])
    P.activation(f64(EBL), flat(b1)[:, 0:64], AF.Exp)
    P.tt(f64(EKW), flat(b1)[:, 0:64], f64(bc), ALU.subtract)
    P.tt(EKW.v(), EKW.v(), it.v(), ALU.add)
    P.activation(EKW.v(), EKW.v(), AF.Exp)
    P.activation(EB.v(), bc.v(), AF.Exp)
    qk = [P.sb("qk", [128, S]) for _ in range(8)]
    for blk in range(8):
        r0 = FM_OFF[C_BQ] + blk * 128
        P.dma("sp", qk[blk].v(), C.fm[r0:r0 + 128, :])
        if blk < 4:
            P.activation(qk[blk].v(), qk[blk].v(), AF.Copy, scale=128 ** -0.5)
    qT, kT = qk[0:4], qk[4:8]
    Cst = P.sb("Cst", [128, 4, 129])
    P.memset(Cst.v(), 0.0)
    k_tm = [P.sb("k_tm", [128, 4, 128]) for _ in range(2)]
    v_ext = [P.sb("v_ext", [128, 4, 129]) for _ in range(2)]
    for b in range(2):
        P.memset(v_ext[b][:, :, 128:129], 1.0)
    kw_tm = P.sb("kw_tm", [128, 4, 128])
    R1 = P.sb("R1", [2, 4, 128])
    R2 = P.sb("R2", [2, 4, 128])
    DT = P.sb("DT", [128, 4, 128])
    SmT = P.sb("SmT", [128, 4, 128])
    htmp = P.sb("htmp", [128, 4, 129])
    htot = P.sb("htot", [128, 4, 129])
    rden = P.sb("rden", [128, 4])
    osb = P.sb("osb", [128, 4, 128])
    OS = out_stage_bufs(P)
    gainB = sm[:, l, SM_MLN:SM_MLN + 128]
    ck, cv = tm_col(C_BK), tm_col(C_BV)
    bkX = SubBanks(bk.t[0:3])
    bkY = SubBanks(bk.t[3:8])
    SmT2 = [SmT, P.sb("SmT", [128, 4, 128])]
    kw2 = [kw_tm, P.sb("kw_tm", [128, 4, 128])]
    zt2 = [P.sb("ztC", [128, 4, 128]) for _ in range(2)]

    def stageX(cg):
        par = cg % 2
        o = slice(cg * 128, (cg + 1) * 128)
        kt, ve = k_tm[par], v_ext[par]
        P.dma("sp", kt.r("p h d -> p (h d)"), C.tm[o, ck:ck + 512])
        P.dma("sp", ve[:, :, 0:128], C.tm[o, cv:cv + 512].r("p (h d) -> p h d", d=128))
        gate_prefetch(P, C, zt2[par], tm_col(C_BO), AF.Sigmoid, cg)
        r_ps = bkX.nxt()
        r2_ps = bkX.nxt()
        for h in range(4):
            P.mm(r_ps[0:2, h, :], G1[:, cg, h, :], cm[:, CM_TRI, :])
        for h in range(4):
            P.mm(r2_ps[0:2, h, :], G2[:, cg, h, :], cm[:, CM_TRI, :], start=True, stop=False)
            P.mm(r2_ps[0:2, h, :], G3[:, cg, h, :], cm[:, CM_IDENT, :], start=False, stop=True)
        yield
        P.copy(R1.v(), r_ps[0:2, :, :], eng="dve")
        P.copy(R2.v(), r2_ps[0:2, :, :], eng="act")
        yield
        dt_ps = bkX.nxt()
        for h in range(4):
            P.mm(dt_ps[:, h, :], R2[:, h, :], R1[:, h, :])
        kq_ps = bkX.nxt()
        for h in range(4):
            P.mm(kq_ps[:, h, :], kT[h][:, o], qT[h][:, o])
        yield
        P.tt(DT.v(), dt_ps.v(), NEGM4, ALU.add)
        yield
        P.activation(DT.v(), DT.v(), AF.Exp)
        for h in range(4):
            scale_cols(P, kw2[par][:, h, :], kt[:, h, :], EKW[:, cg, h:h + 1], ("act", "dve")[h % 2])
        yield
        P.tt(SmT2[par].v(), kq_ps.v(), DT.v(), ALU.mult)

    def stageY(cg):
        par = cg % 2
        o = slice(cg * 128, (cg + 1) * 128)
        ve = v_ext[par]
        hq = [bkY.nxt(), bkY.nxt()]
        hs = [bkY.nxt(), bkY.nxt()]
        for h in range(4):
            P.mm(ext(hq[h // 2], h % 2), qT[h][:, o], Cst[:, h, :])
        for h in range(4):
            P.mm(ext(hs[h // 2], h % 2), SmT2[par][:, h, :], ve[:, h, :])
        yield
        for h in range(4):
            P.activation(htmp[:, h, :], ext(hq[h // 2], h % 2), AF.Copy, scale=EB[:, cg, h:h + 1])
        cps = [bkY.nxt(), bkY.nxt()]
        for h in range(4):
            P.mm(ext(cps[h // 2], h % 2), kw2[par][:, h, :], ve[:, h, :])
        yield
        for h in range(4):
            P.tt(htot[:, h, :], htmp[:, h, :], ext(hs[h // 2], h % 2), ALU.add)
        P.ts(rden.v(), htot[:, :, 128], -1.0, ALU.mult)
        P.tt(rden.v(), rden.v(), htot[:, :, 128], ALU.max)
        P.ts(rden.v(), rden.v(), 1.0, ALU.max)
        P.recip(rden.v(), rden.v())
        for h in range(4):
            scale_cols(P, osb[:, h, :], htot[:, h, 0:128], rden[:, h:h + 1], ("dve", "act")[h % 2])
        for h in range(4):
            P.stt(Cst[:, h, :], Cst[:, h, :], EBL[:, cg, h:h + 1], ext(cps[h // 2], h % 2), ALU.mult, ALU.add)
        yield
        yield from out_stage_gen(P, C, K, bkY, OS, osb, tm_col(C_BO), AF.Sigmoid, gainB, yT, 4, cg, zt=zt2[par])

    run_streams([stageX(0)], [1])
    for cg in range(NT):
        if cg + 1 < NT:
            run_streams([stageX(cg + 1), stageY(cg)], [1, 2])
        else:
            run_streams([stageY(cg)], [1])
    P.release(m)


def phase_B(P, C, K, l, s, yT):
    m = P.mark()
    sm = K.small
    cm = K.cm
    NEGM4, STRICT4, IDENT4 = K.cm4[:, 0], K.cm4[:, 1], K.cm4[:, 2]
    bk = Banks(P, 8)
    rr = RR(("dve", "act", "pool"))
    rr2 = RR(("dve", "act"))
    ab = P.sb("ab", [128, NT, 8])
    ca = tm_col(C_AA)
    for c in range(NT):
        P.dma("sp", ab[:, c, :], C.tm[c * 128:(c + 1) * 128, ca:ca + 8])
    e1 = P.sb("e1", [128, NT, 4])
    g = P.sb("g", [128, NT, 4])
    nega = P.sb("nega", [128, 4])
    P.activation(nega.v(), sm[:, l, SM_ALOG:SM_ALOG + 4], AF.Exp)
    P.ts(nega.v(), nega.v(), -1.0, ALU.mult)
    for h in range(4):
        P.activation(e1[:, :, h], ab[:, :, h], AF.Exp, bias=sm[:, l, SM_DTB + h:SM_DTB + h + 1])
    P.activation(e1.v(), e1.v(), AF.Ln, bias=1.0)
    for h in range(4):
        P.ts(g[:, :, h], e1[:, :, h], nega[:, h:h + 1], ALU.mult)
    beta = P.sb("beta", [128, NT, 4])
    P.activation(beta.v(), ab[:, :, 4:8], AF.Sigmoid)
    nbeta = P.sb("nbeta", [128, NT, 4])
    P.ts(nbeta.v(), beta.v(), -1.0, ALU.mult)
    G1 = P.sb("G1", [128, NT, 4, 2])
    G2 = P.sb("G2", [128, NT, 4, 2])
    P.memset(G1.v(), 0.0)
    P.memset(G2.v(), 0.0)
    P.memset(G1[0:1, :, :, 1], 1.0)
    P.memset(G2[0:1, :, :, 0], 1.0)
    P.copy(G1[:, :, :, 0], g.v())
    P.ts(G2[:, :, :, 1], g.v(), -1.0, ALU.mult)
    gc = P.sb("gc", [128, NT, 4])
    b0 = bk.nxt()
    P.mm(b0.r("p a b -> p (a b)")[:, 0:64], cm[:, CM_TRI, :], g.r("p c h -> p (c h)"))
    P.copy(gc.r("p c h -> p (c h)"), b0.r("p a b -> p (a b)")[:, 0:64])
    b1 = bk.nxt()
    P.mm(b1.r("p a b -> p (a b)")[:, 0:64], cm[:, CM_SEL127, :], gc.r("p c h -> p (c h)"))
    EGL = P.sb("EGL", [128, NT, 4])
    ED = P.sb("ED", [128, NT, 4])
    EG = P.sb("EG", [128, NT, 4])
    P.activation(EGL.r("p c h -> p (c h)"), b1.r("p a b -> p (a b)")[:, 0:64], AF.Exp)
    P.tt(ED.r("p c h -> p (c h)"), b1.r("p a b -> p (a b)")[:, 0:64], gc.r("p c h -> p (c h)"), ALU.subtract)
    P.activation(ED.v(), ED.v(), AF.Exp)
    P.activation(EG.v(), gc.v(), AF.Exp)
    if STOPB == 1:
        P.release(m)
        return
    Sst = P.sb("Sst", [128, 4, 128])
    P.memset(Sst.v(), 0.0)
    HW = 1024
    qkv = [P.sb("qkv", [128, HW]) for _ in range(12)]
    raw = [P.sb("raw", [128, HW + 3]) for _ in range(2)]
    sqt = P.sb("sqt", [128, HW])
    rnt = P.sb("rnt", [128, 512])
    gainB = sm[:, l, SM_GDNN:SM_GDNN + 128]
    for hf in range(2):
        t0 = hf * HW
        def prepA(blk, hf=hf, t0=t0):
            r = raw[blk % 2]
            r0 = FM_OFF[C_AQ] + blk * 128
            if hf == 0:
                P.memset(r[:, 0:3], 0.0)
                P.dma("sp", r[:, 3:], C.fm[r0:r0 + 128, 0:HW])
            else:
                P.dma("sp", r.v(), C.fm[r0:r0 + 128, t0 - 3:t0 + HW])
            dst = qkv[blk]
            e = "dve"
            P.ts(dst.v(), r[:, 0:HW], K.cw[:, l, blk, 0:1], ALU.mult, eng=e)
            for k in range(1, 4):
                P.stt(dst.v(), r[:, k:k + HW], K.cw[:, l, blk, k:k + 1], dst.v(), ALU.mult, ALU.add, eng=e)
            P.activation(dst.v(), dst.v(), AF.Silu)

        def prepB(blk):
            dst = qkv[blk]
            if blk < 8:
                P.activation(sqt.v(), dst.v(), AF.Square)
                for hh in range(2):
                    bb = bk.nxt()
                    bbv = bb.r("p a b -> p (a b)")
                    P.mm(bbv, cm[:, CM_ONES, :], sqt[:, hh * 512:(hh + 1) * 512])
                    P.activation(rnt.v(), bbv, AF.Sqrt, bias=K.eps6.v())
                    P.recip(rnt.v(), rnt.v())
                    sc = (128 ** -0.5) if blk < 4 else 1.0
                    P.stt(dst[:, hh * 512:(hh + 1) * 512], dst[:, hh * 512:(hh + 1) * 512], sc, rnt.v(), ALU.mult, ALU.mult)

        prepA(0)
        for blk in range(12):
            if blk + 1 < 12:
                prepA(blk + 1)
            prepB(blk)
        if STOPB == 2:
            P.release(m)
            return
        qT = qkv[0:4]
        kT = qkv[4:8]
        vT = qkv[8:12]
        if hf == 0:
            bkX = SubBanks(bk.t[0:4])
            bkY = SubBanks(bk.t[4:8])
            v_tm = [P.sb("v_tm", [128, 4, 128]) for _ in range(2)]
            kd_tm = [P.sb("kd_tm", [128, 4, 128]) for _ in range(2)]
            attnT = [P.sb("attnT", [128, 4, 128]) for _ in range(2)]
            nwT = [P.sb("nwT", [128, 4, 128]) for _ in range(2)]
            Rm = [[P.sb("Rm", [128, 4, 128]) for _ in range(2)] for _ in range(2)]
            kg_tm = P.sb("kg_tm", [128, 4, 128])
            R1 = P.sb("R1", [2, 4, 128])
            R2 = P.sb("R2", [2, 4, 128])
            DT = P.sb("DT", [128, 4, 128])
            DTS = P.sb("DTS", [128, 4, 128])
            Bm = P.sb("Bm", [128, 4, 128])
            BT = P.sb("BT", [128, 4, 128])
            Pk = [P.sb("Pk", [128, 4, 128]) for _ in range(2)]
            PkT = [P.sb("PkT", [128, 4, 128]) for _ in range(2)]
            vn = P.sb("vn", [128, 4, 128])
            otmp = P.sb("otmp", [128, 4, 128])
            osb = P.sb("osb", [128, 4, 128])
            OS = out_stage_bufs(P)

        def stageX(c, hf=hf, qT=qT, kT=kT, vT=vT):
            cg = hf * 8 + c
            par = cg % 2
            o = slice(c * 128, (c + 1) * 128)
            kt_ps = bkX.nxt()
            vt_ps = bkX.nxt()
            for h in range(4):
                P.tr(kt_ps[:, h, :], kT[h][:, o], K.identf.v())
                P.tr(vt_ps[:, h, :], vT[h][:, o], K.identf.v())
            yield
            P.copy(v_tm[par].v(), vt_ps.v(), eng="act")
            for h in range(4):
                scale_cols(P, kg_tm[:, h, :], kt_ps[:, h, :], EG[:, cg, h:h + 1], "dve")
                scale_cols(P, kd_tm[par][:, h, :], kt_ps[:, h, :], ED[:, cg, h:h + 1], "dve")
            r_ps = bkX.nxt()
            for h in range(4):
                P.mm(r_ps[0:2, h, :], G1[:, cg, h, :], cm[:, CM_TRI, :])
            r2_ps = bkX.nxt()
            for h in range(4):
                P.mm(r2_ps[0:2, h, :], G2[:, cg, h, :], cm[:, CM_TRI, :])
            yield
            P.copy(R1.v(), r_ps[0:2, :, :], eng="dve")
            P.copy(R2.v(), r2_ps[0:2, :, :], eng="act")
            yield
            dt_ps = bkX.nxt()
            for h in range(4):
                P.mm(dt_ps[:, h, :], R2[:, h, :], R1[:, h, :])
            kq_ps = bkX.nxt()
            kk_ps = bkX.nxt()
            for h in range(4):
                P.mm(kq_ps[:, h, :], kT[h][:, o], qT[h][:, o])
                P.mm(kk_ps[:, h, :], kT[h][:, o], kT[h][:, o])
            yield
            P.tt(DT.v(), dt_ps.v(), NEGM4, ALU.add)
            yield
            P.activation(DT.v(), DT.v(), AF.Exp)
            yield
            P.tt(DTS.v(), DT.v(), STRICT4, ALU.mult)
            P.tt(attnT[par].v(), kq_ps.v(), DT.v(), ALU.mult)
            for h in range(4):
                P.stt(Bm[:, h, :], kk_ps[:, h, :], nbeta[:, cg, h:h + 1], DTS[:, h, :], ALU.mult, ALU.mult)
            yield
            bt_ps = bkX.nxt()
            for h in range(4):
                P.tr(bt_ps[:, h, :], Bm[:, h, :], K.identf.v())
            yield
            P.copy(BT.v(), bt_ps.v(), eng="act")
            P.tt(Rm[par][0].v(), Bm.v(), IDENT4, ALU.add)
            yield
            cur, curT, R = Bm, BT, Rm[par][0]
            for k in range(1, 7):
                nP, nPT, nR = Pk[k % 2], PkT[k % 2], Rm[par][k % 2]
                pT_ps = bkX.nxt()
                for h in range(4):
                    P.mm(pT_ps[:, h, :], cur[:, h, :], curT[:, h, :])
                if k < 6:
                    p_ps = bkX.nxt()
                    for h in range(4):
                        P.mm(p_ps[:, h, :], curT[:, h, :], cur[:, h, :])
                yield
                P.copy(nPT.v(), pT_ps.v(), eng="act")
                if k < 6:
                    P.copy(nP.v(), p_ps.v(), eng="dve")
                yield
                rr_ps = bkX.nxt()
                for h in range(4):
                    P.mm(rr_ps[:, h, :], nPT[:, h, :], R[:, h, :])
                yield
                P.tt(nR.v(), R.v(), rr_ps.v(), ALU.add)
                yield
                cur, curT, R = nP, nPT, nR
            Tt = R
            w_ps = bkX.nxt()
            for h in range(4):
                P.mm(w_ps[:, h, :], kg_tm[:, h, :], Tt[:, h, :])
            yield
            P.activation(nwT[par].v(), w_ps.v(), AF.Copy, scale=-1.0)

        def stageY(c, hf=hf, qT=qT):
            cg = hf * 8 + c
            par = cg % 2
            o = slice(c * 128, (c + 1) * 128)
            Tt = Rm[par][0]
            vn_ps = bkY.nxt()
            o1_ps = bkY.nxt()
            for h in range(4):
                P.mm(vn_ps[:, h, :], Tt[:, h, :], v_tm[par][:, h, :], start=True, stop=False)
                P.mm(vn_ps[:, h, :], nwT[par][:, h, :], Sst[:, h, :], start=False, stop=True)
                P.mm(o1_ps[:, h, :], qT[h][:, o], Sst[:, h, :])
            yield
            for h in range(4):
                scale_cols(P, vn[:, h, :], vn_ps[:, h, :], beta[:, cg, h:h + 1], "dve")
                scale_cols(P, otmp[:, h, :], o1_ps[:, h, :], EG[:, cg, h:h + 1], "act")
            yield
            o2_ps = bkY.nxt()
            s_ps = bkY.nxt()
            for h in range(4):
                P.mm(o2_ps[:, h, :], attnT[par][:, h, :], vn[:, h, :])
                P.mm(s_ps[:, h, :], kd_tm[par][:, h, :], vn[:, h, :])
            yield
            P.tt(osb.v(), otmp.v(), o2_ps.v(), ALU.add)
            for h in range(4):
                P.stt(Sst[:, h, :], Sst[:, h, :], EGL[:, cg, h:h + 1], s_ps[:, h, :], ALU.mult, ALU.add)
            yield
            yield from out_stage_gen(P, C, K, bkY, OS, osb, tm_col(C_AZ), AF.Silu, gainB, yT, 0, cg)

        run_streams([stageX(0)], [1])
        for c in range(8):
            if c + 1 < 8:
                run_streams([stageX(c + 1), stageY(c)], [2, 1])
            else:
                run_streams([stageY(c)], [1])
    P.release(m)


def phase_D(P, C, K, l, s, yT):
    m = P.mark()
    sm = K.small
    cm = K.cm
    bk = Banks(P, 2)
    bkA = Banks(P, 2)
    bkS = Banks(P, 4)
    flat = lambda b: b.r("p a b -> p (a b)")
    kTs = P.sb("kTs", [128, 2, S], BF16)
    kTw = P.sb("kTw", [128, 2, S], BF16)
    P.memset(kTs[64:128, :, :], 0.0)
    P.memset(kTw[64:128, :, :], 0.0)
    vs = P.sb("vs", [128, NT, 2, 65], BF16)
    vw = P.sb("vw", [128, NT, 2, 65], BF16)
    P.memset(vs[:, :, :, 64:65], 1.0)
    P.memset(vw[:, :, :, 64:65], 1.0)
    ckT = P.sb("ckT", [64, 2, 128])
    cvx = P.sb("cvx", [128, 2, 97])
    P.memset(ckT.v(), 0.0)
    P.memset(cvx.v(), 0.0)
    P.memset(cvx[:, :, 64:65], 1.0)
    for g in range(2):
        P.dma("sp", cvx[:, g, 65:97], C.ovm.v())
    gq8 = P.sb("gq8", [128, 8, 64])
    gk = P.sb("gk", [128, 4, 64])
    for h in range(8):
        P.ts(gq8[:, h, :], sm[:, l, SM_QN:SM_QN + 64], 0.125, ALU.mult)
    for a in range(2):
        for g in range(2):
            P.copy(gk[:, 2 * a + g, :], sm[:, l, SM_KN + 64 * (a + 1):SM_KN + 64 * (a + 2)])
    kn0 = P.sb("kn0", [64, 1])
    P.dma("sp", kn0.v(), C.kn0[l])
    ckc = tm_col(C_CKC)
    m1 = P.mark()
    kcT = P.sb("kcT", [64, 2, S])
    vcT = P.sb("vcT", [64, 2, S])
    Wc = P.sb("Wc", [64, 2, 32, 64])
    peT = P.sb("peT", [64, 2, 32])
    P.dma("sp", Wc.v(), C.wcmp[l])
    P.dma("sp", peT.v(), C.peT[l])
    kv = [P.sb("kv", [128, 6, 2, 64]) for _ in range(2)]
    ksq = P.sb("ksq", [128, 2, 2, 64])
    kss = P.sb("kss", [128, 2, 2])
    kn = P.sb("kn", [128, 2, 2, 64])
    kn2 = [kn, P.sb("kn", [128, 2, 2, 64])]

    def d1_stage1(t):
        b = kv[t % 2]
        o = slice(t * 128, (t + 1) * 128)
        knt = kn2[t % 2]
        P.dma("sp", b.r("p a g d -> p (a g d)"), C.tm[o, ckc:ckc + 768])
        P.copy(vs[:, t, :, 0:64], b[:, 3, :, :], eng="act")
        P.copy(vw[:, t, :, 0:64], b[:, 5, :, :], eng="dve")
        P.activation(ksq.v(), b[:, 2:6:2, :, :], AF.Square)
        P.reduce(kss.v(), ksq.v(), ALU.add)
        P.activation(kss.v(), kss.v(), AF.Sqrt, bias=K.epsD.v(), scale=1.0 / 64)
        P.recip(kss.v(), kss.v())
        for a in range(2):
            for g in range(2):
                P.stt(knt[:, a, g, :], b[:, 2 + 2 * a, g, :], kss[:, a, g:g + 1], gk[:, 2 * a + g, :], ALU.mult, ALU.mult)

    def d1_stage2(t):
        b = kv[t % 2]
        o = slice(t * 128, (t + 1) * 128)
        knt = kn2[t % 2]
        ps1 = bk.nxt()
        ps2 = bk.nxt()
        for a in range(2):
            for g in range(2):
                P.tr(ps1[0:64, 2 * a + g, :], knt[:, a, g, :], K.identf.v())
                P.tr(ps2[0:64, 2 * a + g, :], b[:, a, g, :], K.identf.v())
        P.copy(kTs[0:64, :, o], ps1[0:64, 0:2, :], eng="act")
        P.copy(kTw[0:64, :, o], ps1[0:64, 2:4, :], eng="act")
        P.copy(kcT[:, :, o], ps2[0:64, 0:2, :], eng="dve")
        P.copy(vcT[:, :, o], ps2[0:64, 2:4, :], eng="dve")

    d1_stage1(0)
    for t in range(NT):
        if t + 1 < NT:
            d1_stage1(t + 1)
        d1_stage2(t)
    cst = P.sb("cst", [64, 2])
    raw = P.sb("rawc", [64, 127])
    sqc = P.sb("sqc", [64, 127])
    rnc = P.sb("rnc", [64, 127])
    for kvi in range(2):
        pc = bk.nxt()
        for lq in range(32):
            P.mm(flat(pc)[0:64, 0:1], Wc[:, kvi, lq, :], peT[:, kvi, lq:lq + 1], start=(lq == 0), stop=(lq == 31))
        P.copy(cst[:, kvi:kvi + 1], flat(pc)[0:64, 0:1])
    for kvi in range(2):
        src = (kcT, vcT)[kvi]
        for g in range(2):
            ps = bk.nxt()
            pv = flat(ps)[0:64, 0:127]
            for lq in range(32):
                P.mm(pv, Wc[:, kvi, lq, :], src[:, g, lq:lq + 16 * 126 + 1:16], start=(lq == 0), stop=(lq == 31))
            P.ts(raw.v(), pv, cst[:, kvi:kvi + 1], ALU.add)
            if kvi == 0:
                P.activation(sqc.v(), raw.v(), AF.Square)
                p2 = bk.nxt()
                P.mm(flat(p2)[0:64, 0:127], cm[0:64, CM_ONES, 0:64], sqc.v())
                P.activation(rnc.v(), flat(p2)[0:64, 0:127], AF.Sqrt, bias=K.epsD[0:64, :], scale=1.0 / 64)
                P.recip(rnc.v(), rnc.v())
                P.stt(ckT[:, g, 0:127], raw.v(), kn0[:, 0:1], rnc.v(), ALU.mult, ALU.mult)
            else:
                p2 = bk.nxt()
                P.tr(p2[0:127, 0, 0:64], raw.v(), K.identf[0:64, 0:64])
                P.copy(cvx[0:127, g, 0:64], p2[0:127, 0, 0:64])
    P.release(m1)
    qt = [P.sb("qt", [128, 8, 64]) for _ in range(2)]
    gt = [P.sb("gt", [128, 8, 3]) for _ in range(2)]
    qsq = P.sb("qsq", [128, 8, 64])
    qss = P.sb("qss", [128, 8])
    qn = [P.sb("qn", [128, 8, 64]) for _ in range(2)]
    gs = [P.sb("gs", [128, 8, 3]) for _ in range(2)]
    qTf = P.sb("qTf", [64, 4, 128])
    qTb = [P.sb("qTb", [128, 4, 128], BF16) for _ in range(2)]
    for g_ in range(2):
        P.memset(qTb[g_][64:128, :, :], 0.0)
    bct = [P.sb("bct", [128, 4, 128]) for _ in range(2)]
    ec = P.sb("ec", [128, 4, 128])
    esb = [P.sb("esb", [128, 4, 128], BF16) for _ in range(4)]
    rc = P.sb("rc", [128, 3, 4])
    imp = P.sb("imp", [128, 32])
    mx8 = P.sb("mx8", [128, 8])
    selm = P.sb("selm", [128, 32])
    selT = P.sb("selT", [128, 4, 128], BF16)
    P.memset(selT.v(), 0.0)
    yc = [P.sb("yc", [128, 8, 64]) for _ in range(2)]
    cq_, cg_ = tm_col(C_CQ), tm_col(C_CG)
    st_ = dict(ie=0, ib=0)
    LOOK = 2

    def qprep(it):
        o = slice(it * 128, (it + 1) * 128)
        q_, g_ = qt[it % 2], gt[it % 2]
        P.dma("sp", q_.r("p h d -> p (h d)"), C.tm[o, cq_:cq_ + 512])
        P.dma("sp", g_.r("p h k -> p (h k)"), C.tm[o, cg_:cg_ + 24])
        P.activation(qsq.v(), q_.v(), AF.Square)
        P.reduce(qss.v(), qsq.v(), ALU.add)
        P.activation(qss.v(), qss.v(), AF.Sqrt, bias=K.epsD.v(), scale=1.0 / 64)
        P.recip(qss.v(), qss.v())
        for h in range(8):
            P.stt(qn[it % 2][:, h, :], q_[:, h, :], qss[:, h:h + 1], gq8[:, h, :], ALU.mult, ALU.mult)
        P.activation(gs[it % 2].v(), g_.v(), AF.Sigmoid)

    def ytrans(it):
        o = slice(it * 128, (it + 1) * 128)
        y_ps = bk.nxt()
        for c4 in range(4):
            P.tr(y_ps[:, c4, :], yc[it % 2][:, 2 * c4:2 * c4 + 2, :].r("p h d -> p (h d)"), K.identf.v())
        P.copy(yT[:, 8:12, o], y_ps.v(), eng="act")

    qprep(0)
    deferred = None
    for it in range(NT):
        qn_, gs_, yc_ = qn[it % 2], gs[it % 2], yc[it % 2]
        for g in range(2):
            hs_ = slice(4 * g, 4 * g + 4)
            qTb_ = qTb[g]
            qps = bk.nxt()
            for hh in range(4):
                P.tr(qps[0:64, hh, :], qn_[:, 4 * g + hh, :], K.identf.v())
            P.copy(qTf.v(), qps[0:64, :, :], eng="act")
            P.copy(qTb_[0:64, :, :], qps[0:64, :, :], eng="dve")
            bc_ = bct[st_["ib"] % 2]
            st_["ib"] += 1
            P.dma("sp", bc_.v(), C.biasC[it, g])
            sc = bk.nxt()
            P.mm(flat(sc), ckT[:, g, :], flat(qTf), start=True, stop=False)
            P.mm(flat(sc), cm[:, CM_IDENT, :], flat(bc_), start=False, stop=True)
            P.activation(ec.v(), sc.v(), AF.Exp)
            oc = bk.nxt()
            ocv = flat(oc)[:, 0:388].r("p (h c) -> p h c", c=97)
            for hh in range(4):
                P.mm(ocv[:, hh, :], ec[:, hh, :], cvx[:, g, :])
            if deferred is not None:
                ytrans(deferred)
                deferred = None
            P.ts(rc[:, 0, :], ocv[:, :, 64], 1e-30, ALU.max)
            P.recip(rc[:, 0, :], rc[:, 0, :])
            P.ts(imp.v(), ocv[:, 0, 65:97], rc[:, 0, 0:1], ALU.mult)
            for hh in range(1, 4):
                P.stt(imp.v(), ocv[:, hh, 65:97], rc[:, 0, hh:hh + 1], imp.v(), ALU.mult, ALU.add)
            P.tt(rc[:, 0, :], rc[:, 0, :], gs_[:, hs_, 0], ALU.mult)
            for hh in range(4):
                P.ts(yc_[:, 4 * g + hh, :], ocv[:, hh, 0:64], rc[:, 0, hh:hh + 1], ALU.mult)
            P.tt(imp.v(), imp.v(), K.selc[:, it, 0, :], ALU.mult)
            P.tt(imp.v(), imp.v(), K.selc[:, it, 1, :], ALU.add)
            ia, ma = imp.ap, mx8.ap
            P.op("dve", lambda e, ia=ia, ma=ma: e.max(out=ma, in_=ia), [imp], [mx8])
            P.ts(selm.v(), imp.v(), mx8[:, 3:4], ALU.is_ge, -1.0, ALU.add)
            osel = bkA.nxt()
            osv = flat(osel)[:, 0:260].r("p (h c) -> p h c", c=65)
            owin = bkA.nxt()
            owv = flat(owin)[:, 0:260].r("p (h c) -> p h c", c=65)
            j0 = max(0, it - 4)
            items = [("w", jt) for jt in range(j0, it + 1)] + [("s", jt) for jt in range(it + 1)]

            def scores(item):
                br, jt = item
                off = it - jt
                if br == "s" and jt == 0:
                    sps = bk.nxt()
                    P.tr(sps[0:32, 0, :], selm.v(), K.identf.v())
                    for hh in range(4):
                        P.copy(selT[0:32, hh, :], sps[0:32, 0, :], eng=("act", "dve")[hh % 2])
                    if g == 1 and it + 1 < NT:
                        qprep(it + 1)
                sp_ = bkS.nxt()
                if br == "s":
                    ti = min(off, 2)
                    P.mm(flat(sp_), kTs[:, g, jt * 128:(jt + 1) * 128], flat(qTb_), start=True, stop=False)
                    P.mm(flat(sp_), K.identb.v(), K.tbl[:, ti, hs_, :].r("p h i -> p (h i)"), start=False, stop=False)
                    P.mm(flat(sp_), K.rsel[:, jt, :], flat(selT), start=False, stop=True)
                else:
                    ti = (0, 1, 2, 2, 3)[off]
                    P.mm(flat(sp_), kTw[:, g, jt * 128:(jt + 1) * 128], flat(qTb_), start=True, stop=False)
                    P.mm(flat(sp_), K.identb.v(), K.tbl[:, ti, hs_, :].r("p h i -> p (h i)"), start=False, stop=True)
                return sp_

            def finish(item, sp_):
                br, jt = item
                es = esb[st_["ie"] % 4]
                st_["ie"] += 1
                P.activation(es.v(), sp_.v(), AF.Exp)
                if br == "s":
                    for hh in range(4):
                        P.mm(osv[:, hh, :], es[:, hh, :], vs[:, jt, g, :], start=(jt == 0 and hh == 0), stop=(jt == it), skip=True)
                    if jt == it:
                        P.ts(rc[:, 1, :], osv[:, :, 64], 1e-30, ALU.max)
                        P.recip(rc[:, 1, :], rc[:, 1, :])
                        P.tt(rc[:, 1, :], rc[:, 1, :], gs_[:, hs_, 1], ALU.mult)
                        for hh in range(4):
                            P.stt(yc_[:, 4 * g + hh, :], osv[:, hh, 0:64], rc[:, 1, hh:hh + 1], yc_[:, 4 * g + hh, :], ALU.mult, ALU.add)
                else:
                    for hh in range(4):
                        P.mm(owv[:, hh, :], es[:, hh, :], vw[:, jt, g, :], start=(jt == j0 and hh == 0), stop=(jt == it), skip=True)
                    if jt == it:
                        P.ts(rc[:, 2, :], owv[:, :, 64], 1e-30, ALU.max)
                        P.recip(rc[:, 2, :], rc[:, 2, :])
                        P.tt(rc[:, 2, :], rc[:, 2, :], gs_[:, hs_, 2], ALU.mult)
                        for hh in range(4):
                            P.stt(yc_[:, 4 * g + hh, :], owv[:, hh, 0:64], rc[:, 2, hh:hh + 1], yc_[:, 4 * g + hh, :], ALU.mult, ALU.add)

            pending = []
            for item in items:
                pending.append((item, scores(item)))
                if len(pending) > LOOK:
                    finish(*pending.pop(0))
            while pending:
                finish(*pending.pop(0))
        deferred = it
    ytrans(deferred)
    P.release(m)


def phase_E(P, C, K, l, s, uT, yT, hin, hout):
    m0 = P.mark()
    mergedT = P.sb("mergedT", [128, 8, S], BF16)
    m1 = P.mark()
    wsg = WStream(P, 8, 256, nbuf=6)
    wsb = WStream(P, 4, 256, nbuf=6)
    pg = [P.ps("pg", [128, 512]) for _ in range(3)]
    pb = [P.ps("pb", [128, 512]) for _ in range(3)]
    gsb = [P.sb("gsb", [128, 512]) for _ in range(3)]
    acc = [P.sb("acc", [128, 512]) for _ in range(2)]
    it = 0
    k3 = 0

    def ld(q):
        wg, wbr = [], []
        for n in range(3):
            c0 = C_MG + n * 1024 + q * 256
            wg.append(wsg.load(C.w_in[l][:, c0:c0 + 256], 256))
            wbr.append(wsb.load(C.w_branch[l, n][:, q * 256:(q + 1) * 256], 256))
        return wg, wbr
    for q, (wg, wbr) in prefetch_iter(list(range(4)), ld):
        for j in range(2):
            fblk = q * 2 + j
            for tg in range(4):
                a = acc[it % 2]
                tsl = slice(tg * 512, (tg + 1) * 512)
                for n in range(3):
                    g_ps, b_ps, gs = pg[k3 % 3], pb[k3 % 3], gsb[k3 % 3]
                    k3 += 1
                    for kc in range(8):
                        P.mm(g_ps.v(), wg[n][:, kc, j * 128:(j + 1) * 128], uT[:, kc, tsl], start=(kc == 0), stop=(kc == 7))
                    for kc in range(4):
                        P.mm(b_ps.v(), wbr[n][:, kc, j * 128:(j + 1) * 128], yT[:, n * 4 + kc, tsl], start=(kc == 0), stop=(kc == 3))
                    P.activation(gs.v(), g_ps.v(), AF.Sigmoid)
                    if n == 0:
                        P.tt(a.v(), gs.v(), b_ps.v(), ALU.mult)
                    else:
                        P.tt(gs.v(), gs.v(), b_ps.v(), ALU.mult)
                        if n == 1:
                            P.tt(a.v(), a.v(), gs.v(), ALU.add)
                        else:
                            P.tt(mergedT[:, fblk, tsl], a.v(), gs.v(), ALU.add)
                it += 1
    P.release(m1)
    ws = WStream(P, 8, 512, nbuf=2)
    po = [P.ps("po", [128, 512]) for _ in range(3)]
    hsb = [P.sb("hsb", [128, 512]) for _ in range(4)]
    items = [(half, t) for half in range(2) for t in range(NT)]
    wts = [ws.load(C.w_out[l][:, half * 512:(half + 1) * 512], 512) for half in range(2)]

    def ldh(x):
        half, t = x
        hi = hsb[(half * NT + t) % 4]
        P.dma("sp", hi.v(), hin[t][:, half * 512:(half + 1) * 512])
        return hi
    it = 0
    for (half, t), hi in prefetch_iter(items, ldh):
        csl = slice(half * 512, (half + 1) * 512)
        w = wts[half]
        ps = po[it % 3]
        it += 1
        for kc in range(8):
            P.mm(ps.v(), mergedT[:, kc, t * 128:(t + 1) * 128], w[:, kc, :], start=(kc == 0), stop=(kc == 7))
        P.tt(hi.v(), hi.v(), ps.v(), ALU.add)
        P.dma("sp", hout[t][:, csl], hi.v())
    P.release(m0)


def phase_F(P, C, K, l, s, uT, hout):
    norm_transpose(P, K, C, l, 1, hout, uT)
    m = P.mark()
    actT = P.sb("actT", [128, 22, S], BF16)
    m2 = P.mark()
    wsg = WStream(P, 8, 256, nbuf=3)
    wsu = WStream(P, 8, 256, nbuf=3)
    pgt = [P.ps("pgt", [128, 512]) for _ in range(3)]
    pup = [P.ps("pup", [128, 512]) for _ in range(3)]
    sg = [P.sb("sg", [128, 512]) for _ in range(3)]
    it = 0

    def ldf(q):
        return (wsg.load(C.w_ffn_in[l][:, q * 256:(q + 1) * 256], 256),
                wsu.load(C.w_ffn_in[l][:, D_FF + q * 256:D_FF + (q + 1) * 256], 256))
    for q, (wg, wu) in prefetch_iter(list(range(11)), ldf):
        for j in range(2):
            fblk = q * 2 + j
            for tg in range(4):
                tsl = slice(tg * 512, (tg + 1) * 512)
                g_ps, u_ps, sgt = pgt[it % 3], pup[it % 3], sg[it % 3]
                it += 1
                for kc in range(8):
                    P.mm(g_ps.v(), wg[:, kc, j * 128:(j + 1) * 128], uT[:, kc, tsl], start=(kc == 0), stop=(kc == 7))
                for kc in range(8):
                    P.mm(u_ps.v(), wu[:, kc, j * 128:(j + 1) * 128], uT[:, kc, tsl], start=(kc == 0), stop=(kc == 7))
                P.activation(sgt.v(), g_ps.v(), AF.Silu)
                P.tt(actT[:, fblk, tsl], sgt.v(), u_ps.v(), ALU.mult)
    P.release(m2)
    ws = WStream(P, 22, 256, nbuf=2)
    po = [P.ps("po2", [128, 512]) for _ in range(3)]
    hsb = [P.sb("hsb2", [128, 256]) for _ in range(4)]
    items = [(qd, t) for qd in range(4) for t in range(NT)]
    wts = {}

    def ldh2(x):
        qd, t = x
        if t == 0:
            wts[qd] = ws.load(C.w_ffn_out[l][:, qd * 256:(qd + 1) * 256], 256)
        hi = hsb[(qd * NT + t) % 4]
        P.dma("sp", hi.v(), hout[t][:, qd * 256:(qd + 1) * 256])
        return hi
    it = 0
    for (qd, t), hi in prefetch_iter(items, ldh2):
        csl = slice(qd * 256, (qd + 1) * 256)
        w = wts[qd]
        ps = po[it % 3]
        it += 1
        for kc in range(22):
            P.mm(ps[:, 0:256], actT[:, kc, t * 128:(t + 1) * 128], w[:, kc, :], start=(kc == 0), stop=(kc == 21))
        P.tt(hi.v(), hi.v(), ps[:, 0:256], ALU.add)
        P.dma("sp", hout[t][:, csl], hi.v())
    P.release(m)
    norm_transpose(P, K, C, l, 2, hout, uT)
    m = P.mark()
    pTb = P.sb("pTb", [128, 2, S], BF16)
    P.dma("pool", pTb.v(), C.pT[l, s].r("(k p) t -> p k t", p=128))
    wsg = WStream(P, 8, 512, nbuf=2)
    wsp = WStream(P, 2, 512, nbuf=2)
    pg = [P.ps("pg3", [128, 512]) for _ in range(2)]
    pp = [P.ps("pp3", [128, 512]) for _ in range(2)]
    gsb = [P.sb("gsb3", [128, 512]) for _ in range(3)]
    hsb = [P.sb("hsb3", [128, 512]) for _ in range(4)]
    wgs = [wsg.load(C.w_ple_gate[l][:, half * 512:(half + 1) * 512], 512) for half in range(2)]
    wps = [wsp.load(C.w_ple_proj[l][:, half * 512:(half + 1) * 512], 512) for half in range(2)]
    items = [(half, t) for half in range(2) for t in range(NT)]

    def ldh3(x):
        half, t = x
        hi = hsb[(half * NT + t) % 4]
        P.dma("sp", hi.v(), hout[t][:, half * 512:(half + 1) * 512])
        return hi
    it = 0
    for (half, t), hi in prefetch_iter(items, ldh3):
        csl = slice(half * 512, (half + 1) * 512)
        wg, wp = wgs[half], wps[half]
        g_ps, p_ps, gs = pg[it % 2], pp[it % 2], gsb[it % 3]
        it += 1
        for kc in range(8):
            P.mm(g_ps.v(), uT[:, kc, t * 128:(t + 1) * 128], wg[:, kc, :], start=(kc == 0), stop=(kc == 7))
        for kc in range(2):
            P.mm(p_ps.v(), pTb[:, kc, t * 128:(t + 1) * 128], wp[:, kc, :], start=(kc == 0), stop=(kc == 1))
        P.activation(gs.v(), g_ps.v(), AF.Sigmoid)
        P.tt(gs.v(), gs.v(), p_ps.v(), ALU.mult)
        P.tt(hi.v(), hi.v(), gs.v(), ALU.add)
        P.dma("sp", hout[t][:, csl], hi.v())
    P.release(m)


def build(n_layers=DEPTH, n_seq=NSEQ, phases="AE", dbg=(), dbg_yT=False):
    nc = bass.Bass("TRN2", target_bir_lowering=False)
    P = Prog(nc)
    C = declare(P, dbg)
    K = setup_consts(P, C)
    K.epsD = P.sb("epsD", [128, 1])
    P.memset(K.epsD.v(), EPS)
    K.eps6 = P.sb("eps6", [128, 1])
    P.memset(K.eps6.v(), 1e-6)
    if dbg_yT:
        ydbg = P.track(Buf(nc.dram_tensor("yT_dbg", [128, 12, S], F32, kind="ExternalInput").ap(), "yT_dbg"))
    for s in range(n_seq):
        for l in range(n_layers):
            hin = C.xt[s] if l == 0 else C.ot[s]
            my = P.mark()
            yT = P.sb("yT", [128, 12, S], BF16)
            mu = P.mark()
            uT = P.sb("uT", [128, 8, S], BF16)
            if "A" in phases:
                phase_A(P, C, K, l, s, hin, uT)
            P.release(mu)
            if "B" in phases:
                phase_B(P, C, K, l, s, yT)
            if "C" in phases:
                phase_C(P, C, K, l, s, yT)
            if "D" in phases:
                phase_D(P, C, K, l, s, yT)
            if "y" in dbg:
                mm_ = P.mark()
                yf = P.sb("yf", [128, 12, S])
                P.copy(yf.v(), yT.v(), eng="pool")
                P.dma("sp", C.ydbg.v(), yf.v())
                P.release(mm_)
            uT = P.sb("uT", [128, 8, S], BF16)
            norm_transpose(P, K, C, l, 0, hin, uT)
            if dbg_yT:
                m = P.mark()
                yf = P.sb("yf", [128, 12, S])
                P.dma("sp", yf.v(), ydbg.v())
                P.copy(yT.v(), yf.v(), eng="pool")
                P.release(m)
            if "E" in phases:
                phase_E(P, C, K, l, s, uT, yT, hin, C.ot[s])
            P.release(my)
            if "E" in phases:
                uT = P.sb("uT", [128, 8, S], BF16)
                phase_F(P, C, K, l, s, uT, C.ot[s])
                P.release(my)
    st = P.emit()
    P.close()
    return nc, st


import math
NEGB = -30000.0


def rel_bucket_np(dist):
    d = np.maximum(dist, 0)
    df = np.maximum(d, 1).astype(np.float32)
    large = 16 + (np.log(df / np.float32(16)) / np.float32(math.log(128 / 16)) * np.float32(16)).astype(np.int32)
    large = np.minimum(large, 31)
    return np.where(d < 16, d, large).astype(np.int64)


def nsa_consts(inp):
    rb = inp["rel_bias"].astype(np.float32)
    d = {}
    j = np.arange(128)[:, None]
    i = np.arange(128)[None, :]
    neg = np.float32(NEGB)

    def gat(dist, valid):
        g = rb[rel_bucket_np(dist)]
        g = np.where(valid[..., None], g, neg)
        return np.ascontiguousarray(g.transpose(0, 2, 1))
    tb = np.zeros((4, 128, 8, 128), np.float32)
    tb[0] = gat(i - j, (i - j) >= 0)
    tb[1] = gat(128 + i - j, np.ones((128, 128), bool))
    tb[2] = gat(np.full((128, 128), 1000), np.ones((128, 128), bool))
    tb[3] = gat(512 + i - j, i < j)
    d["tbls"] = tb
    n = np.arange(128)[:, None]
    bc = np.zeros((NT, 2, 128, 4, 128), np.float32)
    for it in range(NT):
        tq = it * 128 + np.arange(128)[None, :]
        dist = tq - (16 * n + 31)
        valid = (dist >= 0) & (n < 127)
        g = np.where(valid[..., None], rb[rel_bucket_np(dist)], neg)
        g = g.transpose(0, 2, 1)
        bc[it, 0] = g[:, 0:4]
        bc[it, 1] = g[:, 4:8]
    d["biasC"] = bc
    sc = np.zeros((128, NT, 2, 32), np.float32)
    jj = np.arange(32)[None, :]
    for it in range(NT):
        cur = (it * 128 + np.arange(128)[:, None]) // 64
        forced = (jj == 0) | (jj == cur)
        causal = jj <= cur
        sc[:, it, 0] = (causal & ~forced)
        sc[:, it, 1] = np.where(forced, 1e4, np.where(causal, 0.0, -1.0))
    d["selc"] = sc
    cs = np.arange(127) * 16
    ce = cs + 31
    ss = np.arange(32) * 64
    se = ss + 63
    ov = np.zeros((128, 32), np.float32)
    ov[:127] = ((cs[:, None] <= se[None]) & (ce[:, None] >= ss[None]))
    d["ovm"] = ov
    rs = np.zeros((32, NT, 128), np.float32)
    for jt in range(NT):
        for jr in range(128):
            rs[2 * jt + jr // 64, jt, jr] = 30000.0
    d["rselc"] = rs
    d["wcmp"] = np.ascontiguousarray(inp["nsa_w_cmp"].transpose(0, 3, 1, 2, 4))
    d["peT"] = np.ascontiguousarray(inp["nsa_cmp_pe"].transpose(0, 3, 1, 2))
    d["kn0"] = np.ascontiguousarray(inp["nsa_k_norm"][:, 0, :, None])
    return d


def host_consts(inp):
    d = {}
    j = np.arange(128)[:, None]
    i = np.arange(128)[None, :]
    cm = np.zeros((128, NCM, 128), np.float32)
    cm[:, CM_TRI] = (j <= i)
    cm[:, CM_NEGM] = np.where(i >= j, 0.0, -30000.0)
    cm[:, CM_STRICT] = (i > j)
    cm[:, CM_SEL127] = (j == 127) * np.ones((1, 128))
    cm[:, CM_IDENT] = (i == j)
    cm[:, CM_ONES] = 1.0
    d["cmat"] = cm
    sm = np.zeros((DEPTH, NSMALL), np.float32)
    sm[:, SM_ALOG:SM_ALOG + 4] = inp["gdn_a_log"]
    sm[:, SM_DTB:SM_DTB + 4] = inp["gdn_dt_bias"]
    sm[:, SM_BI:SM_BI + 4] = inp["mlstm_b_i"]
    sm[:, SM_BF:SM_BF + 4] = inp["mlstm_b_f"]
    sm[:, SM_GDNN:SM_GDNN + 128] = inp["gdn_norm"]
    sm[:, SM_MLN:SM_MLN + 128] = inp["mlstm_norm"]
    sm[:, SM_QN:SM_QN + 64] = inp["nsa_q_norm"]
    sm[:, SM_KN:SM_KN + 192] = inp["nsa_k_norm"].reshape(DEPTH, 192)
    d["small"] = np.ascontiguousarray(np.broadcast_to(sm[:, None, :], (DEPTH, 128, NSMALL)))
    d.update(nsa_consts(inp))
    d["cw"] = np.ascontiguousarray(inp["conv_w"].reshape(DEPTH, 4, 12, 128).transpose(0, 3, 2, 1))
    return d


def host_inputs(inp, core=0):
    b0 = core * 2
    d = {}
    d["x"] = np.ascontiguousarray(inp["x"][b0:b0 + 2])
    d["pT"] = np.ascontiguousarray(np.transpose(inp["p"][:, b0:b0 + 2], (0, 1, 3, 2)))
    for k in ["w_in", "w_branch", "w_out", "w_ffn_in", "w_ffn_out", "w_ple_gate", "w_ple_proj"]:
        d[k] = inp[k]
    g = np.stack([inp["norm_mix"], inp["norm_ffn"], inp["norm_ple"]], axis=1)
    d["gains_b"] = np.ascontiguousarray(np.broadcast_to(g[:, :, None, :], (DEPTH, 3, 128, D)), dtype=np.float32)
    d["ident"] = np.eye(128, dtype=np.float32)
    d.update(host_consts(inp))
    return d


_NC_CACHE = {}


def kernel(**inputs):
    inp = {k_: np.asarray(v) for k_, v in inputs.items()}
    if "full" not in _NC_CACHE:
        _NC_CACHE["full"] = build(n_layers=DEPTH, n_seq=NSEQ, phases="ABCDE")[0]
    nc = _NC_CACHE["full"]
    consts = host_consts(inp)
    shared = {}
    for k_ in ["w_in", "w_branch", "w_out", "w_ffn_in", "w_ffn_out", "w_ple_gate", "w_ple_proj"]:
        shared[k_] = np.ascontiguousarray(inp[k_], dtype=np.float32)
    g = np.stack([inp["norm_mix"], inp["norm_ffn"], inp["norm_ple"]], axis=1)
    shared["gains_b"] = np.ascontiguousarray(np.broadcast_to(g[:, :, None, :], (DEPTH, 3, 128, D)), dtype=np.float32)
    shared["ident"] = np.eye(128, dtype=np.float32)
    shared.update(consts)
    in_maps = []
    for c in range(8):
        d = dict(shared)
        d["x"] = np.ascontiguousarray(inp["x"][2 * c:2 * c + 2], dtype=np.float32)
        d["pT"] = np.ascontiguousarray(np.transpose(inp["p"][:, 2 * c:2 * c + 2], (0, 1, 3, 2)), dtype=np.float32)
        in_maps.append(d)
    res = run_bass_kernel_spmd(nc, in_maps, core_ids=list(range(8)))
    out = np.concatenate([np.asarray(r["out"]) for r in res.results], axis=0)
    return out.astype(np.float32)
```
